# Optimizing a Trainium2 kernel written in Bass

```python
import math
import jax, jax.numpy as jnp
from jax import lax
import numpy as np

D_MODEL = 1024
BATCH = 8
SEQ = 2048
DEPTH = 1

GRID_W = 64
CTX_LEN = 256
NA_HEADS = 8
NA_HEAD_DIM = 64
NA_KH = 8
NA_KW = 16
NA_SCALE = NA_HEAD_DIM ** -0.5
DN_HEADS = 8
DN_HEAD_DIM = 64
DN_CONV = 5
DN_CHUNK = 64
ROPE_THETA = 10000.0
N_EXPERTS = 32
TOP_K = 4
D_EXPERT = 1024
SWIGLU_LIMIT = 7.0
SWIGLU_ALPHA = 1.702
MOE_BLOCK = 128
NORM_EPS = 1e-6
NEG_INF = -1e30
NA_WIDTH = NA_HEADS * NA_HEAD_DIM
DN_WIDTH = DN_HEADS * DN_HEAD_DIM
IN_SPLITS = (NA_WIDTH, NA_WIDTH, NA_WIDTH, 3 * DN_WIDTH, DN_WIDTH, 2 * DN_HEADS, 2 * DN_HEADS, D_MODEL, D_MODEL)
D_IN = sum(IN_SPLITS)
IN_OFFSETS = tuple(int(o) for o in np.cumsum(IN_SPLITS)[:-1])

kernel_name = 'hybrid_na_gdn_moe_dit_block'


def rmsnorm(x, w):
    xf = x.astype(jnp.float32)
    y = xf * lax.rsqrt(jnp.mean(xf * xf, axis=-1, keepdims=True) + NORM_EPS)
    return (y * w.astype(jnp.float32)).astype(x.dtype)


def modulate(h, shift, scale):
    return h * (1.0 + scale) + shift


def to_heads(t, heads):
    b, t_len, _ = t.shape
    return t.reshape(b, t_len, heads, -1).transpose(0, 2, 1, 3)


def merge_heads(o):
    b, h, t_len, d = o.shape
    return o.transpose(0, 2, 1, 3).reshape(b, t_len, h * d)


def l2norm(x):
    return x * lax.rsqrt(jnp.sum(x * x, axis=-1, keepdims=True) + NORM_EPS)


def centred_dwconv(x, w):
    taps, ch = w.shape
    return lax.conv_general_dilated(x, w[:, None, :].astype(x.dtype), window_strides=(1,),
                                    padding=[(taps // 2, taps // 2)],
                                    dimension_numbers=('NWC', 'WIO', 'NWC'), feature_group_count=ch)


def axial_rope(x, t_len):
    t = jnp.arange(t_len)
    row = (t // GRID_W).astype(jnp.float32)
    col = (t % GRID_W).astype(jnp.float32)
    half = x.shape[-1] // 2
    n_axis = half // 2
    freqs = ROPE_THETA ** (-jnp.arange(n_axis, dtype=jnp.float32) / n_axis)
    ang = jnp.concatenate([row[:, None] * freqs, col[:, None] * freqs], axis=-1)
    cos, sin = jnp.cos(ang), jnp.sin(ang)
    x1, x2 = x[..., :half], x[..., half:]
    return jnp.concatenate([x1 * cos - x2 * sin, x1 * sin + x2 * cos], axis=-1)


def neighborhood_attention(q, k, v, k_ctx, v_ctx, rpb, rows):
    b, h, t_len, dh = q.shape
    kh = min(NA_KH, rows)
    r = jnp.arange(rows)
    row_idx = jnp.clip(r - kh // 2, 0, rows - kh)[:, None] + jnp.arange(kh)[None, :]
    col = jnp.arange(GRID_W)
    col_start = jnp.clip(col - NA_KW // 2, 0, GRID_W - NA_KW)
    col_mask = (col[None, :] >= col_start[:, None]) & (col[None, :] < col_start[:, None] + NA_KW)
    dr_idx = row_idx - r[:, None] + NA_KH - 1
    dc_idx = jnp.clip(col[None, :] - col[:, None] + NA_KW - 1, 0, 2 * NA_KW - 2)
    bias = rpb[:, dr_idx[:, None, :, None], dc_idx[None, :, None, :]].astype(jnp.float32)
    qg = q.reshape(b, h, rows, GRID_W, dh)
    kg = k.reshape(b, h, rows, GRID_W, dh)[:, :, row_idx]
    vg = v.reshape(b, h, rows, GRID_W, dh)[:, :, row_idx]
    s_lat = jnp.einsum('bhrqd,bhrjkd->bhrqjk', qg, kg).astype(jnp.float32) * NA_SCALE + bias
    s_lat = jnp.where(col_mask[:, None, :], s_lat, NEG_INF)
    s_ctx = jnp.einsum('bhrqd,bhld->bhrql', qg, k_ctx).astype(jnp.float32) * NA_SCALE
    n_lat = kh * GRID_W
    s = jnp.concatenate([s_lat.reshape(b, h, rows, GRID_W, n_lat), s_ctx], axis=-1)
    p = jax.nn.softmax(s, axis=-1).astype(v.dtype)
    p_lat = p[..., :n_lat].reshape(b, h, rows, GRID_W, kh, GRID_W)
    o = (jnp.einsum('bhrqjk,bhrjkd->bhrqd', p_lat, vg)
         + jnp.einsum('bhrql,bhld->bhrqd', p[..., n_lat:], v_ctx))
    return merge_heads(o.reshape(b, h, t_len, dh))


def context_attention(q, k, v):
    s = jnp.einsum('bhqd,bhkd->bhqk', q, k).astype(jnp.float32) * NA_SCALE
    p = jax.nn.softmax(s, axis=-1).astype(v.dtype)
    return merge_heads(jnp.einsum('bhqk,bhkd->bhqd', p, v))


def gated_delta_chunked(q, k, v, g, beta, s0):
    b, h, t_len, dk = q.shape
    dv = v.shape[-1]
    n = t_len // DN_CHUNK
    chunk = lambda t: t.reshape(b, h, n, DN_CHUNK, *t.shape[3:])
    qc, kc, vc, bc = chunk(q * dk ** -0.5), chunk(k), chunk(v), chunk(beta)
    gcum = jnp.cumsum(chunk(g), axis=-1)
    incl = jnp.tril(jnp.ones((DN_CHUNK, DN_CHUNK), dtype=bool))
    strict = jnp.tril(jnp.ones((DN_CHUNK, DN_CHUNK), dtype=bool), -1)
    diff = gcum[..., :, None] - gcum[..., None, :]
    decay = jnp.where(incl, jnp.exp(jnp.where(incl, diff, 0.0)), 0.0)
    kb = kc * bc[..., None]
    a = jnp.where(strict, jnp.einsum('bhnid,bhnjd->bhnij', kb, kc) * decay, 0.0)
    eye = jnp.eye(DN_CHUNK, dtype=a.dtype)
    t_inv = lax.linalg.triangular_solve(a + eye, jnp.broadcast_to(eye, a.shape), left_side=True,
                                        lower=True, unit_diagonal=True)
    u = jnp.einsum('bhnij,bhnjd->bhnid', t_inv, vc * bc[..., None])
    w = jnp.einsum('bhnij,bhnjd->bhnid', t_inv, kb * jnp.exp(gcum)[..., None])
    qk = jnp.einsum('bhnid,bhnjd->bhnij', qc, kc) * decay
    q_dec = qc * jnp.exp(gcum)[..., None]
    g_last = gcum[..., -1]
    k_tail = kc * jnp.exp(g_last[..., None] - gcum)[..., None]

    def step(s, xs):
        u_n, w_n, qk_n, qd_n, kt_n, gl_n = xs
        v_new = u_n - jnp.einsum('bhid,bhde->bhie', w_n, s)
        o_n = jnp.einsum('bhid,bhde->bhie', qd_n, s) + jnp.einsum('bhij,bhje->bhie', qk_n, v_new)
        s = s * jnp.exp(gl_n)[..., None, None] + jnp.einsum('bhid,bhie->bhde', kt_n, v_new)
        return s, o_n

    xs = tuple(jnp.moveaxis(t, 2, 0) for t in (u, w, qk, q_dec, k_tail, g_last))
    s_fin, o = lax.scan(step, s0, xs)
    return s_fin, jnp.moveaxis(o, 0, 2).reshape(b, h, t_len, dv)


def dn_prepare(z_qkv, z_beta, z_a, conv_w, a_log, dt_bias, use_rope):
    qkv = jax.nn.silu(centred_dwconv(z_qkv, conv_w)).astype(jnp.float32)
    q, k, v = jnp.split(qkv, 3, axis=-1)
    q = l2norm(to_heads(q, DN_HEADS))
    k = l2norm(to_heads(k, DN_HEADS))
    v = to_heads(v, DN_HEADS)
    if use_rope:
        q = axial_rope(q, q.shape[2])
        k = axial_rope(k, k.shape[2])
    b, t_len, _ = z_beta.shape
    beta = jax.nn.sigmoid(z_beta.astype(jnp.float32)).reshape(b, t_len, 2, DN_HEADS).transpose(2, 0, 3, 1)
    a = z_a.astype(jnp.float32).reshape(b, t_len, 2, DN_HEADS).transpose(2, 0, 3, 1)
    g = -jnp.exp(a_log.astype(jnp.float32))[:, None, :, None] * jax.nn.softplus(
        a + dt_bias.astype(jnp.float32)[:, None, :, None])
    return q, k, v, g, beta


def dn_bidirectional(ctx_in, lat_in):
    qc, kc, vc, gc, bc = ctx_in
    ql, kl, vl, gl, bl = lat_in
    b, h, _, dk = kc.shape
    s0 = jnp.zeros((b, h, dk, vc.shape[-1]), jnp.float32)
    flip = lambda t: jnp.flip(t, axis=2)
    s_cf, o_cf = gated_delta_chunked(qc, kc, vc, gc[0], bc[0], s0)
    _, o_lf = gated_delta_chunked(ql, kl, vl, gl[0], bl[0], s_cf)
    s_cb, o_cb = gated_delta_chunked(flip(qc), flip(kc), flip(vc), flip(gc[1]), flip(bc[1]), s0)
    _, o_lb = gated_delta_chunked(flip(ql), flip(kl), flip(vl), flip(gl[1]), flip(bl[1]), s_cb)
    return o_cf + flip(o_cb), o_lf + flip(o_lb)


def dn_output(o, z_g, norm_w):
    b, h, t_len, dv = o.shape
    o = rmsnorm(o.transpose(0, 2, 1, 3), norm_w) * jax.nn.silu(z_g.astype(jnp.float32).reshape(b, t_len, h, dv))
    return o.reshape(b, t_len, h * dv).astype(z_g.dtype)


def merge_branches(o_na, o_dn, ga, gb, w_br_a, w_br_b, w_out):
    y = jax.nn.sigmoid(ga) * (o_na @ w_br_a) + jax.nn.sigmoid(gb) * (o_dn @ w_br_b)
    return y @ w_out


def mixer_sublayer(h_lat, h_ctx, w_in, na_rpb, dn_conv_w, dn_a_log, dn_dt_bias, dn_norm_w,
                   w_br_a, w_br_b, w_out, need_ctx_out):
    rows = h_lat.shape[1] // GRID_W
    naq_l, nak_l, nav_l, dqkv_l, dg_l, db_l, da_l, ga_l, gb_l = jnp.split(h_lat @ w_in, IN_OFFSETS, axis=-1)
    naq_c, nak_c, nav_c, dqkv_c, dg_c, db_c, da_c, ga_c, gb_c = jnp.split(h_ctx @ w_in, IN_OFFSETS, axis=-1)
    k_ctx = to_heads(nak_c, NA_HEADS)
    v_ctx = to_heads(nav_c, NA_HEADS)
    o_na_l = neighborhood_attention(to_heads(naq_l, NA_HEADS), to_heads(nak_l, NA_HEADS),
                                    to_heads(nav_l, NA_HEADS), k_ctx, v_ctx, na_rpb, rows)
    dn_c = dn_prepare(dqkv_c, db_c, da_c, dn_conv_w, dn_a_log, dn_dt_bias, False)
    dn_l = dn_prepare(dqkv_l, db_l, da_l, dn_conv_w, dn_a_log, dn_dt_bias, True)
    o_dn_c, o_dn_l = dn_bidirectional(dn_c, dn_l)
    y_lat = merge_branches(o_na_l, dn_output(o_dn_l, dg_l, dn_norm_w), ga_l, gb_l, w_br_a, w_br_b, w_out)
    y_ctx = None
    if need_ctx_out:
        o_na_c = context_attention(to_heads(naq_c, NA_HEADS), k_ctx, v_ctx)
        y_ctx = merge_branches(o_na_c, dn_output(o_dn_c, dg_c, dn_norm_w), ga_c, gb_c, w_br_a, w_br_b, w_out)
    return y_lat, y_ctx


def moe_ffn(h, w_router, b_router, w1, b1, w2, b2):
    b, t_len, d = h.shape
    xf = h.reshape(-1, d)
    n_tok = xf.shape[0]
    logits = (xf @ w_router + b_router).astype(jnp.float32)
    top_val, top_idx = lax.top_k(logits, TOP_K)
    gates = jax.nn.softmax(top_val, axis=-1)
    n_asg = n_tok * TOP_K
    e_flat = top_idx.reshape(-1)
    tok_flat = jnp.repeat(jnp.arange(n_tok, dtype=jnp.int32), TOP_K)
    order = jnp.argsort(e_flat, stable=True)
    e_sorted, tok_sorted, g_sorted = e_flat[order], tok_flat[order], gates.reshape(-1)[order]
    counts = jnp.zeros((N_EXPERTS,), jnp.int32).at[e_flat].add(1)
    starts = jnp.cumsum(counts) - counts
    padded = (counts + MOE_BLOCK - 1) // MOE_BLOCK * MOE_BLOCK
    pad_ends = jnp.cumsum(padded)
    pad_starts = pad_ends - padded
    dest = pad_starts[e_sorted] + (jnp.arange(n_asg, dtype=jnp.int32) - starts[e_sorted])
    m_pad = (n_asg + N_EXPERTS * (MOE_BLOCK - 1) + MOE_BLOCK - 1) // MOE_BLOCK * MOE_BLOCK
    n_blk = m_pad // MOE_BLOCK
    tok_pad = jnp.full((m_pad,), n_tok, jnp.int32).at[dest].set(tok_sorted)
    g_pad = jnp.zeros((m_pad,), jnp.float32).at[dest].set(g_sorted)
    blk_expert = jnp.minimum(jnp.searchsorted(pad_ends, jnp.arange(n_blk, dtype=jnp.int32) * MOE_BLOCK,
                                              side='right'), N_EXPERTS - 1)
    x_pad = jnp.concatenate([xf, jnp.zeros((1, d), xf.dtype)], axis=0)[tok_pad].reshape(n_blk, MOE_BLOCK, d)

    def expert_block(args):
        xb, e = args
        hb = xb @ w1[e] + b1[e]
        gate, up = hb[:, :D_EXPERT], hb[:, D_EXPERT:]
        gate = jnp.minimum(gate, SWIGLU_LIMIT)
        up = jnp.clip(up, -SWIGLU_LIMIT, SWIGLU_LIMIT)
        act = (up + 1.0) * gate * jax.nn.sigmoid(SWIGLU_ALPHA * gate)
        return act @ w2[e] + b2[e]

    y_pad = lax.map(expert_block, (x_pad, blk_expert)).reshape(m_pad, d)
    y = jax.ops.segment_sum(y_pad.astype(jnp.float32) * g_pad[:, None], tok_pad, num_segments=n_tok + 1)[:n_tok]
    return y.reshape(b, t_len, d).astype(h.dtype)


def setup_inputs(seed: int = 0) -> dict:
    key = jax.random.key(seed)
    ks = jax.random.split(key, 24)
    f32 = jnp.float32
    L, D, E, F = DEPTH, D_MODEL, N_EXPERTS, D_EXPERT

    def nrm(k, shape, scale):
        return jax.random.normal(k, shape, f32) * scale

    dt = jnp.exp(jax.random.uniform(ks[10], (L, 2, DN_HEADS), f32, math.log(1e-3), math.log(1e-1)))
    return {
        'x': nrm(ks[0], (BATCH, SEQ, D), 1.0),
        'c': nrm(ks[1], (BATCH, D), 1.0),
        'ctx': nrm(ks[2], (BATCH, CTX_LEN, D), 1.0),
        'c_ctx': nrm(ks[3], (D,), 1.0),
        'w_mod': nrm(ks[4], (L, D, 6 * D), 0.5 * D ** -0.5),
        'b_mod': nrm(ks[5], (L, 6 * D), 0.01),
        'norm1_w': 1.0 + nrm(ks[6], (L, D), 0.01),
        'w_in': nrm(ks[7], (L, D, D_IN), D ** -0.5),
        'na_rpb': nrm(ks[8], (L, NA_HEADS, 2 * NA_KH - 1, 2 * NA_KW - 1), 0.1),
        'dn_conv_w': nrm(ks[9], (L, DN_CONV, 3 * DN_WIDTH), DN_CONV ** -0.5),
        'dn_a_log': jnp.log(jax.random.uniform(ks[11], (L, 2, DN_HEADS), f32, 1.0, 16.0)),
        'dn_dt_bias': dt + jnp.log(-jnp.expm1(-dt)),
        'dn_norm_w': 1.0 + nrm(ks[12], (L, DN_HEAD_DIM), 0.01),
        'w_br_a': nrm(ks[13], (L, NA_WIDTH, D), NA_WIDTH ** -0.5),
        'w_br_b': nrm(ks[14], (L, DN_WIDTH, D), DN_WIDTH ** -0.5),
        'w_out': nrm(ks[15], (L, D, D), D ** -0.5),
        'norm2_w': 1.0 + nrm(ks[16], (L, D), 0.01),
        'w_router': nrm(ks[17], (L, D, E), D ** -0.5),
        'b_router': nrm(ks[18], (L, E), 0.01),
        'w1': nrm(ks[19], (L, E, D, 2 * F), D ** -0.5),
        'b1': nrm(ks[20], (L, E, 2 * F), 0.01),
        'w2': nrm(ks[21], (L, E, F, D), F ** -0.5),
        'b2': nrm(ks[22], (L, E, D), 0.01),
        'final_norm_w': 1.0 + nrm(ks[23], (D,), 0.01),
    }


def reference(x, c, ctx, c_ctx, w_mod, b_mod, norm1_w, w_in, na_rpb, dn_conv_w, dn_a_log, dn_dt_bias,
              dn_norm_w, w_br_a, w_br_b, w_out, norm2_w, w_router, b_router, w1, b1, w2, b2, final_norm_w):
    xl, xc = x, ctx
    for layer in range(DEPTH):
        last = layer == DEPTH - 1
        mod_l = (jax.nn.silu(c) @ w_mod[layer] + b_mod[layer])[:, None, :]
        mod_c = (jax.nn.silu(c_ctx) @ w_mod[layer] + b_mod[layer])[None, None, :]
        sh1_l, sc1_l, g1_l, sh2_l, sc2_l, g2_l = jnp.split(mod_l, 6, axis=-1)
        sh1_c, sc1_c, g1_c, sh2_c, sc2_c, g2_c = jnp.split(mod_c, 6, axis=-1)
        hl = modulate(rmsnorm(xl, norm1_w[layer]), sh1_l, sc1_l)
        hc = modulate(rmsnorm(xc, norm1_w[layer]), sh1_c, sc1_c)
        yl, yc = mixer_sublayer(hl, hc, w_in[layer], na_rpb[layer], dn_conv_w[layer], dn_a_log[layer],
                                dn_dt_bias[layer], dn_norm_w[layer], w_br_a[layer], w_br_b[layer],
                                w_out[layer], not last)
        xl = xl + g1_l * yl
        xl = xl + g2_l * moe_ffn(modulate(rmsnorm(xl, norm2_w[layer]), sh2_l, sc2_l), w_router[layer],
                                 b_router[layer], w1[layer], b1[layer], w2[layer], b2[layer])
        if not last:
            xc = xc + g1_c * yc
            xc = xc + g2_c * moe_ffn(modulate(rmsnorm(xc, norm2_w[layer]), sh2_c, sc2_c), w_router[layer],
                                     b_router[layer], w1[layer], b1[layer], w2[layer], b2[layer])
    return rmsnorm(xl, final_norm_w)
```

```python
import numpy as np
from contextlib import ExitStack
import concourse.bass as bass
import concourse.mybir as mybir
from concourse.bass_utils import run_bass_kernel_spmd

F32 = mybir.dt.float32
BF16 = mybir.dt.bfloat16
I32 = mybir.dt.int32
ALU = mybir.AluOpType
AF = mybir.ActivationFunctionType
AX = mybir.AxisListType

D = 1024
T = 2048
LC = 256
NT = T + LC
NBLK = 96
NEG = -1e30
EPS = 1e-6

C_ID, C_ONE, C_U, C_L, C_IF, C_SF, C_IB, C_SB, C_TRI, C_UT, C_BS, C_KR, C_TOK, C_N = 0, 128, 256, 320, 384, 448, 512, 576, 640, 672, 800, 896, 904, 920


class _StopScan(Exception):
    pass


class KB:
    NDMA = 16

    def __init__(self, nc, es):
        self.nc = nc
        self.eng = {'pe': nc.tensor, 'act': nc.scalar, 'dve': nc.vector, 'pool': nc.gpsimd, 'sp': nc.sync}
        self.sem = {}
        self.cnt = {}
        for e in ('pe', 'act', 'dve', 'pool'):
            self.sem[e] = es.enter_context(nc.semaphore('s_' + e))
            self.cnt[e] = 0
        self.dsem = {}
        self.dcnt = {}
        self.dnext = {}
        for q in ('sp', 'pool'):
            self.dsem[q] = [es.enter_context(nc.semaphore('d_%s%d' % (q, i))) for i in range(self.NDMA)]
            self.dcnt[q] = [0] * self.NDMA
            self.dnext[q] = 0
        self.seen = {e: {} for e in self.eng}
        self.lastw = {}
        self.readers = {}
        self.nins = 0
        self.limit = None
        self.stopped = False

    def _wait(self, e, ev):
        sem, val, src = ev
        if src == e and e == 'pe':
            return
        k = id(sem)
        if self.seen[e].get(k, 0) >= val:
            return
        self.eng[e].wait_ge(sem, val)
        self.seen[e][k] = val

    def _deps(self, e, reads, writes):
        for r in reads:
            ev = self.lastw.get(r)
            if ev is not None:
                self._wait(e, ev)
        for w in writes:
            ev = self.lastw.get(w)
            if ev is not None:
                self._wait(e, ev)
            for ev in self.readers.get(w, {}).values():
                self._wait(e, ev)

    def _record(self, ev, reads, writes):
        for r in reads:
            self.readers.setdefault(r, {})[id(ev[0])] = ev
        for w in writes:
            self.lastw[w] = ev
            self.readers[w] = {}

    def op(self, e, fn, reads=(), writes=()):
        if self.stopped:
            return None
        pr = [r for r in reads if isinstance(r, str) and r[:2] in ('pf', 'pb')]
        if pr:
            writes = list(writes) + pr
        self._deps(e, reads, writes)
        ins = fn(self.eng[e])
        self.cnt[e] += 1
        ins.then_inc(self.sem[e], 1)
        self._record((self.sem[e], self.cnt[e], e), reads, writes)
        self.nins += 1
        if self.limit is not None and self.nins >= self.limit:
            self.stopped = True
        return ins

    def dma(self, q, fn, reads=(), writes=()):
        if self.stopped:
            return None
        i = self.dnext[q]
        self.dnext[q] = (i + 1) % self.NDMA
        sem = self.dsem[q][i]
        if self.dcnt[q][i] > 0:
            self._wait(q, (sem, self.dcnt[q][i], 'dma'))
        self._deps(q, reads, writes)
        ins = fn(self.eng[q])
        self.dcnt[q][i] += 16
        ins.then_inc(sem, 16)
        self._record((sem, self.dcnt[q][i], 'dma'), reads, writes)
        self.nins += 1
        return ins

    def barrier(self):
        if self.stopped:
            return
        for e in self.eng:
            for e2 in ('pe', 'act', 'dve', 'pool'):
                if e2 != e and self.cnt[e2] > 0:
                    self._wait(e, (self.sem[e2], self.cnt[e2], e2))
            for q in self.dsem:
                for i in range(self.NDMA):
                    if self.dcnt[q][i] > 0:
                        self._wait(e, (self.dsem[q][i], self.dcnt[q][i], 'dma'))

    def wait_all(self, e, toks):
        if self.stopped:
            return
        for t in toks:
            if t in self.lastw:
                self._wait(e, self.lastw[t])


def bc(ap, shape):
    return ap.to_broadcast(shape)


def build(dbg=False, stop=None, sub=None):
    nc = bass.Bass("TRN2", target_bir_lowering=False)
    limit = int(sub[1:]) if (sub is not None and sub.startswith('n')) else None
    inp = lambda name, shape, dt=F32: nc.dram_tensor(name, shape, dt, kind="ExternalInput").ap()
    x_d = inp("x", [T, D])
    ctx_d = inp("ctx", [LC, D])
    c2_d = inp("c2", [2, D])
    wmod_d = inp("w_mod", [D, 6 * D])
    bmod_d = inp("b_mod", [1, 6 * D])
    n1_d = inp("norm1_w", [1, D])
    win_d = inp("w_in", [D, 5664])
    natbl_d = inp("na_tbl", [8, 2, 128, 1024])
    convw_d = inp("convw", [128, 12 * 5])
    alog_d = inp("alog", [1, 16])
    dtb_d = inp("dtb", [1, 16])
    dnw_d = inp("dnw", [1, 64])
    wbra_d = inp("w_br_a", [512, D])
    wbrb_d = inp("w_br_b", [512, D])
    wout_d = inp("w_out", [D, D])
    n2_d = inp("norm2_w", [1, D])
    wr_d = inp("w_router", [D, 32])
    br_d = inp("b_router", [1, 32])
    w1_d = inp("w1", [32 * D, 2048])
    b1t_d = inp("b1t", [32 * 128, 16])
    w2_d = inp("w2", [32 * D, D])
    b2_d = inp("b2", [32, D])
    fnw_d = inp("fnw", [1, D])
    rope_d = inp("rope", [64, 2 * 32 * 32])
    cst_d = inp("cst", [128, C_N])
    out_d = nc.dram_tensor("out", [T, D], F32, kind="ExternalOutput").ap()
    scr = lambda name, shape, dt=F32: nc.dram_tensor(name, shape, dt, kind="Internal").ap()
    qkv_s = scr("qkv_s", [36, 64, 1536])
    o_s = scr("o_s", [2, 32, 64, 512])
    xl2_s = scr("xl2_s", [T, D])
    h2_s = scr("h2_s", [T, D])
    slot_s = scr("slot_s", [NBLK * 128, 1], I32)
    ypad_s = scr("ypad_s", [NBLK * 128, D])
    dbg_d = {}
    if dbg:
        for nm, shp in (("d_hT", [128, 8 * NT]), ("d_ona", [128, 4 * T]), ("d_odn", [128, 4 * T]), ("d_lg", [128, 16 * 32])):
            dbg_d[nm] = nc.dram_tensor(nm, shp, F32, kind="ExternalOutput").ap()

    winv = win_d.rearrange("(k p) n -> p k n", p=128)

    with ExitStack() as es:
        kb = KB(nc, es)
        kb.limit = limit
        op, dma = kb.op, kb.dma

        def fin():
            with ExitStack() as sf:
                z = sf.enter_context(nc.sbuf_tensor("t_zout", [128, D], F32))
                op('dve', lambda e: e.memset(z[:], 0.0), writes=['zout'])
                for i in range(16):
                    dma('sp', lambda e, i=i: e.dma_start(out=out_d[i * 128:(i + 1) * 128, :], in_=z[:]), reads=['zout'], writes=[('out', i)])
                kb.barrier()
            print("instructions (stopped at %s):" % stop, kb.nins)
            return nc
        if True:
            G = lambda name, shape, dt=F32: es.enter_context(nc.sbuf_tensor("t_" + name, shape, dt))
            cst = G("cst", [128, C_N])
            identb = G("identb", [128, 128], BF16)
            MODL = G("MODL", [128, 6 * D])
            epsb = G("epsb", [128, 1])
            LG = G("LG", [128, 16, 32])
            mid = ExitStack()
            es.callback(mid.close)
            Gm = lambda name, shape, dt=F32: mid.enter_context(nc.sbuf_tensor("t_" + name, shape, dt))
            hT = Gm("hT", [128, 8, NT], BF16)
            onaT = Gm("onaT", [128, 4, T], BF16)
            odnT = Gm("odnT", [128, 4, T], BF16)

            dma('sp', lambda e: e.dma_start(out=cst[:], in_=cst_d[:, :]), writes=['cst'])
            op('dve', lambda e: e.tensor_copy(out=identb[:], in_=cst[:, C_ID:C_ID + 128]), reads=['cst'], writes=['identb'])
            op('dve', lambda e: e.memset(epsb[:], EPS), writes=['epsb'])
            ident = cst[:, C_ID:C_ID + 128]
            ones = cst[:, C_ONE:C_ONE + 128]

            def rstd_from_ssq(ssq_ap, rs_ap, scale, toks_r, toks_w):
                op('act', lambda e: e.activation(out=rs_ap, in_=ssq_ap, func=AF.Sqrt, scale=scale, bias=epsb[0:rs_ap.partition_size(), :]), reads=list(toks_r) + ['epsb'], writes=toks_w)
                op('dve', lambda e: e.reciprocal(out=rs_ap, in_=rs_ap), reads=toks_w, writes=toks_w)

            with ExitStack() as s1:
                S = lambda name, shape, dt=F32: s1.enter_context(nc.sbuf_tensor("t_" + name, shape, dt))
                PF = [s1.enter_context(nc.psum_tensor("pfA%d" % i, [128, 512], F32)) for i in range(2)]
                c2t = S("c2t", [128, 2, 8])
                MODC = S("MODC", [128, 2 * D])
                Lm = S("Lm", [128, 2, 8, 128], BF16)
                onesb = S("onesb", [128, 128], BF16)
                wm = [S("wm%d" % i, [128, 8, 512], BF16) for i in range(2)]
                nbc = S("nbc", [128, 2 * D])
                dma('sp', lambda e: e.dma_start(out=c2t[:], in_=c2_d.rearrange("t (p k) -> p t k", k=8)), writes=['c2t'])
                dma('sp', lambda e: e.dma_start(out=MODL[:], in_=bmod_d[0:1, :].partition_broadcast(128)), writes=['MODL'])
                dma('sp', lambda e: e.dma_start(out=MODC[:], in_=bmod_d[0:1, 0:2 * D].partition_broadcast(128)), writes=['MODC'])
                dma('sp', lambda e: e.dma_start(out=nbc[:, 0:D], in_=n1_d[0:1, :].partition_broadcast(128)), writes=['nbc0'])
                dma('sp', lambda e: e.dma_start(out=nbc[:, D:2 * D], in_=n2_d[0:1, :].partition_broadcast(128)), writes=['nbc1'])
                op('act', lambda e: e.activation(out=c2t[:], in_=c2t[:], func=AF.Silu), reads=['c2t'], writes=['c2t'])
                op('dve', lambda e: e.memset(onesb[:], 1.0), writes=['onesb'])
                for t in range(2):
                    for k in range(8):
                        op('dve', lambda e, t=t, k=k: e.tensor_scalar(out=Lm[:, t, k, :], in0=onesb[:], scalar1=c2t[:, t, k:k + 1], scalar2=None, op0=ALU.mult),
                           reads=['c2t', 'onesb'], writes=['Lm'])
                wmv = wmod_d.rearrange("(p k) n -> p k n", k=8)
                for j in range(12):
                    w = wm[j % 2]
                    wt = 'wm%d' % (j % 2)
                    dma('pool', lambda e, j=j, w=w: e.dma_start(out=w[:], in_=wmv[:, :, j * 512:(j + 1) * 512]), writes=[wt])
                    for t in range(2 if j < 4 else 1):
                        ps = PF[t]
                        for k in range(8):
                            op('pe', lambda e, t=t, k=k, w=w, ps=ps: e.matmul(out=ps[:], lhsT=Lm[:, t, k, :], rhs=w[:, k, :], start=(k == 0), stop=(k == 7)),
                               reads=['Lm', wt], writes=['pfA%d' % t])
                        M = MODL if t == 0 else MODC
                        op('dve', lambda e, M=M, j=j, ps=ps: e.tensor_tensor(out=M[:, j * 512:(j + 1) * 512], in0=M[:, j * 512:(j + 1) * 512], in1=ps[:], op=ALU.add),
                           reads=['pfA%d' % t, 'MODL', 'MODC'], writes=['MODL' if t == 0 else 'MODC'])
                op('dve', lambda e: e.scalar_tensor_tensor(out=MODL[:, D:2 * D], in0=MODL[:, D:2 * D], scalar=1.0, in1=nbc[:, 0:D], op0=ALU.add, op1=ALU.mult), reads=['MODL', 'nbc0'], writes=['MODL'])
                op('dve', lambda e: e.scalar_tensor_tensor(out=MODC[:, D:2 * D], in0=MODC[:, D:2 * D], scalar=1.0, in1=nbc[:, 0:D], op0=ALU.add, op1=ALU.mult), reads=['MODC', 'nbc0'], writes=['MODC'])
                op('dve', lambda e: e.scalar_tensor_tensor(out=MODL[:, 4 * D:5 * D], in0=MODL[:, 4 * D:5 * D], scalar=1.0, in1=nbc[:, D:2 * D], op0=ALU.add, op1=ALU.mult), reads=['MODL', 'nbc1'], writes=['MODL'])

                PB = [s1.enter_context(nc.psum_tensor("pbB%d" % i, [128, 8, 128], BF16)) for i in range(2)]
                xt = [S("xt%d" % i, [128, D]) for i in range(2)]
                junk = S("junk", [128, D])
                hb = [S("hb%d" % i, [128, D], BF16) for i in range(2)]
                ssq = S("ssq", [128, 18])
                rs = S("rs", [128, 18])
                for i in range(18):
                    xb = xt[i % 2]
                    xtk = 'xt%d' % (i % 2)
                    hbb = hb[i % 2]
                    hbk = 'hb%d' % (i % 2)
                    src = x_d[i * 128:(i + 1) * 128, :] if i < 16 else ctx_d[(i - 16) * 128:(i - 15) * 128, :]
                    M = MODL if i < 16 else MODC
                    Mk = 'MODL' if i < 16 else 'MODC'
                    dma('sp', lambda e, xb=xb, src=src: e.dma_start(out=xb[:], in_=src), writes=[xtk])
                    op('act', lambda e, xb=xb, i=i: e.activation(out=junk[:], in_=xb[:], func=AF.Square, accum_out=ssq[:, i:i + 1]), reads=[xtk], writes=['junk', 'ssq%d' % i])
                    rstd_from_ssq(ssq[:, i:i + 1], rs[:, i:i + 1], 1.0 / D, ['ssq%d' % i], ['rs%d' % i])
                    op('dve', lambda e, xb=xb, i=i, M=M: e.scalar_tensor_tensor(out=junk[:], in0=xb[:], scalar=rs[:, i:i + 1], in1=M[:, D:2 * D], op0=ALU.mult, op1=ALU.mult),
                       reads=[xtk, 'rs%d' % i, Mk], writes=['junk'])
                    op('dve', lambda e, hbb=hbb, M=M: e.tensor_tensor(out=hbb[:], in0=junk[:], in1=M[:, 0:D], op=ALU.add), reads=['junk', Mk], writes=[hbk])
                    pb = PB[i % 2]
                    pbk = 'pbB%d' % (i % 2)
                    for k in range(8):
                        op('pe', lambda e, pb=pb, hbb=hbb, k=k: e.transpose(out=pb[:, k, :], in_=hbb[:, k * 128:(k + 1) * 128], identity=identb[:]), reads=[hbk, 'identb'], writes=[pbk])
                    op('act', lambda e, pb=pb, i=i: e.copy(out=hT[:, :, i * 128:(i + 1) * 128], in_=pb[:]), reads=[pbk], writes=[('hT', i)])
                kb.barrier()
                if stop == 'B':
                    return fin()
            hT_all = [('hT', i) for i in range(18)]
            if dbg:
                with ExitStack() as s1:
                    tmp = s1.enter_context(nc.sbuf_tensor("dbgt", [128, 8 * NT], F32))
                    op('dve', lambda e: e.tensor_copy(out=tmp[:], in_=hT[:].rearrange("p k n -> p (k n)")), reads=hT_all, writes=['dbgt'])
                    dma('sp', lambda e: e.dma_start(out=dbg_d["d_hT"][:, :], in_=tmp[:]), reads=['dbgt'], writes=['d_hT'])
                    kb.barrier()

            with ExitStack() as s1:
                S = lambda name, shape, dt=F32: s1.enter_context(nc.sbuf_tensor("t_" + name, shape, dt))
                PF = [s1.enter_context(nc.psum_tensor("pfC%d" % i, [128, 512], F32)) for i in range(8)]
                wna = S("wna", [128, 8, 1536], BF16)
                qT = S("qT", [128, 4, T], BF16)
                kT = S("kT", [128, 4, NT], BF16)
                V = S("V", [128, 18, 512], BF16)
                onesb = S("onesbC", [128, 64], BF16)
                tb2 = [S("tb2_%d" % i, [128, 2, 1024], BF16) for i in range(2)]
                PT = [S("PT%d" % i, [128, 256], BF16) for i in range(4)]
                rcp = [S("rcp%d" % i, [128, 256]) for i in range(2)]
                op('dve', lambda e: e.memset(onesb[:], 1.0), writes=['onesbC'])
                for j in range(3):
                    dma('pool', lambda e, j=j: e.dma_start(out=wna[:, :, j * 512:(j + 1) * 512], in_=winv[:, :, j * 512:(j + 1) * 512]), writes=[('wna', j)])
                pfi = [0]

                def nextpf():
                    i = pfi[0] % 8
                    pfi[0] += 1
                    return PF[i], 'pfC%d' % i
                for which in range(2):
                    for p in range(4):
                        ntb = 4 if which == 0 else 5
                        for tb in range(ntb):
                            n0 = tb * 512
                            nn = 512 if tb < 4 else 256
                            ps, pk = nextpf()
                            for k in range(8):
                                op('pe', lambda e, ps=ps, k=k, which=which, p=p, n0=n0, nn=nn: e.matmul(out=ps[:, 0:nn], lhsT=wna[:, k, which * 512 + p * 128: which * 512 + (p + 1) * 128],
                                                                                                     rhs=hT[:, k, n0:n0 + nn], start=(k == 0), stop=(k == 7)),
                                   reads=[('wna', which)] + [('hT', t) for t in range(n0 // 128, (n0 + nn) // 128)], writes=[pk])
                            if which == 0:
                                op('act', lambda e, ps=ps, p=p, n0=n0: e.mul(out=qT[:, p, n0:n0 + 512], in_=ps[:], mul=0.125), reads=[pk], writes=[('qT', p, tb)])
                            else:
                                op('dve', lambda e, ps=ps, p=p, n0=n0, nn=nn: e.tensor_copy(out=kT[:, p, n0:n0 + nn], in_=ps[:, 0:nn]), reads=[pk], writes=[('kT', p, tb)])
                for i in range(18):
                    ps, pk = nextpf()
                    for k in range(8):
                        op('pe', lambda e, ps=ps, k=k, i=i: e.matmul(out=ps[:], lhsT=hT[:, k, i * 128:(i + 1) * 128], rhs=wna[:, k, 1024:1536], start=(k == 0), stop=(k == 7)),
                           reads=[('wna', 2), ('hT', i)], writes=[pk])
                    op('act', lambda e, ps=ps, i=i: e.copy(out=V[:, i, :], in_=ps[:]), reads=[pk], writes=[('V', i)])
                PST = PF[0:4]
                PPV = PF[4:6]
                PDN = PF[6:8]
                cnt = 0
                for h in range(8):
                    p, half = h // 2, h % 2
                    hs = slice(half * 64, half * 64 + 64)
                    tbt = tb2[h % 2]
                    tbk = 'tb2_%d' % (h % 2)
                    dma('pool', lambda e, tbt=tbt, h=h: e.dma_start(out=tbt[:], in_=natbl_d[h].rearrange("t p n -> p t n")), writes=[tbk])
                    for qb in range(8):
                        if qb == 0:
                            tiles, tsel = list(range(0, 4)), 0
                        elif qb == 7:
                            tiles, tsel = list(range(12, 16)), 0
                        else:
                            tiles, tsel = list(range(2 * qb - 2, 2 * qb + 4)), 1
                        keyt = [(m, 4 * qb - 2 * m + 7) for m in tiles] + [(16, None), (17, None)]
                        ppv, pdn = PPV[qb % 2], PDN[qb % 2]
                        ppk, pdk = 'pfC%d' % (4 + qb % 2), 'pfC%d' % (6 + qb % 2)
                        q0 = qb * 256
                        for ti, (m, j0) in enumerate(keyt):
                            pst, pstk = PST[cnt % 4], 'pfC%d' % (cnt % 4)
                            ptt, ptk = PT[cnt % 4], 'PT%d' % (cnt % 4)
                            cnt += 1
                            op('pe', lambda e, pst=pst, m=m, j0=j0: e.matmul(out=pst[:, 0:256], lhsT=kT[hs, p, m * 128:(m + 1) * 128], rhs=qT[hs, p, q0:q0 + 256], start=True, stop=(j0 is None)),
                               reads=[('kT', p, min(m // 4, 4)), ('qT', p, qb // 2)], writes=[pstk])
                            if j0 is not None:
                                op('pe', lambda e, pst=pst, j0=j0: e.matmul(out=pst[:, 0:256], lhsT=identb[:], rhs=tbt[:, tsel, j0 * 64:(j0 + 4) * 64], start=False, stop=True),
                                   reads=[tbk, 'identb'], writes=[pstk])
                            op('act', lambda e, pst=pst, ptt=ptt: e.activation(out=ptt[:], in_=pst[:, 0:256], func=AF.Exp), reads=[pstk], writes=[ptk])
                            first, last = ti == 0, ti == len(keyt) - 1
                            op('pe', lambda e, ptt=ptt, m=m, first=first, last=last: e.matmul(out=ppv[hs, 0:256], lhsT=V[:, m, h * 64:(h + 1) * 64], rhs=ptt[:], start=first, stop=last),
                               reads=[ptk, ('V', m)], writes=[ppk])
                            op('pe', lambda e, ptt=ptt, first=first, last=last: e.matmul(out=pdn[hs, 0:256], lhsT=onesb[:], rhs=ptt[:], start=first, stop=last),
                               reads=[ptk, 'onesbC'], writes=[pdk])
                        rc = rcp[qb % 2]
                        rck = 'rcp%d' % (qb % 2)
                        op('dve', lambda e, rc=rc, pdn=pdn: e.reciprocal(out=rc[hs, :], in_=pdn[hs, 0:256]), reads=[pdk], writes=[rck])
                        op('dve', lambda e, rc=rc, ppv=ppv: e.tensor_tensor(out=onaT[hs, p, q0:q0 + 256], in0=ppv[hs, 0:256], in1=rc[hs, :], op=ALU.mult), reads=[ppk, rck], writes=[('onaT', p, qb)])
                kb.barrier()
                if stop == 'C':
                    return fin()
            onaT_all = [('onaT', p, qb) for p in range(4) for qb in range(8)]
            if dbg:
                with ExitStack() as s1:
                    tmp = s1.enter_context(nc.sbuf_tensor("dbgt2", [128, 4 * T], F32))
                    op('dve', lambda e: e.tensor_copy(out=tmp[:], in_=onaT[:].rearrange("p k n -> p (k n)")), reads=onaT_all, writes=['dbgt2'])
                    dma('sp', lambda e: e.dma_start(out=dbg_d["d_ona"][:, :], in_=tmp[:]), reads=['dbgt2'], writes=['d_ona'])
                    kb.barrier()

            id64 = cst[0:64, C_ID:C_ID + 64]
            on64 = cst[0:64, C_ONE:C_ONE + 64]
            b3 = lambda ap8: ap8.unsqueeze(2).to_broadcast([64, 8, 64])
            m3 = lambda ap64: ap64.unsqueeze(1).to_broadcast([64, 8, 64])
            with ExitStack() as s1:
                S = lambda name, shape, dt=F32: s1.enter_context(nc.sbuf_tensor("t_" + name, shape, dt))
                PF = [s1.enter_context(nc.psum_tensor("pfD%d" % i, [128, 512], F32)) for i in range(8)]
                pfi = [0]

                def nextpf():
                    i = pfi[0] % 8
                    pfi[0] += 1
                    return PF[i], 'pfD%d' % i
                wdn = S("wdn", [128, 8, 1536], BF16)
                wba = S("wba", [128, 8, 32], BF16)
                convw = S("convw", [128, 60])
                ropet = S("ropet", [64, 2, 32, 32])
                zl = [S("zl%d" % i, [128, 2052]) for i in range(2)]
                zx = [S("zx%d" % i, [128, 260]) for i in range(2)]
                acc = S("acc", [128, NT])
                sl = S("sl", [128, NT])
                sqt = S("sqt", [64, 512])
                ssq8 = S("ssq8", [64, 8])
                r8 = S("r8", [64, 8])
                st = [S("st%d" % i, [64, 4, 128]) for i in range(2)]
                st2 = [S("st2%d" % i, [64, 4, 128]) for i in range(2)]
                ra = S("ra", [64, 4, 2, 32])
                rb = S("rb", [64, 4, 2, 32])
                BA = S("BA", [64, 36, 32])
                BETA = S("BETA", [64, 36, 16])
                GG = S("GG", [64, 36, 16])
                ta = S("ta", [64, 36, 16])
                tb_ = S("tb_", [64, 36, 16])
                tc_ = S("tc_", [64, 36, 16])
                ab = S("ab", [64, 32])
                one1 = S("one1", [64, 1])
                for j in range(3):
                    dma('pool', lambda e, j=j: e.dma_start(out=wdn[:, :, j * 512:(j + 1) * 512], in_=winv[:, :, 1536 + j * 512:1536 + (j + 1) * 512]), writes=[('wdn', j)])
                dma('pool', lambda e: e.dma_start(out=wba[:], in_=winv[:, :, 3584:3616]), writes=['wba'])
                dma('sp', lambda e: e.dma_start(out=convw[:], in_=convw_d[:, :]), writes=['convw'])
                dma('sp', lambda e: e.dma_start(out=ropet[:], in_=rope_d.rearrange("p (a c f) -> p a c f", a=2, c=32)), writes=['ropet'])
                dma('sp', lambda e: e.dma_start(out=ab[:, 0:16], in_=alog_d[0:1, :].partition_broadcast(64)), writes=['ab'])
                dma('sp', lambda e: e.dma_start(out=ab[:, 16:32], in_=dtb_d[0:1, :].partition_broadcast(64)), reads=[], writes=['ab2'])
                op('dve', lambda e: e.memset(one1[:], 1.0), writes=['one1'])
                for i in range(2):
                    op('dve', lambda e, i=i: e.memset(zl[i][:], 0.0), writes=['zl%d' % i])
                    op('dve', lambda e, i=i: e.memset(zx[i][:], 0.0), writes=['zx%d' % i])
                for g3 in range(3):
                    ps, pk = nextpf()
                    for c in range(12):
                        n = g3 * 12 + c
                        for k in range(8):
                            op('pe', lambda e, ps=ps, c=c, n=n, k=k: e.matmul(out=ps[0:64, c * 32:(c + 1) * 32], lhsT=hT[:, k, n * 64:(n + 1) * 64], rhs=wba[:, k, :], start=(k == 0), stop=(k == 7)),
                               reads=['wba', ('hT', n // 2)], writes=[pk])
                    op('act', lambda e, ps=ps, g3=g3: e.copy(out=BA[:, g3 * 12:(g3 + 1) * 12, :], in_=ps[0:64, 0:384].rearrange("p (c f) -> p c f", f=32)), reads=[pk], writes=['BA'])
                op('act', lambda e: e.activation(out=BETA[:], in_=BA[:, :, 0:16], func=AF.Sigmoid), reads=['BA'], writes=['BETA'])
                op('dve', lambda e: e.tensor_tensor(out=ta[:], in0=BA[:, :, 16:32], in1=ab[:, 16:32].unsqueeze(1).to_broadcast([64, 36, 16]), op=ALU.add), reads=['BA', 'ab2'], writes=['ta'])
                op('dve', lambda e: e.tensor_scalar(out=tb_[:], in0=ta[:], scalar1=-1.0, scalar2=None, op0=ALU.mult), reads=['ta'], writes=['tb_'])
                op('dve', lambda e: e.tensor_tensor(out=tb_[:], in0=tb_[:], in1=ta[:], op=ALU.max), reads=['ta', 'tb_'], writes=['tb_'])
                op('act', lambda e: e.activation(out=tb_[:], in_=tb_[:], func=AF.Exp, scale=-1.0), reads=['tb_'], writes=['tb_'])
                op('act', lambda e: e.activation(out=tb_[:], in_=tb_[:], func=AF.Ln, bias=one1[:]), reads=['tb_', 'one1'], writes=['tb_'])
                op('dve', lambda e: e.tensor_scalar_max(out=tc_[:], in0=ta[:], scalar1=0.0), reads=['ta'], writes=['tc_'])
                op('dve', lambda e: e.tensor_tensor(out=tc_[:], in0=tc_[:], in1=tb_[:], op=ALU.add), reads=['tc_', 'tb_'], writes=['tc_'])
                op('act', lambda e: e.activation(out=ab[:, 0:16], in_=ab[:, 0:16], func=AF.Exp), reads=['ab'], writes=['ab'])
                op('dve', lambda e: e.scalar_tensor_tensor(out=GG[:], in0=tc_[:], scalar=-1.0, in1=ab[:, 0:16].unsqueeze(1).to_broadcast([64, 36, 16]), op0=ALU.mult, op1=ALU.mult),
                   reads=['tc_', 'ab'], writes=['GG'])
                for cc in range(12):
                    z_l, z_x = zl[cc % 2], zx[cc % 2]
                    zlk, zxk = 'zl%d' % (cc % 2), 'zx%d' % (cc % 2)
                    for tb in range(5):
                        n0 = tb * 512
                        nn = 512 if tb < 4 else 256
                        ps, pk = nextpf()
                        for k in range(8):
                            op('pe', lambda e, ps=ps, k=k, n0=n0, nn=nn: e.matmul(out=ps[:, 0:nn], lhsT=wdn[:, k, cc * 128:(cc + 1) * 128], rhs=hT[:, k, n0:n0 + nn], start=(k == 0), stop=(k == 7)),
                               reads=[('wdn', cc // 4)] + [('hT', t) for t in range(n0 // 128, (n0 + nn) // 128)], writes=[pk])
                        if tb < 4:
                            op('act', lambda e, ps=ps, n0=n0: e.copy(out=z_l[:, 2 + n0:2 + n0 + 512], in_=ps[:]), reads=[pk], writes=[zlk])
                        else:
                            op('act', lambda e, ps=ps: e.copy(out=z_x[:, 2:258], in_=ps[:, 0:256]), reads=[pk], writes=[zxk])
                    for (zs, zk, a0, n) in ((z_l, zlk, 0, 2048), (z_x, zxk, 2048, 256)):
                        op('dve', lambda e, zs=zs, a0=a0, n=n: e.tensor_scalar(out=acc[:, a0:a0 + n], in0=zs[:, 0:n], scalar1=convw[:, cc * 5:cc * 5 + 1], scalar2=None, op0=ALU.mult),
                           reads=[zk, 'convw'], writes=['acc'])
                        for tap in range(1, 5):
                            op('dve', lambda e, zs=zs, a0=a0, n=n, tap=tap: e.scalar_tensor_tensor(out=acc[:, a0:a0 + n], in0=zs[:, tap:tap + n], scalar=convw[:, cc * 5 + tap:cc * 5 + tap + 1],
                                                                                               in1=acc[:, a0:a0 + n], op0=ALU.mult, op1=ALU.add), reads=[zk, 'convw', 'acc'], writes=['acc'])
                    op('act', lambda e: e.activation(out=sl[:], in_=acc[:], func=AF.Silu), reads=['acc'], writes=['sl'])
                    for g in range(9):
                        ps, pk = nextpf()
                        for c4 in range(4):
                            n = 4 * g + c4
                            op('pe', lambda e, ps=ps, c4=c4, n=n: e.transpose(out=ps[0:64, c4 * 128:(c4 + 1) * 128], in_=sl[:, n * 64:(n + 1) * 64], identity=ident), reads=['sl', 'cst'], writes=[pk])
                        sti, stk = st[g % 2], 'st%d' % (g % 2)
                        if cc < 8:
                            op('act', lambda e, ps=ps: e.activation(out=sqt[:], in_=ps[0:64, :], func=AF.Square), reads=[pk], writes=['sqt'])
                            op('dve', lambda e: e.tensor_reduce(out=ssq8[:], in_=sqt[:].rearrange("p (g f) -> p g f", f=64), axis=AX.X, op=ALU.add), reads=['sqt'], writes=['ssq8'])
                            rstd_from_ssq(ssq8[:], r8[:], 1.0, ['ssq8'], ['r8'])
                            op('dve', lambda e, ps=ps, sti=sti: e.tensor_tensor(out=sti[:].rearrange("p c (h f) -> p (c h) f", h=2), in0=ps[0:64, :].rearrange("p (g f) -> p g f", f=64),
                                                                            in1=b3(r8[:]), op=ALU.mult), reads=[pk, 'r8'], writes=[stk])
                            if g < 8:
                                s2i, s2k = st2[g % 2], 'st2%d' % (g % 2)
                                x5 = sti[:].rearrange("p c (h t f) -> p c h t f", h=2, t=2)
                                o5 = s2i[:].rearrange("p c (h t f) -> p c h t f", h=2, t=2)
                                cosb = ropet[:, 0, 4 * g:4 * g + 4, :].unsqueeze(2).to_broadcast([64, 4, 2, 32])
                                sinb = ropet[:, 1, 4 * g:4 * g + 4, :].unsqueeze(2).to_broadcast([64, 4, 2, 32])
                                op('dve', lambda e: e.tensor_tensor(out=ra[:], in0=x5[:, :, :, 0, :], in1=cosb, op=ALU.mult), reads=[stk, 'ropet'], writes=['ra'])
                                op('pool', lambda e: e.tensor_tensor(out=rb[:], in0=x5[:, :, :, 1, :], in1=sinb, op=ALU.mult), reads=[stk, 'ropet'], writes=['rb'])
                                op('dve', lambda e: e.tensor_tensor(out=o5[:, :, :, 0, :], in0=ra[:], in1=rb[:], op=ALU.subtract), reads=['ra', 'rb'], writes=[s2k])
                                op('dve', lambda e: e.tensor_tensor(out=ra[:], in0=x5[:, :, :, 0, :], in1=sinb, op=ALU.mult), reads=[stk, 'ropet'], writes=['ra'])
                                op('pool', lambda e: e.tensor_tensor(out=rb[:], in0=x5[:, :, :, 1, :], in1=cosb, op=ALU.mult), reads=[stk, 'ropet'], writes=['rb'])
                                op('dve', lambda e: e.tensor_tensor(out=o5[:, :, :, 1, :], in0=ra[:], in1=rb[:], op=ALU.add), reads=['ra', 'rb'], writes=[s2k])
                                sti, stk = s2i, s2k
                        else:
                            op('act', lambda e, ps=ps, sti=sti: e.copy(out=sti[:].rearrange("p c f -> p (c f)"), in_=ps[0:64, :]), reads=[pk], writes=[stk])
                        dma('sp', lambda e, sti=sti, g=g: e.dma_start(out=qkv_s[4 * g:4 * g + 4, :, cc * 128:(cc + 1) * 128].rearrange("c p f -> p c f"), in_=sti[:]), reads=[stk], writes=[('qkv_s', g)])
                kb.barrier()
                if stop == 'D1':
                    return fin()
            with ExitStack() as s1:
                S = lambda name, shape, dt=F32: s1.enter_context(nc.sbuf_tensor("t_" + name, shape, dt))
                PF = [s1.enter_context(nc.psum_tensor("pfE%d" % i, [128, 512], F32)) for i in range(8)]
                pfi = [0]

                def nextpf():
                    i = pfi[0] % 8
                    pfi[0] += 1
                    return PF[i], 'pfE%d' % i
                wba = S("wba2", [128, 8, 32], BF16)
                BA = S("BA2", [64, 36, 32])
                BETA = S("BETA2", [64, 36, 16])
                GG = S("GG2", [64, 36, 16])
                ta = S("ta2", [64, 36, 16])
                tb_ = S("tb2_", [64, 36, 16])
                tc_ = S("tc2_", [64, 36, 16])
                ab = S("ab2", [64, 32])
                one1 = S("one12", [64, 1])
                dma('pool', lambda e: e.dma_start(out=wba[:], in_=winv[:, :, 3584:3616]), writes=['wba'])
                dma('sp', lambda e: e.dma_start(out=ab[:, 0:16], in_=alog_d[0:1, :].partition_broadcast(64)), writes=['ab'])
                dma('sp', lambda e: e.dma_start(out=ab[:, 16:32], in_=dtb_d[0:1, :].partition_broadcast(64)), reads=[], writes=['ab2'])
                op('dve', lambda e: e.memset(one1[:], 1.0), writes=['one1'])
                for g3 in range(3):
                    ps, pk = nextpf()
                    for c in range(12):
                        n = g3 * 12 + c
                        for k in range(8):
                            op('pe', lambda e, ps=ps, c=c, n=n, k=k: e.matmul(out=ps[0:64, c * 32:(c + 1) * 32], lhsT=hT[:, k, n * 64:(n + 1) * 64], rhs=wba[:, k, :], start=(k == 0), stop=(k == 7)),
                               reads=['wba', ('hT', n // 2)], writes=[pk])
                    op('act', lambda e, ps=ps, g3=g3: e.copy(out=BA[:, g3 * 12:(g3 + 1) * 12, :], in_=ps[0:64, 0:384].rearrange("p (c f) -> p c f", f=32)), reads=[pk], writes=['BA'])
                op('act', lambda e: e.activation(out=BETA[:], in_=BA[:, :, 0:16], func=AF.Sigmoid), reads=['BA'], writes=['BETA'])
                op('dve', lambda e: e.tensor_tensor(out=ta[:], in0=BA[:, :, 16:32], in1=ab[:, 16:32].unsqueeze(1).to_broadcast([64, 36, 16]), op=ALU.add), reads=['BA', 'ab2'], writes=['ta'])
                op('dve', lambda e: e.tensor_scalar(out=tb_[:], in0=ta[:], scalar1=-1.0, scalar2=None, op0=ALU.mult), reads=['ta'], writes=['tb_'])
                op('dve', lambda e: e.tensor_tensor(out=tb_[:], in0=tb_[:], in1=ta[:], op=ALU.max), reads=['ta', 'tb_'], writes=['tb_'])
                op('act', lambda e: e.activation(out=tb_[:], in_=tb_[:], func=AF.Exp, scale=-1.0), reads=['tb_'], writes=['tb_'])
                op('act', lambda e: e.activation(out=tb_[:], in_=tb_[:], func=AF.Ln, bias=one1[:]), reads=['tb_', 'one1'], writes=['tb_'])
                op('dve', lambda e: e.tensor_scalar_max(out=tc_[:], in0=ta[:], scalar1=0.0), reads=['ta'], writes=['tc_'])
                op('dve', lambda e: e.tensor_tensor(out=tc_[:], in0=tc_[:], in1=tb_[:], op=ALU.add), reads=['tc_', 'tb_'], writes=['tc_'])
                op('act', lambda e: e.activation(out=ab[:, 0:16], in_=ab[:, 0:16], func=AF.Exp), reads=['ab'], writes=['ab'])
                op('dve', lambda e: e.scalar_tensor_tensor(out=GG[:], in0=tc_[:], scalar=-1.0, in1=ab[:, 0:16].unsqueeze(1).to_broadcast([64, 36, 16]), op0=ALU.mult, op1=ALU.mult),
                   reads=['tc_', 'ab'], writes=['GG'])

                ld = [S("ld%d" % i, [64, 1536]) for i in range(2)]
                names = ["gc", "gl", "et", "egl", "eg", "bg", "nb", "t8"]
                sm = {n_: S("sm_" + n_, [64, 8]) for n_ in names}
                bigs = ["Rt", "egrow", "Dm", "Dmi", "Dms", "qdT", "NegA", "QKm", "X0", "QKT", "Z", "Xa", "XTa", "Xb", "XTb", "vb", "kbg", "ktail", "u", "wT", "vnew", "Sst", "ost"]
                bg_ = {n_: S("bg_" + n_, [64, 8, 64]) for n_ in bigs}
                qkT = S("qkT", [64, 16, 64])
                fl = lambda t_: t_[:].rearrange("p h f -> p (h f)")

                def headmm(lhs, rhs, reads, start=True, stop=True, ps=None, pk=None):
                    if ps is None:
                        ps, pk = nextpf()
                    for h in range(8):
                        op('pe', lambda e, h=h: e.matmul(out=ps[0:64, h * 64:(h + 1) * 64], lhsT=lhs(h), rhs=rhs(h), start=start, stop=stop), reads=reads, writes=[pk])
                    return ps, pk

                def headtr(src, srck):
                    ps, pk = nextpf()
                    for h in range(8):
                        op('pe', lambda e, h=h: e.transpose(out=ps[0:64, h * 64:(h + 1) * 64], in_=src(h), identity=id64), reads=[srck, 'cst'], writes=[pk])
                    return ps, pk
                p3 = lambda ps: ps[0:64, :].rearrange("p (h f) -> p h f", f=64)
                B = bg_
                if sub == 'g':
                    kb.barrier()
                    return fin()
                for d in range(2 if sub is None else 1):
                    CU = cst[0:64, C_U:C_U + 64] if d == 0 else cst[0:64, C_L:C_L + 64]
                    incl = cst[0:64, C_IF:C_IF + 64] if d == 0 else cst[0:64, C_IB:C_IB + 64]
                    strict = cst[0:64, C_SF:C_SF + 64] if d == 0 else cst[0:64, C_SB:C_SB + 64]
                    last = 63 if d == 0 else 0
                    order = ([32, 33, 34, 35] + list(range(32))) if d == 0 else ([35, 34, 33, 32] + list(range(31, -1, -1)))
                    op('dve', lambda e: e.memset(B["Sst"][:], 0.0), writes=['Sst'])
                    for it, n in enumerate(order):
                        L_, lk = ld[it % 2], 'ld%d' % (it % 2)
                        dma('sp', lambda e, L_=L_, n=n: e.dma_start(out=L_[:], in_=qkv_s[n, :, :]), reads=[('qkv_s', n // 4)], writes=[lk])
                        q3 = L_[:, 0:512].rearrange("p (h f) -> p h f", f=64)
                        k3 = L_[:, 512:1024].rearrange("p (h f) -> p h f", f=64)
                        v3 = L_[:, 1024:1536].rearrange("p (h f) -> p h f", f=64)
                        g = GG[:, n, d * 8:(d + 1) * 8]
                        beta = BETA[:, n, d * 8:(d + 1) * 8]
                        ps, pk = nextpf()
                        op('pe', lambda e, ps=ps: e.matmul(out=ps[0:64, 0:8], lhsT=CU, rhs=g, start=True, stop=True), reads=['GG', 'cst'], writes=[pk])
                        op('dve', lambda e, ps=ps: e.tensor_copy(out=sm["gc"][:], in_=ps[0:64, 0:8]), reads=[pk], writes=['gc'])
                        if sub == 's1':
                            kb.barrier()
                            return fin()
                        op('dve', lambda e: e.tensor_tensor(out=B["Rt"][:], in0=m3(id64), in1=b3(sm["gc"][:]), op=ALU.mult), reads=['gc', 'cst'], writes=['Rt'])
                        psA, pkA = nextpf()
                        op('pe', lambda e: e.matmul(out=psA[0:64, :], lhsT=on64, rhs=fl(B["Rt"]), start=True, stop=True), reads=['Rt', 'cst'], writes=[pkA])
                        op('act', lambda e: e.activation(out=fl(B["egrow"]), in_=psA[0:64, :], func=AF.Exp), reads=[pkA], writes=['egrow'])
                        op('dve', lambda e: e.tensor_tensor(out=B["Dm"][:], in0=b3(sm["gc"][:]), in1=p3(psA), op=ALU.subtract), reads=[pkA, 'gc'], writes=['Dm'])
                        op('act', lambda e: e.copy(out=sm["gl"][:], in_=p3(psA)[:, :, last]), reads=[pkA], writes=['gl'])
                        op('dve', lambda e: e.tensor_scalar_min(out=B["Dm"][:], in0=B["Dm"][:], scalar1=0.0), reads=['Dm'], writes=['Dm'])
                        op('act', lambda e: e.activation(out=B["Dm"][:], in_=B["Dm"][:], func=AF.Exp), reads=['Dm'], writes=['Dm'])
                        op('pool', lambda e: e.tensor_tensor(out=B["Dmi"][:], in0=B["Dm"][:], in1=m3(incl), op=ALU.mult), reads=['Dm', 'cst'], writes=['Dmi'])
                        op('pool', lambda e: e.tensor_tensor(out=B["Dms"][:], in0=B["Dm"][:], in1=m3(strict), op=ALU.mult), reads=['Dm', 'cst'], writes=['Dms'])
                        op('dve', lambda e: e.tensor_tensor(out=sm["t8"][:], in0=sm["gl"][:], in1=sm["gc"][:], op=ALU.subtract), reads=['gl', 'gc'], writes=['t8'])
                        op('act', lambda e: e.activation(out=sm["et"][:], in_=sm["t8"][:], func=AF.Exp), reads=['t8'], writes=['et'])
                        op('act', lambda e: e.activation(out=sm["egl"][:], in_=sm["gl"][:], func=AF.Exp), reads=['gl'], writes=['egl'])
                        op('act', lambda e: e.activation(out=sm["eg"][:], in_=sm["gc"][:], func=AF.Exp), reads=['gc'], writes=['eg'])
                        op('dve', lambda e: e.tensor_tensor(out=sm["bg"][:], in0=sm["eg"][:], in1=beta, op=ALU.mult), reads=['eg', 'BETA'], writes=['bg'])
                        op('dve', lambda e: e.tensor_scalar(out=sm["nb"][:], in0=beta, scalar1=-1.0, scalar2=None, op0=ALU.mult), reads=['BETA'], writes=['nb'])
                        if sub == 's2':
                            kb.barrier()
                            return fin()
                        psQ, pkQ = headtr(lambda h: q3[:, h, :], lk)
                        psK, pkK = headtr(lambda h: k3[:, h, :], lk)
                        op('act', lambda e: e.copy(out=qkT[:, 0:8, :], in_=p3(psQ)), reads=[pkQ], writes=['qT_'])
                        op('dve', lambda e: e.tensor_copy(out=qkT[:, 8:16, :], in_=p3(psK)), reads=[pkK], writes=['kT_'])
                        op('dve', lambda e: e.scalar_tensor_tensor(out=B["qdT"][:], in0=qkT[:, 0:8, :], scalar=0.125, in1=B["egrow"][:], op0=ALU.mult, op1=ALU.mult), reads=['qT_', 'egrow'], writes=['qdT'])
                        if sub == 's3':
                            kb.barrier()
                            return fin()
                        psB, pkB = headmm(lambda h: qkT[:, 8 + h, :], lambda h: qkT[:, 8 + h, :], ['kT_'])
                        psC, pkC = headmm(lambda h: qkT[:, h, :], lambda h: qkT[:, 8 + h, :], ['kT_', 'qT_'])
                        op('dve', lambda e: e.tensor_tensor(out=B["NegA"][:], in0=p3(psB), in1=B["Dms"][:], op=ALU.mult), reads=[pkB, 'Dms'], writes=['NegA'])
                        op('dve', lambda e: e.tensor_tensor(out=B["NegA"][:], in0=B["NegA"][:], in1=b3(sm["nb"][:]), op=ALU.mult), reads=['NegA', 'nb'], writes=['NegA'])
                        op('dve', lambda e: e.scalar_tensor_tensor(out=B["QKm"][:], in0=p3(psC), scalar=0.125, in1=B["Dmi"][:], op0=ALU.mult, op1=ALU.mult), reads=[pkC, 'Dmi'], writes=['QKm'])
                        if sub == 's4':
                            kb.barrier()
                            return fin()
                        psD, pkD = headtr(lambda h: B["NegA"][:, h, :], 'NegA')
                        psE, pkE = headtr(lambda h: B["QKm"][:, h, :], 'QKm')
                        op('act', lambda e: e.copy(out=B["X0"][:], in_=p3(psD)), reads=[pkD], writes=['X0'])
                        op('dve', lambda e: e.tensor_tensor(out=B["Z"][:], in0=p3(psD), in1=m3(id64), op=ALU.add), reads=[pkD, 'cst'], writes=['Z'])
                        op('act', lambda e: e.copy(out=B["QKT"][:], in_=p3(psE)), reads=[pkE], writes=['QKT'])
                        if sub == 's5':
                            kb.barrier()
                            return fin()
                        X, Xk, XT, XTk = B["X0"], 'X0', B["NegA"], 'NegA'
                        for lvl in range(1, 6):
                            nX, nXk = (B["Xa"], 'Xa') if lvl % 2 == 1 else (B["Xb"], 'Xb')
                            nXT, nXTk = (B["XTa"], 'XTa') if lvl % 2 == 1 else (B["XTb"], 'XTb')
                            ps1, pk1 = headmm(lambda h, X=X: X[:, h, :], lambda h, XT=XT: XT[:, h, :], [Xk, XTk])
                            op('act', lambda e, ps1=ps1, nXT=nXT: e.copy(out=nXT[:], in_=p3(ps1)), reads=[pk1], writes=[nXTk])
                            if lvl < 5:
                                ps2, pk2 = headmm(lambda h, XT=XT: XT[:, h, :], lambda h, X=X: X[:, h, :], [Xk, XTk])
                                op('dve', lambda e, ps2=ps2, nX=nX: e.tensor_copy(out=nX[:], in_=p3(ps2)), reads=[pk2], writes=[nXk])
                            ps3, pk3 = headmm(lambda h, nXT=nXT: nXT[:, h, :], lambda h: B["Z"][:, h, :], [nXTk, 'Z'])
                            op('dve', lambda e, ps3=ps3: e.tensor_tensor(out=B["Z"][:], in0=B["Z"][:], in1=p3(ps3), op=ALU.add), reads=[pk3, 'Z'], writes=['Z'])
                            X, Xk, XT, XTk = nX, nXk, nXT, nXTk
                        if sub == 's6':
                            kb.barrier()
                            return fin()
                        op('pool', lambda e: e.tensor_tensor(out=B["vb"][:], in0=v3, in1=b3(beta), op=ALU.mult), reads=[lk, 'BETA'], writes=['vb'])
                        op('pool', lambda e: e.tensor_tensor(out=B["kbg"][:], in0=k3, in1=b3(sm["bg"][:]), op=ALU.mult), reads=[lk, 'bg'], writes=['kbg'])
                        op('pool', lambda e: e.tensor_tensor(out=B["ktail"][:], in0=k3, in1=b3(sm["et"][:]), op=ALU.mult), reads=[lk, 'et'], writes=['ktail'])
                        psU, pkU = headmm(lambda h: B["Z"][:, h, :], lambda h: B["vb"][:, h, :], ['Z', 'vb'])
                        op('act', lambda e: e.copy(out=B["u"][:], in_=p3(psU)), reads=[pkU], writes=['u'])
                        psW, pkW = headmm(lambda h: B["kbg"][:, h, :], lambda h: B["Z"][:, h, :], ['Z', 'kbg'])
                        op('act', lambda e: e.copy(out=B["wT"][:], in_=p3(psW)), reads=[pkW], writes=['wT'])
                        if sub == 's7':
                            kb.barrier()
                            return fin()
                        psP, pkP = headmm(lambda h: B["wT"][:, h, :], lambda h: B["Sst"][:, h, :], ['wT', 'Sst'])
                        op('dve', lambda e: e.tensor_tensor(out=B["vnew"][:], in0=B["u"][:], in1=p3(psP), op=ALU.subtract), reads=[pkP, 'u'], writes=['vnew'])
                        if n < 32:
                            psO, pkO = nextpf()
                            for h in range(8):
                                op('pe', lambda e, h=h: e.matmul(out=psO[0:64, h * 64:(h + 1) * 64], lhsT=B["qdT"][:, h, :], rhs=B["Sst"][:, h, :], start=True, stop=False), reads=['qdT', 'Sst'], writes=[pkO])
                                op('pe', lambda e, h=h: e.matmul(out=psO[0:64, h * 64:(h + 1) * 64], lhsT=B["QKT"][:, h, :], rhs=B["vnew"][:, h, :], start=False, stop=True), reads=['QKT', 'vnew'], writes=[pkO])
                            op('act', lambda e: e.copy(out=B["ost"][:], in_=p3(psO)), reads=[pkO], writes=['ost'])
                            dma('sp', lambda e, n=n: e.dma_start(out=o_s[d, n, :, :], in_=fl(B["ost"])), reads=['ost'], writes=[('o_s', d, n)])
                        psS, pkS = headmm(lambda h: B["ktail"][:, h, :], lambda h: B["vnew"][:, h, :], ['ktail', 'vnew'])
                        op('dve', lambda e: e.tensor_tensor(out=B["Sst"][:], in0=B["Sst"][:], in1=b3(sm["egl"][:]), op=ALU.mult), reads=['Sst', 'egl'], writes=['Sst'])
                        op('dve', lambda e: e.tensor_tensor(out=B["Sst"][:], in0=B["Sst"][:], in1=p3(psS), op=ALU.add), reads=['Sst', pkS], writes=['Sst'])
                        if sub is not None and sub.startswith('it') and it + 1 == int(sub[2:]):
                            kb.barrier()
                            return fin()
                kb.barrier()
                if stop == 'D2':
                    return fin()
            with ExitStack() as s1:
                S = lambda name, shape, dt=F32: s1.enter_context(nc.sbuf_tensor("t_" + name, shape, dt))
                PF = [s1.enter_context(nc.psum_tensor("pfF%d" % i, [128, 512], F32)) for i in range(4)]
                PB = [s1.enter_context(nc.psum_tensor("pbF%d" % i, [128, 4, 256], BF16)) for i in range(2)]
                wdg = S("wdg", [128, 8, 512], BF16)
                dnwb = S("dnwb", [64, 64])
                of = [S("of%d" % i, [64, 512]) for i in range(2)]
                ob = [S("ob%d" % i, [64, 512]) for i in range(2)]
                sq = S("sqF", [64, 512])
                s8 = S("s8F", [64, 8])
                r8 = S("r8F", [64, 8])
                sg = S("sgF", [64, 512])
                odb = [S("odb%d" % i, [64, 512], BF16) for i in range(2)]
                dma('pool', lambda e: e.dma_start(out=wdg[:], in_=winv[:, :, 3072:3584]), writes=['wdg'])
                dma('sp', lambda e: e.dma_start(out=dnwb[:], in_=dnw_d[0:1, :].partition_broadcast(64)), writes=['dnwb'])
                for n in range(32):
                    a, ak = of[n % 2], 'of%d' % (n % 2)
                    b_, bk = ob[n % 2], 'ob%d' % (n % 2)
                    dma('sp', lambda e, a=a, n=n: e.dma_start(out=a[:], in_=o_s[0, n, :, :]), reads=[('o_s', 0, n)], writes=[ak])
                    dma('sp', lambda e, b_=b_, n=n: e.dma_start(out=b_[:], in_=o_s[1, n, :, :]), reads=[('o_s', 1, n)], writes=[bk])
                    op('dve', lambda e, a=a, b_=b_: e.tensor_tensor(out=a[:], in0=a[:], in1=b_[:], op=ALU.add), reads=[ak, bk], writes=[ak])
                    op('act', lambda e, a=a: e.activation(out=sq[:], in_=a[:], func=AF.Square), reads=[ak], writes=['sqF'])
                    op('dve', lambda e: e.tensor_reduce(out=s8[:], in_=sq[:].rearrange("p (g f) -> p g f", f=64), axis=AX.X, op=ALU.add), reads=['sqF'], writes=['s8F'])
                    rstd_from_ssq(s8[:], r8[:], 1.0 / 64, ['s8F'], ['r8F'])
                    a3 = a[:].rearrange("p (g f) -> p g f", f=64)
                    op('dve', lambda e, a3=a3: e.tensor_tensor(out=a3, in0=a3, in1=b3(r8[:]), op=ALU.mult), reads=[ak, 'r8F'], writes=[ak])
                    op('pool', lambda e, a3=a3: e.tensor_tensor(out=a3, in0=a3, in1=m3(dnwb[:]), op=ALU.mult), reads=[ak, 'dnwb'], writes=[ak])
                    ps, pk = PF[n % 4], 'pfF%d' % (n % 4)
                    for k in range(8):
                        op('pe', lambda e, ps=ps, k=k, n=n: e.matmul(out=ps[0:64, :], lhsT=hT[:, k, n * 64:(n + 1) * 64], rhs=wdg[:, k, :], start=(k == 0), stop=(k == 7)), reads=['wdg', ('hT', n // 2)], writes=[pk])
                    op('act', lambda e, ps=ps: e.activation(out=sg[:], in_=ps[0:64, :], func=AF.Silu), reads=[pk], writes=['sgF'])
                    o_, ok_ = odb[n % 2], 'odb%d' % (n % 2)
                    op('dve', lambda e, a=a, o_=o_: e.tensor_tensor(out=o_[:], in0=a[:], in1=sg[:], op=ALU.mult), reads=[ak, 'sgF'], writes=[ok_])
                    pb, pbk = PB[n % 2], 'pbF%d' % (n % 2)
                    for c in range(4):
                        op('pe', lambda e, pb=pb, o_=o_, c=c: e.transpose(out=pb[:, c, 0:64], in_=o_[:, c * 128:(c + 1) * 128], identity=identb[0:64, 0:64]), reads=[ok_, 'identb'], writes=[pbk])
                    op('act', lambda e, pb=pb, n=n: e.copy(out=odnT[:, :, n * 64:(n + 1) * 64], in_=pb[:, :, 0:64]), reads=[pbk], writes=[('odnT', n)])
                kb.barrier()
                if stop == 'D3':
                    return fin()
            if dbg:
                with ExitStack() as s1:
                    tmp = s1.enter_context(nc.sbuf_tensor("dbgt3", [128, 4 * T], F32))
                    op('dve', lambda e: e.tensor_copy(out=tmp[:], in_=odnT[:].rearrange("p k n -> p (k n)")), reads=[('odnT', n) for n in range(32)], writes=['dbgt3'])
                    dma('sp', lambda e: e.dma_start(out=dbg_d["d_odn"][:, :], in_=tmp[:]), reads=['dbgt3'], writes=['d_odn'])
                    kb.barrier()

            with ExitStack() as s1:
                S = lambda name, shape, dt=F32: s1.enter_context(nc.sbuf_tensor("t_" + name, shape, dt))
                PF = [s1.enter_context(nc.psum_tensor("pfG%d" % i, [128, 512], F32)) for i in range(8)]
                pfi = [0]

                def nextpf():
                    i = pfi[0] % 8
                    pfi[0] += 1
                    return PF[i], 'pfG%d' % i
                wg = S("wg", [128, 8, 2048], BF16)
                wa = S("wa", [128, 4, D], BF16)
                wb = S("wb", [128, 4, D], BF16)
                wo = S("wo", [128, 8, D], BF16)
                wr = S("wr", [128, 8, 32])
                brb = S("brb", [128, 32])
                yT = S("yT", [128, 8, 512], BF16)
                sga = S("sga", [128, 512])
                sgb = S("sgb", [128, 512])
                xm = [S("xm0", [128, D])] * 2
                xo = [S("xo%d" % i, [128, D]) for i in range(2)]
                h2 = [S("h2_%d" % i, [128, D]) for i in range(2)]
                h2T = S("h2T", [128, 8, 128])
                junk = S("junkE", [128, D])
                ssq = S("ssqE", [128, 16])
                rs = S("rsE", [128, 16])
                for j in range(4):
                    dma('pool', lambda e, j=j: e.dma_start(out=wg[:, :, j * 512:(j + 1) * 512], in_=winv[:, :, 3616 + j * 512:3616 + (j + 1) * 512]), writes=[('wg', j)])
                dma('pool', lambda e: e.dma_start(out=wa[:], in_=wbra_d.rearrange("(k p) n -> p k n", p=128)), writes=['wa'])
                dma('pool', lambda e: e.dma_start(out=wb[:], in_=wbrb_d.rearrange("(k p) n -> p k n", p=128)), writes=['wb'])
                for j in range(2):
                    dma('pool', lambda e, j=j: e.dma_start(out=wo[:, :, j * 512:(j + 1) * 512], in_=wout_d.rearrange("(k p) n -> p k n", p=128)[:, :, j * 512:(j + 1) * 512]), writes=[('wo', j)])
                dma('sp', lambda e: e.dma_start(out=wr[:], in_=wr_d.rearrange("(k p) n -> p k n", p=128)), writes=['wr'])
                dma('sp', lambda e: e.dma_start(out=brb[:], in_=br_d[0:1, :].partition_broadcast(128)), writes=['brb'])
                for tb in range(4):
                    n0 = tb * 512
                    hts = [('hT', t) for t in range(n0 // 128, n0 // 128 + 4)]
                    for c in range(8):
                        cs = slice(c * 128, (c + 1) * 128)
                        psa, pka = nextpf()
                        for k in range(4):
                            op('pe', lambda e, k=k, psa=psa: e.matmul(out=psa[:], lhsT=wa[:, k, cs], rhs=onaT[:, k, n0:n0 + 512], start=(k == 0), stop=(k == 3)), reads=['wa'] + onaT_all, writes=[pka])
                        psb, pkb = nextpf()
                        for k in range(4):
                            op('pe', lambda e, k=k, psb=psb: e.matmul(out=psb[:], lhsT=wb[:, k, cs], rhs=odnT[:, k, n0:n0 + 512], start=(k == 0), stop=(k == 3)), reads=['wb'] + [('odnT', n) for n in range(tb * 8, tb * 8 + 8)], writes=[pkb])
                        pga, pkga = nextpf()
                        for k in range(8):
                            op('pe', lambda e, k=k, pga=pga: e.matmul(out=pga[:], lhsT=wg[:, k, c * 128:(c + 1) * 128], rhs=hT[:, k, n0:n0 + 512], start=(k == 0), stop=(k == 7)), reads=[('wg', c // 4)] + hts, writes=[pkga])
                        pgb, pkgb = nextpf()
                        for k in range(8):
                            op('pe', lambda e, k=k, pgb=pgb: e.matmul(out=pgb[:], lhsT=wg[:, k, 1024 + c * 128:1024 + (c + 1) * 128], rhs=hT[:, k, n0:n0 + 512], start=(k == 0), stop=(k == 7)), reads=[('wg', 2 + c // 4)] + hts, writes=[pkgb])
                        op('act', lambda e, pga=pga: e.activation(out=sga[:], in_=pga[:], func=AF.Sigmoid), reads=[pkga], writes=['sga'])
                        op('act', lambda e, pgb=pgb: e.activation(out=sgb[:], in_=pgb[:], func=AF.Sigmoid), reads=[pkgb], writes=['sgb'])
                        op('dve', lambda e, psa=psa: e.tensor_tensor(out=sga[:], in0=sga[:], in1=psa[:], op=ALU.mult), reads=['sga', pka], writes=['sga'])
                        op('dve', lambda e, psb=psb: e.tensor_tensor(out=sgb[:], in0=sgb[:], in1=psb[:], op=ALU.mult), reads=['sgb', pkb], writes=['sgb'])
                        op('pool', lambda e, c=c: e.tensor_tensor(out=yT[:, c, :], in0=sga[:], in1=sgb[:], op=ALU.add), reads=['sga', 'sgb'], writes=['yT'])
                    for i4 in range(4):
                        i = tb * 4 + i4
                        xmi, xmk = xm[0], 'xm0'
                        xoi, xok = xo[i % 2], 'xo%d' % (i % 2)
                        h2i, h2k = h2[i % 2], 'h2_%d' % (i % 2)
                        dma('sp', lambda e, xmi=xmi, i=i: e.dma_start(out=xmi[:], in_=x_d[i * 128:(i + 1) * 128, :]), writes=[xmk])
                        for half in range(2):
                            hs_ = slice(half * 512, (half + 1) * 512)
                            ps, pk = nextpf()
                            for k in range(8):
                                op('pe', lambda e, k=k, ps=ps: e.matmul(out=ps[:], lhsT=yT[:, k, i4 * 128:(i4 + 1) * 128], rhs=wo[:, k, hs_], start=(k == 0), stop=(k == 7)), reads=['yT', ('wo', half)], writes=[pk])
                            op('dve', lambda e, ps=ps, xoi=xoi: e.tensor_tensor(out=xoi[:, hs_], in0=ps[:], in1=MODL[:, 2 * D + half * 512:2 * D + (half + 1) * 512], op=ALU.mult), reads=[pk, 'MODL'], writes=[xok])
                            op('dve', lambda e, xoi=xoi, xmi=xmi: e.tensor_tensor(out=xoi[:, hs_], in0=xoi[:, hs_], in1=xmi[:, hs_], op=ALU.add), reads=[xok, xmk], writes=[xok])
                        dma('sp', lambda e, xoi=xoi, i=i: e.dma_start(out=xl2_s[i * 128:(i + 1) * 128, :], in_=xoi[:]), reads=[xok], writes=[('xl2_s', i)])
                        op('act', lambda e, xoi=xoi, i=i: e.activation(out=junk[:], in_=xoi[:], func=AF.Square, accum_out=ssq[:, i:i + 1]), reads=[xok], writes=['junkE', 'ssqE%d' % i])
                        rstd_from_ssq(ssq[:, i:i + 1], rs[:, i:i + 1], 1.0 / D, ['ssqE%d' % i], ['rsE%d' % i])
                        op('dve', lambda e, xoi=xoi, i=i: e.scalar_tensor_tensor(out=junk[:], in0=xoi[:], scalar=rs[:, i:i + 1], in1=MODL[:, 4 * D:5 * D], op0=ALU.mult, op1=ALU.mult), reads=[xok, 'rsE%d' % i, 'MODL', 'junkE'], writes=['junkE'])
                        op('dve', lambda e, h2i=h2i: e.tensor_tensor(out=h2i[:], in0=junk[:], in1=MODL[:, 3 * D:4 * D], op=ALU.add), reads=['junkE', 'MODL'], writes=[h2k])
                        dma('sp', lambda e, h2i=h2i, i=i: e.dma_start(out=h2_s[i * 128:(i + 1) * 128, :], in_=h2i[:]), reads=[h2k], writes=[('h2_s', i)])
                        for hh in range(2):
                            ps, pk = nextpf()
                            for k4 in range(4):
                                k = hh * 4 + k4
                                op('pe', lambda e, ps=ps, k=k, k4=k4, h2i=h2i: e.transpose(out=ps[:, k4 * 128:(k4 + 1) * 128], in_=h2i[:, k * 128:(k + 1) * 128], identity=ident), reads=[h2k, 'cst'], writes=[pk])
                            op('act', lambda e, ps=ps, hh=hh: e.copy(out=h2T[:, hh * 4:(hh + 1) * 4, :], in_=ps[:].rearrange("p (k f) -> p k f", f=128)), reads=[pk], writes=[('h2T', hh)])
                        ps, pk = nextpf()
                        for k in range(8):
                            op('pe', lambda e, ps=ps, k=k: e.matmul(out=ps[:, 0:32], lhsT=h2T[:, k, :], rhs=wr[:, k, :], start=(k == 0), stop=(k == 7)), reads=[('h2T', k // 4), 'wr'], writes=[pk])
                        op('dve', lambda e, ps=ps, i=i: e.tensor_tensor(out=LG[:, i, :], in0=ps[:, 0:32], in1=brb[:], op=ALU.add), reads=[pk, 'brb'], writes=['LG'])
                kb.barrier()
                if stop == 'E':
                    return fin()
            if dbg:
                dma('sp', lambda e: e.dma_start(out=dbg_d["d_lg"][:, :], in_=LG[:].rearrange("p a b -> p (a b)")), reads=['LG'], writes=['d_lg'])
                kb.barrier()

            mid.close()
            with ExitStack() as s1:
                S = lambda name, shape, dt=F32: s1.enter_context(nc.sbuf_tensor("t_" + name, shape, dt))
                PF = [s1.enter_context(nc.psum_tensor("pfH%d" % i, [128, 512], F32)) for i in range(8)]
                A3 = [128, 16, 32]
                DESTi = S("DESTi", [128, 4, 16], I32)
                GATE = S("GATE", [128, 4, 16])
                W1I = S("W1I", [128, NBLK, 8], I32)
                B1I = S("B1I", [128, NBLK], I32)
                B2I = S("B2I", [128, NBLK], I32)
                rt = ExitStack()
                s1.callback(rt.close)
                R_ = lambda name, shape, dt=F32: rt.enter_context(nc.sbuf_tensor("t_" + name, shape, dt))
                LGw = R_("LGw", A3)
                EQ = R_("EQ", [128, 4, 16, 32])
                SEL = R_("SEL", A3)
                GT = R_("GT", A3)
                RANK = R_("RANK", A3)
                tmp3 = R_("tmp3", A3)
                mr = R_("mr", [128, 16])
                m0 = R_("m0", [128, 16])
                den = R_("den", [128, 16])
                CNT = R_("CNT", [128, 32])
                PAD = R_("PAD", [128, 32])
                PADi = R_("PADi", [128, 32], I32)
                PADT = R_("PADT", [32, 128])
                PSb = R_("PSb", [128, 32])
                PEb = R_("PEb", [128, 32])
                DESTf = R_("DESTf", [128, 4, 16])
                cmp_ = R_("cmp_", [128, NBLK, 32])
                BE = R_("BE", [128, NBLK])
                tmpw = R_("tmpw", [128, NBLK, 8])
                tmpb = R_("tmpb", [128, NBLK])
                zi = R_("zi", [128, NBLK], I32)
                tokid = R_("tokid", [128, 16], I32)
                b16 = lambda ap: ap.unsqueeze(2).to_broadcast(A3)
                e16 = lambda ap: ap.unsqueeze(1).to_broadcast(A3)
                op('dve', lambda e: e.tensor_copy(out=LGw[:], in_=LG[:]), reads=['LG'], writes=['LGw'])
                for r in range(4):
                    op('dve', lambda e: e.tensor_reduce(out=mr[:], in_=LGw[:], axis=AX.X, op=ALU.max), reads=['LGw'], writes=['mr'])
                    if r == 0:
                        op('dve', lambda e: e.tensor_copy(out=m0[:], in_=mr[:]), reads=['mr'], writes=['m0'])
                    op('dve', lambda e, r=r: e.tensor_tensor(out=EQ[:, r], in0=LGw[:], in1=b16(mr[:]), op=ALU.is_equal), reads=['LGw', 'mr'], writes=[('EQ', r)])
                    op('dve', lambda e, r=r: e.scalar_tensor_tensor(out=LGw[:], in0=EQ[:, r], scalar=NEG, in1=LGw[:], op0=ALU.mult, op1=ALU.add), reads=[('EQ', r), 'LGw'], writes=['LGw'])
                op('dve', lambda e: e.tensor_tensor(out=SEL[:], in0=EQ[:, 0], in1=EQ[:, 1], op=ALU.add), reads=[('EQ', 0), ('EQ', 1)], writes=['SEL'])
                op('dve', lambda e: e.tensor_tensor(out=SEL[:], in0=SEL[:], in1=EQ[:, 2], op=ALU.add), reads=['SEL', ('EQ', 2)], writes=['SEL'])
                op('dve', lambda e: e.tensor_tensor(out=SEL[:], in0=SEL[:], in1=EQ[:, 3], op=ALU.add), reads=['SEL', ('EQ', 3)], writes=['SEL'])
                op('dve', lambda e: e.tensor_tensor(out=GT[:], in0=LG[:], in1=b16(m0[:]), op=ALU.subtract), reads=['LG', 'm0'], writes=['GT'])
                op('act', lambda e: e.activation(out=GT[:], in_=GT[:], func=AF.Exp), reads=['GT'], writes=['GT'])
                op('dve', lambda e: e.tensor_tensor(out=GT[:], in0=GT[:], in1=SEL[:], op=ALU.mult), reads=['GT', 'SEL'], writes=['GT'])
                op('dve', lambda e: e.tensor_reduce(out=den[:], in_=GT[:], axis=AX.X, op=ALU.add), reads=['GT'], writes=['den'])
                op('dve', lambda e: e.reciprocal(out=den[:], in_=den[:]), reads=['den'], writes=['den'])
                op('dve', lambda e: e.tensor_tensor(out=GT[:], in0=GT[:], in1=b16(den[:]), op=ALU.mult), reads=['GT', 'den'], writes=['GT'])
                op('dve', lambda e: e.memset(CNT[:], 0.0), writes=['CNT'])
                for i in range(16):
                    ps, pk = PF[i % 2], 'pfH%d' % (i % 2)
                    op('pe', lambda e, ps=ps, i=i: e.matmul(out=ps[:, 0:32], lhsT=cst[:, C_UT:C_UT + 128], rhs=SEL[:, i, :], start=True, stop=True), reads=['SEL', 'cst'], writes=[pk])
                    op('dve', lambda e, ps=ps, i=i: e.tensor_tensor(out=RANK[:, i, :], in0=ps[:, 0:32], in1=CNT[:], op=ALU.add), reads=[pk, 'CNT'], writes=['RANK'])
                    ps2, pk2 = PF[2 + i % 2], 'pfH%d' % (2 + i % 2)
                    op('pe', lambda e, ps2=ps2, i=i: e.matmul(out=ps2[:, 0:32], lhsT=ones, rhs=SEL[:, i, :], start=True, stop=True), reads=['SEL', 'cst'], writes=[pk2])
                    op('dve', lambda e, ps2=ps2: e.tensor_tensor(out=CNT[:], in0=CNT[:], in1=ps2[:, 0:32], op=ALU.add), reads=[pk2, 'CNT'], writes=['CNT'])
                op('dve', lambda e: e.tensor_scalar(out=PAD[:], in0=CNT[:], scalar1=127.0, scalar2=None, op0=ALU.add), reads=['CNT'], writes=['PAD'])
                op('dve', lambda e: e.tensor_copy(out=PADi[:], in_=PAD[:]), reads=['PAD'], writes=['PADi'])
                op('dve', lambda e: e.tensor_scalar(out=PADi[:], in0=PADi[:], scalar1=7, scalar2=7, op0=ALU.arith_shift_right, op1=ALU.logical_shift_left), reads=['PADi'], writes=['PADi'])
                op('dve', lambda e: e.tensor_copy(out=PAD[:], in_=PADi[:]), reads=['PADi'], writes=['PAD'])
                op('pe', lambda e: e.transpose(out=PF[4][0:32, 0:128], in_=PAD[:, 0:32], identity=ident), reads=['PAD', 'cst'], writes=['pfH4'])
                op('act', lambda e: e.copy(out=PADT[:], in_=PF[4][0:32, 0:128]), reads=['pfH4'], writes=['PADT'])
                op('pe', lambda e: e.matmul(out=PF[5][:, 0:32], lhsT=PADT[:], rhs=cst[0:32, C_TRI:C_TRI + 32], start=True, stop=True), reads=['PADT', 'cst'], writes=['pfH5'])
                op('act', lambda e: e.copy(out=PSb[:], in_=PF[5][:, 0:32]), reads=['pfH5'], writes=['PSb'])
                op('dve', lambda e: e.tensor_tensor(out=PEb[:], in0=PSb[:], in1=PAD[:], op=ALU.add), reads=['PSb', 'PAD'], writes=['PEb'])
                op('dve', lambda e: e.tensor_tensor(out=RANK[:], in0=RANK[:], in1=e16(PSb[:]), op=ALU.add), reads=['RANK', 'PSb'], writes=['RANK'])
                for r in range(4):
                    op('dve', lambda e, r=r: e.tensor_tensor(out=tmp3[:], in0=EQ[:, r], in1=RANK[:], op=ALU.mult), reads=[('EQ', r), 'RANK'], writes=['tmp3'])
                    op('dve', lambda e, r=r: e.tensor_reduce(out=DESTf[:, r, :], in_=tmp3[:], axis=AX.X, op=ALU.add), reads=['tmp3'], writes=['DESTf'])
                    op('dve', lambda e, r=r: e.tensor_tensor(out=tmp3[:], in0=EQ[:, r], in1=GT[:], op=ALU.mult), reads=[('EQ', r), 'GT'], writes=['tmp3'])
                    op('dve', lambda e, r=r: e.tensor_reduce(out=GATE[:, r, :], in_=tmp3[:], axis=AX.X, op=ALU.add), reads=['tmp3'], writes=['GATE'])
                op('dve', lambda e: e.tensor_copy(out=DESTi[:], in_=DESTf[:]), reads=['DESTf'], writes=['DESTi'])
                op('dve', lambda e: e.tensor_tensor(out=cmp_[:], in0=PEb[:].unsqueeze(1).to_broadcast([128, NBLK, 32]), in1=cst[:, C_BS:C_BS + NBLK].unsqueeze(2).to_broadcast([128, NBLK, 32]), op=ALU.is_le),
                   reads=['PEb', 'cst'], writes=['cmp_'])
                op('dve', lambda e: e.tensor_reduce(out=BE[:], in_=cmp_[:], axis=AX.X, op=ALU.add), reads=['cmp_'], writes=['BE'])
                op('dve', lambda e: e.tensor_scalar_min(out=BE[:], in0=BE[:], scalar1=31.0), reads=['BE'], writes=['BE'])
                op('dve', lambda e: e.tensor_copy(out=B2I[:], in_=BE[:]), reads=['BE'], writes=['B2I'])
                op('dve', lambda e: e.tensor_scalar(out=tmpb[:], in0=BE[:], scalar1=128.0, scalar2=cst[:, C_KR:C_KR + 1], op0=ALU.mult, op1=ALU.add), reads=['BE', 'cst'], writes=['tmpb'])
                op('dve', lambda e: e.tensor_copy(out=B1I[:], in_=tmpb[:]), reads=['tmpb'], writes=['B1I'])
                op('dve', lambda e: e.tensor_scalar(out=tmpb[:], in0=BE[:], scalar1=1024.0, scalar2=None, op0=ALU.mult), reads=['BE', 'B1I'], writes=['tmpb'])
                op('dve', lambda e: e.tensor_tensor(out=tmpw[:], in0=tmpb[:].unsqueeze(2).to_broadcast([128, NBLK, 8]), in1=cst[:, C_KR:C_KR + 8].unsqueeze(1).to_broadcast([128, NBLK, 8]), op=ALU.add),
                   reads=['tmpb', 'cst'], writes=['tmpw'])
                op('dve', lambda e: e.tensor_copy(out=W1I[:], in_=tmpw[:]), reads=['tmpw'], writes=['W1I'])
                op('dve', lambda e: e.memset(zi[:], 0), writes=['zi'])
                op('dve', lambda e: e.tensor_copy(out=tokid[:], in_=cst[:, C_TOK:C_TOK + 16]), reads=['cst'], writes=['tokid'])
                dma('sp', lambda e: e.dma_start(out=slot_s.rearrange("(p j) o -> p (j o)", p=128), in_=zi[:]), reads=['zi'], writes=['slot_s'])
                for r in range(4):
                    for i in range(16):
                        dma('pool', lambda e, r=r, i=i: e.indirect_dma_start(out=slot_s[:, :], out_offset=bass.IndirectOffsetOnAxis(ap=DESTi[:, r, i:i + 1], axis=0), in_=tokid[:, i:i + 1], in_offset=None),
                            reads=['DESTi', 'tokid'], writes=['slot_s'])
                kb.barrier()
                rt.close()
                sidx = [S("sidx%d" % i, [128, 1], I32) for i in range(2)]
                xg = [S("xg%d" % i, [128, D]) for i in range(2)]
                xgT = S("xgT", [128, 8, 128], BF16)
                w1b = [S("w1b%d" % i, [128, 2048], BF16) for i in range(3)]
                w2b = [S("w2b%d" % i, [128, D], BF16) for i in range(3)]
                w1k = [S("w1k%d" % i, [128, 2048]) for i in range(6)]
                w2c = [S("w2c%d" % i, [128, D]) for i in range(8)]
                b1s = [S("b1s%d" % i, [128, 16]) for i in range(2)]
                b2s = [S("b2s%d" % i, [128, D]) for i in range(2)]
                Gt = S("Gt", [128, 4, 128])
                Ut = S("Ut", [128, 4, 128])
                sgm = S("sgm", [128, 4, 128])
                actT = S("actT", [128, 8, 128], BF16)
                ysb = [S("ysb%d" % i, [128, D]) for i in range(2)]
                PH = PF[0:4]
                PY = PF[4:6]
                PTr = PF[6:8]
                wc1 = wc2 = 0
                for j in range(NBLK):
                    si, sik = sidx[j % 2], 'sidx%d' % (j % 2)
                    xgi, xgk = xg[j % 2], 'xg%d' % (j % 2)
                    dma('sp', lambda e, si=si, j=j: e.dma_start(out=si[:], in_=slot_s[j * 128:(j + 1) * 128, :]), reads=['slot_s'], writes=[sik])
                    dma('pool', lambda e, si=si, xgi=xgi: e.indirect_dma_start(out=xgi[:], out_offset=None, in_=h2_s[:, :], in_offset=bass.IndirectOffsetOnAxis(ap=si[:, 0:1], axis=0)),
                        reads=[sik] + [('h2_s', i) for i in range(16)], writes=[xgk])
                    b1i, b1k = b1s[j % 2], 'b1s%d' % (j % 2)
                    b2i, b2k = b2s[j % 2], 'b2s%d' % (j % 2)
                    dma('pool', lambda e, b1i=b1i, j=j: e.indirect_dma_start(out=b1i[:], out_offset=None, in_=b1t_d[:, :], in_offset=bass.IndirectOffsetOnAxis(ap=B1I[:, j:j + 1], axis=0)), reads=['B1I'], writes=[b1k])
                    dma('pool', lambda e, b2i=b2i, j=j: e.indirect_dma_start(out=b2i[:], out_offset=None, in_=b2_d[:, :], in_offset=bass.IndirectOffsetOnAxis(ap=B2I[:, j:j + 1], axis=0)), reads=['B2I'], writes=[b2k])
                    for hh in range(2):
                        ps, pk = PTr[hh], 'pfH%d' % (6 + hh)
                        for k4 in range(4):
                            k = hh * 4 + k4
                            op('pe', lambda e, ps=ps, k=k, k4=k4, xgi=xgi: e.transpose(out=ps[:, k4 * 128:(k4 + 1) * 128], in_=xgi[:, k * 128:(k + 1) * 128], identity=ident), reads=[xgk, 'cst'], writes=[pk])
                        op('act', lambda e, ps=ps, hh=hh: e.copy(out=xgT[:, hh * 4:(hh + 1) * 4, :], in_=ps[:].rearrange("p (k f) -> p k f", f=128)), reads=[pk], writes=[('xgT', hh)])
                    for k in range(8):
                        wt, wk = w1k[wc1 % 6], 'w1k%d' % (wc1 % 6)
                        wc1 += 1
                        dma('pool', lambda e, wt=wt, j=j, k=k: e.indirect_dma_start(out=wt[:], out_offset=None, in_=w1_d[:, :], in_offset=bass.IndirectOffsetOnAxis(ap=W1I[:, j, k:k + 1], axis=0)), reads=['W1I'], writes=[wk])
                        wb_, wbk = w1b[wc1 % 3], 'w1b%d' % (wc1 % 3)
                        if k % 2 == 0:
                            op('dve', lambda e, wt=wt, wb_=wb_: e.tensor_copy(out=wb_[:], in_=wt[:]), reads=[wk], writes=[wbk])
                        else:
                            op('act', lambda e, wt=wt, wb_=wb_: e.copy(out=wb_[:], in_=wt[:]), reads=[wk], writes=[wbk])
                        for c in range(16):
                            op('pe', lambda e, wb_=wb_, k=k, c=c: e.matmul(out=PH[c // 4][:, (c % 4) * 128:(c % 4 + 1) * 128], lhsT=wb_[:, c * 128:(c + 1) * 128], rhs=xgT[:, k, :], start=(k == 0 and c % 4 == 0), stop=(k == 7 and c % 4 == 3)),
                               reads=[wbk, ('xgT', k // 4)], writes=['pfH%d' % (c // 4)])
                    for q in range(2):
                        g3_ = PH[q][:].rearrange("p (c f) -> p c f", f=128)
                        u3_ = PH[q + 2][:].rearrange("p (c f) -> p c f", f=128)
                        bb = lambda lo: b1i[:, lo:lo + 4].unsqueeze(2).to_broadcast([128, 4, 128])
                        op('dve', lambda e: e.tensor_tensor(out=Gt[:], in0=g3_, in1=bb(4 * q), op=ALU.add), reads=['pfH%d' % q, b1k], writes=['Gt'])
                        op('dve', lambda e: e.tensor_scalar_min(out=Gt[:], in0=Gt[:], scalar1=7.0), reads=['Gt'], writes=['Gt'])
                        op('act', lambda e: e.activation(out=sgm[:], in_=Gt[:], func=AF.Sigmoid, scale=1.702), reads=['Gt'], writes=['sgm'])
                        op('dve', lambda e: e.tensor_tensor(out=Ut[:], in0=u3_, in1=bb(8 + 4 * q), op=ALU.add), reads=['pfH%d' % (q + 2), b1k], writes=['Ut'])
                        op('dve', lambda e: e.tensor_scalar(out=Ut[:], in0=Ut[:], scalar1=7.0, scalar2=-7.0, op0=ALU.min, op1=ALU.max), reads=['Ut'], writes=['Ut'])
                        op('dve', lambda e: e.scalar_tensor_tensor(out=Ut[:], in0=Ut[:], scalar=1.0, in1=Gt[:], op0=ALU.add, op1=ALU.mult), reads=['Ut', 'Gt'], writes=['Ut'])
                        op('dve', lambda e: e.tensor_tensor(out=actT[:, 4 * q:4 * q + 4, :], in0=Ut[:], in1=sgm[:], op=ALU.mult), reads=['Ut', 'sgm'], writes=[('actT', q)])
                    for fc in range(8):
                        wt, wk = w2c[wc2 % 8], 'w2c%d' % (wc2 % 8)
                        wc2 += 1
                        dma('pool', lambda e, wt=wt, j=j, fc=fc: e.indirect_dma_start(out=wt[:], out_offset=None, in_=w2_d[:, :], in_offset=bass.IndirectOffsetOnAxis(ap=W1I[:, j, fc:fc + 1], axis=0)), reads=['W1I'], writes=[wk])
                        wb_, wbk = w2b[wc2 % 3], 'w2b%d' % (wc2 % 3)
                        op('act', lambda e, wt=wt, wb_=wb_: e.copy(out=wb_[:], in_=wt[:]), reads=[wk], writes=[wbk])
                        for half in range(2):
                            op('pe', lambda e, wb_=wb_, fc=fc, half=half: e.matmul(out=PY[half][:], lhsT=actT[:, fc, :], rhs=wb_[:, half * 512:(half + 1) * 512], start=(fc == 0), stop=(fc == 7)),
                               reads=[wbk, ('actT', fc // 4)], writes=['pfH%d' % (4 + half)])
                    yi, yk = ysb[j % 2], 'ysb%d' % (j % 2)
                    for half in range(2):
                        op('dve', lambda e, yi=yi, half=half: e.tensor_tensor(out=yi[:, half * 512:(half + 1) * 512], in0=PY[half][:], in1=b2i[:, half * 512:(half + 1) * 512], op=ALU.add),
                           reads=['pfH%d' % (4 + half), b2k], writes=[yk])
                    dma('sp', lambda e, yi=yi, j=j: e.dma_start(out=ypad_s[j * 128:(j + 1) * 128, :], in_=yi[:]), reads=[yk], writes=['ypad_s'])
                yr = [S("yr%d" % i, [128, D]) for i in range(4)]
                xq = [S("xq0", [128, D])] * 2
                ac = [S("ac%d" % i, [128, D]) for i in range(2)]
                fnb = S("fnb", [128, D])
                ssq = S("ssqH", [128, 16])
                rs = S("rsH", [128, 16])
                dma('sp', lambda e: e.dma_start(out=fnb[:], in_=fnw_d[0:1, :].partition_broadcast(128)), writes=['fnb'])
                for i in range(16):
                    aci, ack = ac[i % 2], 'ac%d' % (i % 2)
                    xqi, xqk = xq[0], 'xq0'
                    dma('sp', lambda e, xqi=xqi, i=i: e.dma_start(out=xqi[:], in_=xl2_s[i * 128:(i + 1) * 128, :]), reads=[('xl2_s', i)], writes=[xqk])
                    for r in range(4):
                        dma('pool', lambda e, r=r, i=i: e.indirect_dma_start(out=yr[r][:], out_offset=None, in_=ypad_s[:, :], in_offset=bass.IndirectOffsetOnAxis(ap=DESTi[:, r, i:i + 1], axis=0)),
                            reads=['DESTi', 'ypad_s'], writes=['yr%d' % r])
                        if r == 0:
                            op('dve', lambda e, aci=aci, i=i: e.tensor_scalar(out=aci[:], in0=yr[0][:], scalar1=GATE[:, 0, i:i + 1], scalar2=None, op0=ALU.mult), reads=['yr0', 'GATE'], writes=[ack])
                        else:
                            op('dve', lambda e, aci=aci, i=i, r=r: e.scalar_tensor_tensor(out=aci[:], in0=yr[r][:], scalar=GATE[:, r, i:i + 1], in1=aci[:], op0=ALU.mult, op1=ALU.add), reads=['yr%d' % r, 'GATE', ack], writes=[ack])
                    op('dve', lambda e, aci=aci: e.tensor_tensor(out=aci[:], in0=aci[:], in1=MODL[:, 5 * D:6 * D], op=ALU.mult), reads=[ack, 'MODL'], writes=[ack])
                    op('dve', lambda e, aci=aci, xqi=xqi: e.tensor_tensor(out=aci[:], in0=aci[:], in1=xqi[:], op=ALU.add), reads=[ack, xqk], writes=[ack])
                    op('act', lambda e, aci=aci, i=i: e.activation(out=yr[0][:], in_=aci[:], func=AF.Square, accum_out=ssq[:, i:i + 1]), reads=[ack], writes=['yr0', 'ssqH%d' % i])
                    rstd_from_ssq(ssq[:, i:i + 1], rs[:, i:i + 1], 1.0 / D, ['ssqH%d' % i], ['rsH%d' % i])
                    op('dve', lambda e, aci=aci, i=i: e.scalar_tensor_tensor(out=aci[:], in0=aci[:], scalar=rs[:, i:i + 1], in1=fnb[:], op0=ALU.mult, op1=ALU.mult), reads=[ack, 'rsH%d' % i, 'fnb'], writes=[ack])
                    dma('sp', lambda e, aci=aci, i=i: e.dma_start(out=out_d[i * 128:(i + 1) * 128, :], in_=aci[:]), reads=[ack], writes=[('out', i)])
                kb.barrier()
        if kb.stopped:
            kb.stopped = False
            kb.limit = None
            kb.barrier()
            return fin()
        print("instructions:", kb.nins)
    return nc


def host_consts():
    cst = np.zeros((128, C_N), np.float32)
    cst[:, C_ID:C_ID + 128] = np.eye(128)
    cst[:, C_ONE:C_ONE + 128] = 1.0
    i = np.arange(64)
    cst[:64, C_U:C_U + 64] = (i[:, None] <= i[None, :])
    cst[:64, C_L:C_L + 64] = (i[:, None] >= i[None, :])
    cst[:64, C_IF:C_IF + 64] = (i[:, None] >= i[None, :])
    cst[:64, C_SF:C_SF + 64] = (i[:, None] > i[None, :])
    cst[:64, C_IB:C_IB + 64] = (i[:, None] <= i[None, :])
    cst[:64, C_SB:C_SB + 64] = (i[:, None] < i[None, :])
    e = np.arange(32)
    cst[:32, C_TRI:C_TRI + 32] = (e[:, None] < e[None, :])
    t = np.arange(128)
    cst[:, C_UT:C_UT + 128] = (t[:, None] < t[None, :])
    cst[:, C_BS:C_BS + NBLK] = (np.arange(NBLK) * 128)[None, :]
    cst[:, C_KR:C_KR + 8] = np.arange(8)[None, :] * 128 + t[:, None]
    cst[:, C_TOK:C_TOK + 16] = np.arange(16)[None, :] * 128 + t[:, None]
    return cst


def host_rope():
    tt = np.arange(T)
    row = (tt // 64).astype(np.float32)
    col = (tt % 64).astype(np.float32)
    freqs = (np.float32(10000.0) ** (-np.arange(16, dtype=np.float32) / np.float32(16))).astype(np.float32)
    ang = np.concatenate([row[:, None] * freqs, col[:, None] * freqs], axis=-1).astype(np.float32)
    cos, sin = np.cos(ang).astype(np.float32), np.sin(ang).astype(np.float32)
    r = np.stack([cos.reshape(32, 64, 32).transpose(1, 0, 2), sin.reshape(32, 64, 32).transpose(1, 0, 2)], axis=1)
    return np.ascontiguousarray(r.reshape(64, 2 * 32 * 32))


def host_natbl(rpb):
    kc = np.arange(64)[:, None]
    qc = np.arange(64)[None, :]
    dc = np.clip(kc - qc + 15, 0, 30)
    cs = np.clip(np.arange(64) - 8, 0, 48)
    cmask = (kc >= cs[None, :]) & (kc < cs[None, :] + 16)
    base = np.full((8, 64, 17, 64), NEG, np.float32)
    for jp in range(0, 15):
        g = rpb[:, 14 - jp][:, dc]
        base[:, :, jp + 1, :] = np.where(cmask[None], g, np.float32(NEG))
    base_int = base.copy()
    for jp in range(-1, 16):
        if not (4 <= jp <= 11):
            base_int[:, :, jp + 1, :] = NEG
    out = np.empty((8, 2, 128, 16, 64), np.float32)
    for ti, b in enumerate((base, base_int)):
        for a in range(2):
            out[:, ti, a * 64:(a + 1) * 64, :, :] = b[:, :, (1 - a):(17 - a), :]
    return np.ascontiguousarray(out.reshape(8, 2, 128, 1024))


def make_in_maps(inputs, cores):
    f = lambda a: np.ascontiguousarray(np.asarray(a, dtype=np.float32))
    shared = {
        "w_mod": f(inputs["w_mod"][0]), "b_mod": f(inputs["b_mod"][0]).reshape(1, -1), "norm1_w": f(inputs["norm1_w"][0]).reshape(1, -1),
        "w_in": f(inputs["w_in"][0]), "na_tbl": host_natbl(np.asarray(inputs["na_rpb"][0], np.float32)),
        "convw": f(np.asarray(inputs["dn_conv_w"][0]).reshape(5, 12, 128).transpose(2, 1, 0).reshape(128, 60)),
        "alog": f(inputs["dn_a_log"][0]).reshape(1, 16), "dtb": f(inputs["dn_dt_bias"][0]).reshape(1, 16), "dnw": f(inputs["dn_norm_w"][0]).reshape(1, 64),
        "w_br_a": f(inputs["w_br_a"][0]), "w_br_b": f(inputs["w_br_b"][0]), "w_out": f(inputs["w_out"][0]), "norm2_w": f(inputs["norm2_w"][0]).reshape(1, -1),
        "w_router": f(inputs["w_router"][0]), "b_router": f(inputs["b_router"][0]).reshape(1, 32),
        "w1": f(inputs["w1"][0]).reshape(32 * D, 2048), "b1t": f(np.asarray(inputs["b1"][0]).reshape(32, 16, 128).transpose(0, 2, 1).reshape(32 * 128, 16)),
        "w2": f(inputs["w2"][0]).reshape(32 * D, D), "b2": f(inputs["b2"][0]), "fnw": f(inputs["final_norm_w"]).reshape(1, -1),
        "rope": host_rope(), "cst": host_consts(),
    }
    maps = []
    for b in cores:
        m = dict(shared)
        m["x"] = f(inputs["x"][b])
        m["ctx"] = f(inputs["ctx"][b])
        m["c2"] = f(np.stack([np.asarray(inputs["c"][b]), np.asarray(inputs["c_ctx"])], axis=0))
        maps.append(m)
    return maps


def kernel(**inputs):
    nc = build()
    maps = make_in_maps(inputs, list(range(8)))
    res = run_bass_kernel_spmd(nc, maps, core_ids=list(range(8)))
    return np.stack([np.asarray(r["out"], dtype=np.float32) for r in res.results], axis=0)
```

```python
import numpy as np
from contextlib import ExitStack
import concourse.bass as bass
import concourse.mybir as mybir
from concourse.bass_utils import run_bass_kernel_spmd

F32 = mybir.dt.float32
BF16 = mybir.dt.bfloat16
I32 = mybir.dt.int32
ALU = mybir.AluOpType
AF = mybir.ActivationFunctionType
AX = mybir.AxisListType

D = 1024
T = 2048
LC = 256
NT = T + LC
NBLK = 96
NEG = -1e30
EPS = 1e-6

C_ID, C_ONE, C_U, C_L, C_IF, C_SF, C_IB, C_SB, C_TRI, C_UT, C_BS, C_KR, C_TOK, C_N = 0, 128, 256, 320, 384, 448, 512, 576, 640, 672, 800, 896, 904, 920


class _StopScan(Exception):
    pass


class KB:
    NDMA = 16

    def __init__(self, nc, es):
        self.nc = nc
        self.eng = {'pe': nc.tensor, 'act': nc.scalar, 'dve': nc.vector, 'pool': nc.gpsimd, 'sp': nc.sync}
        self.sem = {}
        self.cnt = {}
        for e in ('pe', 'act', 'dve', 'pool'):
            self.sem[e] = es.enter_context(nc.semaphore('s_' + e))
            self.cnt[e] = 0
        self.dsem = {}
        self.dcnt = {}
        self.dnext = {}
        for q in ('sp', 'pool'):
            self.dsem[q] = [es.enter_context(nc.semaphore('d_%s%d' % (q, i))) for i in range(self.NDMA)]
            self.dcnt[q] = [0] * self.NDMA
            self.dnext[q] = 0
        self.seen = {e: {} for e in self.eng}
        self.lastw = {}
        self.readers = {}
        self.nins = 0
        self.limit = None
        self.stopped = False

    def _wait(self, e, ev):
        sem, val, src = ev
        if src == e and e == 'pe':
            return
        k = id(sem)
        if self.seen[e].get(k, 0) >= val:
            return
        self.eng[e].wait_ge(sem, val)
        self.seen[e][k] = val

    def _deps(self, e, reads, writes):
        for r in reads:
            ev = self.lastw.get(r)
            if ev is not None:
                self._wait(e, ev)
        for w in writes:
            ev = self.lastw.get(w)
            if ev is not None:
                self._wait(e, ev)
            for ev in self.readers.get(w, {}).values():
                self._wait(e, ev)

    def _record(self, ev, reads, writes):
        for r in reads:
            self.readers.setdefault(r, {})[id(ev[0])] = ev
        for w in writes:
            self.lastw[w] = ev
            self.readers[w] = {}

    def op(self, e, fn, reads=(), writes=()):
        if self.stopped:
            return None
        pr = [r for r in reads if isinstance(r, str) and r[:2] in ('pf', 'pb')]
        if pr:
            writes = list(writes) + pr
        self._deps(e, reads, writes)
        ins = fn(self.eng[e])
        self.cnt[e] += 1
        ins.then_inc(self.sem[e], 1)
        self._record((self.sem[e], self.cnt[e], e), reads, writes)
        self.nins += 1
        if self.limit is not None and self.nins >= self.limit:
            self.stopped = True
        return ins

    def dma(self, q, fn, reads=(), writes=()):
        if self.stopped:
            return None
        i = self.dnext[q]
        self.dnext[q] = (i + 1) % self.NDMA
        sem = self.dsem[q][i]
        if self.dcnt[q][i] > 0:
            self._wait(q, (sem, self.dcnt[q][i], 'dma'))
        self._deps(q, reads, writes)
        ins = fn(self.eng[q])
        self.dcnt[q][i] += 16
        ins.then_inc(sem, 16)
        self._record((sem, self.dcnt[q][i], 'dma'), reads, writes)
        self.nins += 1
        return ins

    def barrier(self):
        if self.stopped:
            return
        for e in self.eng:
            for e2 in ('pe', 'act', 'dve', 'pool'):
                if e2 != e and self.cnt[e2] > 0:
                    self._wait(e, (self.sem[e2], self.cnt[e2], e2))
            for q in self.dsem:
                for i in range(self.NDMA):
                    if self.dcnt[q][i] > 0:
                        self._wait(e, (self.dsem[q][i], self.dcnt[q][i], 'dma'))

    def wait_all(self, e, toks):
        if self.stopped:
            return
        for t in toks:
            if t in self.lastw:
                self._wait(e, self.lastw[t])


def bc(ap, shape):
    return ap.to_broadcast(shape)


def build(dbg=False, stop=None, sub=None):
    nc = bass.Bass("TRN2", target_bir_lowering=False)
    limit = int(sub[1:]) if (sub is not None and sub.startswith('n')) else None
    inp = lambda name, shape, dt=F32: nc.dram_tensor(name, shape, dt, kind="ExternalInput").ap()
    x_d = inp("x", [T, D])
    ctx_d = inp("ctx", [LC, D])
    c2_d = inp("c2", [2, D])
    wmod_d = inp("w_mod", [D, 6 * D])
    bmod_d = inp("b_mod", [1, 6 * D])
    n1_d = inp("norm1_w", [1, D])
    win_d = inp("w_in", [D, 5664])
    natbl_d = inp("na_tbl", [8, 2, 128, 1024])
    convw_d = inp("convw", [128, 12 * 5])
    alog_d = inp("alog", [1, 16])
    dtb_d = inp("dtb", [1, 16])
    dnw_d = inp("dnw", [1, 64])
    wbra_d = inp("w_br_a", [512, D])
    wbrb_d = inp("w_br_b", [512, D])
    wout_d = inp("w_out", [D, D])
    n2_d = inp("norm2_w", [1, D])
    wr_d = inp("w_router", [D, 32])
    br_d = inp("b_router", [1, 32])
    w1_d = inp("w1", [32 * D, 2048])
    b1t_d = inp("b1t", [32 * 128, 16])
    w2_d = inp("w2", [32 * D, D])
    b2_d = inp("b2", [32, D])
    fnw_d = inp("fnw", [1, D])
    rope_d = inp("rope", [64, 2 * 32 * 32])
    cst_d = inp("cst", [128, C_N])
    out_d = nc.dram_tensor("out", [T, D], F32, kind="ExternalOutput").ap()
    scr = lambda name, shape, dt=F32: nc.dram_tensor(name, shape, dt, kind="Internal").ap()
    qkv_s = scr("qkv_s", [36, 64, 1536])
    o_s = scr("o_s", [2, 32, 64, 512])
    xl2_s = scr("xl2_s", [T, D])
    h2_s = scr("h2_s", [T, D])
    slot_s = scr("slot_s", [NBLK * 128, 1], I32)
    ypad_s = scr("ypad_s", [NBLK * 128, D])
    dbg_d = {}
    if dbg:
        for nm, shp in (("d_hT", [128, 8 * NT]), ("d_ona", [128, 4 * T]), ("d_odn", [128, 4 * T]), ("d_lg", [128, 16 * 32])):
            dbg_d[nm] = nc.dram_tensor(nm, shp, F32, kind="ExternalOutput").ap()

    winv = win_d.rearrange("(k p) n -> p k n", p=128)

    with ExitStack() as es:
        kb = KB(nc, es)
        kb.limit = limit
        op, dma = kb.op, kb.dma

        def fin():
            with ExitStack() as sf:
                z = sf.enter_context(nc.sbuf_tensor("t_zout", [128, D], F32))
                op('dve', lambda e: e.memset(z[:], 0.0), writes=['zout'])
                for i in range(16):
                    dma('sp', lambda e, i=i: e.dma_start(out=out_d[i * 128:(i + 1) * 128, :], in_=z[:]), reads=['zout'], writes=[('out', i)])
                kb.barrier()
            print("instructions (stopped at %s):" % stop, kb.nins)
            return nc
        if True:
            G = lambda name, shape, dt=F32: es.enter_context(nc.sbuf_tensor("t_" + name, shape, dt))
            cst = G("cst", [128, C_N])
            identb = G("identb", [128, 128], BF16)
            MODL = G("MODL", [128, 6 * D])
            epsb = G("epsb", [128, 1])
            LG = G("LG", [128, 16, 32])
            mid = ExitStack()
            es.callback(mid.close)
            Gm = lambda name, shape, dt=F32: mid.enter_context(nc.sbuf_tensor("t_" + name, shape, dt))
            hT = Gm("hT", [128, 8, NT], BF16)
            onaT = Gm("onaT", [128, 4, T], BF16)
            odnT = Gm("odnT", [128, 4, T], BF16)

            dma('sp', lambda e: e.dma_start(out=cst[:], in_=cst_d[:, :]), writes=['cst'])
            op('dve', lambda e: e.tensor_copy(out=identb[:], in_=cst[:, C_ID:C_ID + 128]), reads=['cst'], writes=['identb'])
            op('dve', lambda e: e.memset(epsb[:], EPS), writes=['epsb'])
            ident = cst[:, C_ID:C_ID + 128]
            ones = cst[:, C_ONE:C_ONE + 128]

            def rstd_from_ssq(ssq_ap, rs_ap, scale, toks_r, toks_w):
                op('act', lambda e: e.activation(out=rs_ap, in_=ssq_ap, func=AF.Sqrt, scale=scale, bias=epsb[0:rs_ap.partition_size(), :]), reads=list(toks_r) + ['epsb'], writes=toks_w)
                op('dve', lambda e: e.reciprocal(out=rs_ap, in_=rs_ap), reads=toks_w, writes=toks_w)

            with ExitStack() as s1:
                S = lambda name, shape, dt=F32: s1.enter_context(nc.sbuf_tensor("t_" + name, shape, dt))
                PF = [s1.enter_context(nc.psum_tensor("pfA%d" % i, [128, 512], F32)) for i in range(2)]
                c2t = S("c2t", [128, 2, 8])
                MODC = S("MODC", [128, 2 * D])
                Lm = S("Lm", [128, 2, 8, 128], BF16)
                onesb = S("onesb", [128, 128], BF16)
                wm = [S("wm%d" % i, [128, 8, 512], BF16) for i in range(2)]
                nbc = S("nbc", [128, 2 * D])
                dma('sp', lambda e: e.dma_start(out=c2t[:], in_=c2_d.rearrange("t (p k) -> p t k", k=8)), writes=['c2t'])
                dma('sp', lambda e: e.dma_start(out=MODL[:], in_=bmod_d[0:1, :].partition_broadcast(128)), writes=['MODL'])
                dma('sp', lambda e: e.dma_start(out=MODC[:], in_=bmod_d[0:1, 0:2 * D].partition_broadcast(128)), writes=['MODC'])
                dma('sp', lambda e: e.dma_start(out=nbc[:, 0:D], in_=n1_d[0:1, :].partition_broadcast(128)), writes=['nbc0'])
                dma('sp', lambda e: e.dma_start(out=nbc[:, D:2 * D], in_=n2_d[0:1, :].partition_broadcast(128)), writes=['nbc1'])
                op('act', lambda e: e.activation(out=c2t[:], in_=c2t[:], func=AF.Silu), reads=['c2t'], writes=['c2t'])
                op('dve', lambda e: e.memset(onesb[:], 1.0), writes=['onesb'])
                for t in range(2):
                    for k in range(8):
                        op('dve', lambda e, t=t, k=k: e.tensor_scalar(out=Lm[:, t, k, :], in0=onesb[:], scalar1=c2t[:, t, k:k + 1], scalar2=None, op0=ALU.mult),
                           reads=['c2t', 'onesb'], writes=['Lm'])
                wmv = wmod_d.rearrange("(p k) n -> p k n", k=8)
                for j in range(12):
                    w = wm[j % 2]
                    wt = 'wm%d' % (j % 2)
                    dma('pool', lambda e, j=j, w=w: e.dma_start(out=w[:], in_=wmv[:, :, j * 512:(j + 1) * 512]), writes=[wt])
                    for t in range(2 if j < 4 else 1):
                        ps = PF[t]
                        for k in range(8):
                            op('pe', lambda e, t=t, k=k, w=w, ps=ps: e.matmul(out=ps[:], lhsT=Lm[:, t, k, :], rhs=w[:, k, :], start=(k == 0), stop=(k == 7)),
                               reads=['Lm', wt], writes=['pfA%d' % t])
                        M = MODL if t == 0 else MODC
                        op('dve', lambda e, M=M, j=j, ps=ps: e.tensor_tensor(out=M[:, j * 512:(j + 1) * 512], in0=M[:, j * 512:(j + 1) * 512], in1=ps[:], op=ALU.add),
                           reads=['pfA%d' % t, 'MODL', 'MODC'], writes=['MODL' if t == 0 else 'MODC'])
                op('dve', lambda e: e.scalar_tensor_tensor(out=MODL[:, D:2 * D], in0=MODL[:, D:2 * D], scalar=1.0, in1=nbc[:, 0:D], op0=ALU.add, op1=ALU.mult), reads=['MODL', 'nbc0'], writes=['MODL'])
                op('dve', lambda e: e.scalar_tensor_tensor(out=MODC[:, D:2 * D], in0=MODC[:, D:2 * D], scalar=1.0, in1=nbc[:, 0:D], op0=ALU.add, op1=ALU.mult), reads=['MODC', 'nbc0'], writes=['MODC'])
                op('dve', lambda e: e.scalar_tensor_tensor(out=MODL[:, 4 * D:5 * D], in0=MODL[:, 4 * D:5 * D], scalar=1.0, in1=nbc[:, D:2 * D], op0=ALU.add, op1=ALU.mult), reads=['MODL', 'nbc1'], writes=['MODL'])

                PB = [s1.enter_context(nc.psum_tensor("pbB%d" % i, [128, 8, 128], BF16)) for i in range(2)]
                xt = [S("xt%d" % i, [128, D]) for i in range(2)]
                junk = S("junk", [128, D])
                hb = [S("hb%d" % i, [128, D], BF16) for i in range(2)]
                ssq = S("ssq", [128, 18])
                rs = S("rs", [128, 18])
                for i in range(18):
                    xb = xt[i % 2]
                    xtk = 'xt%d' % (i % 2)
                    hbb = hb[i % 2]
                    hbk = 'hb%d' % (i % 2)
                    src = x_d[i * 128:(i + 1) * 128, :] if i < 16 else ctx_d[(i - 16) * 128:(i - 15) * 128, :]
                    M = MODL if i < 16 else MODC
                    Mk = 'MODL' if i < 16 else 'MODC'
                    dma('sp', lambda e, xb=xb, src=src: e.dma_start(out=xb[:], in_=src), writes=[xtk])
                    op('act', lambda e, xb=xb, i=i: e.activation(out=junk[:], in_=xb[:], func=AF.Square, accum_out=ssq[:, i:i + 1]), reads=[xtk], writes=['junk', 'ssq%d' % i])
                    rstd_from_ssq(ssq[:, i:i + 1], rs[:, i:i + 1], 1.0 / D, ['ssq%d' % i], ['rs%d' % i])
                    op('dve', lambda e, xb=xb, i=i, M=M: e.scalar_tensor_tensor(out=junk[:], in0=xb[:], scalar=rs[:, i:i + 1], in1=M[:, D:2 * D], op0=ALU.mult, op1=ALU.mult),
                       reads=[xtk, 'rs%d' % i, Mk], writes=['junk'])
                    op('dve', lambda e, hbb=hbb, M=M: e.tensor_tensor(out=hbb[:], in0=junk[:], in1=M[:, 0:D], op=ALU.add), reads=['junk', Mk], writes=[hbk])
                    pb = PB[i % 2]
                    pbk = 'pbB%d' % (i % 2)
                    for k in range(8):
                        op('pe', lambda e, pb=pb, hbb=hbb, k=k: e.transpose(out=pb[:, k, :], in_=hbb[:, k * 128:(k + 1) * 128], identity=identb[:]), reads=[hbk, 'identb'], writes=[pbk])
                    op('act', lambda e, pb=pb, i=i: e.copy(out=hT[:, :, i * 128:(i + 1) * 128], in_=pb[:]), reads=[pbk], writes=[('hT', i)])
                kb.barrier()
                if stop == 'B':
                    return fin()
            hT_all = [('hT', i) for i in range(18)]
            if dbg:
                with ExitStack() as s1:
                    tmp = s1.enter_context(nc.sbuf_tensor("dbgt", [128, 8 * NT], F32))
                    op('dve', lambda e: e.tensor_copy(out=tmp[:], in_=hT[:].rearrange("p k n -> p (k n)")), reads=hT_all, writes=['dbgt'])
                    dma('sp', lambda e: e.dma_start(out=dbg_d["d_hT"][:, :], in_=tmp[:]), reads=['dbgt'], writes=['d_hT'])
                    kb.barrier()

            with ExitStack() as s1:
                S = lambda name, shape, dt=F32: s1.enter_context(nc.sbuf_tensor("t_" + name, shape, dt))
                PF = [s1.enter_context(nc.psum_tensor("pfC%d" % i, [128, 512], F32)) for i in range(8)]
                wna = S("wna", [128, 8, 1536], BF16)
                qT = S("qT", [128, 4, T], BF16)
                kT = S("kT", [128, 4, NT], BF16)
                V = S("V", [128, 18, 512], BF16)
                onesb = S("onesbC", [128, 64], BF16)
                tb2 = [S("tb2_%d" % i, [128, 2, 1024], BF16) for i in range(2)]
                PT = [S("PT%d" % i, [128, 256], BF16) for i in range(4)]
                rcp = [S("rcp%d" % i, [128, 256]) for i in range(2)]
                op('dve', lambda e: e.memset(onesb[:], 1.0), writes=['onesbC'])
                for j in range(3):
                    dma('pool', lambda e, j=j: e.dma_start(out=wna[:, :, j * 512:(j + 1) * 512], in_=winv[:, :, j * 512:(j + 1) * 512]), writes=[('wna', j)])
                pfi = [0]

                def nextpf():
                    i = pfi[0] % 8
                    pfi[0] += 1
                    return PF[i], 'pfC%d' % i
                for which in range(2):
                    for p in range(4):
                        ntb = 4 if which == 0 else 5
                        for tb in range(ntb):
                            n0 = tb * 512
                            nn = 512 if tb < 4 else 256
                            ps, pk = nextpf()
                            for k in range(8):
                                op('pe', lambda e, ps=ps, k=k, which=which, p=p, n0=n0, nn=nn: e.matmul(out=ps[:, 0:nn], lhsT=wna[:, k, which * 512 + p * 128: which * 512 + (p + 1) * 128],
                                                                                                     rhs=hT[:, k, n0:n0 + nn], start=(k == 0), stop=(k == 7)),
                                   reads=[('wna', which)] + [('hT', t) for t in range(n0 // 128, (n0 + nn) // 128)], writes=[pk])
                            if which == 0:
                                op('act', lambda e, ps=ps, p=p, n0=n0: e.mul(out=qT[:, p, n0:n0 + 512], in_=ps[:], mul=0.125), reads=[pk], writes=[('qT', p, tb)])
                            else:
                                op('dve', lambda e, ps=ps, p=p, n0=n0, nn=nn: e.tensor_copy(out=kT[:, p, n0:n0 + nn], in_=ps[:, 0:nn]), reads=[pk], writes=[('kT', p, tb)])
                for i in range(18):
                    ps, pk = nextpf()
                    for k in range(8):
                        op('pe', lambda e, ps=ps, k=k, i=i: e.matmul(out=ps[:], lhsT=hT[:, k, i * 128:(i + 1) * 128], rhs=wna[:, k, 1024:1536], start=(k == 0), stop=(k == 7)),
                           reads=[('wna', 2), ('hT', i)], writes=[pk])
                    op('act', lambda e, ps=ps, i=i: e.copy(out=V[:, i, :], in_=ps[:]), reads=[pk], writes=[('V', i)])
                PST = PF[0:4]
                PPV = PF[4:6]
                PDN = PF[6:8]
                cnt = 0
                for h in range(8):
                    p, half = h // 2, h % 2
                    hs = slice(half * 64, half * 64 + 64)
                    tbt = tb2[h % 2]
                    tbk = 'tb2_%d' % (h % 2)
                    dma('pool', lambda e, tbt=tbt, h=h: e.dma_start(out=tbt[:], in_=natbl_d[h].rearrange("t p n -> p t n")), writes=[tbk])
                    for qb in range(8):
                        if qb == 0:
                            tiles, tsel = list(range(0, 4)), 0
                        elif qb == 7:
                            tiles, tsel = list(range(12, 16)), 0
                        else:
                            tiles, tsel = list(range(2 * qb - 2, 2 * qb + 4)), 1
                        keyt = [(m, 4 * qb - 2 * m + 7) for m in tiles] + [(16, None), (17, None)]
                        ppv, pdn = PPV[qb % 2], PDN[qb % 2]
                        ppk, pdk = 'pfC%d' % (4 + qb % 2), 'pfC%d' % (6 + qb % 2)
                        q0 = qb * 256
                        for ti, (m, j0) in enumerate(keyt):
                            pst, pstk = PST[cnt % 4], 'pfC%d' % (cnt % 4)
                            ptt, ptk = PT[cnt % 4], 'PT%d' % (cnt % 4)
                            cnt += 1
                            op('pe', lambda e, pst=pst, m=m, j0=j0: e.matmul(out=pst[:, 0:256], lhsT=kT[hs, p, m * 128:(m + 1) * 128], rhs=qT[hs, p, q0:q0 + 256], start=True, stop=(j0 is None)),
                               reads=[('kT', p, min(m // 4, 4)), ('qT', p, qb // 2)], writes=[pstk])
                            if j0 is not None:
                                op('pe', lambda e, pst=pst, j0=j0: e.matmul(out=pst[:, 0:256], lhsT=identb[:], rhs=tbt[:, tsel, j0 * 64:(j0 + 4) * 64], start=False, stop=True),
                                   reads=[tbk, 'identb'], writes=[pstk])
                            op('act', lambda e, pst=pst, ptt=ptt: e.activation(out=ptt[:], in_=pst[:, 0:256], func=AF.Exp), reads=[pstk], writes=[ptk])
                            first, last = ti == 0, ti == len(keyt) - 1
                            op('pe', lambda e, ptt=ptt, m=m, first=first, last=last: e.matmul(out=ppv[hs, 0:256], lhsT=V[:, m, h * 64:(h + 1) * 64], rhs=ptt[:], start=first, stop=last),
                               reads=[ptk, ('V', m)], writes=[ppk])
                            op('pe', lambda e, ptt=ptt, first=first, last=last: e.matmul(out=pdn[hs, 0:256], lhsT=onesb[:], rhs=ptt[:], start=first, stop=last),
                               reads=[ptk, 'onesbC'], writes=[pdk])
                        rc = rcp[qb % 2]
                        rck = 'rcp%d' % (qb % 2)
                        op('dve', lambda e, rc=rc, pdn=pdn: e.reciprocal(out=rc[hs, :], in_=pdn[hs, 0:256]), reads=[pdk], writes=[rck])
                        op('dve', lambda e, rc=rc, ppv=ppv: e.tensor_tensor(out=onaT[hs, p, q0:q0 + 256], in0=ppv[hs, 0:256], in1=rc[hs, :], op=ALU.mult), reads=[ppk, rck], writes=[('onaT', p, qb)])
                kb.barrier()
                if stop == 'C':
                    return fin()
            onaT_all = [('onaT', p, qb) for p in range(4) for qb in range(8)]
            if dbg:
                with ExitStack() as s1:
                    tmp = s1.enter_context(nc.sbuf_tensor("dbgt2", [128, 4 * T], F32))
                    op('dve', lambda e: e.tensor_copy(out=tmp[:], in_=onaT[:].rearrange("p k n -> p (k n)")), reads=onaT_all, writes=['dbgt2'])
                    dma('sp', lambda e: e.dma_start(out=dbg_d["d_ona"][:, :], in_=tmp[:]), reads=['dbgt2'], writes=['d_ona'])
                    kb.barrier()

            id64 = cst[0:64, C_ID:C_ID + 64]
            on64 = cst[0:64, C_ONE:C_ONE + 64]
            b3 = lambda ap8: ap8.unsqueeze(2).to_broadcast([64, 8, 64])
            m3 = lambda ap64: ap64.unsqueeze(1).to_broadcast([64, 8, 64])
            with ExitStack() as s1:
                S = lambda name, shape, dt=F32: s1.enter_context(nc.sbuf_tensor("t_" + name, shape, dt))
                PF = [s1.enter_context(nc.psum_tensor("pfD%d" % i, [128, 512], F32)) for i in range(8)]
                pfi = [0]

                def nextpf():
                    i = pfi[0] % 8
                    pfi[0] += 1
                    return PF[i], 'pfD%d' % i
                wdn = S("wdn", [128, 8, 1536], BF16)
                wba = S("wba", [128, 8, 32], BF16)
                convw = S("convw", [128, 60])
                ropet = S("ropet", [64, 2, 32, 32])
                zl = [S("zl%d" % i, [128, 2052]) for i in range(2)]
                zx = [S("zx%d" % i, [128, 260]) for i in range(2)]
                acc = S("acc", [128, NT])
                sl = S("sl", [128, NT])
                sqt = S("sqt", [64, 512])
                ssq8 = S("ssq8", [64, 8])
                r8 = S("r8", [64, 8])
                st = [S("st%d" % i, [64, 4, 128]) for i in range(2)]
                st2 = [S("st2%d" % i, [64, 4, 128]) for i in range(2)]
                ra = S("ra", [64, 4, 2, 32])
                rb = S("rb", [64, 4, 2, 32])
                BA = S("BA", [64, 36, 32])
                BETA = S("BETA", [64, 36, 16])
                GG = S("GG", [64, 36, 16])
                ta = S("ta", [64, 36, 16])
                tb_ = S("tb_", [64, 36, 16])
                tc_ = S("tc_", [64, 36, 16])
                ab = S("ab", [64, 32])
                one1 = S("one1", [64, 1])
                for j in range(3):
                    dma('pool', lambda e, j=j: e.dma_start(out=wdn[:, :, j * 512:(j + 1) * 512], in_=winv[:, :, 1536 + j * 512:1536 + (j + 1) * 512]), writes=[('wdn', j)])
                dma('pool', lambda e: e.dma_start(out=wba[:], in_=winv[:, :, 3584:3616]), writes=['wba'])
                dma('sp', lambda e: e.dma_start(out=convw[:], in_=convw_d[:, :]), writes=['convw'])
                dma('sp', lambda e: e.dma_start(out=ropet[:], in_=rope_d.rearrange("p (a c f) -> p a c f", a=2, c=32)), writes=['ropet'])
                dma('sp', lambda e: e.dma_start(out=ab[:, 0:16], in_=alog_d[0:1, :].partition_broadcast(64)), writes=['ab'])
                dma('sp', lambda e: e.dma_start(out=ab[:, 16:32], in_=dtb_d[0:1, :].partition_broadcast(64)), reads=[], writes=['ab2'])
                op('dve', lambda e: e.memset(one1[:], 1.0), writes=['one1'])
                for i in range(2):
                    op('dve', lambda e, i=i: e.memset(zl[i][:], 0.0), writes=['zl%d' % i])
                    op('dve', lambda e, i=i: e.memset(zx[i][:], 0.0), writes=['zx%d' % i])
                for g3 in range(3):
                    ps, pk = nextpf()
                    for c in range(12):
                        n = g3 * 12 + c
                        for k in range(8):
                            op('pe', lambda e, ps=ps, c=c, n=n, k=k: e.matmul(out=ps[0:64, c * 32:(c + 1) * 32], lhsT=hT[:, k, n * 64:(n + 1) * 64], rhs=wba[:, k, :], start=(k == 0), stop=(k == 7)),
                               reads=['wba', ('hT', n // 2)], writes=[pk])
                    op('act', lambda e, ps=ps, g3=g3: e.copy(out=BA[:, g3 * 12:(g3 + 1) * 12, :], in_=ps[0:64, 0:384].rearrange("p (c f) -> p c f", f=32)), reads=[pk], writes=['BA'])
                op('act', lambda e: e.activation(out=BETA[:], in_=BA[:, :, 0:16], func=AF.Sigmoid), reads=['BA'], writes=['BETA'])
                op('dve', lambda e: e.tensor_tensor(out=ta[:], in0=BA[:, :, 16:32], in1=ab[:, 16:32].unsqueeze(1).to_broadcast([64, 36, 16]), op=ALU.add), reads=['BA', 'ab2'], writes=['ta'])
                op('dve', lambda e: e.tensor_scalar(out=tb_[:], in0=ta[:], scalar1=-1.0, scalar2=None, op0=ALU.mult), reads=['ta'], writes=['tb_'])
                op('dve', lambda e: e.tensor_tensor(out=tb_[:], in0=tb_[:], in1=ta[:], op=ALU.max), reads=['ta', 'tb_'], writes=['tb_'])
                op('act', lambda e: e.activation(out=tb_[:], in_=tb_[:], func=AF.Exp, scale=-1.0), reads=['tb_'], writes=['tb_'])
                op('act', lambda e: e.activation(out=tb_[:], in_=tb_[:], func=AF.Ln, bias=one1[:]), reads=['tb_', 'one1'], writes=['tb_'])
                op('dve', lambda e: e.tensor_scalar_max(out=tc_[:], in0=ta[:], scalar1=0.0), reads=['ta'], writes=['tc_'])
                op('dve', lambda e: e.tensor_tensor(out=tc_[:], in0=tc_[:], in1=tb_[:], op=ALU.add), reads=['tc_', 'tb_'], writes=['tc_'])
                op('act', lambda e: e.activation(out=ab[:, 0:16], in_=ab[:, 0:16], func=AF.Exp), reads=['ab'], writes=['ab'])
                op('dve', lambda e: e.scalar_tensor_tensor(out=GG[:], in0=tc_[:], scalar=-1.0, in1=ab[:, 0:16].unsqueeze(1).to_broadcast([64, 36, 16]), op0=ALU.mult, op1=ALU.mult),
                   reads=['tc_', 'ab'], writes=['GG'])
                for cc in range(12):
                    z_l, z_x = zl[cc % 2], zx[cc % 2]
                    zlk, zxk = 'zl%d' % (cc % 2), 'zx%d' % (cc % 2)
                    for tb in range(5):
                        n0 = tb * 512
                        nn = 512 if tb < 4 else 256
                        ps, pk = nextpf()
                        for k in range(8):
                            op('pe', lambda e, ps=ps, k=k, n0=n0, nn=nn: e.matmul(out=ps[:, 0:nn], lhsT=wdn[:, k, cc * 128:(cc + 1) * 128], rhs=hT[:, k, n0:n0 + nn], start=(k == 0), stop=(k == 7)),
                               reads=[('wdn', cc // 4)] + [('hT', t) for t in range(n0 // 128, (n0 + nn) // 128)], writes=[pk])
                        if tb < 4:
                            op('act', lambda e, ps=ps, n0=n0: e.copy(out=z_l[:, 2 + n0:2 + n0 + 512], in_=ps[:]), reads=[pk], writes=[zlk])
                        else:
                            op('act', lambda e, ps=ps: e.copy(out=z_x[:, 2:258], in_=ps[:, 0:256]), reads=[pk], writes=[zxk])
                    for (zs, zk, a0, n) in ((z_l, zlk, 0, 2048), (z_x, zxk, 2048, 256)):
                        op('dve', lambda e, zs=zs, a0=a0, n=n: e.tensor_scalar(out=acc[:, a0:a0 + n], in0=zs[:, 0:n], scalar1=convw[:, cc * 5:cc * 5 + 1], scalar2=None, op0=ALU.mult),
                           reads=[zk, 'convw'], writes=['acc'])
                        for tap in range(1, 5):
                            op('dve', lambda e, zs=zs, a0=a0, n=n, tap=tap: e.scalar_tensor_tensor(out=acc[:, a0:a0 + n], in0=zs[:, tap:tap + n], scalar=convw[:, cc * 5 + tap:cc * 5 + tap + 1],
                                                                                               in1=acc[:, a0:a0 + n], op0=ALU.mult, op1=ALU.add), reads=[zk, 'convw', 'acc'], writes=['acc'])
                    op('act', lambda e: e.activation(out=sl[:], in_=acc[:], func=AF.Silu), reads=['acc'], writes=['sl'])
                    for g in range(9):
                        ps, pk = nextpf()
                        for c4 in range(4):
                            n = 4 * g + c4
                            op('pe', lambda e, ps=ps, c4=c4, n=n: e.transpose(out=ps[0:64, c4 * 128:(c4 + 1) * 128], in_=sl[:, n * 64:(n + 1) * 64], identity=ident), reads=['sl', 'cst'], writes=[pk])
                        sti, stk = st[g % 2], 'st%d' % (g % 2)
                        if cc < 8:
                            op('act', lambda e, ps=ps: e.activation(out=sqt[:], in_=ps[0:64, :], func=AF.Square), reads=[pk], writes=['sqt'])
                            op('dve', lambda e: e.tensor_reduce(out=ssq8[:], in_=sqt[:].rearrange("p (g f) -> p g f", f=64), axis=AX.X, op=ALU.add), reads=['sqt'], writes=['ssq8'])
                            rstd_from_ssq(ssq8[:], r8[:], 1.0, ['ssq8'], ['r8'])
                            op('dve', lambda e, ps=ps, sti=sti: e.tensor_tensor(out=sti[:].rearrange("p c (h f) -> p (c h) f", h=2), in0=ps[0:64, :].rearrange("p (g f) -> p g f", f=64),
                                                                            in1=b3(r8[:]), op=ALU.mult), reads=[pk, 'r8'], writes=[stk])
                            if g < 8:
                                s2i, s2k = st2[g % 2], 'st2%d' % (g % 2)
                                x5 = sti[:].rearrange("p c (h t f) -> p c h t f", h=2, t=2)
                                o5 = s2i[:].rearrange("p c (h t f) -> p c h t f", h=2, t=2)
                                cosb = ropet[:, 0, 4 * g:4 * g + 4, :].unsqueeze(2).to_broadcast([64, 4, 2, 32])
                                sinb = ropet[:, 1, 4 * g:4 * g + 4, :].unsqueeze(2).to_broadcast([64, 4, 2, 32])
                                op('dve', lambda e: e.tensor_tensor(out=ra[:], in0=x5[:, :, :, 0, :], in1=cosb, op=ALU.mult), reads=[stk, 'ropet'], writes=['ra'])
                                op('pool', lambda e: e.tensor_tensor(out=rb[:], in0=x5[:, :, :, 1, :], in1=sinb, op=ALU.mult), reads=[stk, 'ropet'], writes=['rb'])
                                op('dve', lambda e: e.tensor_tensor(out=o5[:, :, :, 0, :], in0=ra[:], in1=rb[:], op=ALU.subtract), reads=['ra', 'rb'], writes=[s2k])
                                op('dve', lambda e: e.tensor_tensor(out=ra[:], in0=x5[:, :, :, 0, :], in1=sinb, op=ALU.mult), reads=[stk, 'ropet'], writes=['ra'])
                                op('pool', lambda e: e.tensor_tensor(out=rb[:], in0=x5[:, :, :, 1, :], in1=cosb, op=ALU.mult), reads=[stk, 'ropet'], writes=['rb'])
                                op('dve', lambda e: e.tensor_tensor(out=o5[:, :, :, 1, :], in0=ra[:], in1=rb[:], op=ALU.add), reads=['ra', 'rb'], writes=[s2k])
                                sti, stk = s2i, s2k
                        else:
                            op('act', lambda e, ps=ps, sti=sti: e.copy(out=sti[:].rearrange("p c f -> p (c f)"), in_=ps[0:64, :]), reads=[pk], writes=[stk])
                        dma('sp', lambda e, sti=sti, g=g: e.dma_start(out=qkv_s[4 * g:4 * g + 4, :, cc * 128:(cc + 1) * 128].rearrange("c p f -> p c f"), in_=sti[:]), reads=[stk], writes=[('qkv_s', g)])
                kb.barrier()
                if stop == 'D1':
                    return fin()
            with ExitStack() as s1:
                S = lambda name, shape, dt=F32: s1.enter_context(nc.sbuf_tensor("t_" + name, shape, dt))
                PF = [s1.enter_context(nc.psum_tensor("pfE%d" % i, [128, 512], F32)) for i in range(8)]
                pfi = [0]

                def nextpf():
                    i = pfi[0] % 8
                    pfi[0] += 1
                    return PF[i], 'pfE%d' % i
                wba = S("wba2", [128, 8, 32], BF16)
                BA = S("BA2", [64, 36, 32])
                BETA = S("BETA2", [64, 36, 16])
                GG = S("GG2", [64, 36, 16])
                ta = S("ta2", [64, 36, 16])
                tb_ = S("tb2_", [64, 36, 16])
                tc_ = S("tc2_", [64, 36, 16])
                ab = S("ab2", [64, 32])
                one1 = S("one12", [64, 1])
                dma('pool', lambda e: e.dma_start(out=wba[:], in_=winv[:, :, 3584:3616]), writes=['wba'])
                dma('sp', lambda e: e.dma_start(out=ab[:, 0:16], in_=alog_d[0:1, :].partition_broadcast(64)), writes=['ab'])
                dma('sp', lambda e: e.dma_start(out=ab[:, 16:32], in_=dtb_d[0:1, :].partition_broadcast(64)), reads=[], writes=['ab2'])
                op('dve', lambda e: e.memset(one1[:], 1.0), writes=['one1'])
                for g3 in range(3):
                    ps, pk = nextpf()
                    for c in range(12):
                        n = g3 * 12 + c
                        for k in range(8):
                            op('pe', lambda e, ps=ps, c=c, n=n, k=k: e.matmul(out=ps[0:64, c * 32:(c + 1) * 32], lhsT=hT[:, k, n * 64:(n + 1) * 64], rhs=wba[:, k, :], start=(k == 0), stop=(k == 7)),
                               reads=['wba', ('hT', n // 2)], writes=[pk])
                    op('act', lambda e, ps=ps, g3=g3: e.copy(out=BA[:, g3 * 12:(g3 + 1) * 12, :], in_=ps[0:64, 0:384].rearrange("p (c f) -> p c f", f=32)), reads=[pk], writes=['BA'])
                op('act', lambda e: e.activation(out=BETA[:], in_=BA[:, :, 0:16], func=AF.Sigmoid), reads=['BA'], writes=['BETA'])
                op('dve', lambda e: e.tensor_tensor(out=ta[:], in0=BA[:, :, 16:32], in1=ab[:, 16:32].unsqueeze(1).to_broadcast([64, 36, 16]), op=ALU.add), reads=['BA', 'ab2'], writes=['ta'])
                op('dve', lambda e: e.tensor_scalar(out=tb_[:], in0=ta[:], scalar1=-1.0, scalar2=None, op0=ALU.mult), reads=['ta'], writes=['tb_'])
                op('dve', lambda e: e.tensor_tensor(out=tb_[:], in0=tb_[:], in1=ta[:], op=ALU.max), reads=['ta', 'tb_'], writes=['tb_'])
                op('act', lambda e: e.activation(out=tb_[:], in_=tb_[:], func=AF.Exp, scale=-1.0), reads=['tb_'], writes=['tb_'])
                op('act', lambda e: e.activation(out=tb_[:], in_=tb_[:], func=AF.Ln, bias=one1[:]), reads=['tb_', 'one1'], writes=['tb_'])
                op('dve', lambda e: e.tensor_scalar_max(out=tc_[:], in0=ta[:], scalar1=0.0), reads=['ta'], writes=['tc_'])
                op('dve', lambda e: e.tensor_tensor(out=tc_[:], in0=tc_[:], in1=tb_[:], op=ALU.add), reads=['tc_', 'tb_'], writes=['tc_'])
                op('act', lambda e: e.activation(out=ab[:, 0:16], in_=ab[:, 0:16], func=AF.Exp), reads=['ab'], writes=['ab'])
                op('dve', lambda e: e.scalar_tensor_tensor(out=GG[:], in0=tc_[:], scalar=-1.0, in1=ab[:, 0:16].unsqueeze(1).to_broadcast([64, 36, 16]), op0=ALU.mult, op1=ALU.mult),
                   reads=['tc_', 'ab'], writes=['GG'])

                ld = [S("ld%d" % i, [64, 1536]) for i in range(2)]
                names = ["gc", "gl", "et", "egl", "eg", "bg", "nb", "t8"]
                sm = {n_: S("sm_" + n_, [64, 8]) for n_ in names}
                bigs = ["Rt", "egrow", "Dm", "Dmi", "Dms", "qdT", "NegA", "QKm", "X0", "QKT", "Z", "Xa", "XTa", "Xb", "XTb", "vb", "kbg", "ktail", "u", "wT", "vnew", "Sst", "ost"]
                bigs = bigs + ["NegAb", "Zb"]
                NB16 = ("X0", "Xa", "Xb", "XTa", "XTb", "NegAb", "Zb")
                bg_ = {n_: S("bg_" + n_, [64, 8, 64], BF16 if n_ in NB16 else F32) for n_ in bigs}
                qkT = S("qkT", [64, 16, 64])
                fl = lambda t_: t_[:].rearrange("p h f -> p (h f)")

                def headmm(lhs, rhs, reads, start=True, stop=True, ps=None, pk=None):
                    if ps is None:
                        ps, pk = nextpf()
                    for h in range(8):
                        op('pe', lambda e, h=h: e.matmul(out=ps[0:64, h * 64:(h + 1) * 64], lhsT=lhs(h), rhs=rhs(h), start=start, stop=stop), reads=reads, writes=[pk])
                    return ps, pk

                def headtr(src, srck):
                    ps, pk = nextpf()
                    for h in range(8):
                        op('pe', lambda e, h=h: e.transpose(out=ps[0:64, h * 64:(h + 1) * 64], in_=src(h), identity=id64), reads=[srck, 'cst'], writes=[pk])
                    return ps, pk
                p3 = lambda ps: ps[0:64, :].rearrange("p (h f) -> p h f", f=64)
                B = bg_
                if sub == 'g':
                    kb.barrier()
                    return fin()
                for d in range(2 if sub is None else 1):
                    CU = cst[0:64, C_U:C_U + 64] if d == 0 else cst[0:64, C_L:C_L + 64]
                    incl = cst[0:64, C_IF:C_IF + 64] if d == 0 else cst[0:64, C_IB:C_IB + 64]
                    strict = cst[0:64, C_SF:C_SF + 64] if d == 0 else cst[0:64, C_SB:C_SB + 64]
                    last = 63 if d == 0 else 0
                    order = ([32, 33, 34, 35] + list(range(32))) if d == 0 else ([35, 34, 33, 32] + list(range(31, -1, -1)))
                    op('dve', lambda e: e.memset(B["Sst"][:], 0.0), writes=['Sst'])
                    for it, n in enumerate(order):
                        L_, lk = ld[it % 2], 'ld%d' % (it % 2)
                        dma('sp', lambda e, L_=L_, n=n: e.dma_start(out=L_[:], in_=qkv_s[n, :, :]), reads=[('qkv_s', n // 4)], writes=[lk])
                        q3 = L_[:, 0:512].rearrange("p (h f) -> p h f", f=64)
                        k3 = L_[:, 512:1024].rearrange("p (h f) -> p h f", f=64)
                        v3 = L_[:, 1024:1536].rearrange("p (h f) -> p h f", f=64)
                        g = GG[:, n, d * 8:(d + 1) * 8]
                        beta = BETA[:, n, d * 8:(d + 1) * 8]
                        ps, pk = nextpf()
                        op('pe', lambda e, ps=ps: e.matmul(out=ps[0:64, 0:8], lhsT=CU, rhs=g, start=True, stop=True), reads=['GG', 'cst'], writes=[pk])
                        op('dve', lambda e, ps=ps: e.tensor_copy(out=sm["gc"][:], in_=ps[0:64, 0:8]), reads=[pk], writes=['gc'])
                        if sub == 's1':
                            kb.barrier()
                            return fin()
                        op('dve', lambda e: e.tensor_tensor(out=B["Rt"][:], in0=m3(id64), in1=b3(sm["gc"][:]), op=ALU.mult), reads=['gc', 'cst'], writes=['Rt'])
                        psA, pkA = nextpf()
                        op('pe', lambda e: e.matmul(out=psA[0:64, :], lhsT=on64, rhs=fl(B["Rt"]), start=True, stop=True), reads=['Rt', 'cst'], writes=[pkA])
                        op('act', lambda e: e.activation(out=fl(B["egrow"]), in_=psA[0:64, :], func=AF.Exp), reads=[pkA], writes=['egrow'])
                        op('dve', lambda e: e.tensor_tensor(out=B["Dm"][:], in0=b3(sm["gc"][:]), in1=p3(psA), op=ALU.subtract), reads=[pkA, 'gc'], writes=['Dm'])
                        op('act', lambda e: e.copy(out=sm["gl"][:], in_=p3(psA)[:, :, last]), reads=[pkA], writes=['gl'])
                        op('dve', lambda e: e.tensor_scalar_min(out=B["Dm"][:], in0=B["Dm"][:], scalar1=0.0), reads=['Dm'], writes=['Dm'])
                        op('act', lambda e: e.activation(out=B["Dm"][:], in_=B["Dm"][:], func=AF.Exp), reads=['Dm'], writes=['Dm'])
                        op('pool', lambda e: e.tensor_tensor(out=B["Dmi"][:], in0=B["Dm"][:], in1=m3(incl), op=ALU.mult), reads=['Dm', 'cst'], writes=['Dmi'])
                        op('pool', lambda e: e.tensor_tensor(out=B["Dms"][:], in0=B["Dm"][:], in1=m3(strict), op=ALU.mult), reads=['Dm', 'cst'], writes=['Dms'])
                        op('dve', lambda e: e.tensor_tensor(out=sm["t8"][:], in0=sm["gl"][:], in1=sm["gc"][:], op=ALU.subtract), reads=['gl', 'gc'], writes=['t8'])
                        op('act', lambda e: e.activation(out=sm["et"][:], in_=sm["t8"][:], func=AF.Exp), reads=['t8'], writes=['et'])
                        op('act', lambda e: e.activation(out=sm["egl"][:], in_=sm["gl"][:], func=AF.Exp), reads=['gl'], writes=['egl'])
                        op('act', lambda e: e.activation(out=sm["eg"][:], in_=sm["gc"][:], func=AF.Exp), reads=['gc'], writes=['eg'])
                        op('dve', lambda e: e.tensor_tensor(out=sm["bg"][:], in0=sm["eg"][:], in1=beta, op=ALU.mult), reads=['eg', 'BETA'], writes=['bg'])
                        op('dve', lambda e: e.tensor_scalar(out=sm["nb"][:], in0=beta, scalar1=-1.0, scalar2=None, op0=ALU.mult), reads=['BETA'], writes=['nb'])
                        if sub == 's2':
                            kb.barrier()
                            return fin()
                        psQ, pkQ = headtr(lambda h: q3[:, h, :], lk)
                        psK, pkK = headtr(lambda h: k3[:, h, :], lk)
                        op('act', lambda e: e.copy(out=qkT[:, 0:8, :], in_=p3(psQ)), reads=[pkQ], writes=['qT_'])
                        op('dve', lambda e: e.tensor_copy(out=qkT[:, 8:16, :], in_=p3(psK)), reads=[pkK], writes=['kT_'])
                        op('dve', lambda e: e.scalar_tensor_tensor(out=B["qdT"][:], in0=qkT[:, 0:8, :], scalar=0.125, in1=B["egrow"][:], op0=ALU.mult, op1=ALU.mult), reads=['qT_', 'egrow'], writes=['qdT'])
                        if sub == 's3':
                            kb.barrier()
                            return fin()
                        psB, pkB = headmm(lambda h: qkT[:, 8 + h, :], lambda h: qkT[:, 8 + h, :], ['kT_'])
                        psC, pkC = headmm(lambda h: qkT[:, h, :], lambda h: qkT[:, 8 + h, :], ['kT_', 'qT_'])
                        op('dve', lambda e: e.tensor_tensor(out=B["NegA"][:], in0=p3(psB), in1=B["Dms"][:], op=ALU.mult), reads=[pkB, 'Dms'], writes=['NegA'])
                        op('dve', lambda e: e.tensor_tensor(out=B["NegA"][:], in0=B["NegA"][:], in1=b3(sm["nb"][:]), op=ALU.mult), reads=['NegA', 'nb'], writes=['NegA'])
                        op('act', lambda e: e.copy(out=B["NegAb"][:], in_=B["NegA"][:]), reads=['NegA'], writes=['NegAb'])
                        op('dve', lambda e: e.scalar_tensor_tensor(out=B["QKm"][:], in0=p3(psC), scalar=0.125, in1=B["Dmi"][:], op0=ALU.mult, op1=ALU.mult), reads=[pkC, 'Dmi'], writes=['QKm'])
                        if sub == 's4':
                            kb.barrier()
                            return fin()
                        psD, pkD = headtr(lambda h: B["NegA"][:, h, :], 'NegA')
                        psE, pkE = headtr(lambda h: B["QKm"][:, h, :], 'QKm')
                        op('act', lambda e: e.copy(out=B["X0"][:], in_=p3(psD)), reads=[pkD], writes=['X0'])
                        op('dve', lambda e: e.tensor_tensor(out=B["Z"][:], in0=p3(psD), in1=m3(id64), op=ALU.add), reads=[pkD, 'cst'], writes=['Z'])
                        op('act', lambda e: e.copy(out=B["Zb"][:], in_=B["Z"][:]), reads=['Z'], writes=['Zb'])
                        op('act', lambda e: e.copy(out=B["QKT"][:], in_=p3(psE)), reads=[pkE], writes=['QKT'])
                        if sub == 's5':
                            kb.barrier()
                            return fin()
                        X, Xk, XT, XTk = B["X0"], 'X0', B["NegAb"], 'NegAb'
                        for lvl in range(1, 6):
                            nX, nXk = (B["Xa"], 'Xa') if lvl % 2 == 1 else (B["Xb"], 'Xb')
                            nXT, nXTk = (B["XTa"], 'XTa') if lvl % 2 == 1 else (B["XTb"], 'XTb')
                            ps1, pk1 = headmm(lambda h, X=X: X[:, h, :], lambda h, XT=XT: XT[:, h, :], [Xk, XTk])
                            op('act', lambda e, ps1=ps1, nXT=nXT: e.copy(out=nXT[:], in_=p3(ps1)), reads=[pk1], writes=[nXTk])
                            if lvl < 5:
                                ps2, pk2 = headmm(lambda h, XT=XT: XT[:, h, :], lambda h, X=X: X[:, h, :], [Xk, XTk])
                                op('dve', lambda e, ps2=ps2, nX=nX: e.tensor_copy(out=nX[:], in_=p3(ps2)), reads=[pk2], writes=[nXk])
                            ps3, pk3 = headmm(lambda h, nXT=nXT: nXT[:, h, :], lambda h: B["Zb"][:, h, :], [nXTk, 'Zb'])
                            op('dve', lambda e, ps3=ps3: e.tensor_tensor(out=B["Z"][:], in0=B["Z"][:], in1=p3(ps3), op=ALU.add), reads=[pk3, 'Z'], writes=['Z'])
                            if lvl < 5:
                                op('act', lambda e: e.copy(out=B["Zb"][:], in_=B["Z"][:]), reads=['Z'], writes=['Zb'])
                            X, Xk, XT, XTk = nX, nXk, nXT, nXTk
                        if sub == 's6':
                            kb.barrier()
                            return fin()
                        op('pool', lambda e: e.tensor_tensor(out=B["vb"][:], in0=v3, in1=b3(beta), op=ALU.mult), reads=[lk, 'BETA'], writes=['vb'])
                        op('pool', lambda e: e.tensor_tensor(out=B["kbg"][:], in0=k3, in1=b3(sm["bg"][:]), op=ALU.mult), reads=[lk, 'bg'], writes=['kbg'])
                        op('pool', lambda e: e.tensor_tensor(out=B["ktail"][:], in0=k3, in1=b3(sm["et"][:]), op=ALU.mult), reads=[lk, 'et'], writes=['ktail'])
                        psU, pkU = headmm(lambda h: B["Z"][:, h, :], lambda h: B["vb"][:, h, :], ['Z', 'vb'])
                        op('act', lambda e: e.copy(out=B["u"][:], in_=p3(psU)), reads=[pkU], writes=['u'])
                        psW, pkW = headmm(lambda h: B["kbg"][:, h, :], lambda h: B["Z"][:, h, :], ['Z', 'kbg'])
                        op('act', lambda e: e.copy(out=B["wT"][:], in_=p3(psW)), reads=[pkW], writes=['wT'])
                        if sub == 's7':
                            kb.barrier()
                            return fin()
                        psP, pkP = headmm(lambda h: B["wT"][:, h, :], lambda h: B["Sst"][:, h, :], ['wT', 'Sst'])
                        op('dve', lambda e: e.tensor_tensor(out=B["vnew"][:], in0=B["u"][:], in1=p3(psP), op=ALU.subtract), reads=[pkP, 'u'], writes=['vnew'])
                        if n < 32:
                            psO, pkO = nextpf()
                            for h in range(8):
                                op('pe', lambda e, h=h: e.matmul(out=psO[0:64, h * 64:(h + 1) * 64], lhsT=B["qdT"][:, h, :], rhs=B["Sst"][:, h, :], start=True, stop=False), reads=['qdT', 'Sst'], writes=[pkO])
                                op('pe', lambda e, h=h: e.matmul(out=psO[0:64, h * 64:(h + 1) * 64], lhsT=B["QKT"][:, h, :], rhs=B["vnew"][:, h, :], start=False, stop=True), reads=['QKT', 'vnew'], writes=[pkO])
                            op('act', lambda e: e.copy(out=B["ost"][:], in_=p3(psO)), reads=[pkO], writes=['ost'])
                            dma('sp', lambda e, n=n: e.dma_start(out=o_s[d, n, :, :], in_=fl(B["ost"])), reads=['ost'], writes=[('o_s', d, n)])
                        psS, pkS = headmm(lambda h: B["ktail"][:, h, :], lambda h: B["vnew"][:, h, :], ['ktail', 'vnew'])
                        op('dve', lambda e: e.tensor_tensor(out=B["Sst"][:], in0=B["Sst"][:], in1=b3(sm["egl"][:]), op=ALU.mult), reads=['Sst', 'egl'], writes=['Sst'])
                        op('dve', lambda e: e.tensor_tensor(out=B["Sst"][:], in0=B["Sst"][:], in1=p3(psS), op=ALU.add), reads=['Sst', pkS], writes=['Sst'])
                        if sub is not None and sub.startswith('it') and it + 1 == int(sub[2:]):
                            kb.barrier()
                            return fin()
                kb.barrier()
                if stop == 'D2':
                    return fin()
            with ExitStack() as s1:
                S = lambda name, shape, dt=F32: s1.enter_context(nc.sbuf_tensor("t_" + name, shape, dt))
                PF = [s1.enter_context(nc.psum_tensor("pfF%d" % i, [128, 512], F32)) for i in range(4)]
                PB = [s1.enter_context(nc.psum_tensor("pbF%d" % i, [128, 4, 256], BF16)) for i in range(2)]
                wdg = S("wdg", [128, 8, 512], BF16)
                dnwb = S("dnwb", [64, 64])
                of = [S("of%d" % i, [64, 512]) for i in range(2)]
                ob = [S("ob%d" % i, [64, 512]) for i in range(2)]
                sq = S("sqF", [64, 512])
                s8 = S("s8F", [64, 8])
                r8 = S("r8F", [64, 8])
                sg = S("sgF", [64, 512])
                odb = [S("odb%d" % i, [64, 512], BF16) for i in range(2)]
                dma('pool', lambda e: e.dma_start(out=wdg[:], in_=winv[:, :, 3072:3584]), writes=['wdg'])
                dma('sp', lambda e: e.dma_start(out=dnwb[:], in_=dnw_d[0:1, :].partition_broadcast(64)), writes=['dnwb'])
                for n in range(32):
                    a, ak = of[n % 2], 'of%d' % (n % 2)
                    b_, bk = ob[n % 2], 'ob%d' % (n % 2)
                    dma('sp', lambda e, a=a, n=n: e.dma_start(out=a[:], in_=o_s[0, n, :, :]), reads=[('o_s', 0, n)], writes=[ak])
                    dma('sp', lambda e, b_=b_, n=n: e.dma_start(out=b_[:], in_=o_s[1, n, :, :]), reads=[('o_s', 1, n)], writes=[bk])
                    op('dve', lambda e, a=a, b_=b_: e.tensor_tensor(out=a[:], in0=a[:], in1=b_[:], op=ALU.add), reads=[ak, bk], writes=[ak])
                    op('act', lambda e, a=a: e.activation(out=sq[:], in_=a[:], func=AF.Square), reads=[ak], writes=['sqF'])
                    op('dve', lambda e: e.tensor_reduce(out=s8[:], in_=sq[:].rearrange("p (g f) -> p g f", f=64), axis=AX.X, op=ALU.add), reads=['sqF'], writes=['s8F'])
                    rstd_from_ssq(s8[:], r8[:], 1.0 / 64, ['s8F'], ['r8F'])
                    a3 = a[:].rearrange("p (g f) -> p g f", f=64)
                    op('dve', lambda e, a3=a3: e.tensor_tensor(out=a3, in0=a3, in1=b3(r8[:]), op=ALU.mult), reads=[ak, 'r8F'], writes=[ak])
                    op('pool', lambda e, a3=a3: e.tensor_tensor(out=a3, in0=a3, in1=m3(dnwb[:]), op=ALU.mult), reads=[ak, 'dnwb'], writes=[ak])
                    ps, pk = PF[n % 4], 'pfF%d' % (n % 4)
                    for k in range(8):
                        op('pe', lambda e, ps=ps, k=k, n=n: e.matmul(out=ps[0:64, :], lhsT=hT[:, k, n * 64:(n + 1) * 64], rhs=wdg[:, k, :], start=(k == 0), stop=(k == 7)), reads=['wdg', ('hT', n // 2)], writes=[pk])
                    op('act', lambda e, ps=ps: e.activation(out=sg[:], in_=ps[0:64, :], func=AF.Silu), reads=[pk], writes=['sgF'])
                    o_, ok_ = odb[n % 2], 'odb%d' % (n % 2)
                    op('dve', lambda e, a=a, o_=o_: e.tensor_tensor(out=o_[:], in0=a[:], in1=sg[:], op=ALU.mult), reads=[ak, 'sgF'], writes=[ok_])
                    pb, pbk = PB[n % 2], 'pbF%d' % (n % 2)
                    for c in range(4):
                        op('pe', lambda e, pb=pb, o_=o_, c=c: e.transpose(out=pb[:, c, 0:64], in_=o_[:, c * 128:(c + 1) * 128], identity=identb[0:64, 0:64]), reads=[ok_, 'identb'], writes=[pbk])
                    op('act', lambda e, pb=pb, n=n: e.copy(out=odnT[:, :, n * 64:(n + 1) * 64], in_=pb[:, :, 0:64]), reads=[pbk], writes=[('odnT', n)])
                kb.barrier()
                if stop == 'D3':
                    return fin()
            if dbg:
                with ExitStack() as s1:
                    tmp = s1.enter_context(nc.sbuf_tensor("dbgt3", [128, 4 * T], F32))
                    op('dve', lambda e: e.tensor_copy(out=tmp[:], in_=odnT[:].rearrange("p k n -> p (k n)")), reads=[('odnT', n) for n in range(32)], writes=['dbgt3'])
                    dma('sp', lambda e: e.dma_start(out=dbg_d["d_odn"][:, :], in_=tmp[:]), reads=['dbgt3'], writes=['d_odn'])
                    kb.barrier()

            with ExitStack() as s1:
                S = lambda name, shape, dt=F32: s1.enter_context(nc.sbuf_tensor("t_" + name, shape, dt))
                PF = [s1.enter_context(nc.psum_tensor("pfG%d" % i, [128, 512], F32)) for i in range(8)]
                pfi = [0]

                def nextpf():
                    i = pfi[0] % 8
                    pfi[0] += 1
                    return PF[i], 'pfG%d' % i
                wg = S("wg", [128, 8, 2048], BF16)
                wa = S("wa", [128, 4, D], BF16)
                wb = S("wb", [128, 4, D], BF16)
                wo = S("wo", [128, 8, D], BF16)
                wr = S("wr", [128, 8, 32])
                brb = S("brb", [128, 32])
                yT = S("yT", [128, 8, 512], BF16)
                sga = S("sga", [128, 512])
                sgb = S("sgb", [128, 512])
                xm = [S("xm0", [128, D])] * 2
                xo = [S("xo%d" % i, [128, D]) for i in range(2)]
                h2 = [S("h2_%d" % i, [128, D]) for i in range(2)]
                h2T = S("h2T", [128, 8, 128])
                junk = S("junkE", [128, D])
                ssq = S("ssqE", [128, 16])
                rs = S("rsE", [128, 16])
                for j in range(4):
                    dma('pool', lambda e, j=j: e.dma_start(out=wg[:, :, j * 512:(j + 1) * 512], in_=winv[:, :, 3616 + j * 512:3616 + (j + 1) * 512]), writes=[('wg', j)])
                dma('pool', lambda e: e.dma_start(out=wa[:], in_=wbra_d.rearrange("(k p) n -> p k n", p=128)), writes=['wa'])
                dma('pool', lambda e: e.dma_start(out=wb[:], in_=wbrb_d.rearrange("(k p) n -> p k n", p=128)), writes=['wb'])
                for j in range(2):
                    dma('pool', lambda e, j=j: e.dma_start(out=wo[:, :, j * 512:(j + 1) * 512], in_=wout_d.rearrange("(k p) n -> p k n", p=128)[:, :, j * 512:(j + 1) * 512]), writes=[('wo', j)])
                dma('sp', lambda e: e.dma_start(out=wr[:], in_=wr_d.rearrange("(k p) n -> p k n", p=128)), writes=['wr'])
                dma('sp', lambda e: e.dma_start(out=brb[:], in_=br_d[0:1, :].partition_broadcast(128)), writes=['brb'])
                for tb in range(4):
                    n0 = tb * 512
                    hts = [('hT', t) for t in range(n0 // 128, n0 // 128 + 4)]
                    for c in range(8):
                        cs = slice(c * 128, (c + 1) * 128)
                        psa, pka = nextpf()
                        for k in range(4):
                            op('pe', lambda e, k=k, psa=psa: e.matmul(out=psa[:], lhsT=wa[:, k, cs], rhs=onaT[:, k, n0:n0 + 512], start=(k == 0), stop=(k == 3)), reads=['wa'] + onaT_all, writes=[pka])
                        psb, pkb = nextpf()
                        for k in range(4):
                            op('pe', lambda e, k=k, psb=psb: e.matmul(out=psb[:], lhsT=wb[:, k, cs], rhs=odnT[:, k, n0:n0 + 512], start=(k == 0), stop=(k == 3)), reads=['wb'] + [('odnT', n) for n in range(tb * 8, tb * 8 + 8)], writes=[pkb])
                        pga, pkga = nextpf()
                        for k in range(8):
                            op('pe', lambda e, k=k, pga=pga: e.matmul(out=pga[:], lhsT=wg[:, k, c * 128:(c + 1) * 128], rhs=hT[:, k, n0:n0 + 512], start=(k == 0), stop=(k == 7)), reads=[('wg', c // 4)] + hts, writes=[pkga])
                        pgb, pkgb = nextpf()
                        for k in range(8):
                            op('pe', lambda e, k=k, pgb=pgb: e.matmul(out=pgb[:], lhsT=wg[:, k, 1024 + c * 128:1024 + (c + 1) * 128], rhs=hT[:, k, n0:n0 + 512], start=(k == 0), stop=(k == 7)), reads=[('wg', 2 + c // 4)] + hts, writes=[pkgb])
                        op('act', lambda e, pga=pga: e.activation(out=sga[:], in_=pga[:], func=AF.Sigmoid), reads=[pkga], writes=['sga'])
                        op('act', lambda e, pgb=pgb: e.activation(out=sgb[:], in_=pgb[:], func=AF.Sigmoid), reads=[pkgb], writes=['sgb'])
                        op('dve', lambda e, psa=psa: e.tensor_tensor(out=sga[:], in0=sga[:], in1=psa[:], op=ALU.mult), reads=['sga', pka], writes=['sga'])
                        op('dve', lambda e, psb=psb: e.tensor_tensor(out=sgb[:], in0=sgb[:], in1=psb[:], op=ALU.mult), reads=['sgb', pkb], writes=['sgb'])
                        op('pool', lambda e, c=c: e.tensor_tensor(out=yT[:, c, :], in0=sga[:], in1=sgb[:], op=ALU.add), reads=['sga', 'sgb'], writes=['yT'])
                    for i4 in range(4):
                        i = tb * 4 + i4
                        xmi, xmk = xm[0], 'xm0'
                        xoi, xok = xo[i % 2], 'xo%d' % (i % 2)
                        h2i, h2k = h2[i % 2], 'h2_%d' % (i % 2)
                        dma('sp', lambda e, xmi=xmi, i=i: e.dma_start(out=xmi[:], in_=x_d[i * 128:(i + 1) * 128, :]), writes=[xmk])
                        for half in range(2):
                            hs_ = slice(half * 512, (half + 1) * 512)
                            ps, pk = nextpf()
                            for k in range(8):
                                op('pe', lambda e, k=k, ps=ps: e.matmul(out=ps[:], lhsT=yT[:, k, i4 * 128:(i4 + 1) * 128], rhs=wo[:, k, hs_], start=(k == 0), stop=(k == 7)), reads=['yT', ('wo', half)], writes=[pk])
                            op('dve', lambda e, ps=ps, xoi=xoi: e.tensor_tensor(out=xoi[:, hs_], in0=ps[:], in1=MODL[:, 2 * D + half * 512:2 * D + (half + 1) * 512], op=ALU.mult), reads=[pk, 'MODL'], writes=[xok])
                            op('dve', lambda e, xoi=xoi, xmi=xmi: e.tensor_tensor(out=xoi[:, hs_], in0=xoi[:, hs_], in1=xmi[:, hs_], op=ALU.add), reads=[xok, xmk], writes=[xok])
                        dma('sp', lambda e, xoi=xoi, i=i: e.dma_start(out=xl2_s[i * 128:(i + 1) * 128, :], in_=xoi[:]), reads=[xok], writes=[('xl2_s', i)])
                        op('act', lambda e, xoi=xoi, i=i: e.activation(out=junk[:], in_=xoi[:], func=AF.Square, accum_out=ssq[:, i:i + 1]), reads=[xok], writes=['junkE', 'ssqE%d' % i])
                        rstd_from_ssq(ssq[:, i:i + 1], rs[:, i:i + 1], 1.0 / D, ['ssqE%d' % i], ['rsE%d' % i])
                        op('dve', lambda e, xoi=xoi, i=i: e.scalar_tensor_tensor(out=junk[:], in0=xoi[:], scalar=rs[:, i:i + 1], in1=MODL[:, 4 * D:5 * D], op0=ALU.mult, op1=ALU.mult), reads=[xok, 'rsE%d' % i, 'MODL', 'junkE'], writes=['junkE'])
                        op('dve', lambda e, h2i=h2i: e.tensor_tensor(out=h2i[:], in0=junk[:], in1=MODL[:, 3 * D:4 * D], op=ALU.add), reads=['junkE', 'MODL'], writes=[h2k])
                        dma('sp', lambda e, h2i=h2i, i=i: e.dma_start(out=h2_s[i * 128:(i + 1) * 128, :], in_=h2i[:]), reads=[h2k], writes=[('h2_s', i)])
                        for hh in range(2):
                            ps, pk = nextpf()
                            for k4 in range(4):
                                k = hh * 4 + k4
                                op('pe', lambda e, ps=ps, k=k, k4=k4, h2i=h2i: e.transpose(out=ps[:, k4 * 128:(k4 + 1) * 128], in_=h2i[:, k * 128:(k + 1) * 128], identity=ident), reads=[h2k, 'cst'], writes=[pk])
                            op('act', lambda e, ps=ps, hh=hh: e.copy(out=h2T[:, hh * 4:(hh + 1) * 4, :], in_=ps[:].rearrange("p (k f) -> p k f", f=128)), reads=[pk], writes=[('h2T', hh)])
                        ps, pk = nextpf()
                        for k in range(8):
                            op('pe', lambda e, ps=ps, k=k: e.matmul(out=ps[:, 0:32], lhsT=h2T[:, k, :], rhs=wr[:, k, :], start=(k == 0), stop=(k == 7)), reads=[('h2T', k // 4), 'wr'], writes=[pk])
                        op('dve', lambda e, ps=ps, i=i: e.tensor_tensor(out=LG[:, i, :], in0=ps[:, 0:32], in1=brb[:], op=ALU.add), reads=[pk, 'brb'], writes=['LG'])
                kb.barrier()
                if stop == 'E':
                    return fin()
            if dbg:
                dma('sp', lambda e: e.dma_start(out=dbg_d["d_lg"][:, :], in_=LG[:].rearrange("p a b -> p (a b)")), reads=['LG'], writes=['d_lg'])
                kb.barrier()

            mid.close()
            with ExitStack() as s1:
                S = lambda name, shape, dt=F32: s1.enter_context(nc.sbuf_tensor("t_" + name, shape, dt))
                PF = [s1.enter_context(nc.psum_tensor("pfH%d" % i, [128, 512], F32)) for i in range(8)]
                A3 = [128, 16, 32]
                DESTi = S("DESTi", [128, 4, 16], I32)
                GATE = S("GATE", [128, 4, 16])
                W1I = S("W1I", [128, NBLK, 8], I32)
                B1I = S("B1I", [128, NBLK], I32)
                B2I = S("B2I", [128, NBLK], I32)
                rt = ExitStack()
                s1.callback(rt.close)
                R_ = lambda name, shape, dt=F32: rt.enter_context(nc.sbuf_tensor("t_" + name, shape, dt))
                LGw = R_("LGw", A3)
                EQ = R_("EQ", [128, 4, 16, 32])
                SEL = R_("SEL", A3)
                GT = R_("GT", A3)
                RANK = R_("RANK", A3)
                tmp3 = R_("tmp3", A3)
                mr = R_("mr", [128, 16])
                m0 = R_("m0", [128, 16])
                den = R_("den", [128, 16])
                CNT = R_("CNT", [128, 32])
                PAD = R_("PAD", [128, 32])
                PADi = R_("PADi", [128, 32], I32)
                PADT = R_("PADT", [32, 128])
                PSb = R_("PSb", [128, 32])
                PEb = R_("PEb", [128, 32])
                DESTf = R_("DESTf", [128, 4, 16])
                cmp_ = R_("cmp_", [128, NBLK, 32])
                BE = R_("BE", [128, NBLK])
                tmpw = R_("tmpw", [128, NBLK, 8])
                tmpb = R_("tmpb", [128, NBLK])
                zi = R_("zi", [128, NBLK], I32)
                tokid = R_("tokid", [128, 16], I32)
                b16 = lambda ap: ap.unsqueeze(2).to_broadcast(A3)
                e16 = lambda ap: ap.unsqueeze(1).to_broadcast(A3)
                op('dve', lambda e: e.tensor_copy(out=LGw[:], in_=LG[:]), reads=['LG'], writes=['LGw'])
                for r in range(4):
                    op('dve', lambda e: e.tensor_reduce(out=mr[:], in_=LGw[:], axis=AX.X, op=ALU.max), reads=['LGw'], writes=['mr'])
                    if r == 0:
                        op('dve', lambda e: e.tensor_copy(out=m0[:], in_=mr[:]), reads=['mr'], writes=['m0'])
                    op('dve', lambda e, r=r: e.tensor_tensor(out=EQ[:, r], in0=LGw[:], in1=b16(mr[:]), op=ALU.is_equal), reads=['LGw', 'mr'], writes=[('EQ', r)])
                    op('dve', lambda e, r=r: e.scalar_tensor_tensor(out=LGw[:], in0=EQ[:, r], scalar=NEG, in1=LGw[:], op0=ALU.mult, op1=ALU.add), reads=[('EQ', r), 'LGw'], writes=['LGw'])
                op('dve', lambda e: e.tensor_tensor(out=SEL[:], in0=EQ[:, 0], in1=EQ[:, 1], op=ALU.add), reads=[('EQ', 0), ('EQ', 1)], writes=['SEL'])
                op('dve', lambda e: e.tensor_tensor(out=SEL[:], in0=SEL[:], in1=EQ[:, 2], op=ALU.add), reads=['SEL', ('EQ', 2)], writes=['SEL'])
                op('dve', lambda e: e.tensor_tensor(out=SEL[:], in0=SEL[:], in1=EQ[:, 3], op=ALU.add), reads=['SEL', ('EQ', 3)], writes=['SEL'])
                op('dve', lambda e: e.tensor_tensor(out=GT[:], in0=LG[:], in1=b16(m0[:]), op=ALU.subtract), reads=['LG', 'm0'], writes=['GT'])
                op('act', lambda e: e.activation(out=GT[:], in_=GT[:], func=AF.Exp), reads=['GT'], writes=['GT'])
                op('dve', lambda e: e.tensor_tensor(out=GT[:], in0=GT[:], in1=SEL[:], op=ALU.mult), reads=['GT', 'SEL'], writes=['GT'])
                op('dve', lambda e: e.tensor_reduce(out=den[:], in_=GT[:], axis=AX.X, op=ALU.add), reads=['GT'], writes=['den'])
                op('dve', lambda e: e.reciprocal(out=den[:], in_=den[:]), reads=['den'], writes=['den'])
                op('dve', lambda e: e.tensor_tensor(out=GT[:], in0=GT[:], in1=b16(den[:]), op=ALU.mult), reads=['GT', 'den'], writes=['GT'])
                op('dve', lambda e: e.memset(CNT[:], 0.0), writes=['CNT'])
                for i in range(16):
                    ps, pk = PF[i % 2], 'pfH%d' % (i % 2)
                    op('pe', lambda e, ps=ps, i=i: e.matmul(out=ps[:, 0:32], lhsT=cst[:, C_UT:C_UT + 128], rhs=SEL[:, i, :], start=True, stop=True), reads=['SEL', 'cst'], writes=[pk])
                    op('dve', lambda e, ps=ps, i=i: e.tensor_tensor(out=RANK[:, i, :], in0=ps[:, 0:32], in1=CNT[:], op=ALU.add), reads=[pk, 'CNT'], writes=['RANK'])
                    ps2, pk2 = PF[2 + i % 2], 'pfH%d' % (2 + i % 2)
                    op('pe', lambda e, ps2=ps2, i=i: e.matmul(out=ps2[:, 0:32], lhsT=ones, rhs=SEL[:, i, :], start=True, stop=True), reads=['SEL', 'cst'], writes=[pk2])
                    op('dve', lambda e, ps2=ps2: e.tensor_tensor(out=CNT[:], in0=CNT[:], in1=ps2[:, 0:32], op=ALU.add), reads=[pk2, 'CNT'], writes=['CNT'])
                op('dve', lambda e: e.tensor_scalar(out=PAD[:], in0=CNT[:], scalar1=127.0, scalar2=None, op0=ALU.add), reads=['CNT'], writes=['PAD'])
                op('dve', lambda e: e.tensor_copy(out=PADi[:], in_=PAD[:]), reads=['PAD'], writes=['PADi'])
                op('dve', lambda e: e.tensor_scalar(out=PADi[:], in0=PADi[:], scalar1=7, scalar2=7, op0=ALU.arith_shift_right, op1=ALU.logical_shift_left), reads=['PADi'], writes=['PADi'])
                op('dve', lambda e: e.tensor_copy(out=PAD[:], in_=PADi[:]), reads=['PADi'], writes=['PAD'])
                op('pe', lambda e: e.transpose(out=PF[4][0:32, 0:128], in_=PAD[:, 0:32], identity=ident), reads=['PAD', 'cst'], writes=['pfH4'])
                op('act', lambda e: e.copy(out=PADT[:], in_=PF[4][0:32, 0:128]), reads=['pfH4'], writes=['PADT'])
                op('pe', lambda e: e.matmul(out=PF[5][:, 0:32], lhsT=PADT[:], rhs=cst[0:32, C_TRI:C_TRI + 32], start=True, stop=True), reads=['PADT', 'cst'], writes=['pfH5'])
                op('act', lambda e: e.copy(out=PSb[:], in_=PF[5][:, 0:32]), reads=['pfH5'], writes=['PSb'])
                op('dve', lambda e: e.tensor_tensor(out=PEb[:], in0=PSb[:], in1=PAD[:], op=ALU.add), reads=['PSb', 'PAD'], writes=['PEb'])
                op('dve', lambda e: e.tensor_tensor(out=RANK[:], in0=RANK[:], in1=e16(PSb[:]), op=ALU.add), reads=['RANK', 'PSb'], writes=['RANK'])
                for r in range(4):
                    op('dve', lambda e, r=r: e.tensor_tensor(out=tmp3[:], in0=EQ[:, r], in1=RANK[:], op=ALU.mult), reads=[('EQ', r), 'RANK'], writes=['tmp3'])
                    op('dve', lambda e, r=r: e.tensor_reduce(out=DESTf[:, r, :], in_=tmp3[:], axis=AX.X, op=ALU.add), reads=['tmp3'], writes=['DESTf'])
                    op('dve', lambda e, r=r: e.tensor_tensor(out=tmp3[:], in0=EQ[:, r], in1=GT[:], op=ALU.mult), reads=[('EQ', r), 'GT'], writes=['tmp3'])
                    op('dve', lambda e, r=r: e.tensor_reduce(out=GATE[:, r, :], in_=tmp3[:], axis=AX.X, op=ALU.add), reads=['tmp3'], writes=['GATE'])
                op('dve', lambda e: e.tensor_copy(out=DESTi[:], in_=DESTf[:]), reads=['DESTf'], writes=['DESTi'])
                op('dve', lambda e: e.tensor_tensor(out=cmp_[:], in0=PEb[:].unsqueeze(1).to_broadcast([128, NBLK, 32]), in1=cst[:, C_BS:C_BS + NBLK].unsqueeze(2).to_broadcast([128, NBLK, 32]), op=ALU.is_le),
                   reads=['PEb', 'cst'], writes=['cmp_'])
                op('dve', lambda e: e.tensor_reduce(out=BE[:], in_=cmp_[:], axis=AX.X, op=ALU.add), reads=['cmp_'], writes=['BE'])
                op('dve', lambda e: e.tensor_scalar_min(out=BE[:], in0=BE[:], scalar1=31.0), reads=['BE'], writes=['BE'])
                op('dve', lambda e: e.tensor_copy(out=B2I[:], in_=BE[:]), reads=['BE'], writes=['B2I'])
                op('dve', lambda e: e.tensor_scalar(out=tmpb[:], in0=BE[:], scalar1=128.0, scalar2=cst[:, C_KR:C_KR + 1], op0=ALU.mult, op1=ALU.add), reads=['BE', 'cst'], writes=['tmpb'])
                op('dve', lambda e: e.tensor_copy(out=B1I[:], in_=tmpb[:]), reads=['tmpb'], writes=['B1I'])
                op('dve', lambda e: e.tensor_scalar(out=tmpb[:], in0=BE[:], scalar1=1024.0, scalar2=None, op0=ALU.mult), reads=['BE', 'B1I'], writes=['tmpb'])
                op('dve', lambda e: e.tensor_tensor(out=tmpw[:], in0=tmpb[:].unsqueeze(2).to_broadcast([128, NBLK, 8]), in1=cst[:, C_KR:C_KR + 8].unsqueeze(1).to_broadcast([128, NBLK, 8]), op=ALU.add),
                   reads=['tmpb', 'cst'], writes=['tmpw'])
                op('dve', lambda e: e.tensor_copy(out=W1I[:], in_=tmpw[:]), reads=['tmpw'], writes=['W1I'])
                op('dve', lambda e: e.memset(zi[:], 0), writes=['zi'])
                op('dve', lambda e: e.tensor_copy(out=tokid[:], in_=cst[:, C_TOK:C_TOK + 16]), reads=['cst'], writes=['tokid'])
                dma('sp', lambda e: e.dma_start(out=slot_s.rearrange("(p j) o -> p (j o)", p=128), in_=zi[:]), reads=['zi'], writes=['slot_s'])
                for r in range(4):
                    for i in range(16):
                        dma('pool', lambda e, r=r, i=i: e.indirect_dma_start(out=slot_s[:, :], out_offset=bass.IndirectOffsetOnAxis(ap=DESTi[:, r, i:i + 1], axis=0), in_=tokid[:, i:i + 1], in_offset=None),
                            reads=['DESTi', 'tokid'], writes=['slot_s'])
                kb.barrier()
                rt.close()
                sidx = [S("sidx%d" % i, [128, 1], I32) for i in range(2)]
                xg = [S("xg%d" % i, [128, D]) for i in range(2)]
                xgT = S("xgT", [128, 8, 128], BF16)
                w1b = [S("w1b%d" % i, [128, 2048], BF16) for i in range(3)]
                w2b = [S("w2b%d" % i, [128, D], BF16) for i in range(3)]
                w1k = [S("w1k%d" % i, [128, 2048]) for i in range(6)]
                w2c = [S("w2c%d" % i, [128, D]) for i in range(8)]
                b1s = [S("b1s%d" % i, [128, 16]) for i in range(2)]
                b2s = [S("b2s%d" % i, [128, D]) for i in range(2)]
                Gt = S("Gt", [128, 4, 128])
                Ut = S("Ut", [128, 4, 128])
                sgm = S("sgm", [128, 4, 128])
                actT = S("actT", [128, 8, 128], BF16)
                ysb = [S("ysb%d" % i, [128, D]) for i in range(2)]
                PH = PF[0:4]
                PY = PF[4:6]
                PTr = PF[6:8]
                wc1 = wc2 = 0
                for j in range(NBLK):
                    si, sik = sidx[j % 2], 'sidx%d' % (j % 2)
                    xgi, xgk = xg[j % 2], 'xg%d' % (j % 2)
                    dma('sp', lambda e, si=si, j=j: e.dma_start(out=si[:], in_=slot_s[j * 128:(j + 1) * 128, :]), reads=['slot_s'], writes=[sik])
                    dma('pool', lambda e, si=si, xgi=xgi: e.indirect_dma_start(out=xgi[:], out_offset=None, in_=h2_s[:, :], in_offset=bass.IndirectOffsetOnAxis(ap=si[:, 0:1], axis=0)),
                        reads=[sik] + [('h2_s', i) for i in range(16)], writes=[xgk])
                    b1i, b1k = b1s[j % 2], 'b1s%d' % (j % 2)
                    b2i, b2k = b2s[j % 2], 'b2s%d' % (j % 2)
                    dma('pool', lambda e, b1i=b1i, j=j: e.indirect_dma_start(out=b1i[:], out_offset=None, in_=b1t_d[:, :], in_offset=bass.IndirectOffsetOnAxis(ap=B1I[:, j:j + 1], axis=0)), reads=['B1I'], writes=[b1k])
                    dma('pool', lambda e, b2i=b2i, j=j: e.indirect_dma_start(out=b2i[:], out_offset=None, in_=b2_d[:, :], in_offset=bass.IndirectOffsetOnAxis(ap=B2I[:, j:j + 1], axis=0)), reads=['B2I'], writes=[b2k])
                    for hh in range(2):
                        ps, pk = PTr[hh], 'pfH%d' % (6 + hh)
                        for k4 in range(4):
                            k = hh * 4 + k4
                            op('pe', lambda e, ps=ps, k=k, k4=k4, xgi=xgi: e.transpose(out=ps[:, k4 * 128:(k4 + 1) * 128], in_=xgi[:, k * 128:(k + 1) * 128], identity=ident), reads=[xgk, 'cst'], writes=[pk])
                        op('act', lambda e, ps=ps, hh=hh: e.copy(out=xgT[:, hh * 4:(hh + 1) * 4, :], in_=ps[:].rearrange("p (k f) -> p k f", f=128)), reads=[pk], writes=[('xgT', hh)])
                    for k in range(8):
                        wt, wk = w1k[wc1 % 6], 'w1k%d' % (wc1 % 6)
                        wc1 += 1
                        dma('pool', lambda e, wt=wt, j=j, k=k: e.indirect_dma_start(out=wt[:], out_offset=None, in_=w1_d[:, :], in_offset=bass.IndirectOffsetOnAxis(ap=W1I[:, j, k:k + 1], axis=0)), reads=['W1I'], writes=[wk])
                        wb_, wbk = w1b[wc1 % 3], 'w1b%d' % (wc1 % 3)
                        if k % 2 == 0:
                            op('dve', lambda e, wt=wt, wb_=wb_: e.tensor_copy(out=wb_[:], in_=wt[:]), reads=[wk], writes=[wbk])
                        else:
                            op('act', lambda e, wt=wt, wb_=wb_: e.copy(out=wb_[:], in_=wt[:]), reads=[wk], writes=[wbk])
                        for c in range(16):
                            op('pe', lambda e, wb_=wb_, k=k, c=c: e.matmul(out=PH[c // 4][:, (c % 4) * 128:(c % 4 + 1) * 128], lhsT=wb_[:, c * 128:(c + 1) * 128], rhs=xgT[:, k, :], start=(k == 0 and c % 4 == 0), stop=(k == 7 and c % 4 == 3)),
                               reads=[wbk, ('xgT', k // 4)], writes=['pfH%d' % (c // 4)])
                    for q in range(2):
                        g3_ = PH[q][:].rearrange("p (c f) -> p c f", f=128)
                        u3_ = PH[q + 2][:].rearrange("p (c f) -> p c f", f=128)
                        bb = lambda lo: b1i[:, lo:lo + 4].unsqueeze(2).to_broadcast([128, 4, 128])
                        op('dve', lambda e: e.tensor_tensor(out=Gt[:], in0=g3_, in1=bb(4 * q), op=ALU.add), reads=['pfH%d' % q, b1k], writes=['Gt'])
                        op('dve', lambda e: e.tensor_scalar_min(out=Gt[:], in0=Gt[:], scalar1=7.0), reads=['Gt'], writes=['Gt'])
                        op('act', lambda e: e.activation(out=sgm[:], in_=Gt[:], func=AF.Sigmoid, scale=1.702), reads=['Gt'], writes=['sgm'])
                        op('dve', lambda e: e.tensor_tensor(out=Ut[:], in0=u3_, in1=bb(8 + 4 * q), op=ALU.add), reads=['pfH%d' % (q + 2), b1k], writes=['Ut'])
                        op('dve', lambda e: e.tensor_scalar(out=Ut[:], in0=Ut[:], scalar1=7.0, scalar2=-7.0, op0=ALU.min, op1=ALU.max), reads=['Ut'], writes=['Ut'])
                        op('dve', lambda e: e.scalar_tensor_tensor(out=Ut[:], in0=Ut[:], scalar=1.0, in1=Gt[:], op0=ALU.add, op1=ALU.mult), reads=['Ut', 'Gt'], writes=['Ut'])
                        op('dve', lambda e: e.tensor_tensor(out=actT[:, 4 * q:4 * q + 4, :], in0=Ut[:], in1=sgm[:], op=ALU.mult), reads=['Ut', 'sgm'], writes=[('actT', q)])
                    for fc in range(8):
                        wt, wk = w2c[wc2 % 8], 'w2c%d' % (wc2 % 8)
                        wc2 += 1
                        dma('pool', lambda e, wt=wt, j=j, fc=fc: e.indirect_dma_start(out=wt[:], out_offset=None, in_=w2_d[:, :], in_offset=bass.IndirectOffsetOnAxis(ap=W1I[:, j, fc:fc + 1], axis=0)), reads=['W1I'], writes=[wk])
                        wb_, wbk = w2b[wc2 % 3], 'w2b%d' % (wc2 % 3)
                        op('act', lambda e, wt=wt, wb_=wb_: e.copy(out=wb_[:], in_=wt[:]), reads=[wk], writes=[wbk])
                        for half in range(2):
                            op('pe', lambda e, wb_=wb_, fc=fc, half=half: e.matmul(out=PY[half][:], lhsT=actT[:, fc, :], rhs=wb_[:, half * 512:(half + 1) * 512], start=(fc == 0), stop=(fc == 7)),
                               reads=[wbk, ('actT', fc // 4)], writes=['pfH%d' % (4 + half)])
                    yi, yk = ysb[j % 2], 'ysb%d' % (j % 2)
                    for half in range(2):
                        op('dve', lambda e, yi=yi, half=half: e.tensor_tensor(out=yi[:, half * 512:(half + 1) * 512], in0=PY[half][:], in1=b2i[:, half * 512:(half + 1) * 512], op=ALU.add),
                           reads=['pfH%d' % (4 + half), b2k], writes=[yk])
                    dma('sp', lambda e, yi=yi, j=j: e.dma_start(out=ypad_s[j * 128:(j + 1) * 128, :], in_=yi[:]), reads=[yk], writes=['ypad_s'])
                yr = [S("yr%d" % i, [128, D]) for i in range(4)]
                xq = [S("xq0", [128, D])] * 2
                ac = [S("ac%d" % i, [128, D]) for i in range(2)]
                fnb = S("fnb", [128, D])
                ssq = S("ssqH", [128, 16])
                rs = S("rsH", [128, 16])
                dma('sp', lambda e: e.dma_start(out=fnb[:], in_=fnw_d[0:1, :].partition_broadcast(128)), writes=['fnb'])
                for i in range(16):
                    aci, ack = ac[i % 2], 'ac%d' % (i % 2)
                    xqi, xqk = xq[0], 'xq0'
                    dma('sp', lambda e, xqi=xqi, i=i: e.dma_start(out=xqi[:], in_=xl2_s[i * 128:(i + 1) * 128, :]), reads=[('xl2_s', i)], writes=[xqk])
                    for r in range(4):
                        dma('pool', lambda e, r=r, i=i: e.indirect_dma_start(out=yr[r][:], out_offset=None, in_=ypad_s[:, :], in_offset=bass.IndirectOffsetOnAxis(ap=DESTi[:, r, i:i + 1], axis=0)),
                            reads=['DESTi', 'ypad_s'], writes=['yr%d' % r])
                        if r == 0:
                            op('dve', lambda e, aci=aci, i=i: e.tensor_scalar(out=aci[:], in0=yr[0][:], scalar1=GATE[:, 0, i:i + 1], scalar2=None, op0=ALU.mult), reads=['yr0', 'GATE'], writes=[ack])
                        else:
                            op('dve', lambda e, aci=aci, i=i, r=r: e.scalar_tensor_tensor(out=aci[:], in0=yr[r][:], scalar=GATE[:, r, i:i + 1], in1=aci[:], op0=ALU.mult, op1=ALU.add), reads=['yr%d' % r, 'GATE', ack], writes=[ack])
                    op('dve', lambda e, aci=aci: e.tensor_tensor(out=aci[:], in0=aci[:], in1=MODL[:, 5 * D:6 * D], op=ALU.mult), reads=[ack, 'MODL'], writes=[ack])
                    op('dve', lambda e, aci=aci, xqi=xqi: e.tensor_tensor(out=aci[:], in0=aci[:], in1=xqi[:], op=ALU.add), reads=[ack, xqk], writes=[ack])
                    op('act', lambda e, aci=aci, i=i: e.activation(out=yr[0][:], in_=aci[:], func=AF.Square, accum_out=ssq[:, i:i + 1]), reads=[ack], writes=['yr0', 'ssqH%d' % i])
                    rstd_from_ssq(ssq[:, i:i + 1], rs[:, i:i + 1], 1.0 / D, ['ssqH%d' % i], ['rsH%d' % i])
                    op('dve', lambda e, aci=aci, i=i: e.scalar_tensor_tensor(out=aci[:], in0=aci[:], scalar=rs[:, i:i + 1], in1=fnb[:], op0=ALU.mult, op1=ALU.mult), reads=[ack, 'rsH%d' % i, 'fnb'], writes=[ack])
                    dma('sp', lambda e, aci=aci, i=i: e.dma_start(out=out_d[i * 128:(i + 1) * 128, :], in_=aci[:]), reads=[ack], writes=[('out', i)])
                kb.barrier()
        if kb.stopped:
            kb.stopped = False
            kb.limit = None
            kb.barrier()
            return fin()
        print("instructions:", kb.nins)
    return nc


def host_consts():
    cst = np.zeros((128, C_N), np.float32)
    cst[:, C_ID:C_ID + 128] = np.eye(128)
    cst[:, C_ONE:C_ONE + 128] = 1.0
    i = np.arange(64)
    cst[:64, C_U:C_U + 64] = (i[:, None] <= i[None, :])
    cst[:64, C_L:C_L + 64] = (i[:, None] >= i[None, :])
    cst[:64, C_IF:C_IF + 64] = (i[:, None] >= i[None, :])
    cst[:64, C_SF:C_SF + 64] = (i[:, None] > i[None, :])
    cst[:64, C_IB:C_IB + 64] = (i[:, None] <= i[None, :])
    cst[:64, C_SB:C_SB + 64] = (i[:, None] < i[None, :])
    e = np.arange(32)
    cst[:32, C_TRI:C_TRI + 32] = (e[:, None] < e[None, :])
    t = np.arange(128)
    cst[:, C_UT:C_UT + 128] = (t[:, None] < t[None, :])
    cst[:, C_BS:C_BS + NBLK] = (np.arange(NBLK) * 128)[None, :]
    cst[:, C_KR:C_KR + 8] = np.arange(8)[None, :] * 128 + t[:, None]
    cst[:, C_TOK:C_TOK + 16] = np.arange(16)[None, :] * 128 + t[:, None]
    return cst


def host_rope():
    tt = np.arange(T)
    row = (tt // 64).astype(np.float32)
    col = (tt % 64).astype(np.float32)
    freqs = (np.float32(10000.0) ** (-np.arange(16, dtype=np.float32) / np.float32(16))).astype(np.float32)
    ang = np.concatenate([row[:, None] * freqs, col[:, None] * freqs], axis=-1).astype(np.float32)
    cos, sin = np.cos(ang).astype(np.float32), np.sin(ang).astype(np.float32)
    r = np.stack([cos.reshape(32, 64, 32).transpose(1, 0, 2), sin.reshape(32, 64, 32).transpose(1, 0, 2)], axis=1)
    return np.ascontiguousarray(r.reshape(64, 2 * 32 * 32))


def host_natbl(rpb):
    kc = np.arange(64)[:, None]
    qc = np.arange(64)[None, :]
    dc = np.clip(kc - qc + 15, 0, 30)
    cs = np.clip(np.arange(64) - 8, 0, 48)
    cmask = (kc >= cs[None, :]) & (kc < cs[None, :] + 16)
    base = np.full((8, 64, 17, 64), NEG, np.float32)
    for jp in range(0, 15):
        g = rpb[:, 14 - jp][:, dc]
        base[:, :, jp + 1, :] = np.where(cmask[None], g, np.float32(NEG))
    base_int = base.copy()
    for jp in range(-1, 16):
        if not (4 <= jp <= 11):
            base_int[:, :, jp + 1, :] = NEG
    out = np.empty((8, 2, 128, 16, 64), np.float32)
    for ti, b in enumerate((base, base_int)):
        for a in range(2):
            out[:, ti, a * 64:(a + 1) * 64, :, :] = b[:, :, (1 - a):(17 - a), :]
    return np.ascontiguousarray(out.reshape(8, 2, 128, 1024))


def make_in_maps(inputs, cores):
    f = lambda a: np.ascontiguousarray(np.asarray(a, dtype=np.float32))
    shared = {
        "w_mod": f(inputs["w_mod"][0]), "b_mod": f(inputs["b_mod"][0]).reshape(1, -1), "norm1_w": f(inputs["norm1_w"][0]).reshape(1, -1),
        "w_in": f(inputs["w_in"][0]), "na_tbl": host_natbl(np.asarray(inputs["na_rpb"][0], np.float32)),
        "convw": f(np.asarray(inputs["dn_conv_w"][0]).reshape(5, 12, 128).transpose(2, 1, 0).reshape(128, 60)),
        "alog": f(inputs["dn_a_log"][0]).reshape(1, 16), "dtb": f(inputs["dn_dt_bias"][0]).reshape(1, 16), "dnw": f(inputs["dn_norm_w"][0]).reshape(1, 64),
        "w_br_a": f(inputs["w_br_a"][0]), "w_br_b": f(inputs["w_br_b"][0]), "w_out": f(inputs["w_out"][0]), "norm2_w": f(inputs["norm2_w"][0]).reshape(1, -1),
        "w_router": f(inputs["w_router"][0]), "b_router": f(inputs["b_router"][0]).reshape(1, 32),
        "w1": f(inputs["w1"][0]).reshape(32 * D, 2048), "b1t": f(np.asarray(inputs["b1"][0]).reshape(32, 16, 128).transpose(0, 2, 1).reshape(32 * 128, 16)),
        "w2": f(inputs["w2"][0]).reshape(32 * D, D), "b2": f(inputs["b2"][0]), "fnw": f(inputs["final_norm_w"]).reshape(1, -1),
        "rope": host_rope(), "cst": host_consts(),
    }
    maps = []
    for b in cores:
        m = dict(shared)
        m["x"] = f(inputs["x"][b])
        m["ctx"] = f(inputs["ctx"][b])
        m["c2"] = f(np.stack([np.asarray(inputs["c"][b]), np.asarray(inputs["c_ctx"])], axis=0))
        maps.append(m)
    return maps


def kernel(**inputs):
    nc = build()
    maps = make_in_maps(inputs, list(range(8)))
    res = run_bass_kernel_spmd(nc, maps, core_ids=list(range(8)))
    return np.stack([np.asarray(r["out"], dtype=np.float32) for r in res.results], axis=0)
```

```python
import numpy as np
from contextlib import ExitStack
import concourse.bass as bass
import concourse.mybir as mybir
from concourse.bass_utils import run_bass_kernel_spmd

F32 = mybir.dt.float32
BF16 = mybir.dt.bfloat16
I32 = mybir.dt.int32
ALU = mybir.AluOpType
AF = mybir.ActivationFunctionType
AX = mybir.AxisListType

D = 1024
T = 2048
LC = 256
NT = T + LC
NBLK = 96
NEG = -1e30
EPS = 1e-6

C_ID, C_ONE, C_U, C_L, C_IF, C_SF, C_IB, C_SB, C_TRI, C_UT, C_BS, C_KR, C_TOK, C_N = 0, 128, 256, 320, 384, 448, 512, 576, 640, 672, 800, 896, 904, 920


class _StopScan(Exception):
    pass


class KB:
    NDMA = 16

    def __init__(self, nc, es):
        self.nc = nc
        self.eng = {'pe': nc.tensor, 'act': nc.scalar, 'dve': nc.vector, 'pool': nc.gpsimd, 'sp': nc.sync}
        self.sem = {}
        self.cnt = {}
        for e in ('pe', 'act', 'dve', 'pool'):
            self.sem[e] = es.enter_context(nc.semaphore('s_' + e))
            self.cnt[e] = 0
        self.dsem = {}
        self.dcnt = {}
        self.dnext = {}
        for q in ('sp', 'pool'):
            self.dsem[q] = [es.enter_context(nc.semaphore('d_%s%d' % (q, i))) for i in range(self.NDMA)]
            self.dcnt[q] = [0] * self.NDMA
            self.dnext[q] = 0
        self.seen = {e: {} for e in self.eng}
        self.lastw = {}
        self.readers = {}
        self.nins = 0
        self.limit = None
        self.stopped = False

    def _wait(self, e, ev):
        sem, val, src = ev
        if src == e and e == 'pe':
            return
        k = id(sem)
        if self.seen[e].get(k, 0) >= val:
            return
        self.eng[e].wait_ge(sem, val)
        self.seen[e][k] = val

    def _deps(self, e, reads, writes):
        for r in reads:
            ev = self.lastw.get(r)
            if ev is not None:
                self._wait(e, ev)
        for w in writes:
            ev = self.lastw.get(w)
            if ev is not None:
                self._wait(e, ev)
            for ev in self.readers.get(w, {}).values():
                self._wait(e, ev)

    def _record(self, ev, reads, writes):
        for r in reads:
            self.readers.setdefault(r, {})[id(ev[0])] = ev
        for w in writes:
            self.lastw[w] = ev
            self.readers[w] = {}

    def op(self, e, fn, reads=(), writes=()):
        if self.stopped:
            return None
        pr = [r for r in reads if isinstance(r, str) and r[:2] in ('pf', 'pb')]
        if pr:
            writes = list(writes) + pr
        self._deps(e, reads, writes)
        ins = fn(self.eng[e])
        self.cnt[e] += 1
        ins.then_inc(self.sem[e], 1)
        self._record((self.sem[e], self.cnt[e], e), reads, writes)
        self.nins += 1
        if self.limit is not None and self.nins >= self.limit:
            self.stopped = True
        return ins

    def dma(self, q, fn, reads=(), writes=()):
        if self.stopped:
            return None
        i = self.dnext[q]
        self.dnext[q] = (i + 1) % self.NDMA
        sem = self.dsem[q][i]
        if self.dcnt[q][i] > 0:
            self._wait(q, (sem, self.dcnt[q][i], 'dma'))
        self._deps(q, reads, writes)
        ins = fn(self.eng[q])
        self.dcnt[q][i] += 16
        ins.then_inc(sem, 16)
        self._record((sem, self.dcnt[q][i], 'dma'), reads, writes)
        self.nins += 1
        return ins

    def barrier(self):
        if self.stopped:
            return
        for e in self.eng:
            for e2 in ('pe', 'act', 'dve', 'pool'):
                if e2 != e and self.cnt[e2] > 0:
                    self._wait(e, (self.sem[e2], self.cnt[e2], e2))
            for q in self.dsem:
                for i in range(self.NDMA):
                    if self.dcnt[q][i] > 0:
                        self._wait(e, (self.dsem[q][i], self.dcnt[q][i], 'dma'))

    def wait_all(self, e, toks):
        if self.stopped:
            return
        for t in toks:
            if t in self.lastw:
                self._wait(e, self.lastw[t])


def bc(ap, shape):
    return ap.to_broadcast(shape)


def build(dbg=False, stop=None, sub=None):
    nc = bass.Bass("TRN2", target_bir_lowering=False)
    limit = int(sub[1:]) if (sub is not None and sub.startswith('n')) else None
    inp = lambda name, shape, dt=F32: nc.dram_tensor(name, shape, dt, kind="ExternalInput").ap()
    x_d = inp("x", [T, D])
    ctx_d = inp("ctx", [LC, D])
    c2_d = inp("c2", [2, D])
    wmod_d = inp("w_mod", [D, 6 * D])
    bmod_d = inp("b_mod", [1, 6 * D])
    n1_d = inp("norm1_w", [1, D])
    win_d = inp("w_in", [D, 5664])
    natbl_d = inp("na_tbl", [8, 2, 128, 1024])
    convw_d = inp("convw", [128, 12 * 5])
    alog_d = inp("alog", [1, 16])
    dtb_d = inp("dtb", [1, 16])
    dnw_d = inp("dnw", [1, 64])
    wbra_d = inp("w_br_a", [512, D])
    wbrb_d = inp("w_br_b", [512, D])
    wout_d = inp("w_out", [D, D])
    n2_d = inp("norm2_w", [1, D])
    wr_d = inp("w_router", [D, 32])
    br_d = inp("b_router", [1, 32])
    w1_d = inp("w1", [32 * D, 2048])
    b1t_d = inp("b1t", [32 * 128, 16])
    w2_d = inp("w2", [32 * D, D])
    b2_d = inp("b2", [32, D])
    fnw_d = inp("fnw", [1, D])
    rope_d = inp("rope", [64, 2 * 32 * 32])
    cst_d = inp("cst", [128, C_N])
    out_d = nc.dram_tensor("out", [T, D], F32, kind="ExternalOutput").ap()
    scr = lambda name, shape, dt=F32: nc.dram_tensor(name, shape, dt, kind="Internal").ap()
    qkv_s = scr("qkv_s", [36, 64, 1536])
    o_s = scr("o_s", [2, 32, 64, 512])
    xl2_s = scr("xl2_s", [T, D])
    h2_s = scr("h2_s", [T, D])
    slot_s = scr("slot_s", [NBLK * 128, 1], I32)
    ypad_s = scr("ypad_s", [NBLK * 128, D])
    dbg_d = {}
    if dbg:
        for nm, shp in (("d_hT", [128, 8 * NT]), ("d_ona", [128, 4 * T]), ("d_odn", [128, 4 * T]), ("d_lg", [128, 16 * 32])):
            dbg_d[nm] = nc.dram_tensor(nm, shp, F32, kind="ExternalOutput").ap()

    winv = win_d.rearrange("(k p) n -> p k n", p=128)

    with ExitStack() as es:
        kb = KB(nc, es)
        kb.limit = limit
        op, dma = kb.op, kb.dma

        def fin():
            with ExitStack() as sf:
                z = sf.enter_context(nc.sbuf_tensor("t_zout", [128, D], F32))
                op('dve', lambda e: e.memset(z[:], 0.0), writes=['zout'])
                for i in range(16):
                    dma('sp', lambda e, i=i: e.dma_start(out=out_d[i * 128:(i + 1) * 128, :], in_=z[:]), reads=['zout'], writes=[('out', i)])
                kb.barrier()
            print("instructions (stopped at %s):" % stop, kb.nins)
            return nc
        if True:
            G = lambda name, shape, dt=F32: es.enter_context(nc.sbuf_tensor("t_" + name, shape, dt))
            cst = G("cst", [128, C_N])
            identb = G("identb", [128, 128], BF16)
            MODL = G("MODL", [128, 6 * D])
            epsb = G("epsb", [128, 1])
            LG = G("LG", [128, 16, 32])
            mid = ExitStack()
            es.callback(mid.close)
            Gm = lambda name, shape, dt=F32: mid.enter_context(nc.sbuf_tensor("t_" + name, shape, dt))
            hT = Gm("hT", [128, 8, NT], BF16)
            onaT = Gm("onaT", [128, 4, T], BF16)
            odnT = Gm("odnT", [128, 4, T], BF16)

            dma('sp', lambda e: e.dma_start(out=cst[:], in_=cst_d[:, :]), writes=['cst'])
            op('dve', lambda e: e.tensor_copy(out=identb[:], in_=cst[:, C_ID:C_ID + 128]), reads=['cst'], writes=['identb'])
            op('dve', lambda e: e.memset(epsb[:], EPS), writes=['epsb'])
            ident = cst[:, C_ID:C_ID + 128]
            ones = cst[:, C_ONE:C_ONE + 128]

            def rstd_from_ssq(ssq_ap, rs_ap, scale, toks_r, toks_w):
                op('act', lambda e: e.activation(out=rs_ap, in_=ssq_ap, func=AF.Sqrt, scale=scale, bias=epsb[0:rs_ap.partition_size(), :]), reads=list(toks_r) + ['epsb'], writes=toks_w)
                op('dve', lambda e: e.reciprocal(out=rs_ap, in_=rs_ap), reads=toks_w, writes=toks_w)

            with ExitStack() as s1:
                S = lambda name, shape, dt=F32: s1.enter_context(nc.sbuf_tensor("t_" + name, shape, dt))
                PF = [s1.enter_context(nc.psum_tensor("pfA%d" % i, [128, 512], F32)) for i in range(2)]
                c2t = S("c2t", [128, 2, 8])
                MODC = S("MODC", [128, 2 * D])
                Lm = S("Lm", [128, 2, 8, 128], BF16)
                onesb = S("onesb", [128, 128], BF16)
                wm = [S("wm%d" % i, [128, 8, 512], BF16) for i in range(2)]
                nbc = S("nbc", [128, 2 * D])
                dma('sp', lambda e: e.dma_start(out=c2t[:], in_=c2_d.rearrange("t (p k) -> p t k", k=8)), writes=['c2t'])
                dma('sp', lambda e: e.dma_start(out=MODL[:], in_=bmod_d[0:1, :].partition_broadcast(128)), writes=['MODL'])
                dma('sp', lambda e: e.dma_start(out=MODC[:], in_=bmod_d[0:1, 0:2 * D].partition_broadcast(128)), writes=['MODC'])
                dma('sp', lambda e: e.dma_start(out=nbc[:, 0:D], in_=n1_d[0:1, :].partition_broadcast(128)), writes=['nbc0'])
                dma('sp', lambda e: e.dma_start(out=nbc[:, D:2 * D], in_=n2_d[0:1, :].partition_broadcast(128)), writes=['nbc1'])
                op('act', lambda e: e.activation(out=c2t[:], in_=c2t[:], func=AF.Silu), reads=['c2t'], writes=['c2t'])
                op('dve', lambda e: e.memset(onesb[:], 1.0), writes=['onesb'])
                for t in range(2):
                    for k in range(8):
                        op('dve', lambda e, t=t, k=k: e.tensor_scalar(out=Lm[:, t, k, :], in0=onesb[:], scalar1=c2t[:, t, k:k + 1], scalar2=None, op0=ALU.mult),
                           reads=['c2t', 'onesb'], writes=['Lm'])
                wmv = wmod_d.rearrange("(p k) n -> p k n", k=8)
                for j in range(12):
                    w = wm[j % 2]
                    wt = 'wm%d' % (j % 2)
                    dma('pool', lambda e, j=j, w=w: e.dma_start(out=w[:], in_=wmv[:, :, j * 512:(j + 1) * 512]), writes=[wt])
                    for t in range(2 if j < 4 else 1):
                        ps = PF[t]
                        for k in range(8):
                            op('pe', lambda e, t=t, k=k, w=w, ps=ps: e.matmul(out=ps[:], lhsT=Lm[:, t, k, :], rhs=w[:, k, :], start=(k == 0), stop=(k == 7)),
                               reads=['Lm', wt], writes=['pfA%d' % t])
                        M = MODL if t == 0 else MODC
                        op('dve', lambda e, M=M, j=j, ps=ps: e.tensor_tensor(out=M[:, j * 512:(j + 1) * 512], in0=M[:, j * 512:(j + 1) * 512], in1=ps[:], op=ALU.add),
                           reads=['pfA%d' % t, 'MODL', 'MODC'], writes=['MODL' if t == 0 else 'MODC'])
                op('dve', lambda e: e.scalar_tensor_tensor(out=MODL[:, D:2 * D], in0=MODL[:, D:2 * D], scalar=1.0, in1=nbc[:, 0:D], op0=ALU.add, op1=ALU.mult), reads=['MODL', 'nbc0'], writes=['MODL'])
                op('dve', lambda e: e.scalar_tensor_tensor(out=MODC[:, D:2 * D], in0=MODC[:, D:2 * D], scalar=1.0, in1=nbc[:, 0:D], op0=ALU.add, op1=ALU.mult), reads=['MODC', 'nbc0'], writes=['MODC'])
                op('dve', lambda e: e.scalar_tensor_tensor(out=MODL[:, 4 * D:5 * D], in0=MODL[:, 4 * D:5 * D], scalar=1.0, in1=nbc[:, D:2 * D], op0=ALU.add, op1=ALU.mult), reads=['MODL', 'nbc1'], writes=['MODL'])

                PB = [s1.enter_context(nc.psum_tensor("pbB%d" % i, [128, 8, 128], BF16)) for i in range(2)]
                xt = [S("xt%d" % i, [128, D]) for i in range(2)]
                junk = S("junk", [128, D])
                hb = [S("hb%d" % i, [128, D], BF16) for i in range(2)]
                ssq = S("ssq", [128, 18])
                rs = S("rs", [128, 18])
                for i in range(18):
                    xb = xt[i % 2]
                    xtk = 'xt%d' % (i % 2)
                    hbb = hb[i % 2]
                    hbk = 'hb%d' % (i % 2)
                    src = x_d[i * 128:(i + 1) * 128, :] if i < 16 else ctx_d[(i - 16) * 128:(i - 15) * 128, :]
                    M = MODL if i < 16 else MODC
                    Mk = 'MODL' if i < 16 else 'MODC'
                    dma('sp', lambda e, xb=xb, src=src: e.dma_start(out=xb[:], in_=src), writes=[xtk])
                    op('act', lambda e, xb=xb, i=i: e.activation(out=junk[:], in_=xb[:], func=AF.Square, accum_out=ssq[:, i:i + 1]), reads=[xtk], writes=['junk', 'ssq%d' % i])
                    rstd_from_ssq(ssq[:, i:i + 1], rs[:, i:i + 1], 1.0 / D, ['ssq%d' % i], ['rs%d' % i])
                    op('dve', lambda e, xb=xb, i=i, M=M: e.scalar_tensor_tensor(out=junk[:], in0=xb[:], scalar=rs[:, i:i + 1], in1=M[:, D:2 * D], op0=ALU.mult, op1=ALU.mult),
                       reads=[xtk, 'rs%d' % i, Mk], writes=['junk'])
                    op('dve', lambda e, hbb=hbb, M=M: e.tensor_tensor(out=hbb[:], in0=junk[:], in1=M[:, 0:D], op=ALU.add), reads=['junk', Mk], writes=[hbk])
                    pb = PB[i % 2]
                    pbk = 'pbB%d' % (i % 2)
                    for k in range(8):
                        op('pe', lambda e, pb=pb, hbb=hbb, k=k: e.transpose(out=pb[:, k, :], in_=hbb[:, k * 128:(k + 1) * 128], identity=identb[:]), reads=[hbk, 'identb'], writes=[pbk])
                    op('act', lambda e, pb=pb, i=i: e.copy(out=hT[:, :, i * 128:(i + 1) * 128], in_=pb[:]), reads=[pbk], writes=[('hT', i)])
                kb.barrier()
                if stop == 'B':
                    return fin()
            hT_all = [('hT', i) for i in range(18)]
            if dbg:
                with ExitStack() as s1:
                    tmp = s1.enter_context(nc.sbuf_tensor("dbgt", [128, 8 * NT], F32))
                    op('dve', lambda e: e.tensor_copy(out=tmp[:], in_=hT[:].rearrange("p k n -> p (k n)")), reads=hT_all, writes=['dbgt'])
                    dma('sp', lambda e: e.dma_start(out=dbg_d["d_hT"][:, :], in_=tmp[:]), reads=['dbgt'], writes=['d_hT'])
                    kb.barrier()

            with ExitStack() as s1:
                S = lambda name, shape, dt=F32: s1.enter_context(nc.sbuf_tensor("t_" + name, shape, dt))
                PF = [s1.enter_context(nc.psum_tensor("pfC%d" % i, [128, 512], F32)) for i in range(8)]
                wna = S("wna", [128, 8, 1536], BF16)
                qT = S("qT", [128, 4, T], BF16)
                kT = S("kT", [128, 4, NT], BF16)
                V = S("V", [128, 18, 512], BF16)
                onesb = S("onesbC", [128, 64], BF16)
                tb2 = [S("tb2_%d" % i, [128, 2, 1024], BF16) for i in range(2)]
                PT = [S("PT%d" % i, [128, 256], BF16) for i in range(4)]
                rcp = [S("rcp%d" % i, [128, 256]) for i in range(2)]
                op('dve', lambda e: e.memset(onesb[:], 1.0), writes=['onesbC'])
                for j in range(3):
                    dma('pool', lambda e, j=j: e.dma_start(out=wna[:, :, j * 512:(j + 1) * 512], in_=winv[:, :, j * 512:(j + 1) * 512]), writes=[('wna', j)])
                pfi = [0]

                def nextpf():
                    i = pfi[0] % 8
                    pfi[0] += 1
                    return PF[i], 'pfC%d' % i
                for which in range(2):
                    for p in range(4):
                        ntb = 4 if which == 0 else 5
                        for tb in range(ntb):
                            n0 = tb * 512
                            nn = 512 if tb < 4 else 256
                            ps, pk = nextpf()
                            for k in range(8):
                                op('pe', lambda e, ps=ps, k=k, which=which, p=p, n0=n0, nn=nn: e.matmul(out=ps[:, 0:nn], lhsT=wna[:, k, which * 512 + p * 128: which * 512 + (p + 1) * 128],
                                                                                                     rhs=hT[:, k, n0:n0 + nn], start=(k == 0), stop=(k == 7)),
                                   reads=[('wna', which)] + [('hT', t) for t in range(n0 // 128, (n0 + nn) // 128)], writes=[pk])
                            if which == 0:
                                op('act', lambda e, ps=ps, p=p, n0=n0: e.mul(out=qT[:, p, n0:n0 + 512], in_=ps[:], mul=0.125), reads=[pk], writes=[('qT', p, tb)])
                            else:
                                op('dve', lambda e, ps=ps, p=p, n0=n0, nn=nn: e.tensor_copy(out=kT[:, p, n0:n0 + nn], in_=ps[:, 0:nn]), reads=[pk], writes=[('kT', p, tb)])
                for i in range(18):
                    ps, pk = nextpf()
                    for k in range(8):
                        op('pe', lambda e, ps=ps, k=k, i=i: e.matmul(out=ps[:], lhsT=hT[:, k, i * 128:(i + 1) * 128], rhs=wna[:, k, 1024:1536], start=(k == 0), stop=(k == 7)),
                           reads=[('wna', 2), ('hT', i)], writes=[pk])
                    op('act', lambda e, ps=ps, i=i: e.copy(out=V[:, i, :], in_=ps[:]), reads=[pk], writes=[('V', i)])
                PST = PF[0:4]
                PPV = PF[4:6]
                PDN = PF[6:8]
                cnt = 0
                for h in range(8):
                    p, half = h // 2, h % 2
                    hs = slice(half * 64, half * 64 + 64)
                    tbt = tb2[h % 2]
                    tbk = 'tb2_%d' % (h % 2)
                    dma('pool', lambda e, tbt=tbt, h=h: e.dma_start(out=tbt[:], in_=natbl_d[h].rearrange("t p n -> p t n")), writes=[tbk])
                    for qb in range(8):
                        if qb == 0:
                            tiles, tsel = list(range(0, 4)), 0
                        elif qb == 7:
                            tiles, tsel = list(range(12, 16)), 0
                        else:
                            tiles, tsel = list(range(2 * qb - 2, 2 * qb + 4)), 1
                        keyt = [(m, 4 * qb - 2 * m + 7) for m in tiles] + [(16, None), (17, None)]
                        ppv, pdn = PPV[qb % 2], PDN[qb % 2]
                        ppk, pdk = 'pfC%d' % (4 + qb % 2), 'pfC%d' % (6 + qb % 2)
                        q0 = qb * 256
                        for ti, (m, j0) in enumerate(keyt):
                            pst, pstk = PST[cnt % 4], 'pfC%d' % (cnt % 4)
                            ptt, ptk = PT[cnt % 4], 'PT%d' % (cnt % 4)
                            cnt += 1
                            op('pe', lambda e, pst=pst, m=m, j0=j0: e.matmul(out=pst[:, 0:256], lhsT=kT[hs, p, m * 128:(m + 1) * 128], rhs=qT[hs, p, q0:q0 + 256], start=True, stop=(j0 is None)),
                               reads=[('kT', p, min(m // 4, 4)), ('qT', p, qb // 2)], writes=[pstk])
                            if j0 is not None:
                                op('pe', lambda e, pst=pst, j0=j0: e.matmul(out=pst[:, 0:256], lhsT=identb[:], rhs=tbt[:, tsel, j0 * 64:(j0 + 4) * 64], start=False, stop=True),
                                   reads=[tbk, 'identb'], writes=[pstk])
                            op('act', lambda e, pst=pst, ptt=ptt: e.activation(out=ptt[:], in_=pst[:, 0:256], func=AF.Exp), reads=[pstk], writes=[ptk])
                            first, last = ti == 0, ti == len(keyt) - 1
                            op('pe', lambda e, ptt=ptt, m=m, first=first, last=last: e.matmul(out=ppv[hs, 0:256], lhsT=V[:, m, h * 64:(h + 1) * 64], rhs=ptt[:], start=first, stop=last),
                               reads=[ptk, ('V', m)], writes=[ppk])
                            op('pe', lambda e, ptt=ptt, first=first, last=last: e.matmul(out=pdn[hs, 0:256], lhsT=onesb[:], rhs=ptt[:], start=first, stop=last),
                               reads=[ptk, 'onesbC'], writes=[pdk])
                        rc = rcp[qb % 2]
                        rck = 'rcp%d' % (qb % 2)
                        op('dve', lambda e, rc=rc, pdn=pdn: e.reciprocal(out=rc[hs, :], in_=pdn[hs, 0:256]), reads=[pdk], writes=[rck])
                        op('dve', lambda e, rc=rc, ppv=ppv: e.tensor_tensor(out=onaT[hs, p, q0:q0 + 256], in0=ppv[hs, 0:256], in1=rc[hs, :], op=ALU.mult), reads=[ppk, rck], writes=[('onaT', p, qb)])
                kb.barrier()
                if stop == 'C':
                    return fin()
            onaT_all = [('onaT', p, qb) for p in range(4) for qb in range(8)]
            if dbg:
                with ExitStack() as s1:
                    tmp = s1.enter_context(nc.sbuf_tensor("dbgt2", [128, 4 * T], F32))
                    op('dve', lambda e: e.tensor_copy(out=tmp[:], in_=onaT[:].rearrange("p k n -> p (k n)")), reads=onaT_all, writes=['dbgt2'])
                    dma('sp', lambda e: e.dma_start(out=dbg_d["d_ona"][:, :], in_=tmp[:]), reads=['dbgt2'], writes=['d_ona'])
                    kb.barrier()

            id64 = cst[0:64, C_ID:C_ID + 64]
            on64 = cst[0:64, C_ONE:C_ONE + 64]
            b3 = lambda ap8: ap8.unsqueeze(2).to_broadcast([64, 8, 64])
            m3 = lambda ap64: ap64.unsqueeze(1).to_broadcast([64, 8, 64])
            with ExitStack() as s1:
                S = lambda name, shape, dt=F32: s1.enter_context(nc.sbuf_tensor("t_" + name, shape, dt))
                PF = [s1.enter_context(nc.psum_tensor("pfD%d" % i, [128, 512], F32)) for i in range(8)]
                pfi = [0]

                def nextpf():
                    i = pfi[0] % 8
                    pfi[0] += 1
                    return PF[i], 'pfD%d' % i
                wdn = S("wdn", [128, 8, 1536], BF16)
                wba = S("wba", [128, 8, 32], BF16)
                convw = S("convw", [128, 60])
                ropet = S("ropet", [64, 2, 32, 32])
                zl = [S("zl%d" % i, [128, 2052]) for i in range(2)]
                zx = [S("zx%d" % i, [128, 260]) for i in range(2)]
                acc = S("acc", [128, NT])
                sl = S("sl", [128, NT])
                sqt = S("sqt", [64, 512])
                ssq8 = S("ssq8", [64, 8])
                r8 = S("r8", [64, 8])
                st = [S("st%d" % i, [64, 4, 128]) for i in range(2)]
                st2 = [S("st2%d" % i, [64, 4, 128]) for i in range(2)]
                ra = S("ra", [64, 4, 2, 32])
                rb = S("rb", [64, 4, 2, 32])
                BA = S("BA", [64, 36, 32])
                BETA = S("BETA", [64, 36, 16])
                GG = S("GG", [64, 36, 16])
                ta = S("ta", [64, 36, 16])
                tb_ = S("tb_", [64, 36, 16])
                tc_ = S("tc_", [64, 36, 16])
                ab = S("ab", [64, 32])
                one1 = S("one1", [64, 1])
                for j in range(3):
                    dma('pool', lambda e, j=j: e.dma_start(out=wdn[:, :, j * 512:(j + 1) * 512], in_=winv[:, :, 1536 + j * 512:1536 + (j + 1) * 512]), writes=[('wdn', j)])
                dma('pool', lambda e: e.dma_start(out=wba[:], in_=winv[:, :, 3584:3616]), writes=['wba'])
                dma('sp', lambda e: e.dma_start(out=convw[:], in_=convw_d[:, :]), writes=['convw'])
                dma('sp', lambda e: e.dma_start(out=ropet[:], in_=rope_d.rearrange("p (a c f) -> p a c f", a=2, c=32)), writes=['ropet'])
                dma('sp', lambda e: e.dma_start(out=ab[:, 0:16], in_=alog_d[0:1, :].partition_broadcast(64)), writes=['ab'])
                dma('sp', lambda e: e.dma_start(out=ab[:, 16:32], in_=dtb_d[0:1, :].partition_broadcast(64)), reads=[], writes=['ab2'])
                op('dve', lambda e: e.memset(one1[:], 1.0), writes=['one1'])
                for i in range(2):
                    op('dve', lambda e, i=i: e.memset(zl[i][:], 0.0), writes=['zl%d' % i])
                    op('dve', lambda e, i=i: e.memset(zx[i][:], 0.0), writes=['zx%d' % i])
                for g3 in range(3):
                    ps, pk = nextpf()
                    for c in range(12):
                        n = g3 * 12 + c
                        for k in range(8):
                            op('pe', lambda e, ps=ps, c=c, n=n, k=k: e.matmul(out=ps[0:64, c * 32:(c + 1) * 32], lhsT=hT[:, k, n * 64:(n + 1) * 64], rhs=wba[:, k, :], start=(k == 0), stop=(k == 7)),
                               reads=['wba', ('hT', n // 2)], writes=[pk])
                    op('act', lambda e, ps=ps, g3=g3: e.copy(out=BA[:, g3 * 12:(g3 + 1) * 12, :], in_=ps[0:64, 0:384].rearrange("p (c f) -> p c f", f=32)), reads=[pk], writes=['BA'])
                op('act', lambda e: e.activation(out=BETA[:], in_=BA[:, :, 0:16], func=AF.Sigmoid), reads=['BA'], writes=['BETA'])
                op('dve', lambda e: e.tensor_tensor(out=ta[:], in0=BA[:, :, 16:32], in1=ab[:, 16:32].unsqueeze(1).to_broadcast([64, 36, 16]), op=ALU.add), reads=['BA', 'ab2'], writes=['ta'])
                op('dve', lambda e: e.tensor_scalar(out=tb_[:], in0=ta[:], scalar1=-1.0, scalar2=None, op0=ALU.mult), reads=['ta'], writes=['tb_'])
                op('dve', lambda e: e.tensor_tensor(out=tb_[:], in0=tb_[:], in1=ta[:], op=ALU.max), reads=['ta', 'tb_'], writes=['tb_'])
                op('act', lambda e: e.activation(out=tb_[:], in_=tb_[:], func=AF.Exp, scale=-1.0), reads=['tb_'], writes=['tb_'])
                op('act', lambda e: e.activation(out=tb_[:], in_=tb_[:], func=AF.Ln, bias=one1[:]), reads=['tb_', 'one1'], writes=['tb_'])
                op('dve', lambda e: e.tensor_scalar_max(out=tc_[:], in0=ta[:], scalar1=0.0), reads=['ta'], writes=['tc_'])
                op('dve', lambda e: e.tensor_tensor(out=tc_[:], in0=tc_[:], in1=tb_[:], op=ALU.add), reads=['tc_', 'tb_'], writes=['tc_'])
                op('act', lambda e: e.activation(out=ab[:, 0:16], in_=ab[:, 0:16], func=AF.Exp), reads=['ab'], writes=['ab'])
                op('dve', lambda e: e.scalar_tensor_tensor(out=GG[:], in0=tc_[:], scalar=-1.0, in1=ab[:, 0:16].unsqueeze(1).to_broadcast([64, 36, 16]), op0=ALU.mult, op1=ALU.mult),
                   reads=['tc_', 'ab'], writes=['GG'])
                for cc in range(12):
                    z_l, z_x = zl[cc % 2], zx[cc % 2]
                    zlk, zxk = 'zl%d' % (cc % 2), 'zx%d' % (cc % 2)
                    for tb in range(5):
                        n0 = tb * 512
                        nn = 512 if tb < 4 else 256
                        ps, pk = nextpf()
                        for k in range(8):
                            op('pe', lambda e, ps=ps, k=k, n0=n0, nn=nn: e.matmul(out=ps[:, 0:nn], lhsT=wdn[:, k, cc * 128:(cc + 1) * 128], rhs=hT[:, k, n0:n0 + nn], start=(k == 0), stop=(k == 7)),
                               reads=[('wdn', cc // 4)] + [('hT', t) for t in range(n0 // 128, (n0 + nn) // 128)], writes=[pk])
                        if tb < 4:
                            op('act', lambda e, ps=ps, n0=n0: e.copy(out=z_l[:, 2 + n0:2 + n0 + 512], in_=ps[:]), reads=[pk], writes=[zlk])
                        else:
                            op('act', lambda e, ps=ps: e.copy(out=z_x[:, 2:258], in_=ps[:, 0:256]), reads=[pk], writes=[zxk])
                    for (zs, zk, a0, n) in ((z_l, zlk, 0, 2048), (z_x, zxk, 2048, 256)):
                        op('dve', lambda e, zs=zs, a0=a0, n=n: e.tensor_scalar(out=acc[:, a0:a0 + n], in0=zs[:, 0:n], scalar1=convw[:, cc * 5:cc * 5 + 1], scalar2=None, op0=ALU.mult),
                           reads=[zk, 'convw'], writes=['acc'])
                        for tap in range(1, 5):
                            op('dve', lambda e, zs=zs, a0=a0, n=n, tap=tap: e.scalar_tensor_tensor(out=acc[:, a0:a0 + n], in0=zs[:, tap:tap + n], scalar=convw[:, cc * 5 + tap:cc * 5 + tap + 1],
                                                                                               in1=acc[:, a0:a0 + n], op0=ALU.mult, op1=ALU.add), reads=[zk, 'convw', 'acc'], writes=['acc'])
                    op('act', lambda e: e.activation(out=sl[:], in_=acc[:], func=AF.Silu), reads=['acc'], writes=['sl'])
                    for g in range(9):
                        ps, pk = nextpf()
                        for c4 in range(4):
                            n = 4 * g + c4
                            op('pe', lambda e, ps=ps, c4=c4, n=n: e.transpose(out=ps[0:64, c4 * 128:(c4 + 1) * 128], in_=sl[:, n * 64:(n + 1) * 64], identity=ident), reads=['sl', 'cst'], writes=[pk])
                        sti, stk = st[g % 2], 'st%d' % (g % 2)
                        if cc < 8:
                            op('act', lambda e, ps=ps: e.activation(out=sqt[:], in_=ps[0:64, :], func=AF.Square), reads=[pk], writes=['sqt'])
                            op('dve', lambda e: e.tensor_reduce(out=ssq8[:], in_=sqt[:].rearrange("p (g f) -> p g f", f=64), axis=AX.X, op=ALU.add), reads=['sqt'], writes=['ssq8'])
                            rstd_from_ssq(ssq8[:], r8[:], 1.0, ['ssq8'], ['r8'])
                            op('dve', lambda e, ps=ps, sti=sti: e.tensor_tensor(out=sti[:].rearrange("p c (h f) -> p (c h) f", h=2), in0=ps[0:64, :].rearrange("p (g f) -> p g f", f=64),
                                                                            in1=b3(r8[:]), op=ALU.mult), reads=[pk, 'r8'], writes=[stk])
                            if g < 8:
                                s2i, s2k = st2[g % 2], 'st2%d' % (g % 2)
                                x5 = sti[:].rearrange("p c (h t f) -> p c h t f", h=2, t=2)
                                o5 = s2i[:].rearrange("p c (h t f) -> p c h t f", h=2, t=2)
                                cosb = ropet[:, 0, 4 * g:4 * g + 4, :].unsqueeze(2).to_broadcast([64, 4, 2, 32])
                                sinb = ropet[:, 1, 4 * g:4 * g + 4, :].unsqueeze(2).to_broadcast([64, 4, 2, 32])
                                op('dve', lambda e: e.tensor_tensor(out=ra[:], in0=x5[:, :, :, 0, :], in1=cosb, op=ALU.mult), reads=[stk, 'ropet'], writes=['ra'])
                                op('pool', lambda e: e.tensor_tensor(out=rb[:], in0=x5[:, :, :, 1, :], in1=sinb, op=ALU.mult), reads=[stk, 'ropet'], writes=['rb'])
                                op('dve', lambda e: e.tensor_tensor(out=o5[:, :, :, 0, :], in0=ra[:], in1=rb[:], op=ALU.subtract), reads=['ra', 'rb'], writes=[s2k])
                                op('dve', lambda e: e.tensor_tensor(out=ra[:], in0=x5[:, :, :, 0, :], in1=sinb, op=ALU.mult), reads=[stk, 'ropet'], writes=['ra'])
                                op('pool', lambda e: e.tensor_tensor(out=rb[:], in0=x5[:, :, :, 1, :], in1=cosb, op=ALU.mult), reads=[stk, 'ropet'], writes=['rb'])
                                op('dve', lambda e: e.tensor_tensor(out=o5[:, :, :, 1, :], in0=ra[:], in1=rb[:], op=ALU.add), reads=['ra', 'rb'], writes=[s2k])
                                sti, stk = s2i, s2k
                        else:
                            op('act', lambda e, ps=ps, sti=sti: e.copy(out=sti[:].rearrange("p c f -> p (c f)"), in_=ps[0:64, :]), reads=[pk], writes=[stk])
                        dma('sp', lambda e, sti=sti, g=g: e.dma_start(out=qkv_s[4 * g:4 * g + 4, :, cc * 128:(cc + 1) * 128].rearrange("c p f -> p c f"), in_=sti[:]), reads=[stk], writes=[('qkv_s', g)])
                kb.barrier()
                if stop == 'D1':
                    return fin()
            with ExitStack() as s1:
                S = lambda name, shape, dt=F32: s1.enter_context(nc.sbuf_tensor("t_" + name, shape, dt))
                PF = [s1.enter_context(nc.psum_tensor("pfE%d" % i, [128, 512], F32)) for i in range(8)]
                pfi = [0]

                def nextpf():
                    i = pfi[0] % 8
                    pfi[0] += 1
                    return PF[i], 'pfE%d' % i
                wba = S("wba2", [128, 8, 32], BF16)
                BA = S("BA2", [64, 36, 32])
                BETA = S("BETA2", [64, 36, 16])
                GG = S("GG2", [64, 36, 16])
                ta = S("ta2", [64, 36, 16])
                tb_ = S("tb2_", [64, 36, 16])
                tc_ = S("tc2_", [64, 36, 16])
                ab = S("ab2", [64, 32])
                one1 = S("one12", [64, 1])
                dma('pool', lambda e: e.dma_start(out=wba[:], in_=winv[:, :, 3584:3616]), writes=['wba'])
                dma('sp', lambda e: e.dma_start(out=ab[:, 0:16], in_=alog_d[0:1, :].partition_broadcast(64)), writes=['ab'])
                dma('sp', lambda e: e.dma_start(out=ab[:, 16:32], in_=dtb_d[0:1, :].partition_broadcast(64)), reads=[], writes=['ab2'])
                op('dve', lambda e: e.memset(one1[:], 1.0), writes=['one1'])
                for g3 in range(3):
                    ps, pk = nextpf()
                    for c in range(12):
                        n = g3 * 12 + c
                        for k in range(8):
                            op('pe', lambda e, ps=ps, c=c, n=n, k=k: e.matmul(out=ps[0:64, c * 32:(c + 1) * 32], lhsT=hT[:, k, n * 64:(n + 1) * 64], rhs=wba[:, k, :], start=(k == 0), stop=(k == 7)),
                               reads=['wba', ('hT', n // 2)], writes=[pk])
                    op('act', lambda e, ps=ps, g3=g3: e.copy(out=BA[:, g3 * 12:(g3 + 1) * 12, :], in_=ps[0:64, 0:384].rearrange("p (c f) -> p c f", f=32)), reads=[pk], writes=['BA'])
                op('act', lambda e: e.activation(out=BETA[:], in_=BA[:, :, 0:16], func=AF.Sigmoid), reads=['BA'], writes=['BETA'])
                op('dve', lambda e: e.tensor_tensor(out=ta[:], in0=BA[:, :, 16:32], in1=ab[:, 16:32].unsqueeze(1).to_broadcast([64, 36, 16]), op=ALU.add), reads=['BA', 'ab2'], writes=['ta'])
                op('dve', lambda e: e.tensor_scalar(out=tb_[:], in0=ta[:], scalar1=-1.0, scalar2=None, op0=ALU.mult), reads=['ta'], writes=['tb_'])
                op('dve', lambda e: e.tensor_tensor(out=tb_[:], in0=tb_[:], in1=ta[:], op=ALU.max), reads=['ta', 'tb_'], writes=['tb_'])
                op('act', lambda e: e.activation(out=tb_[:], in_=tb_[:], func=AF.Exp, scale=-1.0), reads=['tb_'], writes=['tb_'])
                op('act', lambda e: e.activation(out=tb_[:], in_=tb_[:], func=AF.Ln, bias=one1[:]), reads=['tb_', 'one1'], writes=['tb_'])
                op('dve', lambda e: e.tensor_scalar_max(out=tc_[:], in0=ta[:], scalar1=0.0), reads=['ta'], writes=['tc_'])
                op('dve', lambda e: e.tensor_tensor(out=tc_[:], in0=tc_[:], in1=tb_[:], op=ALU.add), reads=['tc_', 'tb_'], writes=['tc_'])
                op('act', lambda e: e.activation(out=ab[:, 0:16], in_=ab[:, 0:16], func=AF.Exp), reads=['ab'], writes=['ab'])
                op('dve', lambda e: e.scalar_tensor_tensor(out=GG[:], in0=tc_[:], scalar=-1.0, in1=ab[:, 0:16].unsqueeze(1).to_broadcast([64, 36, 16]), op0=ALU.mult, op1=ALU.mult),
                   reads=['tc_', 'ab'], writes=['GG'])

                ld = [S("ld%d" % i, [64, 1536]) for i in range(2)]
                names = ["gc", "gl", "et", "egl", "eg", "bg", "nb", "t8"]
                sm = {n_: S("sm_" + n_, [64, 8]) for n_ in names}
                bigs = ["Rt", "egrow", "Dm", "Dmi", "Dms", "qdT", "NegA", "QKm", "X0", "QKT", "Z", "Xa", "XTa", "Xb", "XTb", "vb", "kbg", "ktail", "u", "wT", "vnew", "Sst", "ost"]
                bigs = bigs + ["NegAb", "Zb", "Sb"]
                NB16 = ("X0", "Xa", "Xb", "XTa", "XTb", "NegAb", "Zb", "Sb", "qdT", "QKT", "vb", "kbg", "ktail", "wT", "vnew")
                bg_ = {n_: S("bg_" + n_, [64, 8, 64], BF16 if n_ in NB16 else F32) for n_ in bigs}
                qkT = S("qkT", [64, 16, 64], BF16)
                fl = lambda t_: t_[:].rearrange("p h f -> p (h f)")

                def headmm(lhs, rhs, reads, start=True, stop=True, ps=None, pk=None):
                    if ps is None:
                        ps, pk = nextpf()
                    for h in range(8):
                        op('pe', lambda e, h=h: e.matmul(out=ps[0:64, h * 64:(h + 1) * 64], lhsT=lhs(h), rhs=rhs(h), start=start, stop=stop), reads=reads, writes=[pk])
                    return ps, pk

                def headtr(src, srck):
                    ps, pk = nextpf()
                    for h in range(8):
                        op('pe', lambda e, h=h: e.transpose(out=ps[0:64, h * 64:(h + 1) * 64], in_=src(h), identity=id64), reads=[srck, 'cst'], writes=[pk])
                    return ps, pk
                p3 = lambda ps: ps[0:64, :].rearrange("p (h f) -> p h f", f=64)
                B = bg_
                if sub == 'g':
                    kb.barrier()
                    return fin()
                for d in range(2 if sub is None else 1):
                    CU = cst[0:64, C_U:C_U + 64] if d == 0 else cst[0:64, C_L:C_L + 64]
                    incl = cst[0:64, C_IF:C_IF + 64] if d == 0 else cst[0:64, C_IB:C_IB + 64]
                    strict = cst[0:64, C_SF:C_SF + 64] if d == 0 else cst[0:64, C_SB:C_SB + 64]
                    last = 63 if d == 0 else 0
                    order = ([32, 33, 34, 35] + list(range(32))) if d == 0 else ([35, 34, 33, 32] + list(range(31, -1, -1)))
                    op('dve', lambda e: e.memset(B["Sst"][:], 0.0), writes=['Sst'])
                    op('dve', lambda e: e.memset(B["Sb"][:], 0.0), writes=['Sb'])
                    for it, n in enumerate(order):
                        L_, lk = ld[it % 2], 'ld%d' % (it % 2)
                        dma('sp', lambda e, L_=L_, n=n: e.dma_start(out=L_[:], in_=qkv_s[n, :, :]), reads=[('qkv_s', n // 4)], writes=[lk])
                        q3 = L_[:, 0:512].rearrange("p (h f) -> p h f", f=64)
                        k3 = L_[:, 512:1024].rearrange("p (h f) -> p h f", f=64)
                        v3 = L_[:, 1024:1536].rearrange("p (h f) -> p h f", f=64)
                        g = GG[:, n, d * 8:(d + 1) * 8]
                        beta = BETA[:, n, d * 8:(d + 1) * 8]
                        ps, pk = nextpf()
                        op('pe', lambda e, ps=ps: e.matmul(out=ps[0:64, 0:8], lhsT=CU, rhs=g, start=True, stop=True), reads=['GG', 'cst'], writes=[pk])
                        op('dve', lambda e, ps=ps: e.tensor_copy(out=sm["gc"][:], in_=ps[0:64, 0:8]), reads=[pk], writes=['gc'])
                        if sub == 's1':
                            kb.barrier()
                            return fin()
                        op('dve', lambda e: e.tensor_tensor(out=B["Rt"][:], in0=m3(id64), in1=b3(sm["gc"][:]), op=ALU.mult), reads=['gc', 'cst'], writes=['Rt'])
                        psA, pkA = nextpf()
                        op('pe', lambda e: e.matmul(out=psA[0:64, :], lhsT=on64, rhs=fl(B["Rt"]), start=True, stop=True), reads=['Rt', 'cst'], writes=[pkA])
                        op('act', lambda e: e.activation(out=fl(B["egrow"]), in_=psA[0:64, :], func=AF.Exp), reads=[pkA], writes=['egrow'])
                        op('dve', lambda e: e.tensor_tensor(out=B["Dm"][:], in0=b3(sm["gc"][:]), in1=p3(psA), op=ALU.subtract), reads=[pkA, 'gc'], writes=['Dm'])
                        op('act', lambda e: e.copy(out=sm["gl"][:], in_=p3(psA)[:, :, last]), reads=[pkA], writes=['gl'])
                        op('dve', lambda e: e.tensor_scalar_min(out=B["Dm"][:], in0=B["Dm"][:], scalar1=0.0), reads=['Dm'], writes=['Dm'])
                        op('act', lambda e: e.activation(out=B["Dm"][:], in_=B["Dm"][:], func=AF.Exp), reads=['Dm'], writes=['Dm'])
                        op('pool', lambda e: e.tensor_tensor(out=B["Dmi"][:], in0=B["Dm"][:], in1=m3(incl), op=ALU.mult), reads=['Dm', 'cst'], writes=['Dmi'])
                        op('pool', lambda e: e.tensor_tensor(out=B["Dms"][:], in0=B["Dm"][:], in1=m3(strict), op=ALU.mult), reads=['Dm', 'cst'], writes=['Dms'])
                        op('dve', lambda e: e.tensor_tensor(out=sm["t8"][:], in0=sm["gl"][:], in1=sm["gc"][:], op=ALU.subtract), reads=['gl', 'gc'], writes=['t8'])
                        op('act', lambda e: e.activation(out=sm["et"][:], in_=sm["t8"][:], func=AF.Exp), reads=['t8'], writes=['et'])
                        op('act', lambda e: e.activation(out=sm["egl"][:], in_=sm["gl"][:], func=AF.Exp), reads=['gl'], writes=['egl'])
                        op('act', lambda e: e.activation(out=sm["eg"][:], in_=sm["gc"][:], func=AF.Exp), reads=['gc'], writes=['eg'])
                        op('dve', lambda e: e.tensor_tensor(out=sm["bg"][:], in0=sm["eg"][:], in1=beta, op=ALU.mult), reads=['eg', 'BETA'], writes=['bg'])
                        op('dve', lambda e: e.tensor_scalar(out=sm["nb"][:], in0=beta, scalar1=-1.0, scalar2=None, op0=ALU.mult), reads=['BETA'], writes=['nb'])
                        if sub == 's2':
                            kb.barrier()
                            return fin()
                        psQ, pkQ = headtr(lambda h: q3[:, h, :], lk)
                        psK, pkK = headtr(lambda h: k3[:, h, :], lk)
                        op('act', lambda e: e.copy(out=qkT[:, 0:8, :], in_=p3(psQ)), reads=[pkQ], writes=['qT_'])
                        op('dve', lambda e: e.tensor_copy(out=qkT[:, 8:16, :], in_=p3(psK)), reads=[pkK], writes=['kT_'])
                        op('dve', lambda e: e.scalar_tensor_tensor(out=B["qdT"][:], in0=qkT[:, 0:8, :], scalar=0.125, in1=B["egrow"][:], op0=ALU.mult, op1=ALU.mult), reads=['qT_', 'egrow'], writes=['qdT'])
                        if sub == 's3':
                            kb.barrier()
                            return fin()
                        psB, pkB = headmm(lambda h: qkT[:, 8 + h, :], lambda h: qkT[:, 8 + h, :], ['kT_'])
                        psC, pkC = headmm(lambda h: qkT[:, h, :], lambda h: qkT[:, 8 + h, :], ['kT_', 'qT_'])
                        op('dve', lambda e: e.tensor_tensor(out=B["NegA"][:], in0=p3(psB), in1=B["Dms"][:], op=ALU.mult), reads=[pkB, 'Dms'], writes=['NegA'])
                        op('dve', lambda e: e.tensor_tensor(out=B["NegA"][:], in0=B["NegA"][:], in1=b3(sm["nb"][:]), op=ALU.mult), reads=['NegA', 'nb'], writes=['NegA'])
                        op('act', lambda e: e.copy(out=B["NegAb"][:], in_=B["NegA"][:]), reads=['NegA'], writes=['NegAb'])
                        op('dve', lambda e: e.scalar_tensor_tensor(out=B["QKm"][:], in0=p3(psC), scalar=0.125, in1=B["Dmi"][:], op0=ALU.mult, op1=ALU.mult), reads=[pkC, 'Dmi'], writes=['QKm'])
                        if sub == 's4':
                            kb.barrier()
                            return fin()
                        psD, pkD = headtr(lambda h: B["NegA"][:, h, :], 'NegA')
                        psE, pkE = headtr(lambda h: B["QKm"][:, h, :], 'QKm')
                        op('act', lambda e: e.copy(out=B["X0"][:], in_=p3(psD)), reads=[pkD], writes=['X0'])
                        op('dve', lambda e: e.tensor_tensor(out=B["Z"][:], in0=p3(psD), in1=m3(id64), op=ALU.add), reads=[pkD, 'cst'], writes=['Z'])
                        op('act', lambda e: e.copy(out=B["Zb"][:], in_=B["Z"][:]), reads=['Z'], writes=['Zb'])
                        op('act', lambda e: e.copy(out=B["QKT"][:], in_=p3(psE)), reads=[pkE], writes=['QKT'])
                        if sub == 's5':
                            kb.barrier()
                            return fin()
                        X, Xk, XT, XTk = B["X0"], 'X0', B["NegAb"], 'NegAb'
                        for lvl in range(1, 6):
                            nX, nXk = (B["Xa"], 'Xa') if lvl % 2 == 1 else (B["Xb"], 'Xb')
                            nXT, nXTk = (B["XTa"], 'XTa') if lvl % 2 == 1 else (B["XTb"], 'XTb')
                            ps1, pk1 = headmm(lambda h, X=X: X[:, h, :], lambda h, XT=XT: XT[:, h, :], [Xk, XTk])
                            op('act', lambda e, ps1=ps1, nXT=nXT: e.copy(out=nXT[:], in_=p3(ps1)), reads=[pk1], writes=[nXTk])
                            if lvl < 5:
                                ps2, pk2 = headmm(lambda h, XT=XT: XT[:, h, :], lambda h, X=X: X[:, h, :], [Xk, XTk])
                                op('dve', lambda e, ps2=ps2, nX=nX: e.tensor_copy(out=nX[:], in_=p3(ps2)), reads=[pk2], writes=[nXk])
                            ps3, pk3 = headmm(lambda h, nXT=nXT: nXT[:, h, :], lambda h: B["Zb"][:, h, :], [nXTk, 'Zb'])
                            op('dve', lambda e, ps3=ps3: e.tensor_tensor(out=B["Z"][:], in0=B["Z"][:], in1=p3(ps3), op=ALU.add), reads=[pk3, 'Z'], writes=['Z'])
                            op('act', lambda e: e.copy(out=B["Zb"][:], in_=B["Z"][:]), reads=['Z'], writes=['Zb'])
                            X, Xk, XT, XTk = nX, nXk, nXT, nXTk
                        if sub == 's6':
                            kb.barrier()
                            return fin()
                        op('pool', lambda e: e.tensor_tensor(out=B["vb"][:], in0=v3, in1=b3(beta), op=ALU.mult), reads=[lk, 'BETA'], writes=['vb'])
                        op('pool', lambda e: e.tensor_tensor(out=B["kbg"][:], in0=k3, in1=b3(sm["bg"][:]), op=ALU.mult), reads=[lk, 'bg'], writes=['kbg'])
                        op('pool', lambda e: e.tensor_tensor(out=B["ktail"][:], in0=k3, in1=b3(sm["et"][:]), op=ALU.mult), reads=[lk, 'et'], writes=['ktail'])
                        psU, pkU = headmm(lambda h: B["Zb"][:, h, :], lambda h: B["vb"][:, h, :], ['Zb', 'vb'])
                        op('act', lambda e: e.copy(out=B["u"][:], in_=p3(psU)), reads=[pkU], writes=['u'])
                        psW, pkW = headmm(lambda h: B["kbg"][:, h, :], lambda h: B["Zb"][:, h, :], ['Zb', 'kbg'])
                        op('act', lambda e: e.copy(out=B["wT"][:], in_=p3(psW)), reads=[pkW], writes=['wT'])
                        if sub == 's7':
                            kb.barrier()
                            return fin()
                        psP, pkP = headmm(lambda h: B["wT"][:, h, :], lambda h: B["Sb"][:, h, :], ['wT', 'Sb'])
                        op('dve', lambda e: e.tensor_tensor(out=B["vnew"][:], in0=B["u"][:], in1=p3(psP), op=ALU.subtract), reads=[pkP, 'u'], writes=['vnew'])
                        if n < 32:
                            psO, pkO = nextpf()
                            for h in range(8):
                                op('pe', lambda e, h=h: e.matmul(out=psO[0:64, h * 64:(h + 1) * 64], lhsT=B["qdT"][:, h, :], rhs=B["Sb"][:, h, :], start=True, stop=False), reads=['qdT', 'Sb'], writes=[pkO])
                                op('pe', lambda e, h=h: e.matmul(out=psO[0:64, h * 64:(h + 1) * 64], lhsT=B["QKT"][:, h, :], rhs=B["vnew"][:, h, :], start=False, stop=True), reads=['QKT', 'vnew'], writes=[pkO])
                            op('act', lambda e: e.copy(out=B["ost"][:], in_=p3(psO)), reads=[pkO], writes=['ost'])
                            dma('sp', lambda e, n=n: e.dma_start(out=o_s[d, n, :, :], in_=fl(B["ost"])), reads=['ost'], writes=[('o_s', d, n)])
                        psS, pkS = headmm(lambda h: B["ktail"][:, h, :], lambda h: B["vnew"][:, h, :], ['ktail', 'vnew'])
                        op('dve', lambda e: e.tensor_tensor(out=B["Sst"][:], in0=B["Sst"][:], in1=b3(sm["egl"][:]), op=ALU.mult), reads=['Sst', 'egl'], writes=['Sst'])
                        op('dve', lambda e: e.tensor_tensor(out=B["Sst"][:], in0=B["Sst"][:], in1=p3(psS), op=ALU.add), reads=['Sst', pkS], writes=['Sst'])
                        op('act', lambda e: e.copy(out=B["Sb"][:], in_=B["Sst"][:]), reads=['Sst'], writes=['Sb'])
                        if sub is not None and sub.startswith('it') and it + 1 == int(sub[2:]):
                            kb.barrier()
                            return fin()
                kb.barrier()
                if stop == 'D2':
                    return fin()
            with ExitStack() as s1:
                S = lambda name, shape, dt=F32: s1.enter_context(nc.sbuf_tensor("t_" + name, shape, dt))
                PF = [s1.enter_context(nc.psum_tensor("pfF%d" % i, [128, 512], F32)) for i in range(4)]
                PB = [s1.enter_context(nc.psum_tensor("pbF%d" % i, [128, 4, 256], BF16)) for i in range(2)]
                wdg = S("wdg", [128, 8, 512], BF16)
                dnwb = S("dnwb", [64, 64])
                of = [S("of%d" % i, [64, 512]) for i in range(2)]
                ob = [S("ob%d" % i, [64, 512]) for i in range(2)]
                sq = S("sqF", [64, 512])
                s8 = S("s8F", [64, 8])
                r8 = S("r8F", [64, 8])
                sg = S("sgF", [64, 512])
                odb = [S("odb%d" % i, [64, 512], BF16) for i in range(2)]
                dma('pool', lambda e: e.dma_start(out=wdg[:], in_=winv[:, :, 3072:3584]), writes=['wdg'])
                dma('sp', lambda e: e.dma_start(out=dnwb[:], in_=dnw_d[0:1, :].partition_broadcast(64)), writes=['dnwb'])
                for n in range(32):
                    a, ak = of[n % 2], 'of%d' % (n % 2)
                    b_, bk = ob[n % 2], 'ob%d' % (n % 2)
                    dma('sp', lambda e, a=a, n=n: e.dma_start(out=a[:], in_=o_s[0, n, :, :]), reads=[('o_s', 0, n)], writes=[ak])
                    dma('sp', lambda e, b_=b_, n=n: e.dma_start(out=b_[:], in_=o_s[1, n, :, :]), reads=[('o_s', 1, n)], writes=[bk])
                    op('dve', lambda e, a=a, b_=b_: e.tensor_tensor(out=a[:], in0=a[:], in1=b_[:], op=ALU.add), reads=[ak, bk], writes=[ak])
                    op('act', lambda e, a=a: e.activation(out=sq[:], in_=a[:], func=AF.Square), reads=[ak], writes=['sqF'])
                    op('dve', lambda e: e.tensor_reduce(out=s8[:], in_=sq[:].rearrange("p (g f) -> p g f", f=64), axis=AX.X, op=ALU.add), reads=['sqF'], writes=['s8F'])
                    rstd_from_ssq(s8[:], r8[:], 1.0 / 64, ['s8F'], ['r8F'])
                    a3 = a[:].rearrange("p (g f) -> p g f", f=64)
                    op('dve', lambda e, a3=a3: e.tensor_tensor(out=a3, in0=a3, in1=b3(r8[:]), op=ALU.mult), reads=[ak, 'r8F'], writes=[ak])
                    op('pool', lambda e, a3=a3: e.tensor_tensor(out=a3, in0=a3, in1=m3(dnwb[:]), op=ALU.mult), reads=[ak, 'dnwb'], writes=[ak])
                    ps, pk = PF[n % 4], 'pfF%d' % (n % 4)
                    for k in range(8):
                        op('pe', lambda e, ps=ps, k=k, n=n: e.matmul(out=ps[0:64, :], lhsT=hT[:, k, n * 64:(n + 1) * 64], rhs=wdg[:, k, :], start=(k == 0), stop=(k == 7)), reads=['wdg', ('hT', n // 2)], writes=[pk])
                    op('act', lambda e, ps=ps: e.activation(out=sg[:], in_=ps[0:64, :], func=AF.Silu), reads=[pk], writes=['sgF'])
                    o_, ok_ = odb[n % 2], 'odb%d' % (n % 2)
                    op('dve', lambda e, a=a, o_=o_: e.tensor_tensor(out=o_[:], in0=a[:], in1=sg[:], op=ALU.mult), reads=[ak, 'sgF'], writes=[ok_])
                    pb, pbk = PB[n % 2], 'pbF%d' % (n % 2)
                    for c in range(4):
                        op('pe', lambda e, pb=pb, o_=o_, c=c: e.transpose(out=pb[:, c, 0:64], in_=o_[:, c * 128:(c + 1) * 128], identity=identb[0:64, 0:64]), reads=[ok_, 'identb'], writes=[pbk])
                    op('act', lambda e, pb=pb, n=n: e.copy(out=odnT[:, :, n * 64:(n + 1) * 64], in_=pb[:, :, 0:64]), reads=[pbk], writes=[('odnT', n)])
                kb.barrier()
                if stop == 'D3':
                    return fin()
            if dbg:
                with ExitStack() as s1:
                    tmp = s1.enter_context(nc.sbuf_tensor("dbgt3", [128, 4 * T], F32))
                    op('dve', lambda e: e.tensor_copy(out=tmp[:], in_=odnT[:].rearrange("p k n -> p (k n)")), reads=[('odnT', n) for n in range(32)], writes=['dbgt3'])
                    dma('sp', lambda e: e.dma_start(out=dbg_d["d_odn"][:, :], in_=tmp[:]), reads=['dbgt3'], writes=['d_odn'])
                    kb.barrier()

            with ExitStack() as s1:
                S = lambda name, shape, dt=F32: s1.enter_context(nc.sbuf_tensor("t_" + name, shape, dt))
                PF = [s1.enter_context(nc.psum_tensor("pfG%d" % i, [128, 512], F32)) for i in range(8)]
                pfi = [0]

                def nextpf():
                    i = pfi[0] % 8
                    pfi[0] += 1
                    return PF[i], 'pfG%d' % i
                wg = S("wg", [128, 8, 2048], BF16)
                wa = S("wa", [128, 4, D], BF16)
                wb = S("wb", [128, 4, D], BF16)
                wo = S("wo", [128, 8, D], BF16)
                wr = S("wr", [128, 8, 32])
                brb = S("brb", [128, 32])
                yT = S("yT", [128, 8, 512], BF16)
                sga = S("sga", [128, 512])
                sgb = S("sgb", [128, 512])
                xm = [S("xm0", [128, D])] * 2
                xo = [S("xo%d" % i, [128, D]) for i in range(2)]
                h2 = [S("h2_%d" % i, [128, D]) for i in range(2)]
                h2T = S("h2T", [128, 8, 128])
                junk = S("junkE", [128, D])
                ssq = S("ssqE", [128, 16])
                rs = S("rsE", [128, 16])
                for j in range(4):
                    dma('pool', lambda e, j=j: e.dma_start(out=wg[:, :, j * 512:(j + 1) * 512], in_=winv[:, :, 3616 + j * 512:3616 + (j + 1) * 512]), writes=[('wg', j)])
                dma('pool', lambda e: e.dma_start(out=wa[:], in_=wbra_d.rearrange("(k p) n -> p k n", p=128)), writes=['wa'])
                dma('pool', lambda e: e.dma_start(out=wb[:], in_=wbrb_d.rearrange("(k p) n -> p k n", p=128)), writes=['wb'])
                for j in range(2):
                    dma('pool', lambda e, j=j: e.dma_start(out=wo[:, :, j * 512:(j + 1) * 512], in_=wout_d.rearrange("(k p) n -> p k n", p=128)[:, :, j * 512:(j + 1) * 512]), writes=[('wo', j)])
                dma('sp', lambda e: e.dma_start(out=wr[:], in_=wr_d.rearrange("(k p) n -> p k n", p=128)), writes=['wr'])
                dma('sp', lambda e: e.dma_start(out=brb[:], in_=br_d[0:1, :].partition_broadcast(128)), writes=['brb'])
                for tb in range(4):
                    n0 = tb * 512
                    hts = [('hT', t) for t in range(n0 // 128, n0 // 128 + 4)]
                    for c in range(8):
                        cs = slice(c * 128, (c + 1) * 128)
                        psa, pka = nextpf()
                        for k in range(4):
                            op('pe', lambda e, k=k, psa=psa: e.matmul(out=psa[:], lhsT=wa[:, k, cs], rhs=onaT[:, k, n0:n0 + 512], start=(k == 0), stop=(k == 3)), reads=['wa'] + onaT_all, writes=[pka])
                        psb, pkb = nextpf()
                        for k in range(4):
                            op('pe', lambda e, k=k, psb=psb: e.matmul(out=psb[:], lhsT=wb[:, k, cs], rhs=odnT[:, k, n0:n0 + 512], start=(k == 0), stop=(k == 3)), reads=['wb'] + [('odnT', n) for n in range(tb * 8, tb * 8 + 8)], writes=[pkb])
                        pga, pkga = nextpf()
                        for k in range(8):
                            op('pe', lambda e, k=k, pga=pga: e.matmul(out=pga[:], lhsT=wg[:, k, c * 128:(c + 1) * 128], rhs=hT[:, k, n0:n0 + 512], start=(k == 0), stop=(k == 7)), reads=[('wg', c // 4)] + hts, writes=[pkga])
                        pgb, pkgb = nextpf()
                        for k in range(8):
                            op('pe', lambda e, k=k, pgb=pgb: e.matmul(out=pgb[:], lhsT=wg[:, k, 1024 + c * 128:1024 + (c + 1) * 128], rhs=hT[:, k, n0:n0 + 512], start=(k == 0), stop=(k == 7)), reads=[('wg', 2 + c // 4)] + hts, writes=[pkgb])
                        op('act', lambda e, pga=pga: e.activation(out=sga[:], in_=pga[:], func=AF.Sigmoid), reads=[pkga], writes=['sga'])
                        op('act', lambda e, pgb=pgb: e.activation(out=sgb[:], in_=pgb[:], func=AF.Sigmoid), reads=[pkgb], writes=['sgb'])
                        op('dve', lambda e, psa=psa: e.tensor_tensor(out=sga[:], in0=sga[:], in1=psa[:], op=ALU.mult), reads=['sga', pka], writes=['sga'])
                        op('dve', lambda e, psb=psb: e.tensor_tensor(out=sgb[:], in0=sgb[:], in1=psb[:], op=ALU.mult), reads=['sgb', pkb], writes=['sgb'])
                        op('pool', lambda e, c=c: e.tensor_tensor(out=yT[:, c, :], in0=sga[:], in1=sgb[:], op=ALU.add), reads=['sga', 'sgb'], writes=['yT'])
                    for i4 in range(4):
                        i = tb * 4 + i4
                        xmi, xmk = xm[0], 'xm0'
                        xoi, xok = xo[i % 2], 'xo%d' % (i % 2)
                        h2i, h2k = h2[i % 2], 'h2_%d' % (i % 2)
                        dma('sp', lambda e, xmi=xmi, i=i: e.dma_start(out=xmi[:], in_=x_d[i * 128:(i + 1) * 128, :]), writes=[xmk])
                        for half in range(2):
                            hs_ = slice(half * 512, (half + 1) * 512)
                            ps, pk = nextpf()
                            for k in range(8):
                                op('pe', lambda e, k=k, ps=ps: e.matmul(out=ps[:], lhsT=yT[:, k, i4 * 128:(i4 + 1) * 128], rhs=wo[:, k, hs_], start=(k == 0), stop=(k == 7)), reads=['yT', ('wo', half)], writes=[pk])
                            op('dve', lambda e, ps=ps, xoi=xoi: e.tensor_tensor(out=xoi[:, hs_], in0=ps[:], in1=MODL[:, 2 * D + half * 512:2 * D + (half + 1) * 512], op=ALU.mult), reads=[pk, 'MODL'], writes=[xok])
                            op('dve', lambda e, xoi=xoi, xmi=xmi: e.tensor_tensor(out=xoi[:, hs_], in0=xoi[:, hs_], in1=xmi[:, hs_], op=ALU.add), reads=[xok, xmk], writes=[xok])
                        dma('sp', lambda e, xoi=xoi, i=i: e.dma_start(out=xl2_s[i * 128:(i + 1) * 128, :], in_=xoi[:]), reads=[xok], writes=[('xl2_s', i)])
                        op('act', lambda e, xoi=xoi, i=i: e.activation(out=junk[:], in_=xoi[:], func=AF.Square, accum_out=ssq[:, i:i + 1]), reads=[xok], writes=['junkE', 'ssqE%d' % i])
                        rstd_from_ssq(ssq[:, i:i + 1], rs[:, i:i + 1], 1.0 / D, ['ssqE%d' % i], ['rsE%d' % i])
                        op('dve', lambda e, xoi=xoi, i=i: e.scalar_tensor_tensor(out=junk[:], in0=xoi[:], scalar=rs[:, i:i + 1], in1=MODL[:, 4 * D:5 * D], op0=ALU.mult, op1=ALU.mult), reads=[xok, 'rsE%d' % i, 'MODL', 'junkE'], writes=['junkE'])
                        op('dve', lambda e, h2i=h2i: e.tensor_tensor(out=h2i[:], in0=junk[:], in1=MODL[:, 3 * D:4 * D], op=ALU.add), reads=['junkE', 'MODL'], writes=[h2k])
                        dma('sp', lambda e, h2i=h2i, i=i: e.dma_start(out=h2_s[i * 128:(i + 1) * 128, :], in_=h2i[:]), reads=[h2k], writes=[('h2_s', i)])
                        for hh in range(2):
                            ps, pk = nextpf()
                            for k4 in range(4):
                                k = hh * 4 + k4
                                op('pe', lambda e, ps=ps, k=k, k4=k4, h2i=h2i: e.transpose(out=ps[:, k4 * 128:(k4 + 1) * 128], in_=h2i[:, k * 128:(k + 1) * 128], identity=ident), reads=[h2k, 'cst'], writes=[pk])
                            op('act', lambda e, ps=ps, hh=hh: e.copy(out=h2T[:, hh * 4:(hh + 1) * 4, :], in_=ps[:].rearrange("p (k f) -> p k f", f=128)), reads=[pk], writes=[('h2T', hh)])
                        ps, pk = nextpf()
                        for k in range(8):
                            op('pe', lambda e, ps=ps, k=k: e.matmul(out=ps[:, 0:32], lhsT=h2T[:, k, :], rhs=wr[:, k, :], start=(k == 0), stop=(k == 7)), reads=[('h2T', k // 4), 'wr'], writes=[pk])
                        op('dve', lambda e, ps=ps, i=i: e.tensor_tensor(out=LG[:, i, :], in0=ps[:, 0:32], in1=brb[:], op=ALU.add), reads=[pk, 'brb'], writes=['LG'])
                kb.barrier()
                if stop == 'E':
                    return fin()
            if dbg:
                dma('sp', lambda e: e.dma_start(out=dbg_d["d_lg"][:, :], in_=LG[:].rearrange("p a b -> p (a b)")), reads=['LG'], writes=['d_lg'])
                kb.barrier()

            mid.close()
            with ExitStack() as s1:
                S = lambda name, shape, dt=F32: s1.enter_context(nc.sbuf_tensor("t_" + name, shape, dt))
                PF = [s1.enter_context(nc.psum_tensor("pfH%d" % i, [128, 512], F32)) for i in range(8)]
                A3 = [128, 16, 32]
                DESTi = S("DESTi", [128, 4, 16], I32)
                GATE = S("GATE", [128, 4, 16])
                W1I = S("W1I", [128, NBLK, 8], I32)
                B1I = S("B1I", [128, NBLK], I32)
                B2I = S("B2I", [128, NBLK], I32)
                rt = ExitStack()
                s1.callback(rt.close)
                R_ = lambda name, shape, dt=F32: rt.enter_context(nc.sbuf_tensor("t_" + name, shape, dt))
                LGw = R_("LGw", A3)
                EQ = R_("EQ", [128, 4, 16, 32])
                SEL = R_("SEL", A3)
                GT = R_("GT", A3)
                RANK = R_("RANK", A3)
                tmp3 = R_("tmp3", A3)
                mr = R_("mr", [128, 16])
                m0 = R_("m0", [128, 16])
                den = R_("den", [128, 16])
                CNT = R_("CNT", [128, 32])
                PAD = R_("PAD", [128, 32])
                PADi = R_("PADi", [128, 32], I32)
                PADT = R_("PADT", [32, 128])
                PSb = R_("PSb", [128, 32])
                PEb = R_("PEb", [128, 32])
                DESTf = R_("DESTf", [128, 4, 16])
                cmp_ = R_("cmp_", [128, NBLK, 32])
                BE = R_("BE", [128, NBLK])
                tmpw = R_("tmpw", [128, NBLK, 8])
                tmpb = R_("tmpb", [128, NBLK])
                zi = R_("zi", [128, NBLK], I32)
                tokid = R_("tokid", [128, 16], I32)
                b16 = lambda ap: ap.unsqueeze(2).to_broadcast(A3)
                e16 = lambda ap: ap.unsqueeze(1).to_broadcast(A3)
                op('dve', lambda e: e.tensor_copy(out=LGw[:], in_=LG[:]), reads=['LG'], writes=['LGw'])
                for r in range(4):
                    op('dve', lambda e: e.tensor_reduce(out=mr[:], in_=LGw[:], axis=AX.X, op=ALU.max), reads=['LGw'], writes=['mr'])
                    if r == 0:
                        op('dve', lambda e: e.tensor_copy(out=m0[:], in_=mr[:]), reads=['mr'], writes=['m0'])
                    op('dve', lambda e, r=r: e.tensor_tensor(out=EQ[:, r], in0=LGw[:], in1=b16(mr[:]), op=ALU.is_equal), reads=['LGw', 'mr'], writes=[('EQ', r)])
                    op('dve', lambda e, r=r: e.scalar_tensor_tensor(out=LGw[:], in0=EQ[:, r], scalar=NEG, in1=LGw[:], op0=ALU.mult, op1=ALU.add), reads=[('EQ', r), 'LGw'], writes=['LGw'])
                op('dve', lambda e: e.tensor_tensor(out=SEL[:], in0=EQ[:, 0], in1=EQ[:, 1], op=ALU.add), reads=[('EQ', 0), ('EQ', 1)], writes=['SEL'])
                op('dve', lambda e: e.tensor_tensor(out=SEL[:], in0=SEL[:], in1=EQ[:, 2], op=ALU.add), reads=['SEL', ('EQ', 2)], writes=['SEL'])
                op('dve', lambda e: e.tensor_tensor(out=SEL[:], in0=SEL[:], in1=EQ[:, 3], op=ALU.add), reads=['SEL', ('EQ', 3)], writes=['SEL'])
                op('dve', lambda e: e.tensor_tensor(out=GT[:], in0=LG[:], in1=b16(m0[:]), op=ALU.subtract), reads=['LG', 'm0'], writes=['GT'])
                op('act', lambda e: e.activation(out=GT[:], in_=GT[:], func=AF.Exp), reads=['GT'], writes=['GT'])
                op('dve', lambda e: e.tensor_tensor(out=GT[:], in0=GT[:], in1=SEL[:], op=ALU.mult), reads=['GT', 'SEL'], writes=['GT'])
                op('dve', lambda e: e.tensor_reduce(out=den[:], in_=GT[:], axis=AX.X, op=ALU.add), reads=['GT'], writes=['den'])
                op('dve', lambda e: e.reciprocal(out=den[:], in_=den[:]), reads=['den'], writes=['den'])
                op('dve', lambda e: e.tensor_tensor(out=GT[:], in0=GT[:], in1=b16(den[:]), op=ALU.mult), reads=['GT', 'den'], writes=['GT'])
                op('dve', lambda e: e.memset(CNT[:], 0.0), writes=['CNT'])
                for i in range(16):
                    ps, pk = PF[i % 2], 'pfH%d' % (i % 2)
                    op('pe', lambda e, ps=ps, i=i: e.matmul(out=ps[:, 0:32], lhsT=cst[:, C_UT:C_UT + 128], rhs=SEL[:, i, :], start=True, stop=True), reads=['SEL', 'cst'], writes=[pk])
                    op('dve', lambda e, ps=ps, i=i: e.tensor_tensor(out=RANK[:, i, :], in0=ps[:, 0:32], in1=CNT[:], op=ALU.add), reads=[pk, 'CNT'], writes=['RANK'])
                    ps2, pk2 = PF[2 + i % 2], 'pfH%d' % (2 + i % 2)
                    op('pe', lambda e, ps2=ps2, i=i: e.matmul(out=ps2[:, 0:32], lhsT=ones, rhs=SEL[:, i, :], start=True, stop=True), reads=['SEL', 'cst'], writes=[pk2])
                    op('dve', lambda e, ps2=ps2: e.tensor_tensor(out=CNT[:], in0=CNT[:], in1=ps2[:, 0:32], op=ALU.add), reads=[pk2, 'CNT'], writes=['CNT'])
                op('dve', lambda e: e.tensor_scalar(out=PAD[:], in0=CNT[:], scalar1=127.0, scalar2=None, op0=ALU.add), reads=['CNT'], writes=['PAD'])
                op('dve', lambda e: e.tensor_copy(out=PADi[:], in_=PAD[:]), reads=['PAD'], writes=['PADi'])
                op('dve', lambda e: e.tensor_scalar(out=PADi[:], in0=PADi[:], scalar1=7, scalar2=7, op0=ALU.arith_shift_right, op1=ALU.logical_shift_left), reads=['PADi'], writes=['PADi'])
                op('dve', lambda e: e.tensor_copy(out=PAD[:], in_=PADi[:]), reads=['PADi'], writes=['PAD'])
                op('pe', lambda e: e.transpose(out=PF[4][0:32, 0:128], in_=PAD[:, 0:32], identity=ident), reads=['PAD', 'cst'], writes=['pfH4'])
                op('act', lambda e: e.copy(out=PADT[:], in_=PF[4][0:32, 0:128]), reads=['pfH4'], writes=['PADT'])
                op('pe', lambda e: e.matmul(out=PF[5][:, 0:32], lhsT=PADT[:], rhs=cst[0:32, C_TRI:C_TRI + 32], start=True, stop=True), reads=['PADT', 'cst'], writes=['pfH5'])
                op('act', lambda e: e.copy(out=PSb[:], in_=PF[5][:, 0:32]), reads=['pfH5'], writes=['PSb'])
                op('dve', lambda e: e.tensor_tensor(out=PEb[:], in0=PSb[:], in1=PAD[:], op=ALU.add), reads=['PSb', 'PAD'], writes=['PEb'])
                op('dve', lambda e: e.tensor_tensor(out=RANK[:], in0=RANK[:], in1=e16(PSb[:]), op=ALU.add), reads=['RANK', 'PSb'], writes=['RANK'])
                for r in range(4):
                    op('dve', lambda e, r=r: e.tensor_tensor(out=tmp3[:], in0=EQ[:, r], in1=RANK[:], op=ALU.mult), reads=[('EQ', r), 'RANK'], writes=['tmp3'])
                    op('dve', lambda e, r=r: e.tensor_reduce(out=DESTf[:, r, :], in_=tmp3[:], axis=AX.X, op=ALU.add), reads=['tmp3'], writes=['DESTf'])
                    op('dve', lambda e, r=r: e.tensor_tensor(out=tmp3[:], in0=EQ[:, r], in1=GT[:], op=ALU.mult), reads=[('EQ', r), 'GT'], writes=['tmp3'])
                    op('dve', lambda e, r=r: e.tensor_reduce(out=GATE[:, r, :], in_=tmp3[:], axis=AX.X, op=ALU.add), reads=['tmp3'], writes=['GATE'])
                op('dve', lambda e: e.tensor_copy(out=DESTi[:], in_=DESTf[:]), reads=['DESTf'], writes=['DESTi'])
                op('dve', lambda e: e.tensor_tensor(out=cmp_[:], in0=PEb[:].unsqueeze(1).to_broadcast([128, NBLK, 32]), in1=cst[:, C_BS:C_BS + NBLK].unsqueeze(2).to_broadcast([128, NBLK, 32]), op=ALU.is_le),
                   reads=['PEb', 'cst'], writes=['cmp_'])
                op('dve', lambda e: e.tensor_reduce(out=BE[:], in_=cmp_[:], axis=AX.X, op=ALU.add), reads=['cmp_'], writes=['BE'])
                op('dve', lambda e: e.tensor_scalar_min(out=BE[:], in0=BE[:], scalar1=31.0), reads=['BE'], writes=['BE'])
                op('dve', lambda e: e.tensor_copy(out=B2I[:], in_=BE[:]), reads=['BE'], writes=['B2I'])
                op('dve', lambda e: e.tensor_scalar(out=tmpb[:], in0=BE[:], scalar1=128.0, scalar2=cst[:, C_KR:C_KR + 1], op0=ALU.mult, op1=ALU.add), reads=['BE', 'cst'], writes=['tmpb'])
                op('dve', lambda e: e.tensor_copy(out=B1I[:], in_=tmpb[:]), reads=['tmpb'], writes=['B1I'])
                op('dve', lambda e: e.tensor_scalar(out=tmpb[:], in0=BE[:], scalar1=1024.0, scalar2=None, op0=ALU.mult), reads=['BE', 'B1I'], writes=['tmpb'])
                op('dve', lambda e: e.tensor_tensor(out=tmpw[:], in0=tmpb[:].unsqueeze(2).to_broadcast([128, NBLK, 8]), in1=cst[:, C_KR:C_KR + 8].unsqueeze(1).to_broadcast([128, NBLK, 8]), op=ALU.add),
                   reads=['tmpb', 'cst'], writes=['tmpw'])
                op('dve', lambda e: e.tensor_copy(out=W1I[:], in_=tmpw[:]), reads=['tmpw'], writes=['W1I'])
                op('dve', lambda e: e.memset(zi[:], 0), writes=['zi'])
                op('dve', lambda e: e.tensor_copy(out=tokid[:], in_=cst[:, C_TOK:C_TOK + 16]), reads=['cst'], writes=['tokid'])
                dma('sp', lambda e: e.dma_start(out=slot_s.rearrange("(p j) o -> p (j o)", p=128), in_=zi[:]), reads=['zi'], writes=['slot_s'])
                for r in range(4):
                    for i in range(16):
                        dma('pool', lambda e, r=r, i=i: e.indirect_dma_start(out=slot_s[:, :], out_offset=bass.IndirectOffsetOnAxis(ap=DESTi[:, r, i:i + 1], axis=0), in_=tokid[:, i:i + 1], in_offset=None),
                            reads=['DESTi', 'tokid'], writes=['slot_s'])
                kb.barrier()
                rt.close()
                sidx = [S("sidx%d" % i, [128, 1], I32) for i in range(2)]
                xg = [S("xg%d" % i, [128, D]) for i in range(2)]
                xgT = S("xgT", [128, 8, 128], BF16)
                w1b = [S("w1b%d" % i, [128, 2048], BF16) for i in range(3)]
                w2b = [S("w2b%d" % i, [128, D], BF16) for i in range(3)]
                w1k = [S("w1k%d" % i, [128, 2048]) for i in range(6)]
                w2c = [S("w2c%d" % i, [128, D]) for i in range(8)]
                b1s = [S("b1s%d" % i, [128, 16]) for i in range(2)]
                b2s = [S("b2s%d" % i, [128, D]) for i in range(2)]
                Gt = S("Gt", [128, 4, 128])
                Ut = S("Ut", [128, 4, 128])
                sgm = S("sgm", [128, 4, 128])
                actT = S("actT", [128, 8, 128], BF16)
                ysb = [S("ysb%d" % i, [128, D]) for i in range(2)]
                PH = PF[0:4]
                PY = PF[4:6]
                PTr = PF[6:8]
                wc1 = wc2 = 0
                for j in range(NBLK):
                    si, sik = sidx[j % 2], 'sidx%d' % (j % 2)
                    xgi, xgk = xg[j % 2], 'xg%d' % (j % 2)
                    dma('sp', lambda e, si=si, j=j: e.dma_start(out=si[:], in_=slot_s[j * 128:(j + 1) * 128, :]), reads=['slot_s'], writes=[sik])
                    dma('pool', lambda e, si=si, xgi=xgi: e.indirect_dma_start(out=xgi[:], out_offset=None, in_=h2_s[:, :], in_offset=bass.IndirectOffsetOnAxis(ap=si[:, 0:1], axis=0)),
                        reads=[sik] + [('h2_s', i) for i in range(16)], writes=[xgk])
                    b1i, b1k = b1s[j % 2], 'b1s%d' % (j % 2)
                    b2i, b2k = b2s[j % 2], 'b2s%d' % (j % 2)
                    dma('pool', lambda e, b1i=b1i, j=j: e.indirect_dma_start(out=b1i[:], out_offset=None, in_=b1t_d[:, :], in_offset=bass.IndirectOffsetOnAxis(ap=B1I[:, j:j + 1], axis=0)), reads=['B1I'], writes=[b1k])
                    dma('pool', lambda e, b2i=b2i, j=j: e.indirect_dma_start(out=b2i[:], out_offset=None, in_=b2_d[:, :], in_offset=bass.IndirectOffsetOnAxis(ap=B2I[:, j:j + 1], axis=0)), reads=['B2I'], writes=[b2k])
                    for hh in range(2):
                        ps, pk = PTr[hh], 'pfH%d' % (6 + hh)
                        for k4 in range(4):
                            k = hh * 4 + k4
                            op('pe', lambda e, ps=ps, k=k, k4=k4, xgi=xgi: e.transpose(out=ps[:, k4 * 128:(k4 + 1) * 128], in_=xgi[:, k * 128:(k + 1) * 128], identity=ident), reads=[xgk, 'cst'], writes=[pk])
                        op('act', lambda e, ps=ps, hh=hh: e.copy(out=xgT[:, hh * 4:(hh + 1) * 4, :], in_=ps[:].rearrange("p (k f) -> p k f", f=128)), reads=[pk], writes=[('xgT', hh)])
                    for k in range(8):
                        wt, wk = w1k[wc1 % 6], 'w1k%d' % (wc1 % 6)
                        wc1 += 1
                        dma('pool', lambda e, wt=wt, j=j, k=k: e.indirect_dma_start(out=wt[:], out_offset=None, in_=w1_d[:, :], in_offset=bass.IndirectOffsetOnAxis(ap=W1I[:, j, k:k + 1], axis=0)), reads=['W1I'], writes=[wk])
                        wb_, wbk = w1b[wc1 % 3], 'w1b%d' % (wc1 % 3)
                        if k % 2 == 0:
                            op('dve', lambda e, wt=wt, wb_=wb_: e.tensor_copy(out=wb_[:], in_=wt[:]), reads=[wk], writes=[wbk])
                        else:
                            op('act', lambda e, wt=wt, wb_=wb_: e.copy(out=wb_[:], in_=wt[:]), reads=[wk], writes=[wbk])
                        for c in range(16):
                            op('pe', lambda e, wb_=wb_, k=k, c=c: e.matmul(out=PH[c // 4][:, (c % 4) * 128:(c % 4 + 1) * 128], lhsT=wb_[:, c * 128:(c + 1) * 128], rhs=xgT[:, k, :], start=(k == 0 and c % 4 == 0), stop=(k == 7 and c % 4 == 3)),
                               reads=[wbk, ('xgT', k // 4)], writes=['pfH%d' % (c // 4)])
                    for q in range(2):
                        g3_ = PH[q][:].rearrange("p (c f) -> p c f", f=128)
                        u3_ = PH[q + 2][:].rearrange("p (c f) -> p c f", f=128)
                        bb = lambda lo: b1i[:, lo:lo + 4].unsqueeze(2).to_broadcast([128, 4, 128])
                        op('dve', lambda e: e.tensor_tensor(out=Gt[:], in0=g3_, in1=bb(4 * q), op=ALU.add), reads=['pfH%d' % q, b1k], writes=['Gt'])
                        op('dve', lambda e: e.tensor_scalar_min(out=Gt[:], in0=Gt[:], scalar1=7.0), reads=['Gt'], writes=['Gt'])
                        op('act', lambda e: e.activation(out=sgm[:], in_=Gt[:], func=AF.Sigmoid, scale=1.702), reads=['Gt'], writes=['sgm'])
                        op('dve', lambda e: e.tensor_tensor(out=Ut[:], in0=u3_, in1=bb(8 + 4 * q), op=ALU.add), reads=['pfH%d' % (q + 2), b1k], writes=['Ut'])
                        op('dve', lambda e: e.tensor_scalar(out=Ut[:], in0=Ut[:], scalar1=7.0, scalar2=-7.0, op0=ALU.min, op1=ALU.max), reads=['Ut'], writes=['Ut'])
                        op('dve', lambda e: e.scalar_tensor_tensor(out=Ut[:], in0=Ut[:], scalar=1.0, in1=Gt[:], op0=ALU.add, op1=ALU.mult), reads=['Ut', 'Gt'], writes=['Ut'])
                        op('dve', lambda e: e.tensor_tensor(out=actT[:, 4 * q:4 * q + 4, :], in0=Ut[:], in1=sgm[:], op=ALU.mult), reads=['Ut', 'sgm'], writes=[('actT', q)])
                    for fc in range(8):
                        wt, wk = w2c[wc2 % 8], 'w2c%d' % (wc2 % 8)
                        wc2 += 1
                        dma('pool', lambda e, wt=wt, j=j, fc=fc: e.indirect_dma_start(out=wt[:], out_offset=None, in_=w2_d[:, :], in_offset=bass.IndirectOffsetOnAxis(ap=W1I[:, j, fc:fc + 1], axis=0)), reads=['W1I'], writes=[wk])
                        wb_, wbk = w2b[wc2 % 3], 'w2b%d' % (wc2 % 3)
                        op('act', lambda e, wt=wt, wb_=wb_: e.copy(out=wb_[:], in_=wt[:]), reads=[wk], writes=[wbk])
                        for half in range(2):
                            op('pe', lambda e, wb_=wb_, fc=fc, half=half: e.matmul(out=PY[half][:], lhsT=actT[:, fc, :], rhs=wb_[:, half * 512:(half + 1) * 512], start=(fc == 0), stop=(fc == 7)),
                               reads=[wbk, ('actT', fc // 4)], writes=['pfH%d' % (4 + half)])
                    yi, yk = ysb[j % 2], 'ysb%d' % (j % 2)
                    for half in range(2):
                        op('dve', lambda e, yi=yi, half=half: e.tensor_tensor(out=yi[:, half * 512:(half + 1) * 512], in0=PY[half][:], in1=b2i[:, half * 512:(half + 1) * 512], op=ALU.add),
                           reads=['pfH%d' % (4 + half), b2k], writes=[yk])
                    dma('sp', lambda e, yi=yi, j=j: e.dma_start(out=ypad_s[j * 128:(j + 1) * 128, :], in_=yi[:]), reads=[yk], writes=['ypad_s'])
                yr = [S("yr%d" % i, [128, D]) for i in range(4)]
                xq = [S("xq0", [128, D])] * 2
                ac = [S("ac%d" % i, [128, D]) for i in range(2)]
                fnb = S("fnb", [128, D])
                ssq = S("ssqH", [128, 16])
                rs = S("rsH", [128, 16])
                dma('sp', lambda e: e.dma_start(out=fnb[:], in_=fnw_d[0:1, :].partition_broadcast(128)), writes=['fnb'])
                for i in range(16):
                    aci, ack = ac[i % 2], 'ac%d' % (i % 2)
                    xqi, xqk = xq[0], 'xq0'
                    dma('sp', lambda e, xqi=xqi, i=i: e.dma_start(out=xqi[:], in_=xl2_s[i * 128:(i + 1) * 128, :]), reads=[('xl2_s', i)], writes=[xqk])
                    for r in range(4):
                        dma('pool', lambda e, r=r, i=i: e.indirect_dma_start(out=yr[r][:], out_offset=None, in_=ypad_s[:, :], in_offset=bass.IndirectOffsetOnAxis(ap=DESTi[:, r, i:i + 1], axis=0)),
                            reads=['DESTi', 'ypad_s'], writes=['yr%d' % r])
                        if r == 0:
                            op('dve', lambda e, aci=aci, i=i: e.tensor_scalar(out=aci[:], in0=yr[0][:], scalar1=GATE[:, 0, i:i + 1], scalar2=None, op0=ALU.mult), reads=['yr0', 'GATE'], writes=[ack])
                        else:
                            op('dve', lambda e, aci=aci, i=i, r=r: e.scalar_tensor_tensor(out=aci[:], in0=yr[r][:], scalar=GATE[:, r, i:i + 1], in1=aci[:], op0=ALU.mult, op1=ALU.add), reads=['yr%d' % r, 'GATE', ack], writes=[ack])
                    op('dve', lambda e, aci=aci: e.tensor_tensor(out=aci[:], in0=aci[:], in1=MODL[:, 5 * D:6 * D], op=ALU.mult), reads=[ack, 'MODL'], writes=[ack])
                    op('dve', lambda e, aci=aci, xqi=xqi: e.tensor_tensor(out=aci[:], in0=aci[:], in1=xqi[:], op=ALU.add), reads=[ack, xqk], writes=[ack])
                    op('act', lambda e, aci=aci, i=i: e.activation(out=yr[0][:], in_=aci[:], func=AF.Square, accum_out=ssq[:, i:i + 1]), reads=[ack], writes=['yr0', 'ssqH%d' % i])
                    rstd_from_ssq(ssq[:, i:i + 1], rs[:, i:i + 1], 1.0 / D, ['ssqH%d' % i], ['rsH%d' % i])
                    op('dve', lambda e, aci=aci, i=i: e.scalar_tensor_tensor(out=aci[:], in0=aci[:], scalar=rs[:, i:i + 1], in1=fnb[:], op0=ALU.mult, op1=ALU.mult), reads=[ack, 'rsH%d' % i, 'fnb'], writes=[ack])
                    dma('sp', lambda e, aci=aci, i=i: e.dma_start(out=out_d[i * 128:(i + 1) * 128, :], in_=aci[:]), reads=[ack], writes=[('out', i)])
                kb.barrier()
        if kb.stopped:
            kb.stopped = False
            kb.limit = None
            kb.barrier()
            return fin()
        print("instructions:", kb.nins)
    return nc


def host_consts():
    cst = np.zeros((128, C_N), np.float32)
    cst[:, C_ID:C_ID + 128] = np.eye(128)
    cst[:, C_ONE:C_ONE + 128] = 1.0
    i = np.arange(64)
    cst[:64, C_U:C_U + 64] = (i[:, None] <= i[None, :])
    cst[:64, C_L:C_L + 64] = (i[:, None] >= i[None, :])
    cst[:64, C_IF:C_IF + 64] = (i[:, None] >= i[None, :])
    cst[:64, C_SF:C_SF + 64] = (i[:, None] > i[None, :])
    cst[:64, C_IB:C_IB + 64] = (i[:, None] <= i[None, :])
    cst[:64, C_SB:C_SB + 64] = (i[:, None] < i[None, :])
    e = np.arange(32)
    cst[:32, C_TRI:C_TRI + 32] = (e[:, None] < e[None, :])
    t = np.arange(128)
    cst[:, C_UT:C_UT + 128] = (t[:, None] < t[None, :])
    cst[:, C_BS:C_BS + NBLK] = (np.arange(NBLK) * 128)[None, :]
    cst[:, C_KR:C_KR + 8] = np.arange(8)[None, :] * 128 + t[:, None]
    cst[:, C_TOK:C_TOK + 16] = np.arange(16)[None, :] * 128 + t[:, None]
    return cst


def host_rope():
    tt = np.arange(T)
    row = (tt // 64).astype(np.float32)
    col = (tt % 64).astype(np.float32)
    freqs = (np.float32(10000.0) ** (-np.arange(16, dtype=np.float32) / np.float32(16))).astype(np.float32)
    ang = np.concatenate([row[:, None] * freqs, col[:, None] * freqs], axis=-1).astype(np.float32)
    cos, sin = np.cos(ang).astype(np.float32), np.sin(ang).astype(np.float32)
    r = np.stack([cos.reshape(32, 64, 32).transpose(1, 0, 2), sin.reshape(32, 64, 32).transpose(1, 0, 2)], axis=1)
    return np.ascontiguousarray(r.reshape(64, 2 * 32 * 32))


def host_natbl(rpb):
    kc = np.arange(64)[:, None]
    qc = np.arange(64)[None, :]
    dc = np.clip(kc - qc + 15, 0, 30)
    cs = np.clip(np.arange(64) - 8, 0, 48)
    cmask = (kc >= cs[None, :]) & (kc < cs[None, :] + 16)
    base = np.full((8, 64, 17, 64), NEG, np.float32)
    for jp in range(0, 15):
        g = rpb[:, 14 - jp][:, dc]
        base[:, :, jp + 1, :] = np.where(cmask[None], g, np.float32(NEG))
    base_int = base.copy()
    for jp in range(-1, 16):
        if not (4 <= jp <= 11):
            base_int[:, :, jp + 1, :] = NEG
    out = np.empty((8, 2, 128, 16, 64), np.float32)
    for ti, b in enumerate((base, base_int)):
        for a in range(2):
            out[:, ti, a * 64:(a + 1) * 64, :, :] = b[:, :, (1 - a):(17 - a), :]
    return np.ascontiguousarray(out.reshape(8, 2, 128, 1024))


def make_in_maps(inputs, cores):
    f = lambda a: np.ascontiguousarray(np.asarray(a, dtype=np.float32))
    shared = {
        "w_mod": f(inputs["w_mod"][0]), "b_mod": f(inputs["b_mod"][0]).reshape(1, -1), "norm1_w": f(inputs["norm1_w"][0]).reshape(1, -1),
        "w_in": f(inputs["w_in"][0]), "na_tbl": host_natbl(np.asarray(inputs["na_rpb"][0], np.float32)),
        "convw": f(np.asarray(inputs["dn_conv_w"][0]).reshape(5, 12, 128).transpose(2, 1, 0).reshape(128, 60)),
        "alog": f(inputs["dn_a_log"][0]).reshape(1, 16), "dtb": f(inputs["dn_dt_bias"][0]).reshape(1, 16), "dnw": f(inputs["dn_norm_w"][0]).reshape(1, 64),
        "w_br_a": f(inputs["w_br_a"][0]), "w_br_b": f(inputs["w_br_b"][0]), "w_out": f(inputs["w_out"][0]), "norm2_w": f(inputs["norm2_w"][0]).reshape(1, -1),
        "w_router": f(inputs["w_router"][0]), "b_router": f(inputs["b_router"][0]).reshape(1, 32),
        "w1": f(inputs["w1"][0]).reshape(32 * D, 2048), "b1t": f(np.asarray(inputs["b1"][0]).reshape(32, 16, 128).transpose(0, 2, 1).reshape(32 * 128, 16)),
        "w2": f(inputs["w2"][0]).reshape(32 * D, D), "b2": f(inputs["b2"][0]), "fnw": f(inputs["final_norm_w"]).reshape(1, -1),
        "rope": host_rope(), "cst": host_consts(),
    }
    maps = []
    for b in cores:
        m = dict(shared)
        m["x"] = f(inputs["x"][b])
        m["ctx"] = f(inputs["ctx"][b])
        m["c2"] = f(np.stack([np.asarray(inputs["c"][b]), np.asarray(inputs["c_ctx"])], axis=0))
        maps.append(m)
    return maps


def kernel(**inputs):
    nc = build()
    maps = make_in_maps(inputs, list(range(8)))
    res = run_bass_kernel_spmd(nc, maps, core_ids=list(range(8)))
    return np.stack([np.asarray(r["out"], dtype=np.float32) for r in res.results], axis=0)
```

```python
import numpy as np
from contextlib import ExitStack
import concourse.bass as bass
import concourse.mybir as mybir
from concourse.bass_utils import run_bass_kernel_spmd

F32 = mybir.dt.float32
BF16 = mybir.dt.bfloat16
I32 = mybir.dt.int32
ALU = mybir.AluOpType
AF = mybir.ActivationFunctionType
AX = mybir.AxisListType

D = 1024
T = 2048
LC = 256
NT = T + LC
NBLK = 96
NEG = -1e30
EPS = 1e-6

C_ID, C_ONE, C_U, C_L, C_IF, C_SF, C_IB, C_SB, C_TRI, C_UT, C_BS, C_KR, C_TOK, C_N = 0, 128, 256, 320, 384, 448, 512, 576, 640, 672, 800, 896, 904, 920


class _StopScan(Exception):
    pass


class KB:
    NDMA = 16

    def __init__(self, nc, es):
        self.nc = nc
        self.eng = {'pe': nc.tensor, 'act': nc.scalar, 'dve': nc.vector, 'pool': nc.gpsimd, 'sp': nc.sync}
        self.sem = {}
        self.cnt = {}
        for e in ('pe', 'act', 'dve', 'pool'):
            self.sem[e] = es.enter_context(nc.semaphore('s_' + e))
            self.cnt[e] = 0
        self.dsem = {}
        self.dcnt = {}
        self.dnext = {}
        for q in ('sp', 'pool'):
            self.dsem[q] = [es.enter_context(nc.semaphore('d_%s%d' % (q, i))) for i in range(self.NDMA)]
            self.dcnt[q] = [0] * self.NDMA
            self.dnext[q] = 0
        self.seen = {e: {} for e in self.eng}
        self.lastw = {}
        self.readers = {}
        self.nins = 0
        self.limit = None
        self.stopped = False

    def _wait(self, e, ev):
        sem, val, src = ev
        if src == e and e == 'pe':
            return
        k = id(sem)
        if self.seen[e].get(k, 0) >= val:
            return
        self.eng[e].wait_ge(sem, val)
        self.seen[e][k] = val

    def _deps(self, e, reads, writes):
        for r in reads:
            ev = self.lastw.get(r)
            if ev is not None:
                self._wait(e, ev)
        for w in writes:
            ev = self.lastw.get(w)
            if ev is not None:
                self._wait(e, ev)
            for ev in self.readers.get(w, {}).values():
                self._wait(e, ev)

    def _record(self, ev, reads, writes):
        for r in reads:
            self.readers.setdefault(r, {})[id(ev[0])] = ev
        for w in writes:
            self.lastw[w] = ev
            self.readers[w] = {}

    def op(self, e, fn, reads=(), writes=()):
        if self.stopped:
            return None
        pr = [r for r in reads if isinstance(r, str) and r[:2] in ('pf', 'pb')]
        if pr:
            writes = list(writes) + pr
        self._deps(e, reads, writes)
        ins = fn(self.eng[e])
        self.cnt[e] += 1
        ins.then_inc(self.sem[e], 1)
        self._record((self.sem[e], self.cnt[e], e), reads, writes)
        self.nins += 1
        if self.limit is not None and self.nins >= self.limit:
            self.stopped = True
        return ins

    def dma(self, q, fn, reads=(), writes=()):
        if self.stopped:
            return None
        i = self.dnext[q]
        self.dnext[q] = (i + 1) % self.NDMA
        sem = self.dsem[q][i]
        if self.dcnt[q][i] > 0:
            self._wait(q, (sem, self.dcnt[q][i], 'dma'))
        self._deps(q, reads, writes)
        ins = fn(self.eng[q])
        self.dcnt[q][i] += 16
        ins.then_inc(sem, 16)
        self._record((sem, self.dcnt[q][i], 'dma'), reads, writes)
        self.nins += 1
        return ins

    def barrier(self):
        if self.stopped:
            return
        for e in self.eng:
            for e2 in ('pe', 'act', 'dve', 'pool'):
                if e2 != e and self.cnt[e2] > 0:
                    self._wait(e, (self.sem[e2], self.cnt[e2], e2))
            for q in self.dsem:
                for i in range(self.NDMA):
                    if self.dcnt[q][i] > 0:
                        self._wait(e, (self.dsem[q][i], self.dcnt[q][i], 'dma'))

    def wait_all(self, e, toks):
        if self.stopped:
            return
        for t in toks:
            if t in self.lastw:
                self._wait(e, self.lastw[t])


def bc(ap, shape):
    return ap.to_broadcast(shape)


def build(dbg=False, stop=None, sub=None):
    nc = bass.Bass("TRN2", target_bir_lowering=False)
    limit = int(sub[1:]) if (sub is not None and sub.startswith('n')) else None
    inp = lambda name, shape, dt=F32: nc.dram_tensor(name, shape, dt, kind="ExternalInput").ap()
    x_d = inp("x", [T, D])
    ctx_d = inp("ctx", [LC, D])
    c2_d = inp("c2", [2, D])
    wmod_d = inp("w_mod", [D, 6 * D])
    bmod_d = inp("b_mod", [1, 6 * D])
    n1_d = inp("norm1_w", [1, D])
    win_d = inp("w_in", [D, 5664])
    natbl_d = inp("na_tbl", [8, 2, 128, 1024])
    convw_d = inp("convw", [128, 12 * 5])
    alog_d = inp("alog", [1, 16])
    dtb_d = inp("dtb", [1, 16])
    dnw_d = inp("dnw", [1, 64])
    wbra_d = inp("w_br_a", [512, D])
    wbrb_d = inp("w_br_b", [512, D])
    wout_d = inp("w_out", [D, D])
    n2_d = inp("norm2_w", [1, D])
    wr_d = inp("w_router", [D, 32])
    br_d = inp("b_router", [1, 32])
    w1_d = inp("w1", [32 * D, 2048])
    b1t_d = inp("b1t", [32 * 128, 16])
    w2_d = inp("w2", [32 * D, D])
    b2_d = inp("b2", [32, D])
    fnw_d = inp("fnw", [1, D])
    rope_d = inp("rope", [64, 2 * 32 * 32])
    cst_d = inp("cst", [128, C_N])
    out_d = nc.dram_tensor("out", [T, D], F32, kind="ExternalOutput").ap()
    scr = lambda name, shape, dt=F32: nc.dram_tensor(name, shape, dt, kind="Internal").ap()
    qkv_s = scr("qkv_s", [36, 64, 1536])
    o_s = scr("o_s", [2, 32, 64, 512])
    xl2_s = scr("xl2_s", [T, D])
    h2_s = scr("h2_s", [T, D])
    slot_s = scr("slot_s", [NBLK * 128, 1], I32)
    ypad_s = scr("ypad_s", [NBLK * 128, D])
    dbg_d = {}
    if dbg:
        for nm, shp in (("d_hT", [128, 8 * NT]), ("d_ona", [128, 4 * T]), ("d_odn", [128, 4 * T]), ("d_lg", [128, 16 * 32])):
            dbg_d[nm] = nc.dram_tensor(nm, shp, F32, kind="ExternalOutput").ap()

    winv = win_d.rearrange("(k p) n -> p k n", p=128)

    with ExitStack() as es:
        kb = KB(nc, es)
        kb.limit = limit
        op, dma = kb.op, kb.dma

        def fin():
            with ExitStack() as sf:
                z = sf.enter_context(nc.sbuf_tensor("t_zout", [128, D], F32))
                op('dve', lambda e: e.memset(z[:], 0.0), writes=['zout'])
                for i in range(16):
                    dma('sp', lambda e, i=i: e.dma_start(out=out_d[i * 128:(i + 1) * 128, :], in_=z[:]), reads=['zout'], writes=[('out', i)])
                kb.barrier()
            print("instructions (stopped at %s):" % stop, kb.nins)
            return nc
        if True:
            G = lambda name, shape, dt=F32: es.enter_context(nc.sbuf_tensor("t_" + name, shape, dt))
            cst = G("cst", [128, C_N])
            identb = G("identb", [128, 128], BF16)
            MODL = G("MODL", [128, 6 * D])
            epsb = G("epsb", [128, 1])
            LG = G("LG", [128, 16, 32])
            mid = ExitStack()
            es.callback(mid.close)
            Gm = lambda name, shape, dt=F32: mid.enter_context(nc.sbuf_tensor("t_" + name, shape, dt))
            hT = Gm("hT", [128, 8, NT], BF16)
            onaT = Gm("onaT", [128, 4, T], BF16)
            odnT = Gm("odnT", [128, 4, T], BF16)

            dma('sp', lambda e: e.dma_start(out=cst[:], in_=cst_d[:, :]), writes=['cst'])
            op('dve', lambda e: e.tensor_copy(out=identb[:], in_=cst[:, C_ID:C_ID + 128]), reads=['cst'], writes=['identb'])
            op('dve', lambda e: e.memset(epsb[:], EPS), writes=['epsb'])
            ident = cst[:, C_ID:C_ID + 128]
            ones = cst[:, C_ONE:C_ONE + 128]

            def rstd_from_ssq(ssq_ap, rs_ap, scale, toks_r, toks_w):
                op('act', lambda e: e.activation(out=rs_ap, in_=ssq_ap, func=AF.Sqrt, scale=scale, bias=epsb[0:rs_ap.partition_size(), :]), reads=list(toks_r) + ['epsb'], writes=toks_w)
                op('dve', lambda e: e.reciprocal(out=rs_ap, in_=rs_ap), reads=toks_w, writes=toks_w)

            with ExitStack() as s1:
                S = lambda name, shape, dt=F32: s1.enter_context(nc.sbuf_tensor("t_" + name, shape, dt))
                PF = [s1.enter_context(nc.psum_tensor("pfA%d" % i, [128, 512], F32)) for i in range(2)]
                c2t = S("c2t", [128, 2, 8])
                MODC = S("MODC", [128, 2 * D])
                Lm = S("Lm", [128, 2, 8, 128], BF16)
                onesb = S("onesb", [128, 128], BF16)
                wm = [S("wm%d" % i, [128, 8, 512], BF16) for i in range(2)]
                nbc = S("nbc", [128, 2 * D])
                dma('sp', lambda e: e.dma_start(out=c2t[:], in_=c2_d.rearrange("t (p k) -> p t k", k=8)), writes=['c2t'])
                dma('sp', lambda e: e.dma_start(out=MODL[:], in_=bmod_d[0:1, :].partition_broadcast(128)), writes=['MODL'])
                dma('sp', lambda e: e.dma_start(out=MODC[:], in_=bmod_d[0:1, 0:2 * D].partition_broadcast(128)), writes=['MODC'])
                dma('sp', lambda e: e.dma_start(out=nbc[:, 0:D], in_=n1_d[0:1, :].partition_broadcast(128)), writes=['nbc0'])
                dma('sp', lambda e: e.dma_start(out=nbc[:, D:2 * D], in_=n2_d[0:1, :].partition_broadcast(128)), writes=['nbc1'])
                op('act', lambda e: e.activation(out=c2t[:], in_=c2t[:], func=AF.Silu), reads=['c2t'], writes=['c2t'])
                op('dve', lambda e: e.memset(onesb[:], 1.0), writes=['onesb'])
                for t in range(2):
                    for k in range(8):
                        op('dve', lambda e, t=t, k=k: e.tensor_scalar(out=Lm[:, t, k, :], in0=onesb[:], scalar1=c2t[:, t, k:k + 1], scalar2=None, op0=ALU.mult),
                           reads=['c2t', 'onesb'], writes=['Lm'])
                wmv = wmod_d.rearrange("(p k) n -> p k n", k=8)
                for j in range(12):
                    w = wm[j % 2]
                    wt = 'wm%d' % (j % 2)
                    dma('pool', lambda e, j=j, w=w: e.dma_start(out=w[:], in_=wmv[:, :, j * 512:(j + 1) * 512]), writes=[wt])
                    for t in range(2 if j < 4 else 1):
                        ps = PF[t]
                        for k in range(8):
                            op('pe', lambda e, t=t, k=k, w=w, ps=ps: e.matmul(out=ps[:], lhsT=Lm[:, t, k, :], rhs=w[:, k, :], start=(k == 0), stop=(k == 7)),
                               reads=['Lm', wt], writes=['pfA%d' % t])
                        M = MODL if t == 0 else MODC
                        op('dve', lambda e, M=M, j=j, ps=ps: e.tensor_tensor(out=M[:, j * 512:(j + 1) * 512], in0=M[:, j * 512:(j + 1) * 512], in1=ps[:], op=ALU.add),
                           reads=['pfA%d' % t, 'MODL', 'MODC'], writes=['MODL' if t == 0 else 'MODC'])
                op('dve', lambda e: e.scalar_tensor_tensor(out=MODL[:, D:2 * D], in0=MODL[:, D:2 * D], scalar=1.0, in1=nbc[:, 0:D], op0=ALU.add, op1=ALU.mult), reads=['MODL', 'nbc0'], writes=['MODL'])
                op('dve', lambda e: e.scalar_tensor_tensor(out=MODC[:, D:2 * D], in0=MODC[:, D:2 * D], scalar=1.0, in1=nbc[:, 0:D], op0=ALU.add, op1=ALU.mult), reads=['MODC', 'nbc0'], writes=['MODC'])
                op('dve', lambda e: e.scalar_tensor_tensor(out=MODL[:, 4 * D:5 * D], in0=MODL[:, 4 * D:5 * D], scalar=1.0, in1=nbc[:, D:2 * D], op0=ALU.add, op1=ALU.mult), reads=['MODL', 'nbc1'], writes=['MODL'])

                PB = [s1.enter_context(nc.psum_tensor("pbB%d" % i, [128, 8, 128], BF16)) for i in range(2)]
                xt = [S("xt%d" % i, [128, D]) for i in range(2)]
                junk = S("junk", [128, D])
                hb = [S("hb%d" % i, [128, D], BF16) for i in range(2)]
                ssq = S("ssq", [128, 18])
                rs = S("rs", [128, 18])
                for i in range(18):
                    xb = xt[i % 2]
                    xtk = 'xt%d' % (i % 2)
                    hbb = hb[i % 2]
                    hbk = 'hb%d' % (i % 2)
                    src = x_d[i * 128:(i + 1) * 128, :] if i < 16 else ctx_d[(i - 16) * 128:(i - 15) * 128, :]
                    M = MODL if i < 16 else MODC
                    Mk = 'MODL' if i < 16 else 'MODC'
                    dma('sp', lambda e, xb=xb, src=src: e.dma_start(out=xb[:], in_=src), writes=[xtk])
                    op('act', lambda e, xb=xb, i=i: e.activation(out=junk[:], in_=xb[:], func=AF.Square, accum_out=ssq[:, i:i + 1]), reads=[xtk], writes=['junk', 'ssq%d' % i])
                    rstd_from_ssq(ssq[:, i:i + 1], rs[:, i:i + 1], 1.0 / D, ['ssq%d' % i], ['rs%d' % i])
                    op('dve', lambda e, xb=xb, i=i, M=M: e.scalar_tensor_tensor(out=junk[:], in0=xb[:], scalar=rs[:, i:i + 1], in1=M[:, D:2 * D], op0=ALU.mult, op1=ALU.mult),
                       reads=[xtk, 'rs%d' % i, Mk], writes=['junk'])
                    op('dve', lambda e, hbb=hbb, M=M: e.tensor_tensor(out=hbb[:], in0=junk[:], in1=M[:, 0:D], op=ALU.add), reads=['junk', Mk], writes=[hbk])
                    pb = PB[i % 2]
                    pbk = 'pbB%d' % (i % 2)
                    for k in range(8):
                        op('pe', lambda e, pb=pb, hbb=hbb, k=k: e.transpose(out=pb[:, k, :], in_=hbb[:, k * 128:(k + 1) * 128], identity=identb[:]), reads=[hbk, 'identb'], writes=[pbk])
                    op('act', lambda e, pb=pb, i=i: e.copy(out=hT[:, :, i * 128:(i + 1) * 128], in_=pb[:]), reads=[pbk], writes=[('hT', i)])
                kb.barrier()
                if stop == 'B':
                    return fin()
            hT_all = [('hT', i) for i in range(18)]
            if dbg:
                with ExitStack() as s1:
                    tmp = s1.enter_context(nc.sbuf_tensor("dbgt", [128, 8 * NT], F32))
                    op('dve', lambda e: e.tensor_copy(out=tmp[:], in_=hT[:].rearrange("p k n -> p (k n)")), reads=hT_all, writes=['dbgt'])
                    dma('sp', lambda e: e.dma_start(out=dbg_d["d_hT"][:, :], in_=tmp[:]), reads=['dbgt'], writes=['d_hT'])
                    kb.barrier()

            with ExitStack() as s1:
                S = lambda name, shape, dt=F32: s1.enter_context(nc.sbuf_tensor("t_" + name, shape, dt))
                PF = [s1.enter_context(nc.psum_tensor("pfC%d" % i, [128, 512], F32)) for i in range(8)]
                wna = S("wna", [128, 8, 1536], BF16)
                qT = S("qT", [128, 4, T], BF16)
                kT = S("kT", [128, 4, NT], BF16)
                V = S("V", [128, 18, 512], BF16)
                onesb = S("onesbC", [128, 64], BF16)
                tb2 = [S("tb2_%d" % i, [128, 2, 1024], BF16) for i in range(2)]
                PT = [S("PT%d" % i, [128, 256], BF16) for i in range(4)]
                rcp = [S("rcp%d" % i, [128, 256]) for i in range(2)]
                op('dve', lambda e: e.memset(onesb[:], 1.0), writes=['onesbC'])
                for j in range(3):
                    dma('pool', lambda e, j=j: e.dma_start(out=wna[:, :, j * 512:(j + 1) * 512], in_=winv[:, :, j * 512:(j + 1) * 512]), writes=[('wna', j)])
                pfi = [0]

                def nextpf():
                    i = pfi[0] % 8
                    pfi[0] += 1
                    return PF[i], 'pfC%d' % i
                for which in range(2):
                    for p in range(4):
                        ntb = 4 if which == 0 else 5
                        for tb in range(ntb):
                            n0 = tb * 512
                            nn = 512 if tb < 4 else 256
                            ps, pk = nextpf()
                            for k in range(8):
                                op('pe', lambda e, ps=ps, k=k, which=which, p=p, n0=n0, nn=nn: e.matmul(out=ps[:, 0:nn], lhsT=wna[:, k, which * 512 + p * 128: which * 512 + (p + 1) * 128],
                                                                                                     rhs=hT[:, k, n0:n0 + nn], start=(k == 0), stop=(k == 7)),
                                   reads=[('wna', which)] + [('hT', t) for t in range(n0 // 128, (n0 + nn) // 128)], writes=[pk])
                            if which == 0:
                                op('act', lambda e, ps=ps, p=p, n0=n0: e.mul(out=qT[:, p, n0:n0 + 512], in_=ps[:], mul=0.125), reads=[pk], writes=[('qT', p, tb)])
                            else:
                                op('dve', lambda e, ps=ps, p=p, n0=n0, nn=nn: e.tensor_copy(out=kT[:, p, n0:n0 + nn], in_=ps[:, 0:nn]), reads=[pk], writes=[('kT', p, tb)])
                for i in range(18):
                    ps, pk = nextpf()
                    for k in range(8):
                        op('pe', lambda e, ps=ps, k=k, i=i: e.matmul(out=ps[:], lhsT=hT[:, k, i * 128:(i + 1) * 128], rhs=wna[:, k, 1024:1536], start=(k == 0), stop=(k == 7)),
                           reads=[('wna', 2), ('hT', i)], writes=[pk])
                    op('act', lambda e, ps=ps, i=i: e.copy(out=V[:, i, :], in_=ps[:]), reads=[pk], writes=[('V', i)])
                PST = PF[0:4]
                PPV = PF[4:6]
                PDN = PF[6:8]
                cnt = 0
                for h in range(8):
                    p, half = h // 2, h % 2
                    hs = slice(half * 64, half * 64 + 64)
                    tbt = tb2[h % 2]
                    tbk = 'tb2_%d' % (h % 2)
                    dma('pool', lambda e, tbt=tbt, h=h: e.dma_start(out=tbt[:], in_=natbl_d[h].rearrange("t p n -> p t n")), writes=[tbk])
                    for qb in range(8):
                        if qb == 0:
                            tiles, tsel = list(range(0, 4)), 0
                        elif qb == 7:
                            tiles, tsel = list(range(12, 16)), 0
                        else:
                            tiles, tsel = list(range(2 * qb - 2, 2 * qb + 4)), 1
                        keyt = [(m, 4 * qb - 2 * m + 7) for m in tiles] + [(16, None), (17, None)]
                        ppv, pdn = PPV[qb % 2], PDN[qb % 2]
                        ppk, pdk = 'pfC%d' % (4 + qb % 2), 'pfC%d' % (6 + qb % 2)
                        q0 = qb * 256
                        for ti, (m, j0) in enumerate(keyt):
                            pst, pstk = PST[cnt % 4], 'pfC%d' % (cnt % 4)
                            ptt, ptk = PT[cnt % 4], 'PT%d' % (cnt % 4)
                            cnt += 1
                            op('pe', lambda e, pst=pst, m=m, j0=j0: e.matmul(out=pst[:, 0:256], lhsT=kT[hs, p, m * 128:(m + 1) * 128], rhs=qT[hs, p, q0:q0 + 256], start=True, stop=(j0 is None)),
                               reads=[('kT', p, min(m // 4, 4)), ('qT', p, qb // 2)], writes=[pstk])
                            if j0 is not None:
                                op('pe', lambda e, pst=pst, j0=j0: e.matmul(out=pst[:, 0:256], lhsT=identb[:], rhs=tbt[:, tsel, j0 * 64:(j0 + 4) * 64], start=False, stop=True),
                                   reads=[tbk, 'identb'], writes=[pstk])
                            op('act', lambda e, pst=pst, ptt=ptt: e.activation(out=ptt[:], in_=pst[:, 0:256], func=AF.Exp), reads=[pstk], writes=[ptk])
                            first, last = ti == 0, ti == len(keyt) - 1
                            op('pe', lambda e, ptt=ptt, m=m, first=first, last=last: e.matmul(out=ppv[hs, 0:256], lhsT=V[:, m, h * 64:(h + 1) * 64], rhs=ptt[:], start=first, stop=last),
                               reads=[ptk, ('V', m)], writes=[ppk])
                            op('pe', lambda e, ptt=ptt, first=first, last=last: e.matmul(out=pdn[hs, 0:256], lhsT=onesb[:], rhs=ptt[:], start=first, stop=last),
                               reads=[ptk, 'onesbC'], writes=[pdk])
                        rc = rcp[qb % 2]
                        rck = 'rcp%d' % (qb % 2)
                        op('dve', lambda e, rc=rc, pdn=pdn: e.reciprocal(out=rc[hs, :], in_=pdn[hs, 0:256]), reads=[pdk], writes=[rck])
                        op('dve', lambda e, rc=rc, ppv=ppv: e.tensor_tensor(out=onaT[hs, p, q0:q0 + 256], in0=ppv[hs, 0:256], in1=rc[hs, :], op=ALU.mult), reads=[ppk, rck], writes=[('onaT', p, qb)])
                kb.barrier()
                if stop == 'C':
                    return fin()
            onaT_all = [('onaT', p, qb) for p in range(4) for qb in range(8)]
            if dbg:
                with ExitStack() as s1:
                    tmp = s1.enter_context(nc.sbuf_tensor("dbgt2", [128, 4 * T], F32))
                    op('dve', lambda e: e.tensor_copy(out=tmp[:], in_=onaT[:].rearrange("p k n -> p (k n)")), reads=onaT_all, writes=['dbgt2'])
                    dma('sp', lambda e: e.dma_start(out=dbg_d["d_ona"][:, :], in_=tmp[:]), reads=['dbgt2'], writes=['d_ona'])
                    kb.barrier()

            id64 = cst[0:64, C_ID:C_ID + 64]
            on64 = cst[0:64, C_ONE:C_ONE + 64]
            b3 = lambda ap8: ap8.unsqueeze(2).to_broadcast([64, 8, 64])
            m3 = lambda ap64: ap64.unsqueeze(1).to_broadcast([64, 8, 64])
            with ExitStack() as s1:
                S = lambda name, shape, dt=F32: s1.enter_context(nc.sbuf_tensor("t_" + name, shape, dt))
                PF = [s1.enter_context(nc.psum_tensor("pfD%d" % i, [128, 512], F32)) for i in range(8)]
                pfi = [0]

                def nextpf():
                    i = pfi[0] % 8
                    pfi[0] += 1
                    return PF[i], 'pfD%d' % i
                wdn = S("wdn", [128, 8, 1536], BF16)
                wba = S("wba", [128, 8, 32], BF16)
                convw = S("convw", [128, 60])
                ropet = S("ropet", [64, 2, 32, 32])
                zl = [S("zl%d" % i, [128, 2052]) for i in range(2)]
                zx = [S("zx%d" % i, [128, 260]) for i in range(2)]
                acc = S("acc", [128, NT])
                sl = S("sl", [128, NT])
                sqt = S("sqt", [64, 512])
                ssq8 = S("ssq8", [64, 8])
                r8 = S("r8", [64, 8])
                st = [S("st%d" % i, [64, 4, 128]) for i in range(2)]
                st2 = [S("st2%d" % i, [64, 4, 128]) for i in range(2)]
                ra = S("ra", [64, 4, 2, 32])
                rb = S("rb", [64, 4, 2, 32])
                BA = S("BA", [64, 36, 32])
                BETA = S("BETA", [64, 36, 16])
                GG = S("GG", [64, 36, 16])
                ta = S("ta", [64, 36, 16])
                tb_ = S("tb_", [64, 36, 16])
                tc_ = S("tc_", [64, 36, 16])
                ab = S("ab", [64, 32])
                one1 = S("one1", [64, 1])
                for j in range(3):
                    dma('pool', lambda e, j=j: e.dma_start(out=wdn[:, :, j * 512:(j + 1) * 512], in_=winv[:, :, 1536 + j * 512:1536 + (j + 1) * 512]), writes=[('wdn', j)])
                dma('pool', lambda e: e.dma_start(out=wba[:], in_=winv[:, :, 3584:3616]), writes=['wba'])
                dma('sp', lambda e: e.dma_start(out=convw[:], in_=convw_d[:, :]), writes=['convw'])
                dma('sp', lambda e: e.dma_start(out=ropet[:], in_=rope_d.rearrange("p (a c f) -> p a c f", a=2, c=32)), writes=['ropet'])
                dma('sp', lambda e: e.dma_start(out=ab[:, 0:16], in_=alog_d[0:1, :].partition_broadcast(64)), writes=['ab'])
                dma('sp', lambda e: e.dma_start(out=ab[:, 16:32], in_=dtb_d[0:1, :].partition_broadcast(64)), reads=[], writes=['ab2'])
                op('dve', lambda e: e.memset(one1[:], 1.0), writes=['one1'])
                for i in range(2):
                    op('dve', lambda e, i=i: e.memset(zl[i][:], 0.0), writes=['zl%d' % i])
                    op('dve', lambda e, i=i: e.memset(zx[i][:], 0.0), writes=['zx%d' % i])
                for g3 in range(3):
                    ps, pk = nextpf()
                    for c in range(12):
                        n = g3 * 12 + c
                        for k in range(8):
                            op('pe', lambda e, ps=ps, c=c, n=n, k=k: e.matmul(out=ps[0:64, c * 32:(c + 1) * 32], lhsT=hT[:, k, n * 64:(n + 1) * 64], rhs=wba[:, k, :], start=(k == 0), stop=(k == 7)),
                               reads=['wba', ('hT', n // 2)], writes=[pk])
                    op('act', lambda e, ps=ps, g3=g3: e.copy(out=BA[:, g3 * 12:(g3 + 1) * 12, :], in_=ps[0:64, 0:384].rearrange("p (c f) -> p c f", f=32)), reads=[pk], writes=['BA'])
                op('act', lambda e: e.activation(out=BETA[:], in_=BA[:, :, 0:16], func=AF.Sigmoid), reads=['BA'], writes=['BETA'])
                op('dve', lambda e: e.tensor_tensor(out=ta[:], in0=BA[:, :, 16:32], in1=ab[:, 16:32].unsqueeze(1).to_broadcast([64, 36, 16]), op=ALU.add), reads=['BA', 'ab2'], writes=['ta'])
                op('dve', lambda e: e.tensor_scalar(out=tb_[:], in0=ta[:], scalar1=-1.0, scalar2=None, op0=ALU.mult), reads=['ta'], writes=['tb_'])
                op('dve', lambda e: e.tensor_tensor(out=tb_[:], in0=tb_[:], in1=ta[:], op=ALU.max), reads=['ta', 'tb_'], writes=['tb_'])
                op('act', lambda e: e.activation(out=tb_[:], in_=tb_[:], func=AF.Exp, scale=-1.0), reads=['tb_'], writes=['tb_'])
                op('act', lambda e: e.activation(out=tb_[:], in_=tb_[:], func=AF.Ln, bias=one1[:]), reads=['tb_', 'one1'], writes=['tb_'])
                op('dve', lambda e: e.tensor_scalar_max(out=tc_[:], in0=ta[:], scalar1=0.0), reads=['ta'], writes=['tc_'])
                op('dve', lambda e: e.tensor_tensor(out=tc_[:], in0=tc_[:], in1=tb_[:], op=ALU.add), reads=['tc_', 'tb_'], writes=['tc_'])
                op('act', lambda e: e.activation(out=ab[:, 0:16], in_=ab[:, 0:16], func=AF.Exp), reads=['ab'], writes=['ab'])
                op('dve', lambda e: e.scalar_tensor_tensor(out=GG[:], in0=tc_[:], scalar=-1.0, in1=ab[:, 0:16].unsqueeze(1).to_broadcast([64, 36, 16]), op0=ALU.mult, op1=ALU.mult),
                   reads=['tc_', 'ab'], writes=['GG'])
                for cc in range(12):
                    z_l, z_x = zl[cc % 2], zx[cc % 2]
                    zlk, zxk = 'zl%d' % (cc % 2), 'zx%d' % (cc % 2)
                    for tb in range(5):
                        n0 = tb * 512
                        nn = 512 if tb < 4 else 256
                        ps, pk = nextpf()
                        for k in range(8):
                            op('pe', lambda e, ps=ps, k=k, n0=n0, nn=nn: e.matmul(out=ps[:, 0:nn], lhsT=wdn[:, k, cc * 128:(cc + 1) * 128], rhs=hT[:, k, n0:n0 + nn], start=(k == 0), stop=(k == 7)),
                               reads=[('wdn', cc // 4)] + [('hT', t) for t in range(n0 // 128, (n0 + nn) // 128)], writes=[pk])
                        if tb < 4:
                            op('act', lambda e, ps=ps, n0=n0: e.copy(out=z_l[:, 2 + n0:2 + n0 + 512], in_=ps[:]), reads=[pk], writes=[zlk])
                        else:
                            op('act', lambda e, ps=ps: e.copy(out=z_x[:, 2:258], in_=ps[:, 0:256]), reads=[pk], writes=[zxk])
                    for (zs, zk, a0, n) in ((z_l, zlk, 0, 2048), (z_x, zxk, 2048, 256)):
                        op('dve', lambda e, zs=zs, a0=a0, n=n: e.tensor_scalar(out=acc[:, a0:a0 + n], in0=zs[:, 0:n], scalar1=convw[:, cc * 5:cc * 5 + 1], scalar2=None, op0=ALU.mult),
                           reads=[zk, 'convw'], writes=['acc'])
                        for tap in range(1, 5):
                            op('dve', lambda e, zs=zs, a0=a0, n=n, tap=tap: e.scalar_tensor_tensor(out=acc[:, a0:a0 + n], in0=zs[:, tap:tap + n], scalar=convw[:, cc * 5 + tap:cc * 5 + tap + 1],
                                                                                               in1=acc[:, a0:a0 + n], op0=ALU.mult, op1=ALU.add), reads=[zk, 'convw', 'acc'], writes=['acc'])
                    op('act', lambda e: e.activation(out=sl[:], in_=acc[:], func=AF.Silu), reads=['acc'], writes=['sl'])
                    for g in range(9):
                        ps, pk = nextpf()
                        for c4 in range(4):
                            n = 4 * g + c4
                            op('pe', lambda e, ps=ps, c4=c4, n=n: e.transpose(out=ps[0:64, c4 * 128:(c4 + 1) * 128], in_=sl[:, n * 64:(n + 1) * 64], identity=ident), reads=['sl', 'cst'], writes=[pk])
                        sti, stk = st[g % 2], 'st%d' % (g % 2)
                        if cc < 8:
                            op('act', lambda e, ps=ps: e.activation(out=sqt[:], in_=ps[0:64, :], func=AF.Square), reads=[pk], writes=['sqt'])
                            op('dve', lambda e: e.tensor_reduce(out=ssq8[:], in_=sqt[:].rearrange("p (g f) -> p g f", f=64), axis=AX.X, op=ALU.add), reads=['sqt'], writes=['ssq8'])
                            rstd_from_ssq(ssq8[:], r8[:], 1.0, ['ssq8'], ['r8'])
                            op('dve', lambda e, ps=ps, sti=sti: e.tensor_tensor(out=sti[:].rearrange("p c (h f) -> p (c h) f", h=2), in0=ps[0:64, :].rearrange("p (g f) -> p g f", f=64),
                                                                            in1=b3(r8[:]), op=ALU.mult), reads=[pk, 'r8'], writes=[stk])
                            if g < 8:
                                s2i, s2k = st2[g % 2], 'st2%d' % (g % 2)
                                x5 = sti[:].rearrange("p c (h t f) -> p c h t f", h=2, t=2)
                                o5 = s2i[:].rearrange("p c (h t f) -> p c h t f", h=2, t=2)
                                cosb = ropet[:, 0, 4 * g:4 * g + 4, :].unsqueeze(2).to_broadcast([64, 4, 2, 32])
                                sinb = ropet[:, 1, 4 * g:4 * g + 4, :].unsqueeze(2).to_broadcast([64, 4, 2, 32])
                                op('dve', lambda e: e.tensor_tensor(out=ra[:], in0=x5[:, :, :, 0, :], in1=cosb, op=ALU.mult), reads=[stk, 'ropet'], writes=['ra'])
                                op('pool', lambda e: e.tensor_tensor(out=rb[:], in0=x5[:, :, :, 1, :], in1=sinb, op=ALU.mult), reads=[stk, 'ropet'], writes=['rb'])
                                op('dve', lambda e: e.tensor_tensor(out=o5[:, :, :, 0, :], in0=ra[:], in1=rb[:], op=ALU.subtract), reads=['ra', 'rb'], writes=[s2k])
                                op('dve', lambda e: e.tensor_tensor(out=ra[:], in0=x5[:, :, :, 0, :], in1=sinb, op=ALU.mult), reads=[stk, 'ropet'], writes=['ra'])
                                op('pool', lambda e: e.tensor_tensor(out=rb[:], in0=x5[:, :, :, 1, :], in1=cosb, op=ALU.mult), reads=[stk, 'ropet'], writes=['rb'])
                                op('dve', lambda e: e.tensor_tensor(out=o5[:, :, :, 1, :], in0=ra[:], in1=rb[:], op=ALU.add), reads=['ra', 'rb'], writes=[s2k])
                                sti, stk = s2i, s2k
                        else:
                            op('act', lambda e, ps=ps, sti=sti: e.copy(out=sti[:].rearrange("p c f -> p (c f)"), in_=ps[0:64, :]), reads=[pk], writes=[stk])
                        dma('sp', lambda e, sti=sti, g=g: e.dma_start(out=qkv_s[4 * g:4 * g + 4, :, cc * 128:(cc + 1) * 128].rearrange("c p f -> p c f"), in_=sti[:]), reads=[stk], writes=[('qkv_s', g)])
                kb.barrier()
                if stop == 'D1':
                    return fin()
            with ExitStack() as s1:
                S = lambda name, shape, dt=F32: s1.enter_context(nc.sbuf_tensor("t_" + name, shape, dt))
                PF = [s1.enter_context(nc.psum_tensor("pfE%d" % i, [128, 512], F32)) for i in range(8)]
                pfi = [0]

                def nextpf():
                    i = pfi[0] % 8
                    pfi[0] += 1
                    return PF[i], 'pfE%d' % i
                wba = S("wba2", [128, 8, 32], BF16)
                BA = S("BA2", [64, 36, 32])
                BETA = S("BETA2", [64, 36, 16])
                GG = S("GG2", [64, 36, 16])
                ta = S("ta2", [64, 36, 16])
                tb_ = S("tb2_", [64, 36, 16])
                tc_ = S("tc2_", [64, 36, 16])
                ab = S("ab2", [64, 32])
                one1 = S("one12", [64, 1])
                dma('pool', lambda e: e.dma_start(out=wba[:], in_=winv[:, :, 3584:3616]), writes=['wba'])
                dma('sp', lambda e: e.dma_start(out=ab[:, 0:16], in_=alog_d[0:1, :].partition_broadcast(64)), writes=['ab'])
                dma('sp', lambda e: e.dma_start(out=ab[:, 16:32], in_=dtb_d[0:1, :].partition_broadcast(64)), reads=[], writes=['ab2'])
                op('dve', lambda e: e.memset(one1[:], 1.0), writes=['one1'])
                for g3 in range(3):
                    ps, pk = nextpf()
                    for c in range(12):
                        n = g3 * 12 + c
                        for k in range(8):
                            op('pe', lambda e, ps=ps, c=c, n=n, k=k: e.matmul(out=ps[0:64, c * 32:(c + 1) * 32], lhsT=hT[:, k, n * 64:(n + 1) * 64], rhs=wba[:, k, :], start=(k == 0), stop=(k == 7)),
                               reads=['wba', ('hT', n // 2)], writes=[pk])
                    op('act', lambda e, ps=ps, g3=g3: e.copy(out=BA[:, g3 * 12:(g3 + 1) * 12, :], in_=ps[0:64, 0:384].rearrange("p (c f) -> p c f", f=32)), reads=[pk], writes=['BA'])
                op('act', lambda e: e.activation(out=BETA[:], in_=BA[:, :, 0:16], func=AF.Sigmoid), reads=['BA'], writes=['BETA'])
                op('dve', lambda e: e.tensor_tensor(out=ta[:], in0=BA[:, :, 16:32], in1=ab[:, 16:32].unsqueeze(1).to_broadcast([64, 36, 16]), op=ALU.add), reads=['BA', 'ab2'], writes=['ta'])
                op('dve', lambda e: e.tensor_scalar(out=tb_[:], in0=ta[:], scalar1=-1.0, scalar2=None, op0=ALU.mult), reads=['ta'], writes=['tb_'])
                op('dve', lambda e: e.tensor_tensor(out=tb_[:], in0=tb_[:], in1=ta[:], op=ALU.max), reads=['ta', 'tb_'], writes=['tb_'])
                op('act', lambda e: e.activation(out=tb_[:], in_=tb_[:], func=AF.Exp, scale=-1.0), reads=['tb_'], writes=['tb_'])
                op('act', lambda e: e.activation(out=tb_[:], in_=tb_[:], func=AF.Ln, bias=one1[:]), reads=['tb_', 'one1'], writes=['tb_'])
                op('dve', lambda e: e.tensor_scalar_max(out=tc_[:], in0=ta[:], scalar1=0.0), reads=['ta'], writes=['tc_'])
                op('dve', lambda e: e.tensor_tensor(out=tc_[:], in0=tc_[:], in1=tb_[:], op=ALU.add), reads=['tc_', 'tb_'], writes=['tc_'])
                op('act', lambda e: e.activation(out=ab[:, 0:16], in_=ab[:, 0:16], func=AF.Exp), reads=['ab'], writes=['ab'])
                op('dve', lambda e: e.scalar_tensor_tensor(out=GG[:], in0=tc_[:], scalar=-1.0, in1=ab[:, 0:16].unsqueeze(1).to_broadcast([64, 36, 16]), op0=ALU.mult, op1=ALU.mult),
                   reads=['tc_', 'ab'], writes=['GG'])

                ld = [S("ld%d" % i, [64, 1536]) for i in range(2)]
                names = ["gc", "gl", "et", "egl", "eg", "bg", "nb", "t8"]
                sm = {n_: S("sm_" + n_, [64, 8]) for n_ in names}
                bigs = ["Rt", "egrow", "Dm", "Dmi", "Dms", "qdT", "NegA", "QKm", "X0", "QKT", "Z", "Xa", "XTa", "Xb", "XTb", "vb", "kbg", "ktail", "u", "wT", "vnew", "Sst", "ost"]
                bigs = bigs + ["NegAb", "Zb", "Sb"]
                NB16 = ("X0", "Xa", "Xb", "XTa", "XTb", "NegAb", "Zb", "Sb", "qdT", "QKT", "vb", "kbg", "ktail", "wT", "vnew")
                bg_ = {n_: S("bg_" + n_, [64, 8, 64], BF16 if n_ in NB16 else F32) for n_ in bigs}
                qkT = S("qkT", [64, 16, 64], BF16)
                fl = lambda t_: t_[:].rearrange("p h f -> p (h f)")

                def headmm(lhs, rhs, reads, start=True, stop=True, ps=None, pk=None):
                    if ps is None:
                        ps, pk = nextpf()
                    for h in range(8):
                        op('pe', lambda e, h=h: e.matmul(out=ps[0:64, h * 64:(h + 1) * 64], lhsT=lhs(h), rhs=rhs(h), start=start, stop=stop), reads=reads, writes=[pk])
                    return ps, pk

                def headtr(src, srck):
                    ps, pk = nextpf()
                    for h in range(8):
                        op('pe', lambda e, h=h: e.transpose(out=ps[0:64, h * 64:(h + 1) * 64], in_=src(h), identity=id64), reads=[srck, 'cst'], writes=[pk])
                    return ps, pk
                p3 = lambda ps: ps[0:64, :].rearrange("p (h f) -> p h f", f=64)
                B = bg_
                if sub == 'g':
                    kb.barrier()
                    return fin()
                for d in range(2 if sub is None else 1):
                    CU = cst[0:64, C_U:C_U + 64] if d == 0 else cst[0:64, C_L:C_L + 64]
                    incl = cst[0:64, C_IF:C_IF + 64] if d == 0 else cst[0:64, C_IB:C_IB + 64]
                    strict = cst[0:64, C_SF:C_SF + 64] if d == 0 else cst[0:64, C_SB:C_SB + 64]
                    last = 63 if d == 0 else 0
                    order = ([32, 33, 34, 35] + list(range(32))) if d == 0 else ([35, 34, 33, 32] + list(range(31, -1, -1)))
                    op('dve', lambda e: e.memset(B["Sst"][:], 0.0), writes=['Sst'])
                    op('dve', lambda e: e.memset(B["Sb"][:], 0.0), writes=['Sb'])
                    for it, n in enumerate(order):
                        L_, lk = ld[it % 2], 'ld%d' % (it % 2)

                        def issue_load(it_):
                            n_, Lb, lkb = order[it_], ld[it_ % 2], 'ld%d' % (it_ % 2)
                            dma('sp', lambda e: e.dma_start(out=Lb[:], in_=qkv_s[n_, :, :]), reads=[('qkv_s', n_ // 4)], writes=[lkb])
                        if it == 0:
                            issue_load(0)
                        if it + 1 < len(order):
                            issue_load(it + 1)
                        q3 = L_[:, 0:512].rearrange("p (h f) -> p h f", f=64)
                        k3 = L_[:, 512:1024].rearrange("p (h f) -> p h f", f=64)
                        v3 = L_[:, 1024:1536].rearrange("p (h f) -> p h f", f=64)
                        g = GG[:, n, d * 8:(d + 1) * 8]
                        beta = BETA[:, n, d * 8:(d + 1) * 8]
                        ps, pk = nextpf()
                        op('pe', lambda e, ps=ps: e.matmul(out=ps[0:64, 0:8], lhsT=CU, rhs=g, start=True, stop=True), reads=['GG', 'cst'], writes=[pk])
                        op('dve', lambda e, ps=ps: e.tensor_copy(out=sm["gc"][:], in_=ps[0:64, 0:8]), reads=[pk], writes=['gc'])
                        if sub == 's1':
                            kb.barrier()
                            return fin()
                        op('dve', lambda e: e.tensor_tensor(out=B["Rt"][:], in0=m3(id64), in1=b3(sm["gc"][:]), op=ALU.mult), reads=['gc', 'cst'], writes=['Rt'])
                        psA, pkA = nextpf()
                        op('pe', lambda e: e.matmul(out=psA[0:64, :], lhsT=on64, rhs=fl(B["Rt"]), start=True, stop=True), reads=['Rt', 'cst'], writes=[pkA])
                        op('act', lambda e: e.activation(out=fl(B["egrow"]), in_=psA[0:64, :], func=AF.Exp), reads=[pkA], writes=['egrow'])
                        op('dve', lambda e: e.tensor_tensor(out=B["Dm"][:], in0=b3(sm["gc"][:]), in1=p3(psA), op=ALU.subtract), reads=[pkA, 'gc'], writes=['Dm'])
                        op('act', lambda e: e.copy(out=sm["gl"][:], in_=p3(psA)[:, :, last]), reads=[pkA], writes=['gl'])
                        op('dve', lambda e: e.tensor_scalar_min(out=B["Dm"][:], in0=B["Dm"][:], scalar1=0.0), reads=['Dm'], writes=['Dm'])
                        op('act', lambda e: e.activation(out=B["Dm"][:], in_=B["Dm"][:], func=AF.Exp), reads=['Dm'], writes=['Dm'])
                        op('pool', lambda e: e.tensor_tensor(out=B["Dmi"][:], in0=B["Dm"][:], in1=m3(incl), op=ALU.mult), reads=['Dm', 'cst'], writes=['Dmi'])
                        op('pool', lambda e: e.tensor_tensor(out=B["Dms"][:], in0=B["Dm"][:], in1=m3(strict), op=ALU.mult), reads=['Dm', 'cst'], writes=['Dms'])
                        op('dve', lambda e: e.tensor_tensor(out=sm["t8"][:], in0=sm["gl"][:], in1=sm["gc"][:], op=ALU.subtract), reads=['gl', 'gc'], writes=['t8'])
                        op('act', lambda e: e.activation(out=sm["et"][:], in_=sm["t8"][:], func=AF.Exp), reads=['t8'], writes=['et'])
                        op('act', lambda e: e.activation(out=sm["egl"][:], in_=sm["gl"][:], func=AF.Exp), reads=['gl'], writes=['egl'])
                        op('act', lambda e: e.activation(out=sm["eg"][:], in_=sm["gc"][:], func=AF.Exp), reads=['gc'], writes=['eg'])
                        op('dve', lambda e: e.tensor_tensor(out=sm["bg"][:], in0=sm["eg"][:], in1=beta, op=ALU.mult), reads=['eg', 'BETA'], writes=['bg'])
                        op('dve', lambda e: e.tensor_scalar(out=sm["nb"][:], in0=beta, scalar1=-1.0, scalar2=None, op0=ALU.mult), reads=['BETA'], writes=['nb'])
                        if sub == 's2':
                            kb.barrier()
                            return fin()
                        psQ, pkQ = headtr(lambda h: q3[:, h, :], lk)
                        psK, pkK = headtr(lambda h: k3[:, h, :], lk)
                        op('act', lambda e: e.copy(out=qkT[:, 0:8, :], in_=p3(psQ)), reads=[pkQ], writes=['qT_'])
                        op('dve', lambda e: e.tensor_copy(out=qkT[:, 8:16, :], in_=p3(psK)), reads=[pkK], writes=['kT_'])
                        op('dve', lambda e: e.scalar_tensor_tensor(out=B["qdT"][:], in0=qkT[:, 0:8, :], scalar=0.125, in1=B["egrow"][:], op0=ALU.mult, op1=ALU.mult), reads=['qT_', 'egrow'], writes=['qdT'])
                        if sub == 's3':
                            kb.barrier()
                            return fin()
                        psB, pkB = headmm(lambda h: qkT[:, 8 + h, :], lambda h: qkT[:, 8 + h, :], ['kT_'])
                        psC, pkC = headmm(lambda h: qkT[:, h, :], lambda h: qkT[:, 8 + h, :], ['kT_', 'qT_'])
                        op('dve', lambda e: e.tensor_tensor(out=B["NegA"][:], in0=p3(psB), in1=B["Dms"][:], op=ALU.mult), reads=[pkB, 'Dms'], writes=['NegA'])
                        op('dve', lambda e: e.tensor_tensor(out=B["NegA"][:], in0=B["NegA"][:], in1=b3(sm["nb"][:]), op=ALU.mult), reads=['NegA', 'nb'], writes=['NegA'])
                        op('act', lambda e: e.copy(out=B["NegAb"][:], in_=B["NegA"][:]), reads=['NegA'], writes=['NegAb'])
                        op('dve', lambda e: e.scalar_tensor_tensor(out=B["QKm"][:], in0=p3(psC), scalar=0.125, in1=B["Dmi"][:], op0=ALU.mult, op1=ALU.mult), reads=[pkC, 'Dmi'], writes=['QKm'])
                        if sub == 's4':
                            kb.barrier()
                            return fin()
                        psD, pkD = headtr(lambda h: B["NegA"][:, h, :], 'NegA')
                        psE, pkE = headtr(lambda h: B["QKm"][:, h, :], 'QKm')
                        op('act', lambda e: e.copy(out=B["X0"][:], in_=p3(psD)), reads=[pkD], writes=['X0'])
                        op('dve', lambda e: e.tensor_tensor(out=B["Z"][:], in0=p3(psD), in1=m3(id64), op=ALU.add), reads=[pkD, 'cst'], writes=['Z'])
                        op('act', lambda e: e.copy(out=B["Zb"][:], in_=B["Z"][:]), reads=['Z'], writes=['Zb'])
                        op('act', lambda e: e.copy(out=B["QKT"][:], in_=p3(psE)), reads=[pkE], writes=['QKT'])
                        if sub == 's5':
                            kb.barrier()
                            return fin()
                        X, Xk, XT, XTk = B["X0"], 'X0', B["NegAb"], 'NegAb'
                        for lvl in range(1, 6):
                            nX, nXk = (B["Xa"], 'Xa') if lvl % 2 == 1 else (B["Xb"], 'Xb')
                            nXT, nXTk = (B["XTa"], 'XTa') if lvl % 2 == 1 else (B["XTb"], 'XTb')
                            ps1, pk1 = headmm(lambda h, X=X: X[:, h, :], lambda h, XT=XT: XT[:, h, :], [Xk, XTk])
                            op('act', lambda e, ps1=ps1, nXT=nXT: e.copy(out=nXT[:], in_=p3(ps1)), reads=[pk1], writes=[nXTk])
                            if lvl < 5:
                                ps2, pk2 = headmm(lambda h, XT=XT: XT[:, h, :], lambda h, X=X: X[:, h, :], [Xk, XTk])
                                op('dve', lambda e, ps2=ps2, nX=nX: e.tensor_copy(out=nX[:], in_=p3(ps2)), reads=[pk2], writes=[nXk])
                            ps3, pk3 = headmm(lambda h, nXT=nXT: nXT[:, h, :], lambda h: B["Zb"][:, h, :], [nXTk, 'Zb'])
                            op('dve', lambda e, ps3=ps3: e.tensor_tensor(out=B["Z"][:], in0=B["Z"][:], in1=p3(ps3), op=ALU.add), reads=[pk3, 'Z'], writes=['Z'])
                            op('act', lambda e: e.copy(out=B["Zb"][:], in_=B["Z"][:]), reads=['Z'], writes=['Zb'])
                            X, Xk, XT, XTk = nX, nXk, nXT, nXTk
                        if sub == 's6':
                            kb.barrier()
                            return fin()
                        op('pool', lambda e: e.tensor_tensor(out=B["vb"][:], in0=v3, in1=b3(beta), op=ALU.mult), reads=[lk, 'BETA'], writes=['vb'])
                        op('pool', lambda e: e.tensor_tensor(out=B["kbg"][:], in0=k3, in1=b3(sm["bg"][:]), op=ALU.mult), reads=[lk, 'bg'], writes=['kbg'])
                        op('pool', lambda e: e.tensor_tensor(out=B["ktail"][:], in0=k3, in1=b3(sm["et"][:]), op=ALU.mult), reads=[lk, 'et'], writes=['ktail'])
                        psU, pkU = headmm(lambda h: B["Zb"][:, h, :], lambda h: B["vb"][:, h, :], ['Zb', 'vb'])
                        op('act', lambda e: e.copy(out=B["u"][:], in_=p3(psU)), reads=[pkU], writes=['u'])
                        psW, pkW = headmm(lambda h: B["kbg"][:, h, :], lambda h: B["Zb"][:, h, :], ['Zb', 'kbg'])
                        op('act', lambda e: e.copy(out=B["wT"][:], in_=p3(psW)), reads=[pkW], writes=['wT'])
                        if sub == 's7':
                            kb.barrier()
                            return fin()
                        psP, pkP = headmm(lambda h: B["wT"][:, h, :], lambda h: B["Sb"][:, h, :], ['wT', 'Sb'])
                        op('dve', lambda e: e.tensor_tensor(out=B["vnew"][:], in0=B["u"][:], in1=p3(psP), op=ALU.subtract), reads=[pkP, 'u'], writes=['vnew'])
                        if n < 32:
                            psO, pkO = nextpf()
                            for h in range(8):
                                op('pe', lambda e, h=h: e.matmul(out=psO[0:64, h * 64:(h + 1) * 64], lhsT=B["qdT"][:, h, :], rhs=B["Sb"][:, h, :], start=True, stop=False), reads=['qdT', 'Sb'], writes=[pkO])
                                op('pe', lambda e, h=h: e.matmul(out=psO[0:64, h * 64:(h + 1) * 64], lhsT=B["QKT"][:, h, :], rhs=B["vnew"][:, h, :], start=False, stop=True), reads=['QKT', 'vnew'], writes=[pkO])
                            op('act', lambda e: e.copy(out=B["ost"][:], in_=p3(psO)), reads=[pkO], writes=['ost'])
                            dma('sp', lambda e, n=n: e.dma_start(out=o_s[d, n, :, :], in_=fl(B["ost"])), reads=['ost'], writes=[('o_s', d, n)])
                        psS, pkS = headmm(lambda h: B["ktail"][:, h, :], lambda h: B["vnew"][:, h, :], ['ktail', 'vnew'])
                        op('dve', lambda e: e.tensor_tensor(out=B["Sst"][:], in0=B["Sst"][:], in1=b3(sm["egl"][:]), op=ALU.mult), reads=['Sst', 'egl'], writes=['Sst'])
                        op('dve', lambda e: e.tensor_tensor(out=B["Sst"][:], in0=B["Sst"][:], in1=p3(psS), op=ALU.add), reads=['Sst', pkS], writes=['Sst'])
                        op('act', lambda e: e.copy(out=B["Sb"][:], in_=B["Sst"][:]), reads=['Sst'], writes=['Sb'])
                        if sub is not None and sub.startswith('it') and it + 1 == int(sub[2:]):
                            kb.barrier()
                            return fin()
                kb.barrier()
                if stop == 'D2':
                    return fin()
            with ExitStack() as s1:
                S = lambda name, shape, dt=F32: s1.enter_context(nc.sbuf_tensor("t_" + name, shape, dt))
                PF = [s1.enter_context(nc.psum_tensor("pfF%d" % i, [128, 512], F32)) for i in range(4)]
                PB = [s1.enter_context(nc.psum_tensor("pbF%d" % i, [128, 4, 256], BF16)) for i in range(2)]
                wdg = S("wdg", [128, 8, 512], BF16)
                dnwb = S("dnwb", [64, 64])
                of = [S("of%d" % i, [64, 512]) for i in range(2)]
                ob = [S("ob%d" % i, [64, 512]) for i in range(2)]
                sq = S("sqF", [64, 512])
                s8 = S("s8F", [64, 8])
                r8 = S("r8F", [64, 8])
                sg = S("sgF", [64, 512])
                odb = [S("odb%d" % i, [64, 512], BF16) for i in range(2)]
                dma('pool', lambda e: e.dma_start(out=wdg[:], in_=winv[:, :, 3072:3584]), writes=['wdg'])
                dma('sp', lambda e: e.dma_start(out=dnwb[:], in_=dnw_d[0:1, :].partition_broadcast(64)), writes=['dnwb'])
                for n in range(32):
                    a, ak = of[n % 2], 'of%d' % (n % 2)
                    b_, bk = ob[n % 2], 'ob%d' % (n % 2)
                    dma('sp', lambda e, a=a, n=n: e.dma_start(out=a[:], in_=o_s[0, n, :, :]), reads=[('o_s', 0, n)], writes=[ak])
                    dma('sp', lambda e, b_=b_, n=n: e.dma_start(out=b_[:], in_=o_s[1, n, :, :]), reads=[('o_s', 1, n)], writes=[bk])
                    op('dve', lambda e, a=a, b_=b_: e.tensor_tensor(out=a[:], in0=a[:], in1=b_[:], op=ALU.add), reads=[ak, bk], writes=[ak])
                    op('act', lambda e, a=a: e.activation(out=sq[:], in_=a[:], func=AF.Square), reads=[ak], writes=['sqF'])
                    op('dve', lambda e: e.tensor_reduce(out=s8[:], in_=sq[:].rearrange("p (g f) -> p g f", f=64), axis=AX.X, op=ALU.add), reads=['sqF'], writes=['s8F'])
                    rstd_from_ssq(s8[:], r8[:], 1.0 / 64, ['s8F'], ['r8F'])
                    a3 = a[:].rearrange("p (g f) -> p g f", f=64)
                    op('dve', lambda e, a3=a3: e.tensor_tensor(out=a3, in0=a3, in1=b3(r8[:]), op=ALU.mult), reads=[ak, 'r8F'], writes=[ak])
                    op('pool', lambda e, a3=a3: e.tensor_tensor(out=a3, in0=a3, in1=m3(dnwb[:]), op=ALU.mult), reads=[ak, 'dnwb'], writes=[ak])
                    ps, pk = PF[n % 4], 'pfF%d' % (n % 4)
                    for k in range(8):
                        op('pe', lambda e, ps=ps, k=k, n=n: e.matmul(out=ps[0:64, :], lhsT=hT[:, k, n * 64:(n + 1) * 64], rhs=wdg[:, k, :], start=(k == 0), stop=(k == 7)), reads=['wdg', ('hT', n // 2)], writes=[pk])
                    op('act', lambda e, ps=ps: e.activation(out=sg[:], in_=ps[0:64, :], func=AF.Silu), reads=[pk], writes=['sgF'])
                    o_, ok_ = odb[n % 2], 'odb%d' % (n % 2)
                    op('dve', lambda e, a=a, o_=o_: e.tensor_tensor(out=o_[:], in0=a[:], in1=sg[:], op=ALU.mult), reads=[ak, 'sgF'], writes=[ok_])
                    pb, pbk = PB[n % 2], 'pbF%d' % (n % 2)
                    for c in range(4):
                        op('pe', lambda e, pb=pb, o_=o_, c=c: e.transpose(out=pb[:, c, 0:64], in_=o_[:, c * 128:(c + 1) * 128], identity=identb[0:64, 0:64]), reads=[ok_, 'identb'], writes=[pbk])
                    op('act', lambda e, pb=pb, n=n: e.copy(out=odnT[:, :, n * 64:(n + 1) * 64], in_=pb[:, :, 0:64]), reads=[pbk], writes=[('odnT', n)])
                kb.barrier()
                if stop == 'D3':
                    return fin()
            if dbg:
                with ExitStack() as s1:
                    tmp = s1.enter_context(nc.sbuf_tensor("dbgt3", [128, 4 * T], F32))
                    op('dve', lambda e: e.tensor_copy(out=tmp[:], in_=odnT[:].rearrange("p k n -> p (k n)")), reads=[('odnT', n) for n in range(32)], writes=['dbgt3'])
                    dma('sp', lambda e: e.dma_start(out=dbg_d["d_odn"][:, :], in_=tmp[:]), reads=['dbgt3'], writes=['d_odn'])
                    kb.barrier()

            with ExitStack() as s1:
                S = lambda name, shape, dt=F32: s1.enter_context(nc.sbuf_tensor("t_" + name, shape, dt))
                PF = [s1.enter_context(nc.psum_tensor("pfG%d" % i, [128, 512], F32)) for i in range(8)]
                pfi = [0]

                def nextpf():
                    i = pfi[0] % 8
                    pfi[0] += 1
                    return PF[i], 'pfG%d' % i
                wg = S("wg", [128, 8, 2048], BF16)
                wa = S("wa", [128, 4, D], BF16)
                wb = S("wb", [128, 4, D], BF16)
                wo = S("wo", [128, 8, D], BF16)
                wr = S("wr", [128, 8, 32])
                brb = S("brb", [128, 32])
                yT = S("yT", [128, 8, 512], BF16)
                sga = S("sga", [128, 512])
                sgb = S("sgb", [128, 512])
                xm = [S("xm0", [128, D])] * 2
                xo = [S("xo%d" % i, [128, D]) for i in range(2)]
                h2 = [S("h2_%d" % i, [128, D]) for i in range(2)]
                h2T = S("h2T", [128, 8, 128])
                junk = S("junkE", [128, D])
                ssq = S("ssqE", [128, 16])
                rs = S("rsE", [128, 16])
                for j in range(4):
                    dma('pool', lambda e, j=j: e.dma_start(out=wg[:, :, j * 512:(j + 1) * 512], in_=winv[:, :, 3616 + j * 512:3616 + (j + 1) * 512]), writes=[('wg', j)])
                dma('pool', lambda e: e.dma_start(out=wa[:], in_=wbra_d.rearrange("(k p) n -> p k n", p=128)), writes=['wa'])
                dma('pool', lambda e: e.dma_start(out=wb[:], in_=wbrb_d.rearrange("(k p) n -> p k n", p=128)), writes=['wb'])
                for j in range(2):
                    dma('pool', lambda e, j=j: e.dma_start(out=wo[:, :, j * 512:(j + 1) * 512], in_=wout_d.rearrange("(k p) n -> p k n", p=128)[:, :, j * 512:(j + 1) * 512]), writes=[('wo', j)])
                dma('sp', lambda e: e.dma_start(out=wr[:], in_=wr_d.rearrange("(k p) n -> p k n", p=128)), writes=['wr'])
                dma('sp', lambda e: e.dma_start(out=brb[:], in_=br_d[0:1, :].partition_broadcast(128)), writes=['brb'])
                for tb in range(4):
                    n0 = tb * 512
                    hts = [('hT', t) for t in range(n0 // 128, n0 // 128 + 4)]
                    for c in range(8):
                        cs = slice(c * 128, (c + 1) * 128)
                        psa, pka = nextpf()
                        for k in range(4):
                            op('pe', lambda e, k=k, psa=psa: e.matmul(out=psa[:], lhsT=wa[:, k, cs], rhs=onaT[:, k, n0:n0 + 512], start=(k == 0), stop=(k == 3)), reads=['wa'] + onaT_all, writes=[pka])
                        psb, pkb = nextpf()
                        for k in range(4):
                            op('pe', lambda e, k=k, psb=psb: e.matmul(out=psb[:], lhsT=wb[:, k, cs], rhs=odnT[:, k, n0:n0 + 512], start=(k == 0), stop=(k == 3)), reads=['wb'] + [('odnT', n) for n in range(tb * 8, tb * 8 + 8)], writes=[pkb])
                        pga, pkga = nextpf()
                        for k in range(8):
                            op('pe', lambda e, k=k, pga=pga: e.matmul(out=pga[:], lhsT=wg[:, k, c * 128:(c + 1) * 128], rhs=hT[:, k, n0:n0 + 512], start=(k == 0), stop=(k == 7)), reads=[('wg', c // 4)] + hts, writes=[pkga])
                        pgb, pkgb = nextpf()
                        for k in range(8):
                            op('pe', lambda e, k=k, pgb=pgb: e.matmul(out=pgb[:], lhsT=wg[:, k, 1024 + c * 128:1024 + (c + 1) * 128], rhs=hT[:, k, n0:n0 + 512], start=(k == 0), stop=(k == 7)), reads=[('wg', 2 + c // 4)] + hts, writes=[pkgb])
                        op('act', lambda e, pga=pga: e.activation(out=sga[:], in_=pga[:], func=AF.Sigmoid), reads=[pkga], writes=['sga'])
                        op('act', lambda e, pgb=pgb: e.activation(out=sgb[:], in_=pgb[:], func=AF.Sigmoid), reads=[pkgb], writes=['sgb'])
                        op('dve', lambda e, psa=psa: e.tensor_tensor(out=sga[:], in0=sga[:], in1=psa[:], op=ALU.mult), reads=['sga', pka], writes=['sga'])
                        op('dve', lambda e, psb=psb: e.tensor_tensor(out=sgb[:], in0=sgb[:], in1=psb[:], op=ALU.mult), reads=['sgb', pkb], writes=['sgb'])
                        op('pool', lambda e, c=c: e.tensor_tensor(out=yT[:, c, :], in0=sga[:], in1=sgb[:], op=ALU.add), reads=['sga', 'sgb'], writes=['yT'])
                    for i4 in range(4):
                        i = tb * 4 + i4
                        xmi, xmk = xm[0], 'xm0'
                        xoi, xok = xo[i % 2], 'xo%d' % (i % 2)
                        h2i, h2k = h2[i % 2], 'h2_%d' % (i % 2)
                        dma('sp', lambda e, xmi=xmi, i=i: e.dma_start(out=xmi[:], in_=x_d[i * 128:(i + 1) * 128, :]), writes=[xmk])
                        for half in range(2):
                            hs_ = slice(half * 512, (half + 1) * 512)
                            ps, pk = nextpf()
                            for k in range(8):
                                op('pe', lambda e, k=k, ps=ps: e.matmul(out=ps[:], lhsT=yT[:, k, i4 * 128:(i4 + 1) * 128], rhs=wo[:, k, hs_], start=(k == 0), stop=(k == 7)), reads=['yT', ('wo', half)], writes=[pk])
                            op('dve', lambda e, ps=ps, xoi=xoi: e.tensor_tensor(out=xoi[:, hs_], in0=ps[:], in1=MODL[:, 2 * D + half * 512:2 * D + (half + 1) * 512], op=ALU.mult), reads=[pk, 'MODL'], writes=[xok])
                            op('dve', lambda e, xoi=xoi, xmi=xmi: e.tensor_tensor(out=xoi[:, hs_], in0=xoi[:, hs_], in1=xmi[:, hs_], op=ALU.add), reads=[xok, xmk], writes=[xok])
                        dma('sp', lambda e, xoi=xoi, i=i: e.dma_start(out=xl2_s[i * 128:(i + 1) * 128, :], in_=xoi[:]), reads=[xok], writes=[('xl2_s', i)])
                        op('act', lambda e, xoi=xoi, i=i: e.activation(out=junk[:], in_=xoi[:], func=AF.Square, accum_out=ssq[:, i:i + 1]), reads=[xok], writes=['junkE', 'ssqE%d' % i])
                        rstd_from_ssq(ssq[:, i:i + 1], rs[:, i:i + 1], 1.0 / D, ['ssqE%d' % i], ['rsE%d' % i])
                        op('dve', lambda e, xoi=xoi, i=i: e.scalar_tensor_tensor(out=junk[:], in0=xoi[:], scalar=rs[:, i:i + 1], in1=MODL[:, 4 * D:5 * D], op0=ALU.mult, op1=ALU.mult), reads=[xok, 'rsE%d' % i, 'MODL', 'junkE'], writes=['junkE'])
                        op('dve', lambda e, h2i=h2i: e.tensor_tensor(out=h2i[:], in0=junk[:], in1=MODL[:, 3 * D:4 * D], op=ALU.add), reads=['junkE', 'MODL'], writes=[h2k])
                        dma('sp', lambda e, h2i=h2i, i=i: e.dma_start(out=h2_s[i * 128:(i + 1) * 128, :], in_=h2i[:]), reads=[h2k], writes=[('h2_s', i)])
                        for hh in range(2):
                            ps, pk = nextpf()
                            for k4 in range(4):
                                k = hh * 4 + k4
                                op('pe', lambda e, ps=ps, k=k, k4=k4, h2i=h2i: e.transpose(out=ps[:, k4 * 128:(k4 + 1) * 128], in_=h2i[:, k * 128:(k + 1) * 128], identity=ident), reads=[h2k, 'cst'], writes=[pk])
                            op('act', lambda e, ps=ps, hh=hh: e.copy(out=h2T[:, hh * 4:(hh + 1) * 4, :], in_=ps[:].rearrange("p (k f) -> p k f", f=128)), reads=[pk], writes=[('h2T', hh)])
                        ps, pk = nextpf()
                        for k in range(8):
                            op('pe', lambda e, ps=ps, k=k: e.matmul(out=ps[:, 0:32], lhsT=h2T[:, k, :], rhs=wr[:, k, :], start=(k == 0), stop=(k == 7)), reads=[('h2T', k // 4), 'wr'], writes=[pk])
                        op('dve', lambda e, ps=ps, i=i: e.tensor_tensor(out=LG[:, i, :], in0=ps[:, 0:32], in1=brb[:], op=ALU.add), reads=[pk, 'brb'], writes=['LG'])
                kb.barrier()
                if stop == 'E':
                    return fin()
            if dbg:
                dma('sp', lambda e: e.dma_start(out=dbg_d["d_lg"][:, :], in_=LG[:].rearrange("p a b -> p (a b)")), reads=['LG'], writes=['d_lg'])
                kb.barrier()

            mid.close()
            with ExitStack() as s1:
                S = lambda name, shape, dt=F32: s1.enter_context(nc.sbuf_tensor("t_" + name, shape, dt))
                PF = [s1.enter_context(nc.psum_tensor("pfH%d" % i, [128, 512], F32)) for i in range(8)]
                A3 = [128, 16, 32]
                DESTi = S("DESTi", [128, 4, 16], I32)
                GATE = S("GATE", [128, 4, 16])
                W1I = S("W1I", [128, NBLK, 8], I32)
                B1I = S("B1I", [128, NBLK], I32)
                B2I = S("B2I", [128, NBLK], I32)
                rt = ExitStack()
                s1.callback(rt.close)
                R_ = lambda name, shape, dt=F32: rt.enter_context(nc.sbuf_tensor("t_" + name, shape, dt))
                LGw = R_("LGw", A3)
                EQ = R_("EQ", [128, 4, 16, 32])
                SEL = R_("SEL", A3)
                GT = R_("GT", A3)
                RANK = R_("RANK", A3)
                tmp3 = R_("tmp3", A3)
                mr = R_("mr", [128, 16])
                m0 = R_("m0", [128, 16])
                den = R_("den", [128, 16])
                CNT = R_("CNT", [128, 32])
                PAD = R_("PAD", [128, 32])
                PADi = R_("PADi", [128, 32], I32)
                PADT = R_("PADT", [32, 128])
                PSb = R_("PSb", [128, 32])
                PEb = R_("PEb", [128, 32])
                DESTf = R_("DESTf", [128, 4, 16])
                cmp_ = R_("cmp_", [128, NBLK, 32])
                BE = R_("BE", [128, NBLK])
                tmpw = R_("tmpw", [128, NBLK, 8])
                tmpb = R_("tmpb", [128, NBLK])
                zi = R_("zi", [128, NBLK], I32)
                tokid = R_("tokid", [128, 16], I32)
                b16 = lambda ap: ap.unsqueeze(2).to_broadcast(A3)
                e16 = lambda ap: ap.unsqueeze(1).to_broadcast(A3)
                op('dve', lambda e: e.tensor_copy(out=LGw[:], in_=LG[:]), reads=['LG'], writes=['LGw'])
                for r in range(4):
                    op('dve', lambda e: e.tensor_reduce(out=mr[:], in_=LGw[:], axis=AX.X, op=ALU.max), reads=['LGw'], writes=['mr'])
                    if r == 0:
                        op('dve', lambda e: e.tensor_copy(out=m0[:], in_=mr[:]), reads=['mr'], writes=['m0'])
                    op('dve', lambda e, r=r: e.tensor_tensor(out=EQ[:, r], in0=LGw[:], in1=b16(mr[:]), op=ALU.is_equal), reads=['LGw', 'mr'], writes=[('EQ', r)])
                    op('dve', lambda e, r=r: e.scalar_tensor_tensor(out=LGw[:], in0=EQ[:, r], scalar=NEG, in1=LGw[:], op0=ALU.mult, op1=ALU.add), reads=[('EQ', r), 'LGw'], writes=['LGw'])
                op('dve', lambda e: e.tensor_tensor(out=SEL[:], in0=EQ[:, 0], in1=EQ[:, 1], op=ALU.add), reads=[('EQ', 0), ('EQ', 1)], writes=['SEL'])
                op('dve', lambda e: e.tensor_tensor(out=SEL[:], in0=SEL[:], in1=EQ[:, 2], op=ALU.add), reads=['SEL', ('EQ', 2)], writes=['SEL'])
                op('dve', lambda e: e.tensor_tensor(out=SEL[:], in0=SEL[:], in1=EQ[:, 3], op=ALU.add), reads=['SEL', ('EQ', 3)], writes=['SEL'])
                op('dve', lambda e: e.tensor_tensor(out=GT[:], in0=LG[:], in1=b16(m0[:]), op=ALU.subtract), reads=['LG', 'm0'], writes=['GT'])
                op('act', lambda e: e.activation(out=GT[:], in_=GT[:], func=AF.Exp), reads=['GT'], writes=['GT'])
                op('dve', lambda e: e.tensor_tensor(out=GT[:], in0=GT[:], in1=SEL[:], op=ALU.mult), reads=['GT', 'SEL'], writes=['GT'])
                op('dve', lambda e: e.tensor_reduce(out=den[:], in_=GT[:], axis=AX.X, op=ALU.add), reads=['GT'], writes=['den'])
                op('dve', lambda e: e.reciprocal(out=den[:], in_=den[:]), reads=['den'], writes=['den'])
                op('dve', lambda e: e.tensor_tensor(out=GT[:], in0=GT[:], in1=b16(den[:]), op=ALU.mult), reads=['GT', 'den'], writes=['GT'])
                op('dve', lambda e: e.memset(CNT[:], 0.0), writes=['CNT'])
                for i in range(16):
                    ps, pk = PF[i % 2], 'pfH%d' % (i % 2)
                    op('pe', lambda e, ps=ps, i=i: e.matmul(out=ps[:, 0:32], lhsT=cst[:, C_UT:C_UT + 128], rhs=SEL[:, i, :], start=True, stop=True), reads=['SEL', 'cst'], writes=[pk])
                    op('dve', lambda e, ps=ps, i=i: e.tensor_tensor(out=RANK[:, i, :], in0=ps[:, 0:32], in1=CNT[:], op=ALU.add), reads=[pk, 'CNT'], writes=['RANK'])
                    ps2, pk2 = PF[2 + i % 2], 'pfH%d' % (2 + i % 2)
                    op('pe', lambda e, ps2=ps2, i=i: e.matmul(out=ps2[:, 0:32], lhsT=ones, rhs=SEL[:, i, :], start=True, stop=True), reads=['SEL', 'cst'], writes=[pk2])
                    op('dve', lambda e, ps2=ps2: e.tensor_tensor(out=CNT[:], in0=CNT[:], in1=ps2[:, 0:32], op=ALU.add), reads=[pk2, 'CNT'], writes=['CNT'])
                op('dve', lambda e: e.tensor_scalar(out=PAD[:], in0=CNT[:], scalar1=127.0, scalar2=None, op0=ALU.add), reads=['CNT'], writes=['PAD'])
                op('dve', lambda e: e.tensor_copy(out=PADi[:], in_=PAD[:]), reads=['PAD'], writes=['PADi'])
                op('dve', lambda e: e.tensor_scalar(out=PADi[:], in0=PADi[:], scalar1=7, scalar2=7, op0=ALU.arith_shift_right, op1=ALU.logical_shift_left), reads=['PADi'], writes=['PADi'])
                op('dve', lambda e: e.tensor_copy(out=PAD[:], in_=PADi[:]), reads=['PADi'], writes=['PAD'])
                op('pe', lambda e: e.transpose(out=PF[4][0:32, 0:128], in_=PAD[:, 0:32], identity=ident), reads=['PAD', 'cst'], writes=['pfH4'])
                op('act', lambda e: e.copy(out=PADT[:], in_=PF[4][0:32, 0:128]), reads=['pfH4'], writes=['PADT'])
                op('pe', lambda e: e.matmul(out=PF[5][:, 0:32], lhsT=PADT[:], rhs=cst[0:32, C_TRI:C_TRI + 32], start=True, stop=True), reads=['PADT', 'cst'], writes=['pfH5'])
                op('act', lambda e: e.copy(out=PSb[:], in_=PF[5][:, 0:32]), reads=['pfH5'], writes=['PSb'])
                op('dve', lambda e: e.tensor_tensor(out=PEb[:], in0=PSb[:], in1=PAD[:], op=ALU.add), reads=['PSb', 'PAD'], writes=['PEb'])
                op('dve', lambda e: e.tensor_tensor(out=RANK[:], in0=RANK[:], in1=e16(PSb[:]), op=ALU.add), reads=['RANK', 'PSb'], writes=['RANK'])
                for r in range(4):
                    op('dve', lambda e, r=r: e.tensor_tensor(out=tmp3[:], in0=EQ[:, r], in1=RANK[:], op=ALU.mult), reads=[('EQ', r), 'RANK'], writes=['tmp3'])
                    op('dve', lambda e, r=r: e.tensor_reduce(out=DESTf[:, r, :], in_=tmp3[:], axis=AX.X, op=ALU.add), reads=['tmp3'], writes=['DESTf'])
                    op('dve', lambda e, r=r: e.tensor_tensor(out=tmp3[:], in0=EQ[:, r], in1=GT[:], op=ALU.mult), reads=[('EQ', r), 'GT'], writes=['tmp3'])
                    op('dve', lambda e, r=r: e.tensor_reduce(out=GATE[:, r, :], in_=tmp3[:], axis=AX.X, op=ALU.add), reads=['tmp3'], writes=['GATE'])
                op('dve', lambda e: e.tensor_copy(out=DESTi[:], in_=DESTf[:]), reads=['DESTf'], writes=['DESTi'])
                op('dve', lambda e: e.tensor_tensor(out=cmp_[:], in0=PEb[:].unsqueeze(1).to_broadcast([128, NBLK, 32]), in1=cst[:, C_BS:C_BS + NBLK].unsqueeze(2).to_broadcast([128, NBLK, 32]), op=ALU.is_le),
                   reads=['PEb', 'cst'], writes=['cmp_'])
                op('dve', lambda e: e.tensor_reduce(out=BE[:], in_=cmp_[:], axis=AX.X, op=ALU.add), reads=['cmp_'], writes=['BE'])
                op('dve', lambda e: e.tensor_scalar_min(out=BE[:], in0=BE[:], scalar1=31.0), reads=['BE'], writes=['BE'])
                op('dve', lambda e: e.tensor_copy(out=B2I[:], in_=BE[:]), reads=['BE'], writes=['B2I'])
                op('dve', lambda e: e.tensor_scalar(out=tmpb[:], in0=BE[:], scalar1=128.0, scalar2=cst[:, C_KR:C_KR + 1], op0=ALU.mult, op1=ALU.add), reads=['BE', 'cst'], writes=['tmpb'])
                op('dve', lambda e: e.tensor_copy(out=B1I[:], in_=tmpb[:]), reads=['tmpb'], writes=['B1I'])
                op('dve', lambda e: e.tensor_scalar(out=tmpb[:], in0=BE[:], scalar1=1024.0, scalar2=None, op0=ALU.mult), reads=['BE', 'B1I'], writes=['tmpb'])
                op('dve', lambda e: e.tensor_tensor(out=tmpw[:], in0=tmpb[:].unsqueeze(2).to_broadcast([128, NBLK, 8]), in1=cst[:, C_KR:C_KR + 8].unsqueeze(1).to_broadcast([128, NBLK, 8]), op=ALU.add),
                   reads=['tmpb', 'cst'], writes=['tmpw'])
                op('dve', lambda e: e.tensor_copy(out=W1I[:], in_=tmpw[:]), reads=['tmpw'], writes=['W1I'])
                op('dve', lambda e: e.memset(zi[:], 0), writes=['zi'])
                op('dve', lambda e: e.tensor_copy(out=tokid[:], in_=cst[:, C_TOK:C_TOK + 16]), reads=['cst'], writes=['tokid'])
                dma('sp', lambda e: e.dma_start(out=slot_s.rearrange("(p j) o -> p (j o)", p=128), in_=zi[:]), reads=['zi'], writes=['slot_s'])
                for r in range(4):
                    for i in range(16):
                        dma('pool', lambda e, r=r, i=i: e.indirect_dma_start(out=slot_s[:, :], out_offset=bass.IndirectOffsetOnAxis(ap=DESTi[:, r, i:i + 1], axis=0), in_=tokid[:, i:i + 1], in_offset=None),
                            reads=['DESTi', 'tokid'], writes=['slot_s'])
                kb.barrier()
                rt.close()
                sidx = [S("sidx%d" % i, [128, 1], I32) for i in range(2)]
                xg = [S("xg%d" % i, [128, D]) for i in range(2)]
                xgT = S("xgT", [128, 8, 128], BF16)
                w1b = [S("w1b%d" % i, [128, 2048], BF16) for i in range(3)]
                w2b = [S("w2b%d" % i, [128, D], BF16) for i in range(3)]
                w1k = [S("w1k%d" % i, [128, 2048]) for i in range(6)]
                w2c = [S("w2c%d" % i, [128, D]) for i in range(8)]
                b1s = [S("b1s%d" % i, [128, 16]) for i in range(2)]
                b2s = [S("b2s%d" % i, [128, D]) for i in range(2)]
                Gt = S("Gt", [128, 4, 128])
                Ut = S("Ut", [128, 4, 128])
                sgm = S("sgm", [128, 4, 128])
                actT = S("actT", [128, 8, 128], BF16)
                ysb = [S("ysb%d" % i, [128, D]) for i in range(2)]
                PH = PF[0:4]
                PY = PF[4:6]
                PTr = PF[6:8]
                wc1 = wc2 = 0
                for j in range(NBLK):
                    si, sik = sidx[j % 2], 'sidx%d' % (j % 2)
                    xgi, xgk = xg[j % 2], 'xg%d' % (j % 2)
                    dma('sp', lambda e, si=si, j=j: e.dma_start(out=si[:], in_=slot_s[j * 128:(j + 1) * 128, :]), reads=['slot_s'], writes=[sik])
                    dma('pool', lambda e, si=si, xgi=xgi: e.indirect_dma_start(out=xgi[:], out_offset=None, in_=h2_s[:, :], in_offset=bass.IndirectOffsetOnAxis(ap=si[:, 0:1], axis=0)),
                        reads=[sik] + [('h2_s', i) for i in range(16)], writes=[xgk])
                    b1i, b1k = b1s[j % 2], 'b1s%d' % (j % 2)
                    b2i, b2k = b2s[j % 2], 'b2s%d' % (j % 2)
                    dma('pool', lambda e, b1i=b1i, j=j: e.indirect_dma_start(out=b1i[:], out_offset=None, in_=b1t_d[:, :], in_offset=bass.IndirectOffsetOnAxis(ap=B1I[:, j:j + 1], axis=0)), reads=['B1I'], writes=[b1k])
                    dma('pool', lambda e, b2i=b2i, j=j: e.indirect_dma_start(out=b2i[:], out_offset=None, in_=b2_d[:, :], in_offset=bass.IndirectOffsetOnAxis(ap=B2I[:, j:j + 1], axis=0)), reads=['B2I'], writes=[b2k])
                    for hh in range(2):
                        ps, pk = PTr[hh], 'pfH%d' % (6 + hh)
                        for k4 in range(4):
                            k = hh * 4 + k4
                            op('pe', lambda e, ps=ps, k=k, k4=k4, xgi=xgi: e.transpose(out=ps[:, k4 * 128:(k4 + 1) * 128], in_=xgi[:, k * 128:(k + 1) * 128], identity=ident), reads=[xgk, 'cst'], writes=[pk])
                        op('act', lambda e, ps=ps, hh=hh: e.copy(out=xgT[:, hh * 4:(hh + 1) * 4, :], in_=ps[:].rearrange("p (k f) -> p k f", f=128)), reads=[pk], writes=[('xgT', hh)])
                    for k in range(8):
                        wt, wk = w1k[wc1 % 6], 'w1k%d' % (wc1 % 6)
                        wc1 += 1
                        dma('pool', lambda e, wt=wt, j=j, k=k: e.indirect_dma_start(out=wt[:], out_offset=None, in_=w1_d[:, :], in_offset=bass.IndirectOffsetOnAxis(ap=W1I[:, j, k:k + 1], axis=0)), reads=['W1I'], writes=[wk])
                        wb_, wbk = w1b[wc1 % 3], 'w1b%d' % (wc1 % 3)
                        if k % 2 == 0:
                            op('dve', lambda e, wt=wt, wb_=wb_: e.tensor_copy(out=wb_[:], in_=wt[:]), reads=[wk], writes=[wbk])
                        else:
                            op('act', lambda e, wt=wt, wb_=wb_: e.copy(out=wb_[:], in_=wt[:]), reads=[wk], writes=[wbk])
                        for c in range(16):
                            op('pe', lambda e, wb_=wb_, k=k, c=c: e.matmul(out=PH[c // 4][:, (c % 4) * 128:(c % 4 + 1) * 128], lhsT=wb_[:, c * 128:(c + 1) * 128], rhs=xgT[:, k, :], start=(k == 0 and c % 4 == 0), stop=(k == 7 and c % 4 == 3)),
                               reads=[wbk, ('xgT', k // 4)], writes=['pfH%d' % (c // 4)])
                    for q in range(2):
                        g3_ = PH[q][:].rearrange("p (c f) -> p c f", f=128)
                        u3_ = PH[q + 2][:].rearrange("p (c f) -> p c f", f=128)
                        bb = lambda lo: b1i[:, lo:lo + 4].unsqueeze(2).to_broadcast([128, 4, 128])
                        op('dve', lambda e: e.tensor_tensor(out=Gt[:], in0=g3_, in1=bb(4 * q), op=ALU.add), reads=['pfH%d' % q, b1k], writes=['Gt'])
                        op('dve', lambda e: e.tensor_scalar_min(out=Gt[:], in0=Gt[:], scalar1=7.0), reads=['Gt'], writes=['Gt'])
                        op('act', lambda e: e.activation(out=sgm[:], in_=Gt[:], func=AF.Sigmoid, scale=1.702), reads=['Gt'], writes=['sgm'])
                        op('dve', lambda e: e.tensor_tensor(out=Ut[:], in0=u3_, in1=bb(8 + 4 * q), op=ALU.add), reads=['pfH%d' % (q + 2), b1k], writes=['Ut'])
                        op('dve', lambda e: e.tensor_scalar(out=Ut[:], in0=Ut[:], scalar1=7.0, scalar2=-7.0, op0=ALU.min, op1=ALU.max), reads=['Ut'], writes=['Ut'])
                        op('dve', lambda e: e.scalar_tensor_tensor(out=Ut[:], in0=Ut[:], scalar=1.0, in1=Gt[:], op0=ALU.add, op1=ALU.mult), reads=['Ut', 'Gt'], writes=['Ut'])
                        op('dve', lambda e: e.tensor_tensor(out=actT[:, 4 * q:4 * q + 4, :], in0=Ut[:], in1=sgm[:], op=ALU.mult), reads=['Ut', 'sgm'], writes=[('actT', q)])
                    for fc in range(8):
                        wt, wk = w2c[wc2 % 8], 'w2c%d' % (wc2 % 8)
                        wc2 += 1
                        dma('pool', lambda e, wt=wt, j=j, fc=fc: e.indirect_dma_start(out=wt[:], out_offset=None, in_=w2_d[:, :], in_offset=bass.IndirectOffsetOnAxis(ap=W1I[:, j, fc:fc + 1], axis=0)), reads=['W1I'], writes=[wk])
                        wb_, wbk = w2b[wc2 % 3], 'w2b%d' % (wc2 % 3)
                        op('act', lambda e, wt=wt, wb_=wb_: e.copy(out=wb_[:], in_=wt[:]), reads=[wk], writes=[wbk])
                        for half in range(2):
                            op('pe', lambda e, wb_=wb_, fc=fc, half=half: e.matmul(out=PY[half][:], lhsT=actT[:, fc, :], rhs=wb_[:, half * 512:(half + 1) * 512], start=(fc == 0), stop=(fc == 7)),
                               reads=[wbk, ('actT', fc // 4)], writes=['pfH%d' % (4 + half)])
                    yi, yk = ysb[j % 2], 'ysb%d' % (j % 2)
                    for half in range(2):
                        op('dve', lambda e, yi=yi, half=half: e.tensor_tensor(out=yi[:, half * 512:(half + 1) * 512], in0=PY[half][:], in1=b2i[:, half * 512:(half + 1) * 512], op=ALU.add),
                           reads=['pfH%d' % (4 + half), b2k], writes=[yk])
                    dma('sp', lambda e, yi=yi, j=j: e.dma_start(out=ypad_s[j * 128:(j + 1) * 128, :], in_=yi[:]), reads=[yk], writes=['ypad_s'])
                yr = [S("yr%d" % i, [128, D]) for i in range(4)]
                xq = [S("xq0", [128, D])] * 2
                ac = [S("ac%d" % i, [128, D]) for i in range(2)]
                fnb = S("fnb", [128, D])
                ssq = S("ssqH", [128, 16])
                rs = S("rsH", [128, 16])
                dma('sp', lambda e: e.dma_start(out=fnb[:], in_=fnw_d[0:1, :].partition_broadcast(128)), writes=['fnb'])
                for i in range(16):
                    aci, ack = ac[i % 2], 'ac%d' % (i % 2)
                    xqi, xqk = xq[0], 'xq0'
                    dma('sp', lambda e, xqi=xqi, i=i: e.dma_start(out=xqi[:], in_=xl2_s[i * 128:(i + 1) * 128, :]), reads=[('xl2_s', i)], writes=[xqk])
                    for r in range(4):
                        dma('pool', lambda e, r=r, i=i: e.indirect_dma_start(out=yr[r][:], out_offset=None, in_=ypad_s[:, :], in_offset=bass.IndirectOffsetOnAxis(ap=DESTi[:, r, i:i + 1], axis=0)),
                            reads=['DESTi', 'ypad_s'], writes=['yr%d' % r])
                        if r == 0:
                            op('dve', lambda e, aci=aci, i=i: e.tensor_scalar(out=aci[:], in0=yr[0][:], scalar1=GATE[:, 0, i:i + 1], scalar2=None, op0=ALU.mult), reads=['yr0', 'GATE'], writes=[ack])
                        else:
                            op('dve', lambda e, aci=aci, i=i, r=r: e.scalar_tensor_tensor(out=aci[:], in0=yr[r][:], scalar=GATE[:, r, i:i + 1], in1=aci[:], op0=ALU.mult, op1=ALU.add), reads=['yr%d' % r, 'GATE', ack], writes=[ack])
                    op('dve', lambda e, aci=aci: e.tensor_tensor(out=aci[:], in0=aci[:], in1=MODL[:, 5 * D:6 * D], op=ALU.mult), reads=[ack, 'MODL'], writes=[ack])
                    op('dve', lambda e, aci=aci, xqi=xqi: e.tensor_tensor(out=aci[:], in0=aci[:], in1=xqi[:], op=ALU.add), reads=[ack, xqk], writes=[ack])
                    op('act', lambda e, aci=aci, i=i: e.activation(out=yr[0][:], in_=aci[:], func=AF.Square, accum_out=ssq[:, i:i + 1]), reads=[ack], writes=['yr0', 'ssqH%d' % i])
                    rstd_from_ssq(ssq[:, i:i + 1], rs[:, i:i + 1], 1.0 / D, ['ssqH%d' % i], ['rsH%d' % i])
                    op('dve', lambda e, aci=aci, i=i: e.scalar_tensor_tensor(out=aci[:], in0=aci[:], scalar=rs[:, i:i + 1], in1=fnb[:], op0=ALU.mult, op1=ALU.mult), reads=[ack, 'rsH%d' % i, 'fnb'], writes=[ack])
                    dma('sp', lambda e, aci=aci, i=i: e.dma_start(out=out_d[i * 128:(i + 1) * 128, :], in_=aci[:]), reads=[ack], writes=[('out', i)])
                kb.barrier()
        if kb.stopped:
            kb.stopped = False
            kb.limit = None
            kb.barrier()
            return fin()
        print("instructions:", kb.nins)
    return nc


def host_consts():
    cst = np.zeros((128, C_N), np.float32)
    cst[:, C_ID:C_ID + 128] = np.eye(128)
    cst[:, C_ONE:C_ONE + 128] = 1.0
    i = np.arange(64)
    cst[:64, C_U:C_U + 64] = (i[:, None] <= i[None, :])
    cst[:64, C_L:C_L + 64] = (i[:, None] >= i[None, :])
    cst[:64, C_IF:C_IF + 64] = (i[:, None] >= i[None, :])
    cst[:64, C_SF:C_SF + 64] = (i[:, None] > i[None, :])
    cst[:64, C_IB:C_IB + 64] = (i[:, None] <= i[None, :])
    cst[:64, C_SB:C_SB + 64] = (i[:, None] < i[None, :])
    e = np.arange(32)
    cst[:32, C_TRI:C_TRI + 32] = (e[:, None] < e[None, :])
    t = np.arange(128)
    cst[:, C_UT:C_UT + 128] = (t[:, None] < t[None, :])
    cst[:, C_BS:C_BS + NBLK] = (np.arange(NBLK) * 128)[None, :]
    cst[:, C_KR:C_KR + 8] = np.arange(8)[None, :] * 128 + t[:, None]
    cst[:, C_TOK:C_TOK + 16] = np.arange(16)[None, :] * 128 + t[:, None]
    return cst


def host_rope():
    tt = np.arange(T)
    row = (tt // 64).astype(np.float32)
    col = (tt % 64).astype(np.float32)
    freqs = (np.float32(10000.0) ** (-np.arange(16, dtype=np.float32) / np.float32(16))).astype(np.float32)
    ang = np.concatenate([row[:, None] * freqs, col[:, None] * freqs], axis=-1).astype(np.float32)
    cos, sin = np.cos(ang).astype(np.float32), np.sin(ang).astype(np.float32)
    r = np.stack([cos.reshape(32, 64, 32).transpose(1, 0, 2), sin.reshape(32, 64, 32).transpose(1, 0, 2)], axis=1)
    return np.ascontiguousarray(r.reshape(64, 2 * 32 * 32))


def host_natbl(rpb):
    kc = np.arange(64)[:, None]
    qc = np.arange(64)[None, :]
    dc = np.clip(kc - qc + 15, 0, 30)
    cs = np.clip(np.arange(64) - 8, 0, 48)
    cmask = (kc >= cs[None, :]) & (kc < cs[None, :] + 16)
    base = np.full((8, 64, 17, 64), NEG, np.float32)
    for jp in range(0, 15):
        g = rpb[:, 14 - jp][:, dc]
        base[:, :, jp + 1, :] = np.where(cmask[None], g, np.float32(NEG))
    base_int = base.copy()
    for jp in range(-1, 16):
        if not (4 <= jp <= 11):
            base_int[:, :, jp + 1, :] = NEG
    out = np.empty((8, 2, 128, 16, 64), np.float32)
    for ti, b in enumerate((base, base_int)):
        for a in range(2):
            out[:, ti, a * 64:(a + 1) * 64, :, :] = b[:, :, (1 - a):(17 - a), :]
    return np.ascontiguousarray(out.reshape(8, 2, 128, 1024))


def make_in_maps(inputs, cores):
    f = lambda a: np.ascontiguousarray(np.asarray(a, dtype=np.float32))
    shared = {
        "w_mod": f(inputs["w_mod"][0]), "b_mod": f(inputs["b_mod"][0]).reshape(1, -1), "norm1_w": f(inputs["norm1_w"][0]).reshape(1, -1),
        "w_in": f(inputs["w_in"][0]), "na_tbl": host_natbl(np.asarray(inputs["na_rpb"][0], np.float32)),
        "convw": f(np.asarray(inputs["dn_conv_w"][0]).reshape(5, 12, 128).transpose(2, 1, 0).reshape(128, 60)),
        "alog": f(inputs["dn_a_log"][0]).reshape(1, 16), "dtb": f(inputs["dn_dt_bias"][0]).reshape(1, 16), "dnw": f(inputs["dn_norm_w"][0]).reshape(1, 64),
        "w_br_a": f(inputs["w_br_a"][0]), "w_br_b": f(inputs["w_br_b"][0]), "w_out": f(inputs["w_out"][0]), "norm2_w": f(inputs["norm2_w"][0]).reshape(1, -1),
        "w_router": f(inputs["w_router"][0]), "b_router": f(inputs["b_router"][0]).reshape(1, 32),
        "w1": f(inputs["w1"][0]).reshape(32 * D, 2048), "b1t": f(np.asarray(inputs["b1"][0]).reshape(32, 16, 128).transpose(0, 2, 1).reshape(32 * 128, 16)),
        "w2": f(inputs["w2"][0]).reshape(32 * D, D), "b2": f(inputs["b2"][0]), "fnw": f(inputs["final_norm_w"]).reshape(1, -1),
        "rope": host_rope(), "cst": host_consts(),
    }
    maps = []
    for b in cores:
        m = dict(shared)
        m["x"] = f(inputs["x"][b])
        m["ctx"] = f(inputs["ctx"][b])
        m["c2"] = f(np.stack([np.asarray(inputs["c"][b]), np.asarray(inputs["c_ctx"])], axis=0))
        maps.append(m)
    return maps


def kernel(**inputs):
    nc = build()
    maps = make_in_maps(inputs, list(range(8)))
    res = run_bass_kernel_spmd(nc, maps, core_ids=list(range(8)))
    return np.stack([np.asarray(r["out"], dtype=np.float32) for r in res.results], axis=0)
```

```python
import numpy as np
from contextlib import ExitStack
import concourse.bass as bass
import concourse.mybir as mybir
from concourse.bass_utils import run_bass_kernel_spmd

F32 = mybir.dt.float32
BF16 = mybir.dt.bfloat16
I32 = mybir.dt.int32
ALU = mybir.AluOpType
AF = mybir.ActivationFunctionType
AX = mybir.AxisListType

D = 1024
T = 2048
LC = 256
NT = T + LC
NBLK = 96
NEG = -1e30
EPS = 1e-6

C_ID, C_ONE, C_U, C_L, C_IF, C_SF, C_IB, C_SB, C_TRI, C_UT, C_BS, C_KR, C_TOK, C_N = 0, 128, 256, 320, 384, 448, 512, 576, 640, 672, 800, 896, 904, 920


class _StopScan(Exception):
    pass


class KB:
    NDMA = 16

    def __init__(self, nc, es):
        self.nc = nc
        self.eng = {'pe': nc.tensor, 'act': nc.scalar, 'dve': nc.vector, 'pool': nc.gpsimd, 'sp': nc.sync}
        self.sem = {}
        self.cnt = {}
        for e in ('pe', 'act', 'dve', 'pool'):
            self.sem[e] = es.enter_context(nc.semaphore('s_' + e))
            self.cnt[e] = 0
        self.dsem = {}
        self.dcnt = {}
        self.dnext = {}
        for q in ('sp', 'pool'):
            self.dsem[q] = [es.enter_context(nc.semaphore('d_%s%d' % (q, i))) for i in range(self.NDMA)]
            self.dcnt[q] = [0] * self.NDMA
            self.dnext[q] = 0
        self.seen = {e: {} for e in self.eng}
        self.lastw = {}
        self.readers = {}
        self.nins = 0
        self.limit = None
        self.stopped = False

    def _wait(self, e, ev):
        sem, val, src = ev
        if src == e and e == 'pe':
            return
        k = id(sem)
        if self.seen[e].get(k, 0) >= val:
            return
        self.eng[e].wait_ge(sem, val)
        self.seen[e][k] = val

    def _deps(self, e, reads, writes):
        for r in reads:
            ev = self.lastw.get(r)
            if ev is not None:
                self._wait(e, ev)
        for w in writes:
            ev = self.lastw.get(w)
            if ev is not None:
                self._wait(e, ev)
            for ev in self.readers.get(w, {}).values():
                self._wait(e, ev)

    def _record(self, ev, reads, writes):
        for r in reads:
            self.readers.setdefault(r, {})[id(ev[0])] = ev
        for w in writes:
            self.lastw[w] = ev
            self.readers[w] = {}

    def op(self, e, fn, reads=(), writes=()):
        if self.stopped:
            return None
        pr = [r for r in reads if isinstance(r, str) and r[:2] in ('pf', 'pb')]
        if pr:
            writes = list(writes) + pr
        self._deps(e, reads, writes)
        ins = fn(self.eng[e])
        self.cnt[e] += 1
        ins.then_inc(self.sem[e], 1)
        self._record((self.sem[e], self.cnt[e], e), reads, writes)
        self.nins += 1
        if self.limit is not None and self.nins >= self.limit:
            self.stopped = True
        return ins

    def dma(self, q, fn, reads=(), writes=()):
        if self.stopped:
            return None
        i = self.dnext[q]
        self.dnext[q] = (i + 1) % self.NDMA
        sem = self.dsem[q][i]
        if self.dcnt[q][i] > 0:
            self._wait(q, (sem, self.dcnt[q][i], 'dma'))
        self._deps(q, reads, writes)
        ins = fn(self.eng[q])
        self.dcnt[q][i] += 16
        ins.then_inc(sem, 16)
        self._record((sem, self.dcnt[q][i], 'dma'), reads, writes)
        self.nins += 1
        return ins

    def barrier(self):
        if self.stopped:
            return
        for e in self.eng:
            for e2 in ('pe', 'act', 'dve', 'pool'):
                if e2 != e and self.cnt[e2] > 0:
                    self._wait(e, (self.sem[e2], self.cnt[e2], e2))
            for q in self.dsem:
                for i in range(self.NDMA):
                    if self.dcnt[q][i] > 0:
                        self._wait(e, (self.dsem[q][i], self.dcnt[q][i], 'dma'))

    def wait_all(self, e, toks):
        if self.stopped:
            return
        for t in toks:
            if t in self.lastw:
                self._wait(e, self.lastw[t])


def bc(ap, shape):
    return ap.to_broadcast(shape)


def build(dbg=False, stop=None, sub=None):
    nc = bass.Bass("TRN2", target_bir_lowering=False)
    limit = int(sub[1:]) if (sub is not None and sub.startswith('n')) else None
    inp = lambda name, shape, dt=F32: nc.dram_tensor(name, shape, dt, kind="ExternalInput").ap()
    x_d = inp("x", [T, D])
    ctx_d = inp("ctx", [LC, D])
    c2_d = inp("c2", [2, D])
    wmod_d = inp("w_mod", [D, 6 * D])
    bmod_d = inp("b_mod", [1, 6 * D])
    n1_d = inp("norm1_w", [1, D])
    win_d = inp("w_in", [D, 5664])
    natbl_d = inp("na_tbl", [8, 2, 128, 1024])
    convw_d = inp("convw", [128, 12 * 5])
    alog_d = inp("alog", [1, 16])
    dtb_d = inp("dtb", [1, 16])
    dnw_d = inp("dnw", [1, 64])
    wbra_d = inp("w_br_a", [512, D])
    wbrb_d = inp("w_br_b", [512, D])
    wout_d = inp("w_out", [D, D])
    n2_d = inp("norm2_w", [1, D])
    wr_d = inp("w_router", [D, 32])
    br_d = inp("b_router", [1, 32])
    w1_d = inp("w1", [32 * D, 2048])
    b1t_d = inp("b1t", [32 * 128, 16])
    w2_d = inp("w2", [32 * D, D])
    b2_d = inp("b2", [32, D])
    fnw_d = inp("fnw", [1, D])
    rope_d = inp("rope", [64, 2 * 32 * 32])
    cst_d = inp("cst", [128, C_N])
    out_d = nc.dram_tensor("out", [T, D], F32, kind="ExternalOutput").ap()
    scr = lambda name, shape, dt=F32: nc.dram_tensor(name, shape, dt, kind="Internal").ap()
    qkv_s = scr("qkv_s", [36, 64, 1536])
    o_s = scr("o_s", [2, 32, 64, 512])
    xl2_s = scr("xl2_s", [T, D])
    h2_s = scr("h2_s", [T, D])
    slot_s = scr("slot_s", [NBLK * 128, 1], I32)
    ypad_s = scr("ypad_s", [NBLK * 128, D])
    dbg_d = {}
    if dbg:
        for nm, shp in (("d_hT", [128, 8 * NT]), ("d_ona", [128, 4 * T]), ("d_odn", [128, 4 * T]), ("d_lg", [128, 16 * 32])):
            dbg_d[nm] = nc.dram_tensor(nm, shp, F32, kind="ExternalOutput").ap()

    winv = win_d.rearrange("(k p) n -> p k n", p=128)

    with ExitStack() as es:
        kb = KB(nc, es)
        kb.limit = limit
        op, dma = kb.op, kb.dma

        def fin():
            with ExitStack() as sf:
                z = sf.enter_context(nc.sbuf_tensor("t_zout", [128, D], F32))
                op('dve', lambda e: e.memset(z[:], 0.0), writes=['zout'])
                for i in range(16):
                    dma('sp', lambda e, i=i: e.dma_start(out=out_d[i * 128:(i + 1) * 128, :], in_=z[:]), reads=['zout'], writes=[('out', i)])
                kb.barrier()
            print("instructions (stopped at %s):" % stop, kb.nins)
            return nc
        if True:
            G = lambda name, shape, dt=F32: es.enter_context(nc.sbuf_tensor("t_" + name, shape, dt))
            cst = G("cst", [128, C_N])
            identb = G("identb", [128, 128], BF16)
            MODL = G("MODL", [128, 6 * D])
            epsb = G("epsb", [128, 1])
            LG = G("LG", [128, 16, 32])
            mid = ExitStack()
            es.callback(mid.close)
            Gm = lambda name, shape, dt=F32: mid.enter_context(nc.sbuf_tensor("t_" + name, shape, dt))
            hT = Gm("hT", [128, 8, NT], BF16)
            onaT = Gm("onaT", [128, 4, T], BF16)
            odnT = Gm("odnT", [128, 4, T], BF16)

            dma('sp', lambda e: e.dma_start(out=cst[:], in_=cst_d[:, :]), writes=['cst'])
            op('dve', lambda e: e.tensor_copy(out=identb[:], in_=cst[:, C_ID:C_ID + 128]), reads=['cst'], writes=['identb'])
            op('dve', lambda e: e.memset(epsb[:], EPS), writes=['epsb'])
            ident = cst[:, C_ID:C_ID + 128]
            ones = cst[:, C_ONE:C_ONE + 128]

            def rstd_from_ssq(ssq_ap, rs_ap, scale, toks_r, toks_w):
                op('act', lambda e: e.activation(out=rs_ap, in_=ssq_ap, func=AF.Sqrt, scale=scale, bias=epsb[0:rs_ap.partition_size(), :]), reads=list(toks_r) + ['epsb'], writes=toks_w)
                op('dve', lambda e: e.reciprocal(out=rs_ap, in_=rs_ap), reads=toks_w, writes=toks_w)

            with ExitStack() as s1:
                S = lambda name, shape, dt=F32: s1.enter_context(nc.sbuf_tensor("t_" + name, shape, dt))
                PF = [s1.enter_context(nc.psum_tensor("pfA%d" % i, [128, 512], F32)) for i in range(2)]
                c2t = S("c2t", [128, 2, 8])
                MODC = S("MODC", [128, 2 * D])
                Lm = S("Lm", [128, 2, 8, 128], BF16)
                onesb = S("onesb", [128, 128], BF16)
                wm = [S("wm%d" % i, [128, 8, 512], BF16) for i in range(2)]
                nbc = S("nbc", [128, 2 * D])
                dma('sp', lambda e: e.dma_start(out=c2t[:], in_=c2_d.rearrange("t (p k) -> p t k", k=8)), writes=['c2t'])
                dma('sp', lambda e: e.dma_start(out=MODL[:], in_=bmod_d[0:1, :].partition_broadcast(128)), writes=['MODL'])
                dma('sp', lambda e: e.dma_start(out=MODC[:], in_=bmod_d[0:1, 0:2 * D].partition_broadcast(128)), writes=['MODC'])
                dma('sp', lambda e: e.dma_start(out=nbc[:, 0:D], in_=n1_d[0:1, :].partition_broadcast(128)), writes=['nbc0'])
                dma('sp', lambda e: e.dma_start(out=nbc[:, D:2 * D], in_=n2_d[0:1, :].partition_broadcast(128)), writes=['nbc1'])
                op('act', lambda e: e.activation(out=c2t[:], in_=c2t[:], func=AF.Silu), reads=['c2t'], writes=['c2t'])
                op('dve', lambda e: e.memset(onesb[:], 1.0), writes=['onesb'])
                for t in range(2):
                    for k in range(8):
                        op('dve', lambda e, t=t, k=k: e.tensor_scalar(out=Lm[:, t, k, :], in0=onesb[:], scalar1=c2t[:, t, k:k + 1], scalar2=None, op0=ALU.mult),
                           reads=['c2t', 'onesb'], writes=['Lm'])
                wmv = wmod_d.rearrange("(p k) n -> p k n", k=8)
                for j in range(12):
                    w = wm[j % 2]
                    wt = 'wm%d' % (j % 2)
                    dma('pool', lambda e, j=j, w=w: e.dma_start(out=w[:], in_=wmv[:, :, j * 512:(j + 1) * 512]), writes=[wt])
                    for t in range(2 if j < 4 else 1):
                        ps = PF[t]
                        for k in range(8):
                            op('pe', lambda e, t=t, k=k, w=w, ps=ps: e.matmul(out=ps[:], lhsT=Lm[:, t, k, :], rhs=w[:, k, :], start=(k == 0), stop=(k == 7)),
                               reads=['Lm', wt], writes=['pfA%d' % t])
                        M = MODL if t == 0 else MODC
                        op('dve', lambda e, M=M, j=j, ps=ps: e.tensor_tensor(out=M[:, j * 512:(j + 1) * 512], in0=M[:, j * 512:(j + 1) * 512], in1=ps[:], op=ALU.add),
                           reads=['pfA%d' % t, 'MODL', 'MODC'], writes=['MODL' if t == 0 else 'MODC'])
                op('dve', lambda e: e.scalar_tensor_tensor(out=MODL[:, D:2 * D], in0=MODL[:, D:2 * D], scalar=1.0, in1=nbc[:, 0:D], op0=ALU.add, op1=ALU.mult), reads=['MODL', 'nbc0'], writes=['MODL'])
                op('dve', lambda e: e.scalar_tensor_tensor(out=MODC[:, D:2 * D], in0=MODC[:, D:2 * D], scalar=1.0, in1=nbc[:, 0:D], op0=ALU.add, op1=ALU.mult), reads=['MODC', 'nbc0'], writes=['MODC'])
                op('dve', lambda e: e.scalar_tensor_tensor(out=MODL[:, 4 * D:5 * D], in0=MODL[:, 4 * D:5 * D], scalar=1.0, in1=nbc[:, D:2 * D], op0=ALU.add, op1=ALU.mult), reads=['MODL', 'nbc1'], writes=['MODL'])

                PB = [s1.enter_context(nc.psum_tensor("pbB%d" % i, [128, 8, 128], BF16)) for i in range(2)]
                xt = [S("xt%d" % i, [128, D]) for i in range(2)]
                junk = S("junk", [128, D])
                hb = [S("hb%d" % i, [128, D], BF16) for i in range(2)]
                ssq = S("ssq", [128, 18])
                rs = S("rs", [128, 18])
                for i in range(18):
                    xb = xt[i % 2]
                    xtk = 'xt%d' % (i % 2)
                    hbb = hb[i % 2]
                    hbk = 'hb%d' % (i % 2)
                    src = x_d[i * 128:(i + 1) * 128, :] if i < 16 else ctx_d[(i - 16) * 128:(i - 15) * 128, :]
                    M = MODL if i < 16 else MODC
                    Mk = 'MODL' if i < 16 else 'MODC'
                    dma('sp', lambda e, xb=xb, src=src: e.dma_start(out=xb[:], in_=src), writes=[xtk])
                    op('act', lambda e, xb=xb, i=i: e.activation(out=junk[:], in_=xb[:], func=AF.Square, accum_out=ssq[:, i:i + 1]), reads=[xtk], writes=['junk', 'ssq%d' % i])
                    rstd_from_ssq(ssq[:, i:i + 1], rs[:, i:i + 1], 1.0 / D, ['ssq%d' % i], ['rs%d' % i])
                    op('dve', lambda e, xb=xb, i=i, M=M: e.scalar_tensor_tensor(out=junk[:], in0=xb[:], scalar=rs[:, i:i + 1], in1=M[:, D:2 * D], op0=ALU.mult, op1=ALU.mult),
                       reads=[xtk, 'rs%d' % i, Mk], writes=['junk'])
                    op('dve', lambda e, hbb=hbb, M=M: e.tensor_tensor(out=hbb[:], in0=junk[:], in1=M[:, 0:D], op=ALU.add), reads=['junk', Mk], writes=[hbk])
                    pb = PB[i % 2]
                    pbk = 'pbB%d' % (i % 2)
                    for k in range(8):
                        op('pe', lambda e, pb=pb, hbb=hbb, k=k: e.transpose(out=pb[:, k, :], in_=hbb[:, k * 128:(k + 1) * 128], identity=identb[:]), reads=[hbk, 'identb'], writes=[pbk])
                    op('act', lambda e, pb=pb, i=i: e.copy(out=hT[:, :, i * 128:(i + 1) * 128], in_=pb[:]), reads=[pbk], writes=[('hT', i)])
                kb.barrier()
                if stop == 'B':
                    return fin()
            hT_all = [('hT', i) for i in range(18)]
            if dbg:
                with ExitStack() as s1:
                    tmp = s1.enter_context(nc.sbuf_tensor("dbgt", [128, 8 * NT], F32))
                    op('dve', lambda e: e.tensor_copy(out=tmp[:], in_=hT[:].rearrange("p k n -> p (k n)")), reads=hT_all, writes=['dbgt'])
                    dma('sp', lambda e: e.dma_start(out=dbg_d["d_hT"][:, :], in_=tmp[:]), reads=['dbgt'], writes=['d_hT'])
                    kb.barrier()

            with ExitStack() as s1:
                S = lambda name, shape, dt=F32: s1.enter_context(nc.sbuf_tensor("t_" + name, shape, dt))
                PF = [s1.enter_context(nc.psum_tensor("pfC%d" % i, [128, 512], F32)) for i in range(8)]
                wna = S("wna", [128, 8, 1536], BF16)
                qT = S("qT", [128, 4, T], BF16)
                kT = S("kT", [128, 4, NT], BF16)
                V = S("V", [128, 18, 512], BF16)
                onesb = S("onesbC", [128, 64], BF16)
                tb2 = [S("tb2_%d" % i, [128, 2, 1024], BF16) for i in range(2)]
                PT = [S("PT%d" % i, [128, 256], BF16) for i in range(4)]
                rcp = [S("rcp%d" % i, [128, 256]) for i in range(2)]
                op('dve', lambda e: e.memset(onesb[:], 1.0), writes=['onesbC'])
                for j in range(3):
                    dma('pool', lambda e, j=j: e.dma_start(out=wna[:, :, j * 512:(j + 1) * 512], in_=winv[:, :, j * 512:(j + 1) * 512]), writes=[('wna', j)])
                pfi = [0]

                def nextpf():
                    i = pfi[0] % 8
                    pfi[0] += 1
                    return PF[i], 'pfC%d' % i
                for which in range(2):
                    for p in range(4):
                        ntb = 4 if which == 0 else 5
                        for tb in range(ntb):
                            n0 = tb * 512
                            nn = 512 if tb < 4 else 256
                            ps, pk = nextpf()
                            for k in range(8):
                                op('pe', lambda e, ps=ps, k=k, which=which, p=p, n0=n0, nn=nn: e.matmul(out=ps[:, 0:nn], lhsT=wna[:, k, which * 512 + p * 128: which * 512 + (p + 1) * 128],
                                                                                                     rhs=hT[:, k, n0:n0 + nn], start=(k == 0), stop=(k == 7)),
                                   reads=[('wna', which)] + [('hT', t) for t in range(n0 // 128, (n0 + nn) // 128)], writes=[pk])
                            if which == 0:
                                op('act', lambda e, ps=ps, p=p, n0=n0: e.mul(out=qT[:, p, n0:n0 + 512], in_=ps[:], mul=0.125), reads=[pk], writes=[('qT', p, tb)])
                            else:
                                op('dve', lambda e, ps=ps, p=p, n0=n0, nn=nn: e.tensor_copy(out=kT[:, p, n0:n0 + nn], in_=ps[:, 0:nn]), reads=[pk], writes=[('kT', p, tb)])
                for i in range(18):
                    ps, pk = nextpf()
                    for k in range(8):
                        op('pe', lambda e, ps=ps, k=k, i=i: e.matmul(out=ps[:], lhsT=hT[:, k, i * 128:(i + 1) * 128], rhs=wna[:, k, 1024:1536], start=(k == 0), stop=(k == 7)),
                           reads=[('wna', 2), ('hT', i)], writes=[pk])
                    op('act', lambda e, ps=ps, i=i: e.copy(out=V[:, i, :], in_=ps[:]), reads=[pk], writes=[('V', i)])
                PST = PF[0:4]
                PPV = PF[4:6]
                PDN = PF[6:8]
                cnt = 0
                for h in range(8):
                    p, half = h // 2, h % 2
                    hs = slice(half * 64, half * 64 + 64)
                    tbt = tb2[h % 2]
                    tbk = 'tb2_%d' % (h % 2)
                    dma('pool', lambda e, tbt=tbt, h=h: e.dma_start(out=tbt[:], in_=natbl_d[h].rearrange("t p n -> p t n")), writes=[tbk])
                    for qb in range(8):
                        if qb == 0:
                            tiles, tsel = list(range(0, 4)), 0
                        elif qb == 7:
                            tiles, tsel = list(range(12, 16)), 0
                        else:
                            tiles, tsel = list(range(2 * qb - 2, 2 * qb + 4)), 1
                        keyt = [(m, 4 * qb - 2 * m + 7) for m in tiles] + [(16, None), (17, None)]
                        ppv, pdn = PPV[qb % 2], PDN[qb % 2]
                        ppk, pdk = 'pfC%d' % (4 + qb % 2), 'pfC%d' % (6 + qb % 2)
                        q0 = qb * 256
                        for ti, (m, j0) in enumerate(keyt):
                            pst, pstk = PST[cnt % 4], 'pfC%d' % (cnt % 4)
                            ptt, ptk = PT[cnt % 4], 'PT%d' % (cnt % 4)
                            cnt += 1
                            op('pe', lambda e, pst=pst, m=m, j0=j0: e.matmul(out=pst[:, 0:256], lhsT=kT[hs, p, m * 128:(m + 1) * 128], rhs=qT[hs, p, q0:q0 + 256], start=True, stop=(j0 is None)),
                               reads=[('kT', p, min(m // 4, 4)), ('qT', p, qb // 2)], writes=[pstk])
                            if j0 is not None:
                                op('pe', lambda e, pst=pst, j0=j0: e.matmul(out=pst[:, 0:256], lhsT=identb[:], rhs=tbt[:, tsel, j0 * 64:(j0 + 4) * 64], start=False, stop=True),
                                   reads=[tbk, 'identb'], writes=[pstk])
                            op('act', lambda e, pst=pst, ptt=ptt: e.activation(out=ptt[:], in_=pst[:, 0:256], func=AF.Exp), reads=[pstk], writes=[ptk])
                            first, last = ti == 0, ti == len(keyt) - 1
                            op('pe', lambda e, ptt=ptt, m=m, first=first, last=last: e.matmul(out=ppv[hs, 0:256], lhsT=V[:, m, h * 64:(h + 1) * 64], rhs=ptt[:], start=first, stop=last),
                               reads=[ptk, ('V', m)], writes=[ppk])
                            op('pe', lambda e, ptt=ptt, first=first, last=last: e.matmul(out=pdn[hs, 0:256], lhsT=onesb[:], rhs=ptt[:], start=first, stop=last),
                               reads=[ptk, 'onesbC'], writes=[pdk])
                        rc = rcp[qb % 2]
                        rck = 'rcp%d' % (qb % 2)
                        op('dve', lambda e, rc=rc, pdn=pdn: e.reciprocal(out=rc[hs, :], in_=pdn[hs, 0:256]), reads=[pdk], writes=[rck])
                        op('dve', lambda e, rc=rc, ppv=ppv: e.tensor_tensor(out=onaT[hs, p, q0:q0 + 256], in0=ppv[hs, 0:256], in1=rc[hs, :], op=ALU.mult), reads=[ppk, rck], writes=[('onaT', p, qb)])
                kb.barrier()
                if stop == 'C':
                    return fin()
            onaT_all = [('onaT', p, qb) for p in range(4) for qb in range(8)]
            if dbg:
                with ExitStack() as s1:
                    tmp = s1.enter_context(nc.sbuf_tensor("dbgt2", [128, 4 * T], F32))
                    op('dve', lambda e: e.tensor_copy(out=tmp[:], in_=onaT[:].rearrange("p k n -> p (k n)")), reads=onaT_all, writes=['dbgt2'])
                    dma('sp', lambda e: e.dma_start(out=dbg_d["d_ona"][:, :], in_=tmp[:]), reads=['dbgt2'], writes=['d_ona'])
                    kb.barrier()

            id64 = cst[0:64, C_ID:C_ID + 64]
            on64 = cst[0:64, C_ONE:C_ONE + 64]
            b3 = lambda ap8: ap8.unsqueeze(2).to_broadcast([64, 8, 64])
            m3 = lambda ap64: ap64.unsqueeze(1).to_broadcast([64, 8, 64])
            with ExitStack() as s1:
                S = lambda name, shape, dt=F32: s1.enter_context(nc.sbuf_tensor("t_" + name, shape, dt))
                PF = [s1.enter_context(nc.psum_tensor("pfD%d" % i, [128, 512], F32)) for i in range(8)]
                pfi = [0]

                def nextpf():
                    i = pfi[0] % 8
                    pfi[0] += 1
                    return PF[i], 'pfD%d' % i
                wdn = S("wdn", [128, 8, 1536], BF16)
                wba = S("wba", [128, 8, 32], BF16)
                convw = S("convw", [128, 60])
                ropet = S("ropet", [64, 2, 32, 32])
                zl = [S("zl%d" % i, [128, 2052]) for i in range(2)]
                zx = [S("zx%d" % i, [128, 260]) for i in range(2)]
                acc = S("acc", [128, NT])
                sl = S("sl", [128, NT])
                sqt = S("sqt", [64, 512])
                ssq8 = S("ssq8", [64, 8])
                r8 = S("r8", [64, 8])
                st = [S("st%d" % i, [64, 4, 128]) for i in range(2)]
                st2 = [S("st2%d" % i, [64, 4, 128]) for i in range(2)]
                ra = S("ra", [64, 4, 2, 32])
                rb = S("rb", [64, 4, 2, 32])
                BA = S("BA", [64, 36, 32])
                BETA = S("BETA", [64, 36, 16])
                GG = S("GG", [64, 36, 16])
                ta = S("ta", [64, 36, 16])
                tb_ = S("tb_", [64, 36, 16])
                tc_ = S("tc_", [64, 36, 16])
                ab = S("ab", [64, 32])
                one1 = S("one1", [64, 1])
                for j in range(3):
                    dma('pool', lambda e, j=j: e.dma_start(out=wdn[:, :, j * 512:(j + 1) * 512], in_=winv[:, :, 1536 + j * 512:1536 + (j + 1) * 512]), writes=[('wdn', j)])
                dma('pool', lambda e: e.dma_start(out=wba[:], in_=winv[:, :, 3584:3616]), writes=['wba'])
                dma('sp', lambda e: e.dma_start(out=convw[:], in_=convw_d[:, :]), writes=['convw'])
                dma('sp', lambda e: e.dma_start(out=ropet[:], in_=rope_d.rearrange("p (a c f) -> p a c f", a=2, c=32)), writes=['ropet'])
                dma('sp', lambda e: e.dma_start(out=ab[:, 0:16], in_=alog_d[0:1, :].partition_broadcast(64)), writes=['ab'])
                dma('sp', lambda e: e.dma_start(out=ab[:, 16:32], in_=dtb_d[0:1, :].partition_broadcast(64)), reads=[], writes=['ab2'])
                op('dve', lambda e: e.memset(one1[:], 1.0), writes=['one1'])
                for i in range(2):
                    op('dve', lambda e, i=i: e.memset(zl[i][:], 0.0), writes=['zl%d' % i])
                    op('dve', lambda e, i=i: e.memset(zx[i][:], 0.0), writes=['zx%d' % i])
                for g3 in range(3):
                    ps, pk = nextpf()
                    for c in range(12):
                        n = g3 * 12 + c
                        for k in range(8):
                            op('pe', lambda e, ps=ps, c=c, n=n, k=k: e.matmul(out=ps[0:64, c * 32:(c + 1) * 32], lhsT=hT[:, k, n * 64:(n + 1) * 64], rhs=wba[:, k, :], start=(k == 0), stop=(k == 7)),
                               reads=['wba', ('hT', n // 2)], writes=[pk])
                    op('act', lambda e, ps=ps, g3=g3: e.copy(out=BA[:, g3 * 12:(g3 + 1) * 12, :], in_=ps[0:64, 0:384].rearrange("p (c f) -> p c f", f=32)), reads=[pk], writes=['BA'])
                op('act', lambda e: e.activation(out=BETA[:], in_=BA[:, :, 0:16], func=AF.Sigmoid), reads=['BA'], writes=['BETA'])
                op('dve', lambda e: e.tensor_tensor(out=ta[:], in0=BA[:, :, 16:32], in1=ab[:, 16:32].unsqueeze(1).to_broadcast([64, 36, 16]), op=ALU.add), reads=['BA', 'ab2'], writes=['ta'])
                op('dve', lambda e: e.tensor_scalar(out=tb_[:], in0=ta[:], scalar1=-1.0, scalar2=None, op0=ALU.mult), reads=['ta'], writes=['tb_'])
                op('dve', lambda e: e.tensor_tensor(out=tb_[:], in0=tb_[:], in1=ta[:], op=ALU.max), reads=['ta', 'tb_'], writes=['tb_'])
                op('act', lambda e: e.activation(out=tb_[:], in_=tb_[:], func=AF.Exp, scale=-1.0), reads=['tb_'], writes=['tb_'])
                op('act', lambda e: e.activation(out=tb_[:], in_=tb_[:], func=AF.Ln, bias=one1[:]), reads=['tb_', 'one1'], writes=['tb_'])
                op('dve', lambda e: e.tensor_scalar_max(out=tc_[:], in0=ta[:], scalar1=0.0), reads=['ta'], writes=['tc_'])
                op('dve', lambda e: e.tensor_tensor(out=tc_[:], in0=tc_[:], in1=tb_[:], op=ALU.add), reads=['tc_', 'tb_'], writes=['tc_'])
                op('act', lambda e: e.activation(out=ab[:, 0:16], in_=ab[:, 0:16], func=AF.Exp), reads=['ab'], writes=['ab'])
                op('dve', lambda e: e.scalar_tensor_tensor(out=GG[:], in0=tc_[:], scalar=-1.0, in1=ab[:, 0:16].unsqueeze(1).to_broadcast([64, 36, 16]), op0=ALU.mult, op1=ALU.mult),
                   reads=['tc_', 'ab'], writes=['GG'])
                for cc in range(12):
                    z_l, z_x = zl[cc % 2], zx[cc % 2]
                    zlk, zxk = 'zl%d' % (cc % 2), 'zx%d' % (cc % 2)
                    for tb in range(5):
                        n0 = tb * 512
                        nn = 512 if tb < 4 else 256
                        ps, pk = nextpf()
                        for k in range(8):
                            op('pe', lambda e, ps=ps, k=k, n0=n0, nn=nn: e.matmul(out=ps[:, 0:nn], lhsT=wdn[:, k, cc * 128:(cc + 1) * 128], rhs=hT[:, k, n0:n0 + nn], start=(k == 0), stop=(k == 7)),
                               reads=[('wdn', cc // 4)] + [('hT', t) for t in range(n0 // 128, (n0 + nn) // 128)], writes=[pk])
                        if tb < 4:
                            op('act', lambda e, ps=ps, n0=n0: e.copy(out=z_l[:, 2 + n0:2 + n0 + 512], in_=ps[:]), reads=[pk], writes=[zlk])
                        else:
                            op('act', lambda e, ps=ps: e.copy(out=z_x[:, 2:258], in_=ps[:, 0:256]), reads=[pk], writes=[zxk])
                    for (zs, zk, a0, n) in ((z_l, zlk, 0, 2048), (z_x, zxk, 2048, 256)):
                        op('dve', lambda e, zs=zs, a0=a0, n=n: e.tensor_scalar(out=acc[:, a0:a0 + n], in0=zs[:, 0:n], scalar1=convw[:, cc * 5:cc * 5 + 1], scalar2=None, op0=ALU.mult),
                           reads=[zk, 'convw'], writes=['acc'])
                        for tap in range(1, 5):
                            op('dve', lambda e, zs=zs, a0=a0, n=n, tap=tap: e.scalar_tensor_tensor(out=acc[:, a0:a0 + n], in0=zs[:, tap:tap + n], scalar=convw[:, cc * 5 + tap:cc * 5 + tap + 1],
                                                                                               in1=acc[:, a0:a0 + n], op0=ALU.mult, op1=ALU.add), reads=[zk, 'convw', 'acc'], writes=['acc'])
                    op('act', lambda e: e.activation(out=sl[:], in_=acc[:], func=AF.Silu), reads=['acc'], writes=['sl'])
                    for g in range(9):
                        ps, pk = nextpf()
                        for c4 in range(4):
                            n = 4 * g + c4
                            op('pe', lambda e, ps=ps, c4=c4, n=n: e.transpose(out=ps[0:64, c4 * 128:(c4 + 1) * 128], in_=sl[:, n * 64:(n + 1) * 64], identity=ident), reads=['sl', 'cst'], writes=[pk])
                        sti, stk = st[g % 2], 'st%d' % (g % 2)
                        if cc < 8:
                            op('act', lambda e, ps=ps: e.activation(out=sqt[:], in_=ps[0:64, :], func=AF.Square), reads=[pk], writes=['sqt'])
                            op('dve', lambda e: e.tensor_reduce(out=ssq8[:], in_=sqt[:].rearrange("p (g f) -> p g f", f=64), axis=AX.X, op=ALU.add), reads=['sqt'], writes=['ssq8'])
                            rstd_from_ssq(ssq8[:], r8[:], 1.0, ['ssq8'], ['r8'])
                            op('dve', lambda e, ps=ps, sti=sti: e.tensor_tensor(out=sti[:].rearrange("p c (h f) -> p (c h) f", h=2), in0=ps[0:64, :].rearrange("p (g f) -> p g f", f=64),
                                                                            in1=b3(r8[:]), op=ALU.mult), reads=[pk, 'r8'], writes=[stk])
                            if g < 8:
                                s2i, s2k = st2[g % 2], 'st2%d' % (g % 2)
                                x5 = sti[:].rearrange("p c (h t f) -> p c h t f", h=2, t=2)
                                o5 = s2i[:].rearrange("p c (h t f) -> p c h t f", h=2, t=2)
                                cosb = ropet[:, 0, 4 * g:4 * g + 4, :].unsqueeze(2).to_broadcast([64, 4, 2, 32])
                                sinb = ropet[:, 1, 4 * g:4 * g + 4, :].unsqueeze(2).to_broadcast([64, 4, 2, 32])
                                op('dve', lambda e: e.tensor_tensor(out=ra[:], in0=x5[:, :, :, 0, :], in1=cosb, op=ALU.mult), reads=[stk, 'ropet'], writes=['ra'])
                                op('pool', lambda e: e.tensor_tensor(out=rb[:], in0=x5[:, :, :, 1, :], in1=sinb, op=ALU.mult), reads=[stk, 'ropet'], writes=['rb'])
                                op('dve', lambda e: e.tensor_tensor(out=o5[:, :, :, 0, :], in0=ra[:], in1=rb[:], op=ALU.subtract), reads=['ra', 'rb'], writes=[s2k])
                                op('dve', lambda e: e.tensor_tensor(out=ra[:], in0=x5[:, :, :, 0, :], in1=sinb, op=ALU.mult), reads=[stk, 'ropet'], writes=['ra'])
                                op('pool', lambda e: e.tensor_tensor(out=rb[:], in0=x5[:, :, :, 1, :], in1=cosb, op=ALU.mult), reads=[stk, 'ropet'], writes=['rb'])
                                op('dve', lambda e: e.tensor_tensor(out=o5[:, :, :, 1, :], in0=ra[:], in1=rb[:], op=ALU.add), reads=['ra', 'rb'], writes=[s2k])
                                sti, stk = s2i, s2k
                        else:
                            op('act', lambda e, ps=ps, sti=sti: e.copy(out=sti[:].rearrange("p c f -> p (c f)"), in_=ps[0:64, :]), reads=[pk], writes=[stk])
                        dma('sp', lambda e, sti=sti, g=g: e.dma_start(out=qkv_s[4 * g:4 * g + 4, :, cc * 128:(cc + 1) * 128].rearrange("c p f -> p c f"), in_=sti[:]), reads=[stk], writes=[('qkv_s', g)])
                kb.barrier()
                if stop == 'D1':
                    return fin()
            with ExitStack() as s1:
                S = lambda name, shape, dt=F32: s1.enter_context(nc.sbuf_tensor("t_" + name, shape, dt))
                PF = [s1.enter_context(nc.psum_tensor("pfE%d" % i, [128, 512], F32)) for i in range(8)]
                pfi = [0]

                def nextpf():
                    i = pfi[0] % 8
                    pfi[0] += 1
                    return PF[i], 'pfE%d' % i
                wba = S("wba2", [128, 8, 32], BF16)
                BA = S("BA2", [64, 36, 32])
                BETA = S("BETA2", [64, 36, 16])
                GG = S("GG2", [64, 36, 16])
                ta = S("ta2", [64, 36, 16])
                tb_ = S("tb2_", [64, 36, 16])
                tc_ = S("tc2_", [64, 36, 16])
                ab = S("ab2", [64, 32])
                one1 = S("one12", [64, 1])
                dma('pool', lambda e: e.dma_start(out=wba[:], in_=winv[:, :, 3584:3616]), writes=['wba'])
                dma('sp', lambda e: e.dma_start(out=ab[:, 0:16], in_=alog_d[0:1, :].partition_broadcast(64)), writes=['ab'])
                dma('sp', lambda e: e.dma_start(out=ab[:, 16:32], in_=dtb_d[0:1, :].partition_broadcast(64)), reads=[], writes=['ab2'])
                op('dve', lambda e: e.memset(one1[:], 1.0), writes=['one1'])
                for g3 in range(3):
                    ps, pk = nextpf()
                    for c in range(12):
                        n = g3 * 12 + c
                        for k in range(8):
                            op('pe', lambda e, ps=ps, c=c, n=n, k=k: e.matmul(out=ps[0:64, c * 32:(c + 1) * 32], lhsT=hT[:, k, n * 64:(n + 1) * 64], rhs=wba[:, k, :], start=(k == 0), stop=(k == 7)),
                               reads=['wba', ('hT', n // 2)], writes=[pk])
                    op('act', lambda e, ps=ps, g3=g3: e.copy(out=BA[:, g3 * 12:(g3 + 1) * 12, :], in_=ps[0:64, 0:384].rearrange("p (c f) -> p c f", f=32)), reads=[pk], writes=['BA'])
                op('act', lambda e: e.activation(out=BETA[:], in_=BA[:, :, 0:16], func=AF.Sigmoid), reads=['BA'], writes=['BETA'])
                op('dve', lambda e: e.tensor_tensor(out=ta[:], in0=BA[:, :, 16:32], in1=ab[:, 16:32].unsqueeze(1).to_broadcast([64, 36, 16]), op=ALU.add), reads=['BA', 'ab2'], writes=['ta'])
                op('dve', lambda e: e.tensor_scalar(out=tb_[:], in0=ta[:], scalar1=-1.0, scalar2=None, op0=ALU.mult), reads=['ta'], writes=['tb_'])
                op('dve', lambda e: e.tensor_tensor(out=tb_[:], in0=tb_[:], in1=ta[:], op=ALU.max), reads=['ta', 'tb_'], writes=['tb_'])
                op('act', lambda e: e.activation(out=tb_[:], in_=tb_[:], func=AF.Exp, scale=-1.0), reads=['tb_'], writes=['tb_'])
                op('act', lambda e: e.activation(out=tb_[:], in_=tb_[:], func=AF.Ln, bias=one1[:]), reads=['tb_', 'one1'], writes=['tb_'])
                op('dve', lambda e: e.tensor_scalar_max(out=tc_[:], in0=ta[:], scalar1=0.0), reads=['ta'], writes=['tc_'])
                op('dve', lambda e: e.tensor_tensor(out=tc_[:], in0=tc_[:], in1=tb_[:], op=ALU.add), reads=['tc_', 'tb_'], writes=['tc_'])
                op('act', lambda e: e.activation(out=ab[:, 0:16], in_=ab[:, 0:16], func=AF.Exp), reads=['ab'], writes=['ab'])
                op('dve', lambda e: e.scalar_tensor_tensor(out=GG[:], in0=tc_[:], scalar=-1.0, in1=ab[:, 0:16].unsqueeze(1).to_broadcast([64, 36, 16]), op0=ALU.mult, op1=ALU.mult),
                   reads=['tc_', 'ab'], writes=['GG'])

                ld = [S("ld%d" % i, [64, 1536]) for i in range(2)]
                names = ["gc", "gl", "et", "egl", "eg", "bg", "nb", "t8"]
                sm = {n_: S("sm_" + n_, [64, 8]) for n_ in names}
                bigs = ["Rt", "egrow", "Dm", "Dmi", "Dms", "qdT", "NegA", "QKm", "X0", "QKT", "Z", "Xa", "XTa", "Xb", "XTb", "vb", "kbg", "ktail", "u", "wT", "vnew", "Sst", "ost"]
                bigs = bigs + ["NegAb", "Zb", "Sb"]
                NB16 = ("X0", "Xa", "Xb", "XTa", "XTb", "NegAb", "Zb", "Sb", "qdT", "QKT", "vb", "kbg", "ktail", "wT", "vnew")
                bg_ = {n_: S("bg_" + n_, [64, 8, 64], BF16 if n_ in NB16 else F32) for n_ in bigs}
                qkT = S("qkT", [64, 16, 64], BF16)
                fl = lambda t_: t_[:].rearrange("p h f -> p (h f)")

                def headmm(lhs, rhs, reads, start=True, stop=True, ps=None, pk=None):
                    if ps is None:
                        ps, pk = nextpf()
                    for h in range(8):
                        op('pe', lambda e, h=h: e.matmul(out=ps[0:64, h * 64:(h + 1) * 64], lhsT=lhs(h), rhs=rhs(h), start=start, stop=stop), reads=reads, writes=[pk])
                    return ps, pk

                def headtr(src, srck):
                    ps, pk = nextpf()
                    for h in range(8):
                        op('pe', lambda e, h=h: e.transpose(out=ps[0:64, h * 64:(h + 1) * 64], in_=src(h), identity=id64), reads=[srck, 'cst'], writes=[pk])
                    return ps, pk
                p3 = lambda ps: ps[0:64, :].rearrange("p (h f) -> p h f", f=64)
                B = bg_
                if sub == 'g':
                    kb.barrier()
                    return fin()
                for d in range(2 if sub is None else 1):
                    CU = cst[0:64, C_U:C_U + 64] if d == 0 else cst[0:64, C_L:C_L + 64]
                    incl = cst[0:64, C_IF:C_IF + 64] if d == 0 else cst[0:64, C_IB:C_IB + 64]
                    strict = cst[0:64, C_SF:C_SF + 64] if d == 0 else cst[0:64, C_SB:C_SB + 64]
                    last = 63 if d == 0 else 0
                    order = ([32, 33, 34, 35] + list(range(32))) if d == 0 else ([35, 34, 33, 32] + list(range(31, -1, -1)))
                    op('dve', lambda e: e.memset(B["Sst"][:], 0.0), writes=['Sst'])
                    op('dve', lambda e: e.memset(B["Sb"][:], 0.0), writes=['Sb'])
                    for it, n in enumerate(order):
                        L_, lk = ld[it % 2], 'ld%d' % (it % 2)

                        def issue_load(it_):
                            n_, Lb, lkb = order[it_], ld[it_ % 2], 'ld%d' % (it_ % 2)
                            dma('sp', lambda e: e.dma_start(out=Lb[:], in_=qkv_s[n_, :, :]), reads=[('qkv_s', n_ // 4)], writes=[lkb])
                        if it == 0:
                            issue_load(0)
                        if it + 1 < len(order):
                            issue_load(it + 1)
                        q3 = L_[:, 0:512].rearrange("p (h f) -> p h f", f=64)
                        k3 = L_[:, 512:1024].rearrange("p (h f) -> p h f", f=64)
                        v3 = L_[:, 1024:1536].rearrange("p (h f) -> p h f", f=64)
                        g = GG[:, n, d * 8:(d + 1) * 8]
                        beta = BETA[:, n, d * 8:(d + 1) * 8]
                        ps, pk = nextpf()
                        op('pe', lambda e, ps=ps: e.matmul(out=ps[0:64, 0:8], lhsT=CU, rhs=g, start=True, stop=True), reads=['GG', 'cst'], writes=[pk])
                        op('dve', lambda e, ps=ps: e.tensor_copy(out=sm["gc"][:], in_=ps[0:64, 0:8]), reads=[pk], writes=['gc'])
                        if sub == 's1':
                            kb.barrier()
                            return fin()
                        op('dve', lambda e: e.tensor_tensor(out=B["Rt"][:], in0=m3(id64), in1=b3(sm["gc"][:]), op=ALU.mult), reads=['gc', 'cst'], writes=['Rt'])
                        psA, pkA = nextpf()
                        op('pe', lambda e: e.matmul(out=psA[0:64, :], lhsT=on64, rhs=fl(B["Rt"]), start=True, stop=True), reads=['Rt', 'cst'], writes=[pkA])
                        op('act', lambda e: e.activation(out=fl(B["egrow"]), in_=psA[0:64, :], func=AF.Exp), reads=[pkA], writes=['egrow'])
                        op('dve', lambda e: e.tensor_tensor(out=B["Dm"][:], in0=b3(sm["gc"][:]), in1=p3(psA), op=ALU.subtract), reads=[pkA, 'gc'], writes=['Dm'])
                        op('act', lambda e: e.copy(out=sm["gl"][:], in_=p3(psA)[:, :, last]), reads=[pkA], writes=['gl'])
                        op('dve', lambda e: e.tensor_scalar_min(out=B["Dm"][:], in0=B["Dm"][:], scalar1=0.0), reads=['Dm'], writes=['Dm'])
                        op('act', lambda e: e.activation(out=B["Dm"][:], in_=B["Dm"][:], func=AF.Exp), reads=['Dm'], writes=['Dm'])
                        op('pool', lambda e: e.tensor_tensor(out=B["Dmi"][:], in0=B["Dm"][:], in1=m3(incl), op=ALU.mult), reads=['Dm', 'cst'], writes=['Dmi'])
                        op('pool', lambda e: e.tensor_tensor(out=B["Dms"][:], in0=B["Dm"][:], in1=m3(strict), op=ALU.mult), reads=['Dm', 'cst'], writes=['Dms'])
                        op('dve', lambda e: e.tensor_tensor(out=sm["t8"][:], in0=sm["gl"][:], in1=sm["gc"][:], op=ALU.subtract), reads=['gl', 'gc'], writes=['t8'])
                        op('act', lambda e: e.activation(out=sm["et"][:], in_=sm["t8"][:], func=AF.Exp), reads=['t8'], writes=['et'])
                        op('act', lambda e: e.activation(out=sm["egl"][:], in_=sm["gl"][:], func=AF.Exp), reads=['gl'], writes=['egl'])
                        op('act', lambda e: e.activation(out=sm["eg"][:], in_=sm["gc"][:], func=AF.Exp), reads=['gc'], writes=['eg'])
                        op('dve', lambda e: e.tensor_tensor(out=sm["bg"][:], in0=sm["eg"][:], in1=beta, op=ALU.mult), reads=['eg', 'BETA'], writes=['bg'])
                        op('dve', lambda e: e.tensor_scalar(out=sm["nb"][:], in0=beta, scalar1=-1.0, scalar2=None, op0=ALU.mult), reads=['BETA'], writes=['nb'])
                        if sub == 's2':
                            kb.barrier()
                            return fin()
                        psQ, pkQ = headtr(lambda h: q3[:, h, :], lk)
                        psK, pkK = headtr(lambda h: k3[:, h, :], lk)
                        op('act', lambda e: e.copy(out=qkT[:, 0:8, :], in_=p3(psQ)), reads=[pkQ], writes=['qT_'])
                        op('dve', lambda e: e.tensor_copy(out=qkT[:, 8:16, :], in_=p3(psK)), reads=[pkK], writes=['kT_'])
                        op('dve', lambda e: e.scalar_tensor_tensor(out=B["qdT"][:], in0=qkT[:, 0:8, :], scalar=0.125, in1=B["egrow"][:], op0=ALU.mult, op1=ALU.mult), reads=['qT_', 'egrow'], writes=['qdT'])
                        if sub == 's3':
                            kb.barrier()
                            return fin()
                        psB, pkB = headmm(lambda h: qkT[:, 8 + h, :], lambda h: qkT[:, 8 + h, :], ['kT_'])
                        psC, pkC = headmm(lambda h: qkT[:, h, :], lambda h: qkT[:, 8 + h, :], ['kT_', 'qT_'])
                        op('dve', lambda e: e.tensor_tensor(out=B["NegA"][:], in0=p3(psB), in1=B["Dms"][:], op=ALU.mult), reads=[pkB, 'Dms'], writes=['NegA'])
                        op('dve', lambda e: e.tensor_tensor(out=B["NegA"][:], in0=B["NegA"][:], in1=b3(sm["nb"][:]), op=ALU.mult), reads=['NegA', 'nb'], writes=['NegA'])
                        op('act', lambda e: e.copy(out=B["NegAb"][:], in_=B["NegA"][:]), reads=['NegA'], writes=['NegAb'])
                        op('dve', lambda e: e.scalar_tensor_tensor(out=B["QKm"][:], in0=p3(psC), scalar=0.125, in1=B["Dmi"][:], op0=ALU.mult, op1=ALU.mult), reads=[pkC, 'Dmi'], writes=['QKm'])
                        if sub == 's4':
                            kb.barrier()
                            return fin()
                        psD, pkD = headtr(lambda h: B["NegA"][:, h, :], 'NegA')
                        psE, pkE = headtr(lambda h: B["QKm"][:, h, :], 'QKm')
                        op('act', lambda e: e.copy(out=B["X0"][:], in_=p3(psD)), reads=[pkD], writes=['X0'])
                        op('dve', lambda e: e.tensor_tensor(out=B["Z"][:], in0=p3(psD), in1=m3(id64), op=ALU.add), reads=[pkD, 'cst'], writes=['Z'])
                        op('act', lambda e: e.copy(out=B["Zb"][:], in_=B["Z"][:]), reads=['Z'], writes=['Zb'])
                        op('act', lambda e: e.copy(out=B["QKT"][:], in_=p3(psE)), reads=[pkE], writes=['QKT'])
                        if sub == 's5':
                            kb.barrier()
                            return fin()
                        X, Xk, XT, XTk = B["X0"], 'X0', B["NegAb"], 'NegAb'
                        for lvl in range(1, 6):
                            nX, nXk = (B["Xa"], 'Xa') if lvl % 2 == 1 else (B["Xb"], 'Xb')
                            nXT, nXTk = (B["XTa"], 'XTa') if lvl % 2 == 1 else (B["XTb"], 'XTb')
                            ps1, pk1 = headmm(lambda h, X=X: X[:, h, :], lambda h, XT=XT: XT[:, h, :], [Xk, XTk])
                            op('act', lambda e, ps1=ps1, nXT=nXT: e.copy(out=nXT[:], in_=p3(ps1)), reads=[pk1], writes=[nXTk])
                            if lvl < 5:
                                ps2, pk2 = headmm(lambda h, XT=XT: XT[:, h, :], lambda h, X=X: X[:, h, :], [Xk, XTk])
                                op('dve', lambda e, ps2=ps2, nX=nX: e.tensor_copy(out=nX[:], in_=p3(ps2)), reads=[pk2], writes=[nXk])
                            ps3, pk3 = headmm(lambda h, nXT=nXT: nXT[:, h, :], lambda h: B["Zb"][:, h, :], [nXTk, 'Zb'])
                            op('dve', lambda e, ps3=ps3: e.tensor_tensor(out=B["Z"][:], in0=B["Z"][:], in1=p3(ps3), op=ALU.add), reads=[pk3, 'Z'], writes=['Z'])
                            op('act', lambda e: e.copy(out=B["Zb"][:], in_=B["Z"][:]), reads=['Z'], writes=['Zb'])
                            X, Xk, XT, XTk = nX, nXk, nXT, nXTk
                        if sub == 's6':
                            kb.barrier()
                            return fin()
                        op('pool', lambda e: e.tensor_tensor(out=B["vb"][:], in0=v3, in1=b3(beta), op=ALU.mult), reads=[lk, 'BETA'], writes=['vb'])
                        op('pool', lambda e: e.tensor_tensor(out=B["kbg"][:], in0=k3, in1=b3(sm["bg"][:]), op=ALU.mult), reads=[lk, 'bg'], writes=['kbg'])
                        op('pool', lambda e: e.tensor_tensor(out=B["ktail"][:], in0=k3, in1=b3(sm["et"][:]), op=ALU.mult), reads=[lk, 'et'], writes=['ktail'])
                        psU, pkU = headmm(lambda h: B["Zb"][:, h, :], lambda h: B["vb"][:, h, :], ['Zb', 'vb'])
                        op('act', lambda e: e.copy(out=B["u"][:], in_=p3(psU)), reads=[pkU], writes=['u'])
                        psW, pkW = headmm(lambda h: B["kbg"][:, h, :], lambda h: B["Zb"][:, h, :], ['Zb', 'kbg'])
                        op('act', lambda e: e.copy(out=B["wT"][:], in_=p3(psW)), reads=[pkW], writes=['wT'])
                        if sub == 's7':
                            kb.barrier()
                            return fin()
                        psP, pkP = headmm(lambda h: B["wT"][:, h, :], lambda h: B["Sb"][:, h, :], ['wT', 'Sb'])
                        op('dve', lambda e: e.tensor_tensor(out=B["vnew"][:], in0=B["u"][:], in1=p3(psP), op=ALU.subtract), reads=[pkP, 'u'], writes=['vnew'])
                        if n < 32:
                            psO, pkO = nextpf()
                            for h in range(8):
                                op('pe', lambda e, h=h: e.matmul(out=psO[0:64, h * 64:(h + 1) * 64], lhsT=B["qdT"][:, h, :], rhs=B["Sb"][:, h, :], start=True, stop=False), reads=['qdT', 'Sb'], writes=[pkO])
                                op('pe', lambda e, h=h: e.matmul(out=psO[0:64, h * 64:(h + 1) * 64], lhsT=B["QKT"][:, h, :], rhs=B["vnew"][:, h, :], start=False, stop=True), reads=['QKT', 'vnew'], writes=[pkO])
                            op('act', lambda e: e.copy(out=B["ost"][:], in_=p3(psO)), reads=[pkO], writes=['ost'])
                            dma('sp', lambda e, n=n: e.dma_start(out=o_s[d, n, :, :], in_=fl(B["ost"])), reads=['ost'], writes=[('o_s', d, n)])
                        psS, pkS = headmm(lambda h: B["ktail"][:, h, :], lambda h: B["vnew"][:, h, :], ['ktail', 'vnew'])
                        op('dve', lambda e: e.tensor_tensor(out=B["Sst"][:], in0=B["Sst"][:], in1=b3(sm["egl"][:]), op=ALU.mult), reads=['Sst', 'egl'], writes=['Sst'])
                        op('dve', lambda e: e.tensor_tensor(out=B["Sst"][:], in0=B["Sst"][:], in1=p3(psS), op=ALU.add), reads=['Sst', pkS], writes=['Sst'])
                        op('act', lambda e: e.copy(out=B["Sb"][:], in_=B["Sst"][:]), reads=['Sst'], writes=['Sb'])
                        if sub is not None and sub.startswith('it') and it + 1 == int(sub[2:]):
                            kb.barrier()
                            return fin()
                kb.barrier()
                if stop == 'D2':
                    return fin()
            with ExitStack() as s1:
                S = lambda name, shape, dt=F32: s1.enter_context(nc.sbuf_tensor("t_" + name, shape, dt))
                PF = [s1.enter_context(nc.psum_tensor("pfF%d" % i, [128, 512], F32)) for i in range(4)]
                PB = [s1.enter_context(nc.psum_tensor("pbF%d" % i, [128, 4, 256], BF16)) for i in range(2)]
                wdg = S("wdg", [128, 8, 512], BF16)
                dnwb = S("dnwb", [64, 64])
                of = [S("of%d" % i, [64, 512]) for i in range(2)]
                ob = [S("ob%d" % i, [64, 512]) for i in range(2)]
                sq = S("sqF", [64, 512])
                s8 = S("s8F", [64, 8])
                r8 = S("r8F", [64, 8])
                sg = S("sgF", [64, 512])
                odb = [S("odb%d" % i, [64, 512], BF16) for i in range(2)]
                dma('pool', lambda e: e.dma_start(out=wdg[:], in_=winv[:, :, 3072:3584]), writes=['wdg'])
                dma('sp', lambda e: e.dma_start(out=dnwb[:], in_=dnw_d[0:1, :].partition_broadcast(64)), writes=['dnwb'])
                for n in range(32):
                    a, ak = of[n % 2], 'of%d' % (n % 2)
                    b_, bk = ob[n % 2], 'ob%d' % (n % 2)
                    dma('sp', lambda e, a=a, n=n: e.dma_start(out=a[:], in_=o_s[0, n, :, :]), reads=[('o_s', 0, n)], writes=[ak])
                    dma('sp', lambda e, b_=b_, n=n: e.dma_start(out=b_[:], in_=o_s[1, n, :, :]), reads=[('o_s', 1, n)], writes=[bk])
                    op('dve', lambda e, a=a, b_=b_: e.tensor_tensor(out=a[:], in0=a[:], in1=b_[:], op=ALU.add), reads=[ak, bk], writes=[ak])
                    op('act', lambda e, a=a: e.activation(out=sq[:], in_=a[:], func=AF.Square), reads=[ak], writes=['sqF'])
                    op('dve', lambda e: e.tensor_reduce(out=s8[:], in_=sq[:].rearrange("p (g f) -> p g f", f=64), axis=AX.X, op=ALU.add), reads=['sqF'], writes=['s8F'])
                    rstd_from_ssq(s8[:], r8[:], 1.0 / 64, ['s8F'], ['r8F'])
                    a3 = a[:].rearrange("p (g f) -> p g f", f=64)
                    op('dve', lambda e, a3=a3: e.tensor_tensor(out=a3, in0=a3, in1=b3(r8[:]), op=ALU.mult), reads=[ak, 'r8F'], writes=[ak])
                    op('pool', lambda e, a3=a3: e.tensor_tensor(out=a3, in0=a3, in1=m3(dnwb[:]), op=ALU.mult), reads=[ak, 'dnwb'], writes=[ak])
                    ps, pk = PF[n % 4], 'pfF%d' % (n % 4)
                    for k in range(8):
                        op('pe', lambda e, ps=ps, k=k, n=n: e.matmul(out=ps[0:64, :], lhsT=hT[:, k, n * 64:(n + 1) * 64], rhs=wdg[:, k, :], start=(k == 0), stop=(k == 7)), reads=['wdg', ('hT', n // 2)], writes=[pk])
                    op('act', lambda e, ps=ps: e.activation(out=sg[:], in_=ps[0:64, :], func=AF.Silu), reads=[pk], writes=['sgF'])
                    o_, ok_ = odb[n % 2], 'odb%d' % (n % 2)
                    op('dve', lambda e, a=a, o_=o_: e.tensor_tensor(out=o_[:], in0=a[:], in1=sg[:], op=ALU.mult), reads=[ak, 'sgF'], writes=[ok_])
                    pb, pbk = PB[n % 2], 'pbF%d' % (n % 2)
                    for c in range(4):
                        op('pe', lambda e, pb=pb, o_=o_, c=c: e.transpose(out=pb[:, c, 0:64], in_=o_[:, c * 128:(c + 1) * 128], identity=identb[0:64, 0:64]), reads=[ok_, 'identb'], writes=[pbk])
                    op('act', lambda e, pb=pb, n=n: e.copy(out=odnT[:, :, n * 64:(n + 1) * 64], in_=pb[:, :, 0:64]), reads=[pbk], writes=[('odnT', n)])
                kb.barrier()
                if stop == 'D3':
                    return fin()
            if dbg:
                with ExitStack() as s1:
                    tmp = s1.enter_context(nc.sbuf_tensor("dbgt3", [128, 4 * T], F32))
                    op('dve', lambda e: e.tensor_copy(out=tmp[:], in_=odnT[:].rearrange("p k n -> p (k n)")), reads=[('odnT', n) for n in range(32)], writes=['dbgt3'])
                    dma('sp', lambda e: e.dma_start(out=dbg_d["d_odn"][:, :], in_=tmp[:]), reads=['dbgt3'], writes=['d_odn'])
                    kb.barrier()

            with ExitStack() as s1:
                S = lambda name, shape, dt=F32: s1.enter_context(nc.sbuf_tensor("t_" + name, shape, dt))
                PF = [s1.enter_context(nc.psum_tensor("pfG%d" % i, [128, 512], F32)) for i in range(8)]
                pfi = [0]

                def nextpf():
                    i = pfi[0] % 8
                    pfi[0] += 1
                    return PF[i], 'pfG%d' % i
                wg = S("wg", [128, 8, 2048], BF16)
                wa = S("wa", [128, 4, D], BF16)
                wb = S("wb", [128, 4, D], BF16)
                wo = S("wo", [128, 8, D], BF16)
                wr = S("wr", [128, 8, 32])
                brb = S("brb", [128, 32])
                yT = S("yT", [128, 8, 512], BF16)
                sga = S("sga", [128, 512])
                sgb = S("sgb", [128, 512])
                xm = [S("xm0", [128, D])] * 2
                xo = [S("xo%d" % i, [128, D]) for i in range(2)]
                h2 = [S("h2_%d" % i, [128, D]) for i in range(2)]
                h2T = S("h2T", [128, 8, 128])
                junk = S("junkE", [128, D])
                ssq = S("ssqE", [128, 16])
                rs = S("rsE", [128, 16])
                for j in range(4):
                    dma('pool', lambda e, j=j: e.dma_start(out=wg[:, :, j * 512:(j + 1) * 512], in_=winv[:, :, 3616 + j * 512:3616 + (j + 1) * 512]), writes=[('wg', j)])
                dma('pool', lambda e: e.dma_start(out=wa[:], in_=wbra_d.rearrange("(k p) n -> p k n", p=128)), writes=['wa'])
                dma('pool', lambda e: e.dma_start(out=wb[:], in_=wbrb_d.rearrange("(k p) n -> p k n", p=128)), writes=['wb'])
                for j in range(2):
                    dma('pool', lambda e, j=j: e.dma_start(out=wo[:, :, j * 512:(j + 1) * 512], in_=wout_d.rearrange("(k p) n -> p k n", p=128)[:, :, j * 512:(j + 1) * 512]), writes=[('wo', j)])
                dma('sp', lambda e: e.dma_start(out=wr[:], in_=wr_d.rearrange("(k p) n -> p k n", p=128)), writes=['wr'])
                dma('sp', lambda e: e.dma_start(out=brb[:], in_=br_d[0:1, :].partition_broadcast(128)), writes=['brb'])
                for tb in range(4):
                    n0 = tb * 512
                    hts = [('hT', t) for t in range(n0 // 128, n0 // 128 + 4)]
                    for c in range(8):
                        cs = slice(c * 128, (c + 1) * 128)
                        psa, pka = nextpf()
                        for k in range(4):
                            op('pe', lambda e, k=k, psa=psa: e.matmul(out=psa[:], lhsT=wa[:, k, cs], rhs=onaT[:, k, n0:n0 + 512], start=(k == 0), stop=(k == 3)), reads=['wa'] + onaT_all, writes=[pka])
                        psb, pkb = nextpf()
                        for k in range(4):
                            op('pe', lambda e, k=k, psb=psb: e.matmul(out=psb[:], lhsT=wb[:, k, cs], rhs=odnT[:, k, n0:n0 + 512], start=(k == 0), stop=(k == 3)), reads=['wb'] + [('odnT', n) for n in range(tb * 8, tb * 8 + 8)], writes=[pkb])
                        pga, pkga = nextpf()
                        for k in range(8):
                            op('pe', lambda e, k=k, pga=pga: e.matmul(out=pga[:], lhsT=wg[:, k, c * 128:(c + 1) * 128], rhs=hT[:, k, n0:n0 + 512], start=(k == 0), stop=(k == 7)), reads=[('wg', c // 4)] + hts, writes=[pkga])
                        pgb, pkgb = nextpf()
                        for k in range(8):
                            op('pe', lambda e, k=k, pgb=pgb: e.matmul(out=pgb[:], lhsT=wg[:, k, 1024 + c * 128:1024 + (c + 1) * 128], rhs=hT[:, k, n0:n0 + 512], start=(k == 0), stop=(k == 7)), reads=[('wg', 2 + c // 4)] + hts, writes=[pkgb])
                        op('act', lambda e, pga=pga: e.activation(out=sga[:], in_=pga[:], func=AF.Sigmoid), reads=[pkga], writes=['sga'])
                        op('act', lambda e, pgb=pgb: e.activation(out=sgb[:], in_=pgb[:], func=AF.Sigmoid), reads=[pkgb], writes=['sgb'])
                        op('dve', lambda e, psa=psa: e.tensor_tensor(out=sga[:], in0=sga[:], in1=psa[:], op=ALU.mult), reads=['sga', pka], writes=['sga'])
                        op('dve', lambda e, psb=psb: e.tensor_tensor(out=sgb[:], in0=sgb[:], in1=psb[:], op=ALU.mult), reads=['sgb', pkb], writes=['sgb'])
                        op('pool', lambda e, c=c: e.tensor_tensor(out=yT[:, c, :], in0=sga[:], in1=sgb[:], op=ALU.add), reads=['sga', 'sgb'], writes=['yT'])
                    for i4 in range(4):
                        i = tb * 4 + i4
                        xmi, xmk = xm[0], 'xm0'
                        xoi, xok = xo[i % 2], 'xo%d' % (i % 2)
                        h2i, h2k = h2[i % 2], 'h2_%d' % (i % 2)
                        dma('sp', lambda e, xmi=xmi, i=i: e.dma_start(out=xmi[:], in_=x_d[i * 128:(i + 1) * 128, :]), writes=[xmk])
                        for half in range(2):
                            hs_ = slice(half * 512, (half + 1) * 512)
                            ps, pk = nextpf()
                            for k in range(8):
                                op('pe', lambda e, k=k, ps=ps: e.matmul(out=ps[:], lhsT=yT[:, k, i4 * 128:(i4 + 1) * 128], rhs=wo[:, k, hs_], start=(k == 0), stop=(k == 7)), reads=['yT', ('wo', half)], writes=[pk])
                            op('dve', lambda e, ps=ps, xoi=xoi: e.tensor_tensor(out=xoi[:, hs_], in0=ps[:], in1=MODL[:, 2 * D + half * 512:2 * D + (half + 1) * 512], op=ALU.mult), reads=[pk, 'MODL'], writes=[xok])
                            op('dve', lambda e, xoi=xoi, xmi=xmi: e.tensor_tensor(out=xoi[:, hs_], in0=xoi[:, hs_], in1=xmi[:, hs_], op=ALU.add), reads=[xok, xmk], writes=[xok])
                        dma('sp', lambda e, xoi=xoi, i=i: e.dma_start(out=xl2_s[i * 128:(i + 1) * 128, :], in_=xoi[:]), reads=[xok], writes=[('xl2_s', i)])
                        op('act', lambda e, xoi=xoi, i=i: e.activation(out=junk[:], in_=xoi[:], func=AF.Square, accum_out=ssq[:, i:i + 1]), reads=[xok], writes=['junkE', 'ssqE%d' % i])
                        rstd_from_ssq(ssq[:, i:i + 1], rs[:, i:i + 1], 1.0 / D, ['ssqE%d' % i], ['rsE%d' % i])
                        op('dve', lambda e, xoi=xoi, i=i: e.scalar_tensor_tensor(out=junk[:], in0=xoi[:], scalar=rs[:, i:i + 1], in1=MODL[:, 4 * D:5 * D], op0=ALU.mult, op1=ALU.mult), reads=[xok, 'rsE%d' % i, 'MODL', 'junkE'], writes=['junkE'])
                        op('dve', lambda e, h2i=h2i: e.tensor_tensor(out=h2i[:], in0=junk[:], in1=MODL[:, 3 * D:4 * D], op=ALU.add), reads=['junkE', 'MODL'], writes=[h2k])
                        dma('sp', lambda e, h2i=h2i, i=i: e.dma_start(out=h2_s[i * 128:(i + 1) * 128, :], in_=h2i[:]), reads=[h2k], writes=[('h2_s', i)])
                        for hh in range(2):
                            ps, pk = nextpf()
                            for k4 in range(4):
                                k = hh * 4 + k4
                                op('pe', lambda e, ps=ps, k=k, k4=k4, h2i=h2i: e.transpose(out=ps[:, k4 * 128:(k4 + 1) * 128], in_=h2i[:, k * 128:(k + 1) * 128], identity=ident), reads=[h2k, 'cst'], writes=[pk])
                            op('act', lambda e, ps=ps, hh=hh: e.copy(out=h2T[:, hh * 4:(hh + 1) * 4, :], in_=ps[:].rearrange("p (k f) -> p k f", f=128)), reads=[pk], writes=[('h2T', hh)])
                        ps, pk = nextpf()
                        for k in range(8):
                            op('pe', lambda e, ps=ps, k=k: e.matmul(out=ps[:, 0:32], lhsT=h2T[:, k, :], rhs=wr[:, k, :], start=(k == 0), stop=(k == 7)), reads=[('h2T', k // 4), 'wr'], writes=[pk])
                        op('dve', lambda e, ps=ps, i=i: e.tensor_tensor(out=LG[:, i, :], in0=ps[:, 0:32], in1=brb[:], op=ALU.add), reads=[pk, 'brb'], writes=['LG'])
                kb.barrier()
                if stop == 'E':
                    return fin()
            if dbg:
                dma('sp', lambda e: e.dma_start(out=dbg_d["d_lg"][:, :], in_=LG[:].rearrange("p a b -> p (a b)")), reads=['LG'], writes=['d_lg'])
                kb.barrier()

            mid.close()
            with ExitStack() as s1:
                S = lambda name, shape, dt=F32: s1.enter_context(nc.sbuf_tensor("t_" + name, shape, dt))
                PF = [s1.enter_context(nc.psum_tensor("pfH%d" % i, [128, 512], F32)) for i in range(8)]
                A3 = [128, 16, 32]
                DESTi = S("DESTi", [128, 4, 16], I32)
                GATE = S("GATE", [128, 4, 16])
                W1I = S("W1I", [128, NBLK, 8], I32)
                B1I = S("B1I", [128, NBLK], I32)
                B2I = S("B2I", [128, NBLK], I32)
                rt = ExitStack()
                s1.callback(rt.close)
                R_ = lambda name, shape, dt=F32: rt.enter_context(nc.sbuf_tensor("t_" + name, shape, dt))
                LGw = R_("LGw", A3)
                EQ = R_("EQ", [128, 4, 16, 32])
                SEL = R_("SEL", A3)
                GT = R_("GT", A3)
                RANK = R_("RANK", A3)
                tmp3 = R_("tmp3", A3)
                mr = R_("mr", [128, 16])
                m0 = R_("m0", [128, 16])
                den = R_("den", [128, 16])
                CNT = R_("CNT", [128, 32])
                PAD = R_("PAD", [128, 32])
                PADi = R_("PADi", [128, 32], I32)
                PADT = R_("PADT", [32, 128])
                PSb = R_("PSb", [128, 32])
                PEb = R_("PEb", [128, 32])
                DESTf = R_("DESTf", [128, 4, 16])
                cmp_ = R_("cmp_", [128, NBLK, 32])
                BE = R_("BE", [128, NBLK])
                tmpw = R_("tmpw", [128, NBLK, 8])
                tmpb = R_("tmpb", [128, NBLK])
                zi = R_("zi", [128, NBLK], I32)
                tokid = R_("tokid", [128, 16], I32)
                b16 = lambda ap: ap.unsqueeze(2).to_broadcast(A3)
                e16 = lambda ap: ap.unsqueeze(1).to_broadcast(A3)
                op('dve', lambda e: e.tensor_copy(out=LGw[:], in_=LG[:]), reads=['LG'], writes=['LGw'])
                for r in range(4):
                    op('dve', lambda e: e.tensor_reduce(out=mr[:], in_=LGw[:], axis=AX.X, op=ALU.max), reads=['LGw'], writes=['mr'])
                    if r == 0:
                        op('dve', lambda e: e.tensor_copy(out=m0[:], in_=mr[:]), reads=['mr'], writes=['m0'])
                    op('dve', lambda e, r=r: e.tensor_tensor(out=EQ[:, r], in0=LGw[:], in1=b16(mr[:]), op=ALU.is_equal), reads=['LGw', 'mr'], writes=[('EQ', r)])
                    op('dve', lambda e, r=r: e.scalar_tensor_tensor(out=LGw[:], in0=EQ[:, r], scalar=NEG, in1=LGw[:], op0=ALU.mult, op1=ALU.add), reads=[('EQ', r), 'LGw'], writes=['LGw'])
                op('dve', lambda e: e.tensor_tensor(out=SEL[:], in0=EQ[:, 0], in1=EQ[:, 1], op=ALU.add), reads=[('EQ', 0), ('EQ', 1)], writes=['SEL'])
                op('dve', lambda e: e.tensor_tensor(out=SEL[:], in0=SEL[:], in1=EQ[:, 2], op=ALU.add), reads=['SEL', ('EQ', 2)], writes=['SEL'])
                op('dve', lambda e: e.tensor_tensor(out=SEL[:], in0=SEL[:], in1=EQ[:, 3], op=ALU.add), reads=['SEL', ('EQ', 3)], writes=['SEL'])
                op('dve', lambda e: e.tensor_tensor(out=GT[:], in0=LG[:], in1=b16(m0[:]), op=ALU.subtract), reads=['LG', 'm0'], writes=['GT'])
                op('act', lambda e: e.activation(out=GT[:], in_=GT[:], func=AF.Exp), reads=['GT'], writes=['GT'])
                op('dve', lambda e: e.tensor_tensor(out=GT[:], in0=GT[:], in1=SEL[:], op=ALU.mult), reads=['GT', 'SEL'], writes=['GT'])
                op('dve', lambda e: e.tensor_reduce(out=den[:], in_=GT[:], axis=AX.X, op=ALU.add), reads=['GT'], writes=['den'])
                op('dve', lambda e: e.reciprocal(out=den[:], in_=den[:]), reads=['den'], writes=['den'])
                op('dve', lambda e: e.tensor_tensor(out=GT[:], in0=GT[:], in1=b16(den[:]), op=ALU.mult), reads=['GT', 'den'], writes=['GT'])
                op('dve', lambda e: e.memset(CNT[:], 0.0), writes=['CNT'])
                for i in range(16):
                    ps, pk = PF[i % 2], 'pfH%d' % (i % 2)
                    op('pe', lambda e, ps=ps, i=i: e.matmul(out=ps[:, 0:32], lhsT=cst[:, C_UT:C_UT + 128], rhs=SEL[:, i, :], start=True, stop=True), reads=['SEL', 'cst'], writes=[pk])
                    op('dve', lambda e, ps=ps, i=i: e.tensor_tensor(out=RANK[:, i, :], in0=ps[:, 0:32], in1=CNT[:], op=ALU.add), reads=[pk, 'CNT'], writes=['RANK'])
                    ps2, pk2 = PF[2 + i % 2], 'pfH%d' % (2 + i % 2)
                    op('pe', lambda e, ps2=ps2, i=i: e.matmul(out=ps2[:, 0:32], lhsT=ones, rhs=SEL[:, i, :], start=True, stop=True), reads=['SEL', 'cst'], writes=[pk2])
                    op('dve', lambda e, ps2=ps2: e.tensor_tensor(out=CNT[:], in0=CNT[:], in1=ps2[:, 0:32], op=ALU.add), reads=[pk2, 'CNT'], writes=['CNT'])
                op('dve', lambda e: e.tensor_scalar(out=PAD[:], in0=CNT[:], scalar1=127.0, scalar2=None, op0=ALU.add), reads=['CNT'], writes=['PAD'])
                op('dve', lambda e: e.tensor_copy(out=PADi[:], in_=PAD[:]), reads=['PAD'], writes=['PADi'])
                op('dve', lambda e: e.tensor_scalar(out=PADi[:], in0=PADi[:], scalar1=7, scalar2=7, op0=ALU.arith_shift_right, op1=ALU.logical_shift_left), reads=['PADi'], writes=['PADi'])
                op('dve', lambda e: e.tensor_copy(out=PAD[:], in_=PADi[:]), reads=['PADi'], writes=['PAD'])
                op('pe', lambda e: e.transpose(out=PF[4][0:32, 0:128], in_=PAD[:, 0:32], identity=ident), reads=['PAD', 'cst'], writes=['pfH4'])
                op('act', lambda e: e.copy(out=PADT[:], in_=PF[4][0:32, 0:128]), reads=['pfH4'], writes=['PADT'])
                op('pe', lambda e: e.matmul(out=PF[5][:, 0:32], lhsT=PADT[:], rhs=cst[0:32, C_TRI:C_TRI + 32], start=True, stop=True), reads=['PADT', 'cst'], writes=['pfH5'])
                op('act', lambda e: e.copy(out=PSb[:], in_=PF[5][:, 0:32]), reads=['pfH5'], writes=['PSb'])
                op('dve', lambda e: e.tensor_tensor(out=PEb[:], in0=PSb[:], in1=PAD[:], op=ALU.add), reads=['PSb', 'PAD'], writes=['PEb'])
                op('dve', lambda e: e.tensor_tensor(out=RANK[:], in0=RANK[:], in1=e16(PSb[:]), op=ALU.add), reads=['RANK', 'PSb'], writes=['RANK'])
                for r in range(4):
                    op('dve', lambda e, r=r: e.tensor_tensor(out=tmp3[:], in0=EQ[:, r], in1=RANK[:], op=ALU.mult), reads=[('EQ', r), 'RANK'], writes=['tmp3'])
                    op('dve', lambda e, r=r: e.tensor_reduce(out=DESTf[:, r, :], in_=tmp3[:], axis=AX.X, op=ALU.add), reads=['tmp3'], writes=['DESTf'])
                    op('dve', lambda e, r=r: e.tensor_tensor(out=tmp3[:], in0=EQ[:, r], in1=GT[:], op=ALU.mult), reads=[('EQ', r), 'GT'], writes=['tmp3'])
                    op('dve', lambda e, r=r: e.tensor_reduce(out=GATE[:, r, :], in_=tmp3[:], axis=AX.X, op=ALU.add), reads=['tmp3'], writes=['GATE'])
                op('dve', lambda e: e.tensor_copy(out=DESTi[:], in_=DESTf[:]), reads=['DESTf'], writes=['DESTi'])
                op('dve', lambda e: e.tensor_tensor(out=cmp_[:], in0=PEb[:].unsqueeze(1).to_broadcast([128, NBLK, 32]), in1=cst[:, C_BS:C_BS + NBLK].unsqueeze(2).to_broadcast([128, NBLK, 32]), op=ALU.is_le),
                   reads=['PEb', 'cst'], writes=['cmp_'])
                op('dve', lambda e: e.tensor_reduce(out=BE[:], in_=cmp_[:], axis=AX.X, op=ALU.add), reads=['cmp_'], writes=['BE'])
                op('dve', lambda e: e.tensor_scalar_min(out=BE[:], in0=BE[:], scalar1=31.0), reads=['BE'], writes=['BE'])
                op('dve', lambda e: e.tensor_copy(out=B2I[:], in_=BE[:]), reads=['BE'], writes=['B2I'])
                op('dve', lambda e: e.tensor_scalar(out=tmpb[:], in0=BE[:], scalar1=128.0, scalar2=cst[:, C_KR:C_KR + 1], op0=ALU.mult, op1=ALU.add), reads=['BE', 'cst'], writes=['tmpb'])
                op('dve', lambda e: e.tensor_copy(out=B1I[:], in_=tmpb[:]), reads=['tmpb'], writes=['B1I'])
                op('dve', lambda e: e.tensor_scalar(out=tmpb[:], in0=BE[:], scalar1=1024.0, scalar2=None, op0=ALU.mult), reads=['BE', 'B1I'], writes=['tmpb'])
                op('dve', lambda e: e.tensor_tensor(out=tmpw[:], in0=tmpb[:].unsqueeze(2).to_broadcast([128, NBLK, 8]), in1=cst[:, C_KR:C_KR + 8].unsqueeze(1).to_broadcast([128, NBLK, 8]), op=ALU.add),
                   reads=['tmpb', 'cst'], writes=['tmpw'])
                op('dve', lambda e: e.tensor_copy(out=W1I[:], in_=tmpw[:]), reads=['tmpw'], writes=['W1I'])
                op('dve', lambda e: e.memset(zi[:], 0), writes=['zi'])
                op('dve', lambda e: e.tensor_copy(out=tokid[:], in_=cst[:, C_TOK:C_TOK + 16]), reads=['cst'], writes=['tokid'])
                dma('sp', lambda e: e.dma_start(out=slot_s.rearrange("(p j) o -> p (j o)", p=128), in_=zi[:]), reads=['zi'], writes=['slot_s'])
                for r in range(4):
                    for i in range(16):
                        dma('pool', lambda e, r=r, i=i: e.indirect_dma_start(out=slot_s[:, :], out_offset=bass.IndirectOffsetOnAxis(ap=DESTi[:, r, i:i + 1], axis=0), in_=tokid[:, i:i + 1], in_offset=None),
                            reads=['DESTi', 'tokid'], writes=['slot_s'])
                kb.barrier()
                rt.close()
                sidx = [S("sidx%d" % i, [128, 1], I32) for i in range(2)]
                xg = [S("xg%d" % i, [128, D]) for i in range(2)]
                xgT = S("xgT", [128, 8, 128], BF16)
                w1b = [S("w1b%d" % i, [128, 2048], BF16) for i in range(3)]
                w2b = [S("w2b%d" % i, [128, D], BF16) for i in range(3)]
                w1k = [S("w1k%d" % i, [128, 2048]) for i in range(6)]
                w2c = [S("w2c%d" % i, [128, D]) for i in range(8)]
                b1s = [S("b1s%d" % i, [128, 16]) for i in range(2)]
                b2s = [S("b2s%d" % i, [128, D]) for i in range(2)]
                Gt = S("Gt", [128, 4, 128])
                Ut = S("Ut", [128, 4, 128])
                sgm = S("sgm", [128, 4, 128])
                actT = S("actT", [128, 8, 128], BF16)
                ysb = [S("ysb%d" % i, [128, D]) for i in range(2)]
                PH = PF[0:4]
                PY = PF[4:6]
                PTr = PF[6:8]
                wc1 = wc2 = 0
                for j in range(NBLK):
                    si, sik = sidx[j % 2], 'sidx%d' % (j % 2)
                    xgi, xgk = xg[j % 2], 'xg%d' % (j % 2)

                    def issue_sidx(j_):
                        sb_, sbk = sidx[j_ % 2], 'sidx%d' % (j_ % 2)
                        dma('sp', lambda e: e.dma_start(out=sb_[:], in_=slot_s[j_ * 128:(j_ + 1) * 128, :]), reads=['slot_s'], writes=[sbk])
                    if j == 0:
                        issue_sidx(0)
                    if j + 1 < NBLK:
                        issue_sidx(j + 1)
                    dma('pool', lambda e, si=si, xgi=xgi: e.indirect_dma_start(out=xgi[:], out_offset=None, in_=h2_s[:, :], in_offset=bass.IndirectOffsetOnAxis(ap=si[:, 0:1], axis=0)),
                        reads=[sik] + [('h2_s', i) for i in range(16)], writes=[xgk])
                    b1i, b1k = b1s[j % 2], 'b1s%d' % (j % 2)
                    b2i, b2k = b2s[j % 2], 'b2s%d' % (j % 2)
                    dma('pool', lambda e, b1i=b1i, j=j: e.indirect_dma_start(out=b1i[:], out_offset=None, in_=b1t_d[:, :], in_offset=bass.IndirectOffsetOnAxis(ap=B1I[:, j:j + 1], axis=0)), reads=['B1I'], writes=[b1k])
                    dma('pool', lambda e, b2i=b2i, j=j: e.indirect_dma_start(out=b2i[:], out_offset=None, in_=b2_d[:, :], in_offset=bass.IndirectOffsetOnAxis(ap=B2I[:, j:j + 1], axis=0)), reads=['B2I'], writes=[b2k])
                    for hh in range(2):
                        ps, pk = PTr[hh], 'pfH%d' % (6 + hh)
                        for k4 in range(4):
                            k = hh * 4 + k4
                            op('pe', lambda e, ps=ps, k=k, k4=k4, xgi=xgi: e.transpose(out=ps[:, k4 * 128:(k4 + 1) * 128], in_=xgi[:, k * 128:(k + 1) * 128], identity=ident), reads=[xgk, 'cst'], writes=[pk])
                        op('act', lambda e, ps=ps, hh=hh: e.copy(out=xgT[:, hh * 4:(hh + 1) * 4, :], in_=ps[:].rearrange("p (k f) -> p k f", f=128)), reads=[pk], writes=[('xgT', hh)])
                    for k in range(8):
                        wt, wk = w1k[wc1 % 6], 'w1k%d' % (wc1 % 6)
                        wc1 += 1
                        dma('pool', lambda e, wt=wt, j=j, k=k: e.indirect_dma_start(out=wt[:], out_offset=None, in_=w1_d[:, :], in_offset=bass.IndirectOffsetOnAxis(ap=W1I[:, j, k:k + 1], axis=0)), reads=['W1I'], writes=[wk])
                        wb_, wbk = w1b[wc1 % 3], 'w1b%d' % (wc1 % 3)
                        if k % 2 == 0:
                            op('dve', lambda e, wt=wt, wb_=wb_: e.tensor_copy(out=wb_[:], in_=wt[:]), reads=[wk], writes=[wbk])
                        else:
                            op('act', lambda e, wt=wt, wb_=wb_: e.copy(out=wb_[:], in_=wt[:]), reads=[wk], writes=[wbk])
                        for c in range(16):
                            op('pe', lambda e, wb_=wb_, k=k, c=c: e.matmul(out=PH[c // 4][:, (c % 4) * 128:(c % 4 + 1) * 128], lhsT=wb_[:, c * 128:(c + 1) * 128], rhs=xgT[:, k, :], start=(k == 0 and c % 4 == 0), stop=(k == 7 and c % 4 == 3)),
                               reads=[wbk, ('xgT', k // 4)], writes=['pfH%d' % (c // 4)])
                    for q in range(2):
                        g3_ = PH[q][:].rearrange("p (c f) -> p c f", f=128)
                        u3_ = PH[q + 2][:].rearrange("p (c f) -> p c f", f=128)
                        bb = lambda lo: b1i[:, lo:lo + 4].unsqueeze(2).to_broadcast([128, 4, 128])
                        op('dve', lambda e: e.tensor_tensor(out=Gt[:], in0=g3_, in1=bb(4 * q), op=ALU.add), reads=['pfH%d' % q, b1k], writes=['Gt'])
                        op('dve', lambda e: e.tensor_scalar_min(out=Gt[:], in0=Gt[:], scalar1=7.0), reads=['Gt'], writes=['Gt'])
                        op('act', lambda e: e.activation(out=sgm[:], in_=Gt[:], func=AF.Sigmoid, scale=1.702), reads=['Gt'], writes=['sgm'])
                        op('dve', lambda e: e.tensor_tensor(out=Ut[:], in0=u3_, in1=bb(8 + 4 * q), op=ALU.add), reads=['pfH%d' % (q + 2), b1k], writes=['Ut'])
                        op('dve', lambda e: e.tensor_scalar(out=Ut[:], in0=Ut[:], scalar1=7.0, scalar2=-7.0, op0=ALU.min, op1=ALU.max), reads=['Ut'], writes=['Ut'])
                        op('dve', lambda e: e.scalar_tensor_tensor(out=Ut[:], in0=Ut[:], scalar=1.0, in1=Gt[:], op0=ALU.add, op1=ALU.mult), reads=['Ut', 'Gt'], writes=['Ut'])
                        op('dve', lambda e: e.tensor_tensor(out=actT[:, 4 * q:4 * q + 4, :], in0=Ut[:], in1=sgm[:], op=ALU.mult), reads=['Ut', 'sgm'], writes=[('actT', q)])
                    for fc in range(8):
                        wt, wk = w2c[wc2 % 8], 'w2c%d' % (wc2 % 8)
                        wc2 += 1
                        dma('pool', lambda e, wt=wt, j=j, fc=fc: e.indirect_dma_start(out=wt[:], out_offset=None, in_=w2_d[:, :], in_offset=bass.IndirectOffsetOnAxis(ap=W1I[:, j, fc:fc + 1], axis=0)), reads=['W1I'], writes=[wk])
                        wb_, wbk = w2b[wc2 % 3], 'w2b%d' % (wc2 % 3)
                        op('act', lambda e, wt=wt, wb_=wb_: e.copy(out=wb_[:], in_=wt[:]), reads=[wk], writes=[wbk])
                        for half in range(2):
                            op('pe', lambda e, wb_=wb_, fc=fc, half=half: e.matmul(out=PY[half][:], lhsT=actT[:, fc, :], rhs=wb_[:, half * 512:(half + 1) * 512], start=(fc == 0), stop=(fc == 7)),
                               reads=[wbk, ('actT', fc // 4)], writes=['pfH%d' % (4 + half)])
                    yi, yk = ysb[j % 2], 'ysb%d' % (j % 2)
                    for half in range(2):
                        op('dve', lambda e, yi=yi, half=half: e.tensor_tensor(out=yi[:, half * 512:(half + 1) * 512], in0=PY[half][:], in1=b2i[:, half * 512:(half + 1) * 512], op=ALU.add),
                           reads=['pfH%d' % (4 + half), b2k], writes=[yk])
                    dma('sp', lambda e, yi=yi, j=j: e.dma_start(out=ypad_s[j * 128:(j + 1) * 128, :], in_=yi[:]), reads=[yk], writes=['ypad_s'])
                yr = [S("yr%d" % i, [128, D]) for i in range(4)]
                xq = [S("xq0", [128, D])] * 2
                ac = [S("ac%d" % i, [128, D]) for i in range(2)]
                fnb = S("fnb", [128, D])
                ssq = S("ssqH", [128, 16])
                rs = S("rsH", [128, 16])
                dma('sp', lambda e: e.dma_start(out=fnb[:], in_=fnw_d[0:1, :].partition_broadcast(128)), writes=['fnb'])
                for i in range(16):
                    aci, ack = ac[i % 2], 'ac%d' % (i % 2)
                    xqi, xqk = xq[0], 'xq0'
                    dma('sp', lambda e, xqi=xqi, i=i: e.dma_start(out=xqi[:], in_=xl2_s[i * 128:(i + 1) * 128, :]), reads=[('xl2_s', i)], writes=[xqk])
                    for r in range(4):
                        dma('pool', lambda e, r=r, i=i: e.indirect_dma_start(out=yr[r][:], out_offset=None, in_=ypad_s[:, :], in_offset=bass.IndirectOffsetOnAxis(ap=DESTi[:, r, i:i + 1], axis=0)),
                            reads=['DESTi', 'ypad_s'], writes=['yr%d' % r])
                        if r == 0:
                            op('dve', lambda e, aci=aci, i=i: e.tensor_scalar(out=aci[:], in0=yr[0][:], scalar1=GATE[:, 0, i:i + 1], scalar2=None, op0=ALU.mult), reads=['yr0', 'GATE'], writes=[ack])
                        else:
                            op('dve', lambda e, aci=aci, i=i, r=r: e.scalar_tensor_tensor(out=aci[:], in0=yr[r][:], scalar=GATE[:, r, i:i + 1], in1=aci[:], op0=ALU.mult, op1=ALU.add), reads=['yr%d' % r, 'GATE', ack], writes=[ack])
                    op('dve', lambda e, aci=aci: e.tensor_tensor(out=aci[:], in0=aci[:], in1=MODL[:, 5 * D:6 * D], op=ALU.mult), reads=[ack, 'MODL'], writes=[ack])
                    op('dve', lambda e, aci=aci, xqi=xqi: e.tensor_tensor(out=aci[:], in0=aci[:], in1=xqi[:], op=ALU.add), reads=[ack, xqk], writes=[ack])
                    op('act', lambda e, aci=aci, i=i: e.activation(out=yr[0][:], in_=aci[:], func=AF.Square, accum_out=ssq[:, i:i + 1]), reads=[ack], writes=['yr0', 'ssqH%d' % i])
                    rstd_from_ssq(ssq[:, i:i + 1], rs[:, i:i + 1], 1.0 / D, ['ssqH%d' % i], ['rsH%d' % i])
                    op('dve', lambda e, aci=aci, i=i: e.scalar_tensor_tensor(out=aci[:], in0=aci[:], scalar=rs[:, i:i + 1], in1=fnb[:], op0=ALU.mult, op1=ALU.mult), reads=[ack, 'rsH%d' % i, 'fnb'], writes=[ack])
                    dma('sp', lambda e, aci=aci, i=i: e.dma_start(out=out_d[i * 128:(i + 1) * 128, :], in_=aci[:]), reads=[ack], writes=[('out', i)])
                kb.barrier()
        if kb.stopped:
            kb.stopped = False
            kb.limit = None
            kb.barrier()
            return fin()
        print("instructions:", kb.nins)
    return nc


def host_consts():
    cst = np.zeros((128, C_N), np.float32)
    cst[:, C_ID:C_ID + 128] = np.eye(128)
    cst[:, C_ONE:C_ONE + 128] = 1.0
    i = np.arange(64)
    cst[:64, C_U:C_U + 64] = (i[:, None] <= i[None, :])
    cst[:64, C_L:C_L + 64] = (i[:, None] >= i[None, :])
    cst[:64, C_IF:C_IF + 64] = (i[:, None] >= i[None, :])
    cst[:64, C_SF:C_SF + 64] = (i[:, None] > i[None, :])
    cst[:64, C_IB:C_IB + 64] = (i[:, None] <= i[None, :])
    cst[:64, C_SB:C_SB + 64] = (i[:, None] < i[None, :])
    e = np.arange(32)
    cst[:32, C_TRI:C_TRI + 32] = (e[:, None] < e[None, :])
    t = np.arange(128)
    cst[:, C_UT:C_UT + 128] = (t[:, None] < t[None, :])
    cst[:, C_BS:C_BS + NBLK] = (np.arange(NBLK) * 128)[None, :]
    cst[:, C_KR:C_KR + 8] = np.arange(8)[None, :] * 128 + t[:, None]
    cst[:, C_TOK:C_TOK + 16] = np.arange(16)[None, :] * 128 + t[:, None]
    return cst


def host_rope():
    tt = np.arange(T)
    row = (tt // 64).astype(np.float32)
    col = (tt % 64).astype(np.float32)
    freqs = (np.float32(10000.0) ** (-np.arange(16, dtype=np.float32) / np.float32(16))).astype(np.float32)
    ang = np.concatenate([row[:, None] * freqs, col[:, None] * freqs], axis=-1).astype(np.float32)
    cos, sin = np.cos(ang).astype(np.float32), np.sin(ang).astype(np.float32)
    r = np.stack([cos.reshape(32, 64, 32).transpose(1, 0, 2), sin.reshape(32, 64, 32).transpose(1, 0, 2)], axis=1)
    return np.ascontiguousarray(r.reshape(64, 2 * 32 * 32))


def host_natbl(rpb):
    kc = np.arange(64)[:, None]
    qc = np.arange(64)[None, :]
    dc = np.clip(kc - qc + 15, 0, 30)
    cs = np.clip(np.arange(64) - 8, 0, 48)
    cmask = (kc >= cs[None, :]) & (kc < cs[None, :] + 16)
    base = np.full((8, 64, 17, 64), NEG, np.float32)
    for jp in range(0, 15):
        g = rpb[:, 14 - jp][:, dc]
        base[:, :, jp + 1, :] = np.where(cmask[None], g, np.float32(NEG))
    base_int = base.copy()
    for jp in range(-1, 16):
        if not (4 <= jp <= 11):
            base_int[:, :, jp + 1, :] = NEG
    out = np.empty((8, 2, 128, 16, 64), np.float32)
    for ti, b in enumerate((base, base_int)):
        for a in range(2):
            out[:, ti, a * 64:(a + 1) * 64, :, :] = b[:, :, (1 - a):(17 - a), :]
    return np.ascontiguousarray(out.reshape(8, 2, 128, 1024))


def make_in_maps(inputs, cores):
    f = lambda a: np.ascontiguousarray(np.asarray(a, dtype=np.float32))
    shared = {
        "w_mod": f(inputs["w_mod"][0]), "b_mod": f(inputs["b_mod"][0]).reshape(1, -1), "norm1_w": f(inputs["norm1_w"][0]).reshape(1, -1),
        "w_in": f(inputs["w_in"][0]), "na_tbl": host_natbl(np.asarray(inputs["na_rpb"][0], np.float32)),
        "convw": f(np.asarray(inputs["dn_conv_w"][0]).reshape(5, 12, 128).transpose(2, 1, 0).reshape(128, 60)),
        "alog": f(inputs["dn_a_log"][0]).reshape(1, 16), "dtb": f(inputs["dn_dt_bias"][0]).reshape(1, 16), "dnw": f(inputs["dn_norm_w"][0]).reshape(1, 64),
        "w_br_a": f(inputs["w_br_a"][0]), "w_br_b": f(inputs["w_br_b"][0]), "w_out": f(inputs["w_out"][0]), "norm2_w": f(inputs["norm2_w"][0]).reshape(1, -1),
        "w_router": f(inputs["w_router"][0]), "b_router": f(inputs["b_router"][0]).reshape(1, 32),
        "w1": f(inputs["w1"][0]).reshape(32 * D, 2048), "b1t": f(np.asarray(inputs["b1"][0]).reshape(32, 16, 128).transpose(0, 2, 1).reshape(32 * 128, 16)),
        "w2": f(inputs["w2"][0]).reshape(32 * D, D), "b2": f(inputs["b2"][0]), "fnw": f(inputs["final_norm_w"]).reshape(1, -1),
        "rope": host_rope(), "cst": host_consts(),
    }
    maps = []
    for b in cores:
        m = dict(shared)
        m["x"] = f(inputs["x"][b])
        m["ctx"] = f(inputs["ctx"][b])
        m["c2"] = f(np.stack([np.asarray(inputs["c"][b]), np.asarray(inputs["c_ctx"])], axis=0))
        maps.append(m)
    return maps


def kernel(**inputs):
    nc = build()
    maps = make_in_maps(inputs, list(range(8)))
    res = run_bass_kernel_spmd(nc, maps, core_ids=list(range(8)))
    return np.stack([np.asarray(r["out"], dtype=np.float32) for r in res.results], axis=0)
```
